# Optimizing a Trainium2 kernel written in Bass

```python
import jax
import jax.numpy as jnp
from jax import lax
import numpy as np

D_MODEL = 1024
BATCH = 16
SEQ = 4096
DEPTH = 4

GRID_W = 64
CTX_LEN = 256
N_MIXERS = 3
ALPHA = (2.0 * DEPTH) ** 0.25
BETA = (8.0 * DEPTH) ** -0.25
LN_EPS = 1e-5
RMS_EPS = 1e-6
ROPE_BASE = 10000.0

RET_HEADS = 4
RET_DK = D_MODEL // RET_HEADS
RET_DV = 2 * RET_DK
RET_QK = RET_HEADS * RET_DK
RET_VW = RET_HEADS * RET_DV
RET_CHUNK = 128

MLA_HEADS = 8
MLA_Q_LORA = 3 * D_MODEL // 8
MLA_KV_LORA = D_MODEL // 4
MLA_NOPE = 128
MLA_ROPE = 64
MLA_V = 128
Q_BLOCK = 128

CONV_WIDTH = 3

MOE_GROUPS = 4
MOE_EXPERTS_PER_GROUP = 8
MOE_EXPERTS = MOE_GROUPS * MOE_EXPERTS_PER_GROUP
MOE_TOPK = 2
MOE_HIDDEN = D_MODEL // 2
MOE_BLOCK = 256

N_RET = (DEPTH + 2) // 3
N_MLA = (DEPTH + 1) // 3
N_CONV = DEPTH // 3

kernel_name = 'hybrid_retention_mla_shortconv_hmoe_dit'


def layer_norm(x, g, b):
    xf = x.astype(jnp.float32)
    mu = jnp.mean(xf, -1, keepdims=True)
    var = jnp.mean(jnp.square(xf - mu), -1, keepdims=True)
    y = (xf - mu) * lax.rsqrt(var + LN_EPS)
    return (y * g.astype(jnp.float32) + b.astype(jnp.float32)).astype(x.dtype)


def rms_norm(x, g):
    xf = x.astype(jnp.float32)
    y = xf * lax.rsqrt(jnp.mean(xf * xf, -1, keepdims=True) + RMS_EPS)
    return (y * g.astype(jnp.float32)).astype(x.dtype)


def modulate(h, shift, scale):
    return h * (1.0 + scale) + shift


def axial_rope_tables(n_tokens, dim):
    n_rows = n_tokens // GRID_W
    rows = jnp.repeat(jnp.arange(n_rows, dtype=jnp.float32), GRID_W)
    cols = jnp.tile(jnp.arange(GRID_W, dtype=jnp.float32), n_rows)
    quarter = dim // 4
    inv_freq = ROPE_BASE ** (-jnp.arange(quarter, dtype=jnp.float32) / quarter)
    ang = jnp.concatenate([rows[:, None] * inv_freq, cols[:, None] * inv_freq], axis=-1)
    return jnp.cos(ang), jnp.sin(ang)


def apply_rope(x, cos, sin):
    half = x.shape[-1] // 2
    cos = cos.astype(x.dtype)
    sin = sin.astype(x.dtype)
    x1, x2 = x[..., :half], x[..., half:]
    return jnp.concatenate([x1 * cos - x2 * sin, x1 * sin + x2 * cos], axis=-1)


def retention_scan(k, v, log_gamma, state0, q=None):
    b, h, t, _ = k.shape
    L = RET_CHUNK
    nc = t // L
    chunks = lambda a: jnp.moveaxis(a.reshape(b, h, nc, L, a.shape[-1]), 2, 0)
    idx = jnp.arange(L, dtype=jnp.float32)
    lg = log_gamma[:, None]
    k_dec = jnp.exp((L - 1.0 - idx) * lg)[..., None]
    c_dec = jnp.exp(L * lg)[..., None]

    def update(state, kc, vc):
        return state * c_dec + jnp.einsum('bhjd,bhje->bhde', kc * k_dec, vc)

    if q is None:
        final, _ = lax.scan(lambda st, kv: (update(st, kv[0], kv[1]), None), state0, (chunks(k), chunks(v)))
        return None, final
    rel = idx[:, None] - idx[None, :]
    intra = jnp.where(rel >= 0, jnp.exp(jnp.maximum(rel, 0.0) * lg[:, :, None]), 0.0)
    q_dec = jnp.exp((idx + 1.0) * lg)[..., None]

    def step(state, qkv):
        qc, kc, vc = qkv
        scores = jnp.einsum('bhid,bhjd->bhij', qc, kc) * intra
        out = jnp.einsum('bhij,bhje->bhie', scores, vc) + jnp.einsum('bhid,bhde->bhie', qc * q_dec, state)
        return update(state, kc, vc), out

    final, outs = lax.scan(step, state0, (chunks(q), chunks(k), chunks(v)))
    return jnp.moveaxis(outs, 0, 2).reshape(b, h, t, v.shape[-1]), final


def retention_mixer(u_ctx, u_lat, w_in, decay_logit, gn_g, w_out, need_ctx):
    b, s, _ = u_lat.shape
    w_q, w_k, w_v, w_g = jnp.split(w_in, [RET_QK, 2 * RET_QK, 2 * RET_QK + RET_VW], axis=1)

    def heads(z, dh):
        return z.reshape(b, -1, RET_HEADS, dh).transpose(0, 2, 1, 3).astype(jnp.float32)

    cos, sin = axial_rope_tables(s, RET_DK)
    q_l = apply_rope(heads(u_lat @ w_q, RET_DK), cos, sin)
    k_l = apply_rope(heads(u_lat @ w_k, RET_DK), cos, sin) * RET_DK ** -0.5
    v_l = heads(u_lat @ w_v, RET_DV)
    k_c = heads(u_ctx @ w_k, RET_DK) * RET_DK ** -0.5
    v_c = heads(u_ctx @ w_v, RET_DV)
    q_c = heads(u_ctx @ w_q, RET_DK) if need_ctx else None
    log_gamma = jax.nn.log_sigmoid(decay_logit.astype(jnp.float32))
    zero = jnp.zeros((b, RET_HEADS, RET_DK, RET_DV), jnp.float32)
    rev = lambda a: None if a is None else a[:, :, ::-1]
    o_cf, s_cf = retention_scan(k_c, v_c, log_gamma[0], zero, q_c)
    o_cb, s_cb = retention_scan(rev(k_c), rev(v_c), log_gamma[1], zero, rev(q_c))
    o_lf, _ = retention_scan(k_l, v_l, log_gamma[0], s_cf, q_l)
    o_lb, _ = retention_scan(rev(k_l), rev(v_l), log_gamma[1], s_cb, rev(q_l))

    def finish(o, u):
        mu = jnp.mean(o, -1, keepdims=True)
        var = jnp.mean(jnp.square(o - mu), -1, keepdims=True)
        on = ((o - mu) * lax.rsqrt(var + LN_EPS)).transpose(0, 2, 1, 3).reshape(b, -1, RET_VW)
        on = on * gn_g.astype(jnp.float32)
        return (jax.nn.silu(u @ w_g) * on.astype(u.dtype)) @ w_out

    y_lat = finish(o_lf + rev(o_lb), u_lat)
    y_ctx = finish(o_cf + rev(o_cb), u_ctx) if need_ctx else None
    return y_ctx, y_lat


def mla_mixer(u_ctx, u_lat, w_down, q_norm, kv_norm, w_uq, w_ukv, w_out, need_ctx):
    b, s, _ = u_lat.shape
    w_dq, w_dkv, w_kr = jnp.split(w_down, [MLA_Q_LORA, MLA_Q_LORA + MLA_KV_LORA], axis=1)
    cos, sin = axial_rope_tables(s, MLA_ROPE)
    scale = (MLA_NOPE + MLA_ROPE) ** -0.5

    def heads(z, dh):
        return z.reshape(b, -1, MLA_HEADS, dh).transpose(0, 2, 1, 3)

    def project_q(u):
        q = heads(rms_norm(u @ w_dq, q_norm) @ w_uq, MLA_NOPE + MLA_ROPE)
        return q[..., :MLA_NOPE], q[..., MLA_NOPE:]

    def project_kv(u):
        kv = heads(rms_norm(u @ w_dkv, kv_norm) @ w_ukv, MLA_NOPE + MLA_V)
        return kv[..., :MLA_NOPE], u @ w_kr, kv[..., MLA_NOPE:]

    def attend(qn, qr, kn, kr, v):
        sc = jnp.einsum('bhqd,bhkd->bhqk', qn, kn) + jnp.einsum('bhqd,bkd->bhqk', qr, kr)
        p = jax.nn.softmax(sc.astype(jnp.float32) * scale, axis=-1).astype(v.dtype)
        return jnp.einsum('bhqk,bhkd->bhqd', p, v)

    def merge(o):
        return o.transpose(0, 2, 1, 3).reshape(b, -1, MLA_HEADS * MLA_V) @ w_out

    qn_l, qr_l = project_q(u_lat)
    qr_l = apply_rope(qr_l, cos, sin)
    kn_l, kr_l, v_l = project_kv(u_lat)
    kr_l = apply_rope(kr_l, cos, sin)
    kn_c, kr_c, v_c = project_kv(u_ctx)
    kn_all = jnp.concatenate([kn_c, kn_l], axis=2)
    kr_all = jnp.concatenate([kr_c, kr_l], axis=1)
    v_all = jnp.concatenate([v_c, v_l], axis=2)
    nb = s // Q_BLOCK
    blocks = lambda a: jnp.moveaxis(a.reshape(b, MLA_HEADS, nb, Q_BLOCK, a.shape[-1]), 2, 0)
    o = lax.map(lambda qq: attend(qq[0], qq[1], kn_all, kr_all, v_all), (blocks(qn_l), blocks(qr_l)))
    y_lat = merge(jnp.moveaxis(o, 0, 2).reshape(b, MLA_HEADS, s, MLA_V))
    if not need_ctx:
        return None, y_lat
    qn_c, qr_c = project_q(u_ctx)
    y_ctx = merge(attend(qn_c, qr_c, kn_c, kr_c, v_c))
    return y_ctx, y_lat


def short_conv(h, w, bias):
    y = lax.conv_general_dilated(h, w[:, None, :].astype(h.dtype), window_strides=(1,),
                                 padding=((CONV_WIDTH // 2, CONV_WIDTH // 2),),
                                 dimension_numbers=('NWC', 'WIO', 'NWC'), feature_group_count=h.shape[-1])
    return y + bias


def conv_mixer(u, w_in, w, bias, w_out):
    gate_b, gate_c, h = jnp.split(u @ w_in, 3, axis=-1)
    return (gate_b * short_conv(gate_c * h, w, bias)) @ w_out


def hier_moe(t, w_group, b_group, w_expert, b_expert, w1, w3, w2):
    n, d = t.shape
    g_prob = jax.nn.softmax((t @ w_group).astype(jnp.float32) + b_group.astype(jnp.float32), axis=-1)
    g_p, g_idx = lax.top_k(g_prob, 1)
    e_logits = ((t @ w_expert).astype(jnp.float32) + b_expert.astype(jnp.float32)).reshape(n, MOE_GROUPS, MOE_EXPERTS_PER_GROUP)
    e_logits = jnp.take_along_axis(e_logits, g_idx[:, :, None], axis=1)[:, 0]
    e_p, e_idx = lax.top_k(jax.nn.softmax(e_logits, axis=-1), MOE_TOPK)
    gate = g_p * e_p / jnp.sum(e_p, -1, keepdims=True)
    expert = g_idx * MOE_EXPERTS_PER_GROUP + e_idx
    nk = n * MOE_TOPK
    flat_e = expert.reshape(nk)
    flat_tok = jnp.repeat(jnp.arange(n, dtype=jnp.int32), MOE_TOPK)
    flat_w = gate.reshape(nk)
    order = jnp.argsort(flat_e)
    se, stok, sw = flat_e[order], flat_tok[order], flat_w[order]
    counts = jnp.bincount(flat_e, length=MOE_EXPERTS)
    starts = jnp.cumsum(counts) - counts
    padded = (counts + MOE_BLOCK - 1) // MOE_BLOCK * MOE_BLOCK
    pad_ends = jnp.cumsum(padded)
    pad_starts = pad_ends - padded
    dest = pad_starts[se] + jnp.arange(nk) - starts[se]
    n_blocks = -(-nk // MOE_BLOCK) + MOE_EXPERTS
    n_rows = n_blocks * MOE_BLOCK
    row_tok = jnp.full((n_rows,), n, jnp.int32).at[dest].set(stok)
    row_w = jnp.zeros((n_rows,), jnp.float32).at[dest].set(sw)
    block_e = jnp.minimum(jnp.searchsorted(pad_ends, jnp.arange(n_blocks) * MOE_BLOCK, side='right'), MOE_EXPERTS - 1)
    xs = jnp.concatenate([t, jnp.zeros((1, d), t.dtype)], axis=0)[row_tok].reshape(n_blocks, MOE_BLOCK, d)

    def expert_block(args):
        xb, e = args
        hdn = jax.nn.silu(xb @ w1[e]) * (xb @ w3[e])
        return hdn @ w2[e]

    ys = lax.map(expert_block, (xs, block_e)).reshape(n_rows, d)
    out = jax.ops.segment_sum(ys * row_w[:, None].astype(ys.dtype), row_tok, num_segments=n + 1)
    return out[:n]


def setup_inputs(seed: int = 0) -> dict:
    key = jax.random.key(seed)
    ks = jax.random.split(key, 32)
    nrm = lambda k, shape, sc: jax.random.normal(k, shape, jnp.float32) * sc
    D = D_MODEL
    base_logit = jnp.log(2.0 ** (5.0 + jnp.arange(RET_HEADS, dtype=jnp.float32)) - 1.0)
    return {
        'x': nrm(ks[0], (BATCH, SEQ, D), 1.0),
        'c': nrm(ks[1], (BATCH, D), 1.0),
        'ctx': nrm(ks[2], (BATCH, CTX_LEN, D), 1.0),
        'c_ctx': nrm(ks[3], (D,), 1.0),
        'ada_w': nrm(ks[4], (DEPTH, D, 6 * D), 0.5 * D ** -0.5),
        'ada_b': nrm(ks[5], (DEPTH, 6 * D), 0.02),
        'ln_g': 1.0 + nrm(ks[6], (DEPTH, 2, D), 0.02),
        'ln_b': nrm(ks[7], (DEPTH, 2, D), 0.02),
        'ret_w_in': nrm(ks[8], (N_RET, D, 2 * RET_QK + 2 * RET_VW), D ** -0.5),
        'ret_decay': base_logit + nrm(ks[9], (N_RET, 2, RET_HEADS), 0.1),
        'ret_gn_g': 1.0 + nrm(ks[10], (N_RET, RET_VW), 0.02),
        'ret_w_out': nrm(ks[11], (N_RET, RET_VW, D), BETA * RET_VW ** -0.5),
        'mla_w_down': nrm(ks[12], (N_MLA, D, MLA_Q_LORA + MLA_KV_LORA + MLA_ROPE), D ** -0.5),
        'mla_q_norm': 1.0 + nrm(ks[13], (N_MLA, MLA_Q_LORA), 0.02),
        'mla_kv_norm': 1.0 + nrm(ks[14], (N_MLA, MLA_KV_LORA), 0.02),
        'mla_w_uq': nrm(ks[15], (N_MLA, MLA_Q_LORA, MLA_HEADS * (MLA_NOPE + MLA_ROPE)), MLA_Q_LORA ** -0.5),
        'mla_w_ukv': nrm(ks[16], (N_MLA, MLA_KV_LORA, MLA_HEADS * (MLA_NOPE + MLA_V)), MLA_KV_LORA ** -0.5),
        'mla_w_out': nrm(ks[17], (N_MLA, MLA_HEADS * MLA_V, D), BETA * (MLA_HEADS * MLA_V) ** -0.5),
        'conv_w_in': nrm(ks[18], (N_CONV, D, 3 * D), D ** -0.5),
        'conv_w': nrm(ks[19], (N_CONV, CONV_WIDTH, D), CONV_WIDTH ** -0.5),
        'conv_b': nrm(ks[20], (N_CONV, D), 0.02),
        'conv_w_out': nrm(ks[21], (N_CONV, D, D), BETA * D ** -0.5),
        'moe_w_group': nrm(ks[22], (DEPTH, D, MOE_GROUPS), D ** -0.5),
        'moe_b_group': nrm(ks[23], (DEPTH, MOE_GROUPS), 0.01),
        'moe_w_expert': nrm(ks[24], (DEPTH, D, MOE_EXPERTS), D ** -0.5),
        'moe_b_expert': nrm(ks[25], (DEPTH, MOE_EXPERTS), 0.01),
        'moe_w1': nrm(ks[26], (DEPTH, MOE_EXPERTS, D, MOE_HIDDEN), D ** -0.5),
        'moe_w3': nrm(ks[27], (DEPTH, MOE_EXPERTS, D, MOE_HIDDEN), D ** -0.5),
        'moe_w2': nrm(ks[28], (DEPTH, MOE_EXPERTS, MOE_HIDDEN, D), BETA * MOE_HIDDEN ** -0.5),
    }


def reference(x, c, ctx, c_ctx, ada_w, ada_b, ln_g, ln_b, ret_w_in, ret_decay, ret_gn_g, ret_w_out,
              mla_w_down, mla_q_norm, mla_kv_norm, mla_w_uq, mla_w_ukv, mla_w_out,
              conv_w_in, conv_w, conv_b, conv_w_out,
              moe_w_group, moe_b_group, moe_w_expert, moe_b_expert, moe_w1, moe_w3, moe_w2):
    b, s, d = x.shape
    n_ctx = ctx.shape[1]
    h_lat, h_ctx = x, ctx
    act_lat = jax.nn.silu(c)[:, None, :]
    act_ctx = jax.nn.silu(c_ctx)[None, None, :]
    for i in range(DEPTH):
        need_ctx = i < DEPTH - 1
        kind, j = i % N_MIXERS, i // N_MIXERS
        m_lat = jnp.split(act_lat @ ada_w[i] + ada_b[i], 6, axis=-1)
        m_ctx = jnp.split(act_ctx @ ada_w[i] + ada_b[i], 6, axis=-1)
        u_lat = modulate(h_lat, m_lat[0], m_lat[1])
        u_ctx = modulate(h_ctx, m_ctx[0], m_ctx[1])
        if kind == 0:
            y_ctx, y_lat = retention_mixer(u_ctx, u_lat, ret_w_in[j], ret_decay[j], ret_gn_g[j], ret_w_out[j], need_ctx)
        elif kind == 1:
            y_ctx, y_lat = mla_mixer(u_ctx, u_lat, mla_w_down[j], mla_q_norm[j], mla_kv_norm[j],
                                     mla_w_uq[j], mla_w_ukv[j], mla_w_out[j], need_ctx)
        else:
            y_lat = conv_mixer(u_lat, conv_w_in[j], conv_w[j], conv_b[j], conv_w_out[j])
            y_ctx = conv_mixer(u_ctx, conv_w_in[j], conv_w[j], conv_b[j], conv_w_out[j]) if need_ctx else None
        h_lat = layer_norm(ALPHA * h_lat + m_lat[2] * y_lat, ln_g[i, 0], ln_b[i, 0])
        v_lat = modulate(h_lat, m_lat[3], m_lat[4]).reshape(b * s, d)
        if need_ctx:
            h_ctx = layer_norm(ALPHA * h_ctx + m_ctx[2] * y_ctx, ln_g[i, 0], ln_b[i, 0])
            v_ctx = modulate(h_ctx, m_ctx[3], m_ctx[4]).reshape(b * n_ctx, d)
            tokens = jnp.concatenate([v_ctx, v_lat], axis=0)
        else:
            tokens = v_lat
        f = hier_moe(tokens, moe_w_group[i], moe_b_group[i], moe_w_expert[i], moe_b_expert[i],
                     moe_w1[i], moe_w3[i], moe_w2[i])
        f_lat = f[f.shape[0] - b * s:].reshape(b, s, d)
        h_lat = layer_norm(ALPHA * h_lat + m_lat[5] * f_lat, ln_g[i, 1], ln_b[i, 1])
        if need_ctx:
            f_ctx = f[:b * n_ctx].reshape(b, n_ctx, d)
            h_ctx = layer_norm(ALPHA * h_ctx + m_ctx[5] * f_ctx, ln_g[i, 1], ln_b[i, 1])
    return h_lat
```

```python
import contextlib
import numpy as np
import concourse.bass as bass
import concourse.mybir as mybir
from concourse.bass_utils import run_bass_kernel_spmd

F32 = mybir.dt.float32
BF16 = mybir.dt.bfloat16
AF = mybir.ActivationFunctionType
ALU = mybir.AluOpType

EPOCH_D = 1500
EPOCH_C = 30000
PH = "__phase__"

D = 1024
NBC = 2
SEQ = 4096
CTX = 256
TT = SEQ + CTX
DEPTH = 4
ALPHA = (2.0 * DEPTH) ** 0.25
LN_EPS = 1e-5
RMS_EPS = 1e-6
NE = 32


def _key(x):
    if isinstance(x, (tuple, str)):
        return x
    t = getattr(x, "tensor", None)
    if t is not None:
        return t.name
    return x.name


class Sched:
    def __init__(self, nc):
        self.nc = nc
        self.ops = []

    def op(self, eng, fn, r=(), w=(), dma=False):
        rk = [_key(k) for k in r]
        wk = [_key(k) for k in w]
        for kk in rk:
            if isinstance(kk, str) and kk.startswith("ps") and kk not in wk:
                wk.append(kk)
        rk.append(PH)
        self.ops.append([eng, fn, tuple(rk), tuple(wk), dma])

    def dma(self, eng, out, in_, r=None, w=None, store=False, skey=None, **kw):
        r = [in_] if r is None else r
        w = [out] if w is None else w
        if skey is None:
            skey = _key(in_) if store else _key(out)
        self.op(eng, lambda e, out=out, in_=in_, kw=kw: e.dma_start(out=out, in_=in_, **kw), r, w, dma=skey)

    def barrier(self):
        self.ops.append(["sp", lambda e: e.nop(), (), (PH,), False])

    def finish(self):
        nc = self.nc
        ops = self.ops
        n = len(ops)
        last_w = {}
        rd_eng = {}
        rd_dma = {}
        deps_all = [None] * n
        for i, (eng, fn, r, w, dma) in enumerate(ops):
            deps = set()
            for k in r:
                j = last_w.get(k)
                if j is not None:
                    deps.add(j)
            for k in w:
                j = last_w.get(k)
                if j is not None:
                    deps.add(j)
                d = rd_eng.get(k)
                if d:
                    deps.update(d.values())
                d = rd_dma.get(k)
                if d:
                    deps.update(d)
            for k in r:
                if dma:
                    rd_dma.setdefault(k, []).append(i)
                else:
                    rd_eng.setdefault(k, {})[eng] = i
            for k in w:
                last_w[k] = i
                rd_eng[k] = {}
                rd_dma[k] = []
            deps.discard(i)
            deps_all[i] = deps
        need = [False] * n
        for i, (eng, fn, r, w, dma) in enumerate(ops):
            keep = set()
            for j in deps_all[i]:
                je, _, _, _, jd = ops[j]
                if jd:
                    keep.add(j)
                elif je != eng or eng != "pe":
                    keep.add(j)
            deps_all[i] = keep
            for j in keep:
                need[j] = True
        pos = [None] * n
        cnt = {}
        for i, (eng, fn, r, w, dma) in enumerate(ops):
            if dma:
                stream = ("dma", dma)
            elif need[i]:
                stream = ("eng", eng)
            else:
                continue
            c = cnt.get(stream, 0)
            pos[i] = (stream, c)
            cnt[stream] = c + 1
        phase = [0] * n
        ph = 0
        for i, o in enumerate(ops):
            if o[3] == (PH,):
                ph += 1
            phase[i] = ph
        seg = {}
        for i in range(n):
            if pos[i] is None:
                continue
            stream, c = pos[i]
            EP = EPOCH_D if stream[0] == "dma" else EPOCH_C
            sk = (stream, c // EP)
            g = seg.get(sk)
            if g is None:
                seg[sk] = [phase[i], phase[i], 1]
            else:
                g[1] = phase[i]
                g[2] += 1
        es = contextlib.ExitStack()
        hw = []
        base = {}
        semof = {}
        for sk in sorted(seg, key=lambda q: seg[q][0]):
            pf, pl, c = seg[sk]
            inc = 16 if sk[0][0] == "dma" else 1
            pick = None
            for hwi in hw:
                if hwi[2] < pf and hwi[1] + c * inc <= 30000:
                    pick = hwi
                    break
            if pick is None:
                pick = [es.enter_context(nc.semaphore(f"s{len(hw)}")), 0, -1]
                hw.append(pick)
            base[sk] = pick[1]
            semof[sk] = pick[0]
            pick[1] += c * inc
            pick[2] = pl
        self.nsem = len(hw)
        by_eng = {}
        for i, o in enumerate(ops):
            by_eng.setdefault(o[0], []).append(i)

        def emit(e, eng):
            waited = {}
            for i in by_eng.get(eng, []):
                _, fn, r, w, dma = ops[i]
                reqs = {}
                for j in deps_all[i]:
                    stream, c = pos[j]
                    isd = stream[0] == "dma"
                    EP = EPOCH_D if isd else EPOCH_C
                    sk = (stream, c // EP)
                    v = base[sk] + (c % EP + 1) * (16 if isd else 1)
                    sm = semof[sk]
                    if reqs.get(id(sm), (0, None))[0] < v:
                        reqs[id(sm)] = (v, sm)
                for sid, (v, sm) in reqs.items():
                    if waited.get(sid, 0) < v:
                        e.wait_ge(sm, v)
                        waited[sid] = v
                ins = fn(e)
                if pos[i] is not None:
                    stream, c = pos[i]
                    isd = stream[0] == "dma"
                    EP = EPOCH_D if isd else EPOCH_C
                    ins.then_inc(semof[(stream, c // EP)], 16 if isd else 1)

        with nc.Block() as block:
            @block.tensor
            def _(e): emit(e, "pe")

            @block.scalar
            def _(e): emit(e, "act")

            @block.vector
            def _(e): emit(e, "dve")

            @block.gpsimd
            def _(e): emit(e, "pool")

            @block.sync
            def _(e): emit(e, "sp")
        es.close()


class KB:
    def __init__(self, nc):
        self.nc = nc
        self.S = Sched(nc)
        self.gs = contextlib.ExitStack()

    def sb(self, es, name, shape, dt=F32):
        self.uid = getattr(self, "uid", 0) + 1
        return es.enter_context(self.nc.sbuf_tensor(f"{name}_u{self.uid}", list(shape), dt))

    def dram(self, name, shape, dt, kind):
        return self.nc.dram_tensor(name, list(shape), dt, kind=kind).ap()

    @staticmethod
    def _rw(out, ins, r, w):
        if r is None:
            r = [a for a in ins if not isinstance(a, (int, float)) and a is not None]
        if w is None:
            w = [out]
        return r, w

    def mm(self, out, lhsT, rhs, start, stop, r=None, w=None):
        r, w = self._rw(out, [lhsT, rhs], r, w)
        self.S.op("pe", lambda e: e.matmul(out, lhsT=lhsT, rhs=rhs, start=start, stop=stop), r, w)

    def tr(self, out, in_, ident, r=None, w=None):
        r, w = self._rw(out, [in_, ident], r, w)
        self.S.op("pe", lambda e: e.transpose(out, in_, ident), r, w)

    def act(self, out, in_, func, scale=None, bias=None, r=None, w=None):
        r, w = self._rw(out, [in_, scale, bias], r, w)
        kw = {}
        if scale is not None:
            kw["scale"] = scale
        if bias is not None:
            kw["bias"] = bias
        self.S.op("act", lambda e: e.activation(out=out, in_=in_, func=func, **kw), r, w)

    def tt(self, eng, out, in0, in1, op, r=None, w=None):
        r, w = self._rw(out, [in0, in1], r, w)
        self.S.op(eng, lambda e: e.tensor_tensor(out=out, in0=in0, in1=in1, op=op), r, w)

    def ts(self, eng, out, in0, s1, s2, op0, op1=None, r=None, w=None):
        r, w = self._rw(out, [in0, s1, s2], r, w)
        if op1 is None:
            self.S.op(eng, lambda e: e.tensor_scalar(out=out, in0=in0, scalar1=s1, scalar2=None, op0=op0), r, w)
        else:
            self.S.op(eng, lambda e: e.tensor_scalar(out=out, in0=in0, scalar1=s1, scalar2=s2, op0=op0, op1=op1), r, w)

    def stt(self, out, in0, scalar, in1, op0, op1, r=None, w=None):
        r, w = self._rw(out, [in0, scalar, in1], r, w)
        self.S.op("dve", lambda e: e.scalar_tensor_tensor(out=out, in0=in0, scalar=scalar, in1=in1, op0=op0, op1=op1), r, w)

    def copy(self, eng, out, in_, r=None, w=None):
        r, w = self._rw(out, [in_], r, w)
        if eng == "act":
            self.S.op("act", lambda e: e.copy(out=out, in_=in_), r, w)
        else:
            self.S.op(eng, lambda e: e.tensor_copy(out=out, in_=in_), r, w)

    def memset(self, eng, ap, val, w=None):
        self.S.op(eng, lambda e: e.memset(ap, val), (), [ap] if w is None else w)

    def fn(self, eng, f, r, w):
        self.S.op(eng, f, r, w)

    def dma(self, eng, out, in_, **kw):
        self.S.dma(eng, out, in_, **kw)


def tiles_of(b, with_ctx=True):
    t = []
    if with_ctx:
        t.append((b, 0, CTX, 2))
    for j in range(SEQ // 512):
        t.append((b, CTX + 512 * j, 512, b))
    return t


class Prog:
    def __init__(self, layers=(0, 1, 2, 3), h_from_input=True, dbg=None):
        self.layers = list(layers)
        self.dbg = dbg
        NL = len(self.layers)
        self.li = {l: j for j, l in enumerate(self.layers)}
        nc = bass.Bass("TRN2", target_bir_lowering=False)
        self.nc = nc
        k = KB(nc)
        self.k = k
        NEd = dbg[2] if isinstance(dbg, tuple) else NE
        shapes = {
            "hin": [NBC, TT, D], "cc": [3, D],
            "ada_w": [NL, D, 6 * D], "ada_b": [NL, 1, 6 * D], "ln_g": [NL, 2, D], "ln_b": [NL, 2, D],
            "ret_w_in": [2, D, 6 * D], "ret_decay": [2, 8], "ret_gn_g": [2, 2 * D], "ret_w_out": [2, 2 * D, D],
            "ret_cos": [128, SEQ], "ret_sin": [128, SEQ],
            "mla_wd": [D, 768], "mla_norms": [5, 128], "mla_wuq": [384, 2048], "mla_wukv": [256, 2048],
            "mla_w_out": [D, D], "mla_c1": [128, SEQ], "mla_c2": [128, SEQ],
            "conv_w_in": [1, D, 3 * D], "conv_wb": [1, 4, D], "conv_w_out": [1, D, D],
            "moe_wr": [NL, D, 64], "moe_bg": [NL, 4], "moe_be": [NL, 32],
            "moe_w1": [NL, NEd, D, 512], "moe_w3": [NL, NEd, D, 512], "moe_w2": [NL, NEd, 512, D],
        }

        class Lazy(dict):
            def __missing__(d, name):
                d[name] = k.dram(name, shapes[name], F32, "ExternalInput")
                return d[name]
        I = Lazy()
        self.I = I
        self.out = k.dram("out", [NBC, SEQ, D], F32, "ExternalOutput")
        self.HA = k.dram("HA", [NBC, TT, D], F32, "ExternalOutput" if dbg else "Internal")
        self.HB = k.dram("HB", [NBC, TT, D], F32, "ExternalOutput" if dbg else "Internal")
        self.SC1 = k.dram("SC1", [NBC, D, SEQ + 2], F32, "Internal")
        self.SC2 = k.dram("SC2", [NBC, D, SEQ], F32, "Internal")
        self.SC1c = k.dram("SC1c", [NBC, D, CTX + 2], F32, "Internal")
        self.SC2c = k.dram("SC2c", [NBC, D, CTX], F32, "Internal")
        if any(l % 3 == 0 for l in self.layers):
            for nm in ("QT", "KT", "QTF", "QTB"):
                setattr(self, nm, k.dram(nm, [NBC, D, TT], BF16, "Internal"))
            for nm in ("KF", "KB"):
                setattr(self, nm, k.dram(nm, [NBC, TT, D], BF16, "Internal"))
            for nm in ("RV", "RG"):
                setattr(self, nm, k.dram(nm, [NBC, TT, 2 * D], BF16, "Internal"))
            self.SBD = k.dram("SBD", [NBC, TT // 128, 128, 8 * 512], BF16, "Internal")
        if any(l % 3 == 1 for l in self.layers):
            self.QN = k.dram("QN", [NBC, 8, 128, TT], BF16, "Internal")
            self.QR = k.dram("QR", [NBC, 4, 128, TT], BF16, "Internal")
            self.KN = k.dram("KN", [NBC, 8, 128, TT], BF16, "Internal")
            self.KR = k.dram("KR", [NBC, 64, TT], BF16, "Internal")
            self.MV = k.dram("MV", [NBC, TT, D], BF16, "Internal")
            self.OT = k.dram("OT", [NBC, D, TT], BF16, "Internal")
        gs = k.gs
        self.PS = [gs.enter_context(nc.psum_tensor(f"ps{i}", [128, 512], F32)) for i in range(8)]
        self.ident = k.sb(gs, "ident", [128, 128])
        self.actT = k.sb(gs, "actT", [128, 8, 4])
        self.REP = k.sb(gs, "REP", [128, 3, 8, 128])
        self.ones = k.sb(gs, "ones", [1, 512])
        self.negh = k.sb(gs, "negh", [128, 512])
        self.onesf = k.sb(gs, "onesf", [128, 128])
        self.onesb = k.sb(gs, "onesb", [128, 128], BF16)
        self.MP = k.sb(gs, "MP", [128, 4, 8, 4])
        self.GB = k.sb(gs, "GB", [128, 3, 2, 1024])
        self.LNP = k.sb(gs, "LNP", [128, 4, 1024])
        self.setup()
        first = True
        for i in self.layers:
            src = I["hin"] if (first and h_from_input) else self.HA
            first = False
            self.mod_params(i)
            if isinstance(dbg, tuple) and dbg[0] == "moe":
                self.moe_layer(i, src, self.HA, False, dbg_ns=dbg[1], dbg_ne=dbg[2])
                break
            if dbg == "mod":
                d1 = k.dram("dbg_MP", [128, 4 * 8 * 4], F32, "ExternalOutput")
                d2 = k.dram("dbg_GB", [128, 3 * 2 * 1024], F32, "ExternalOutput")
                d3 = k.dram("dbg_LNP", [128, 4 * 1024], F32, "ExternalOutput")
                k.dma("sp", d1, self.MP[:].rearrange("p a b c -> p (a b c)"), store=True)
                k.dma("sp", d2, self.GB[:].rearrange("p a b c -> p (a b c)"), store=True)
                k.dma("sp", d3, self.LNP[:].rearrange("p a c -> p (a c)"), store=True)
                break
            kind = i % 3
            if kind == 2:
                self.conv_layer(i, src, self.HB)
            elif kind == 0:
                self.ret_layer(i, src, self.HB)
            elif kind == 1:
                self.mla_layer(i, src, self.HB)
            else:
                raise NotImplementedError
            if dbg == "mix":
                break
            last = (i == DEPTH - 1)
            self.moe_layer(i, self.HB, self.HA, last)
        import os
        mo = int(os.environ.get("MAXOPS", "0"))
        if mo:
            for o in k.S.ops[mo:mo + 3]:
                print("TRUNC next ops:", o[0], o[2][:3], o[3])
            del k.S.ops[mo:]
        k.S.barrier()
        k.S.op("sp", lambda e: e.nop(), (), ())
        k.S.finish()
        gs.close()

    def setup(self):
        k = self.k
        nc = self.nc
        ident = self.ident
        k.memset("pool", ident[:], 0.0)
        k.fn("pool", lambda e: e.affine_select(out=ident[:], in_=ident[:], pattern=[[-1, 128]], compare_op=ALU.not_equal,
                                               fill=1.0, base=0, channel_multiplier=1), [ident], [ident])
        k.memset("dve", self.ones[:], 1.0)
        k.memset("dve", self.negh[:], -0.5)
        k.memset("dve", self.onesf[:], 1.0)
        k.memset("dve", self.onesb[:], 1.0)
        with contextlib.ExitStack() as es:
            craw = k.sb(es, "craw", [4, D])
            k.memset("dve", craw[:], 0.0)
            k.dma("sp", craw[0:3, :], self.I["cc"][:, :])
            k.act(craw[:], craw[:], AF.Silu)
            ps = self.PS[0]
            for kk in range(8):
                k.tr(ps[:, kk * 4:(kk + 1) * 4], craw[0:4, kk * 128:(kk + 1) * 128], ident[0:4, 0:4])
            k.copy("dve", self.actT[:], ps[:, 0:32].rearrange("p (k f) -> p k f", f=4))
            for r in range(3):
                for kk in range(8):
                    k.copy("dve", self.REP[:, r, kk, :], self.actT[:, kk, r:r + 1].broadcast_to([128, 128]))
            k.S.barrier()

    def mod_params(self, i):
        k = self.k
        I = self.I
        i = self.li[i]
        with contextlib.ExitStack() as es:
            wblk = [k.sb(es, f"mp_w{j}", [128, 8, 1024]) for j in range(2)]
            brow = k.sb(es, "mp_brow", [1, 6 * D])
            k.dma("sp", brow[:], I["ada_b"][i])
            for j in range(4):
                k.dma("sp", self.LNP[:, j, :], (I["ln_g"] if j % 2 == 0 else I["ln_b"])[i, j // 2].partition_broadcast(128))
            pi = 0
            for wi in range(6):
                buf = wblk[wi % 2]
                k.dma("sp", buf[:], I["ada_w"][i][:, wi * 1024:(wi + 1) * 1024].rearrange("(k p) n -> p k n", p=128))
                if wi in (0, 1, 3, 4):
                    slot = {0: 0, 1: 1, 3: 2, 4: 3}[wi]
                    ps = self.PS[pi % 8]
                    pi += 1
                    for ko in range(8):
                        o = ps[:, ko * 4:ko * 4 + 3]
                        for ki in range(8):
                            k.mm(o, buf[:, ki, ko * 128:(ko + 1) * 128], self.actT[:, ki, 0:3], ki == 0, False)
                        k.mm(o, brow[0:1, wi * 1024 + ko * 128: wi * 1024 + (ko + 1) * 128], self.ones[0:1, 0:3], False, True)
                    src = ps[:, 0:32].rearrange("p (k f) -> p k f", f=4)[:, :, 0:3]
                    if wi in (1, 4):
                        k.ts("dve", self.MP[:, slot, :, 0:3], src, 1.0, None, ALU.add)
                    else:
                        k.copy("dve", self.MP[:, slot, :, 0:3], src)
                else:
                    gi = 0 if wi == 2 else 1
                    for r in range(3):
                        for half in range(2):
                            ps = self.PS[pi % 8]
                            pi += 1
                            for ki in range(8):
                                k.mm(ps[:], self.REP[:, r, ki, :], buf[:, ki, half * 512:(half + 1) * 512], ki == 0, False)
                            k.mm(ps[:], self.ones[0:1, 0:128], brow[0:1, wi * 1024 + half * 512: wi * 1024 + (half + 1) * 512], False, True)
                            k.copy("act", self.GB[:, r, gi, half * 512:(half + 1) * 512], ps[:])
            k.S.barrier()

    def prologue(self, src, b, t0, n, r, slot_sh, stg, uT, psb, v32=None):
        k = self.k
        nt = n // 128
        k.dma("sp", stg[:, 0:nt, :], src[b, t0:t0 + n, :].rearrange("(t p) d -> p t d", p=128),
              r=[(_key(src), b, t0)])
        for kk in range(8):
            ps = psb[kk % len(psb)]
            for t in range(nt):
                k.tr(ps[:, t * 128:(t + 1) * 128], stg[:, t, kk * 128:(kk + 1) * 128], self.ident[:])
            k.act(uT[:, kk, 0:n], ps[:, 0:n], AF.Identity, scale=self.MP[:, slot_sh + 1, kk, r:r + 1],
                  bias=self.MP[:, slot_sh, kk, r:r + 1])
            if v32 is not None:
                k.ts("dve", v32[:, kk, 0:n], ps[:, 0:n], self.MP[:, slot_sh + 1, kk, r:r + 1],
                     self.MP[:, slot_sh, kk, r:r + 1], ALU.mult, ALU.add)

    def ln_epilogue(self, ysrc, stg, nt, r, gi, li, zb, st, mv, rs, dst_rows, dst_key):
        k = self.k
        for t in range(nt):
            z = zb[:, t, :]
            for half in range(2):
                k.tt("dve", zb[:, t, half * 512:(half + 1) * 512], ysrc(t, half), self.GB[:, r, gi, half * 512:(half + 1) * 512], ALU.mult)
            k.stt(z, stg[:, t, :], ALPHA, z, ALU.mult, ALU.add)
            for c in range(2):
                k.fn("dve", (lambda e, t=t, c=c: e.bn_stats(out=st[:, t, c * 6:(c + 1) * 6], in_=zb[:, t, c * 512:(c + 1) * 512])), [zb], [st])
            k.fn("dve", (lambda e, t=t: e.bn_aggr(out=mv[:, t, :], in_=st[:, t, :])), [st], [mv])
        k.ts("pool", rs[:, 0:nt], mv[:, 0:nt, 1], LN_EPS, None, ALU.add)
        k.tt("pool", rs[:, 0:nt], rs[:, 0:nt], self.negh[:, 0:nt], ALU.pow)
        for t in range(nt):
            z = zb[:, t, :]
            k.ts("dve", z, z, mv[:, t, 0:1], rs[:, t:t + 1], ALU.subtract, ALU.mult)
            k.tt("pool", z, z, self.LNP[:, 2 * li, :], ALU.mult)
            k.tt("pool", z, z, self.LNP[:, 2 * li + 1, :], ALU.add)
        k.dma("sp", dst_rows.rearrange("(t p) d -> p t d", p=128), zb[:, 0:nt, :], store=True, w=[dst_key])

    def conv_layer(self, i, src, dst):
        k = self.k
        I = self.I
        PS = self.PS
        need_ctx = i < DEPTH - 1
        with contextlib.ExitStack() as es:
            win = k.sb(es, "cv_win", [128, 8, 3 * D], BF16)
            k.dma("pool", win[:], I["conv_w_in"][0].rearrange("(k p) n -> p k n", p=128))
            stg = [k.sb(es, "cv_stg0", [128, 4, D])]
            uT = [k.sb(es, f"cv_uT{j}", [128, 8, 512], BF16) for j in range(2)]
            sbuf = [k.sb(es, f"cv_s{j}", [128, 8, 512]) for j in range(2)]
            gbuf = [k.sb(es, f"cv_g{j}", [128, 8, 512]) for j in range(2)]
            gct = [k.sb(es, f"cv_gc{j}", [128, 512]) for j in range(2)]
            zero = k.sb(es, "cv_zero", [128, 8, 1])
            k.memset("dve", zero[:], 0.0)
            it = 0
            for b in range(NBC):
                for (bb, t0, n, r) in tiles_of(b, need_ctx):
                    isctx = (r == 2)
                    s1 = (self.SC1c if isctx else self.SC1)
                    s2 = (self.SC2c if isctx else self.SC2)
                    c0 = t0 if isctx else t0 - CTX
                    sg, u, sbf, gbf = stg[0], uT[it % 2], sbuf[it % 2], gbuf[it % 2]
                    self.prologue(src, b, t0, n, r, 0, sg, u, [PS[6], PS[7]])
                    for j in range(8):
                        pb, pc, ph = PS[(3 * j) % 6], PS[(3 * j + 1) % 6], PS[(3 * j + 2) % 6]
                        for (pp, col) in ((pb, j), (pc, 8 + j), (ph, 16 + j)):
                            for kk in range(8):
                                k.mm(pp[:, 0:n], win[:, kk, col * 128:(col + 1) * 128], u[:, kk, 0:n], kk == 0, kk == 7)
                        g = gct[j % 2]
                        k.copy("act", gbf[:, j, 0:n], pb[:, 0:n], w=[(_key(gbf), j)])
                        k.copy("act", g[:, 0:n], pc[:, 0:n])
                        k.tt("dve", sbf[:, j, 0:n], g[:, 0:n], ph[:, 0:n], ALU.mult, w=[(_key(sbf), j)])
                    k.dma("sp", s1[b, :, 1 + c0:1 + c0 + n].rearrange("(k p) t -> p k t", p=128), sbf[:, :, 0:n], store=True,
                          r=[(_key(sbf), j) for j in range(8)], w=[("SC1", b, isctx, c0)])
                    k.dma("sp", s2[b, :, c0:c0 + n].rearrange("(k p) t -> p k t", p=128), gbf[:, :, 0:n], store=True,
                          r=[(_key(gbf), j) for j in range(8)], w=[("SC2", b, isctx, c0)])
                    it += 1
                for (s1, L) in (((self.SC1c, CTX), (self.SC1, SEQ)) if need_ctx else ((self.SC1, SEQ),)):
                    for col in (0, L + 1):
                        k.dma("sp", s1[b, :, col:col + 1].rearrange("(k p) t -> p k t", p=128), zero[:], store=True,
                              w=[("SC1h", b, L, col)], allow_slow_non_contiguous=True)
            k.S.barrier()
        with contextlib.ExitStack() as es:
            wout = k.sb(es, "cv_wout", [128, 8, D], BF16)
            cw = k.sb(es, "cv_cw", [128, 4, 8])
            craw = k.sb(es, "cv_craw", [32, 128])
            k.dma("pool", wout[:], I["conv_w_out"][0].rearrange("(k p) n -> p k n", p=128))
            k.dma("sp", craw[:], I["conv_wb"][0].rearrange("j (k p) -> (j k) p", p=128))
            k.tr(PS[7][:, 0:32], craw[:, :], self.ident[0:32, 0:32])
            k.copy("dve", cw[:], PS[7][:, 0:32].rearrange("p (j k) -> p j k", k=8))
            stg = [k.sb(es, f"cv_stg{j}", [128, 4, D]) for j in range(2)]
            gbuf = [k.sb(es, f"cv_g{j}", [128, 8, 512]) for j in range(2)]
            zb = k.sb(es, "cv_zb", [128, 4, D])
            st = k.sb(es, "cv_st", [128, 4, 12])
            mv = k.sb(es, "cv_mv", [128, 4, 2])
            rs = k.sb(es, "cv_rs", [128, 4])
            sx = [k.sb(es, f"cv_sx{j}", [128, 8, 514]) for j in range(2)]
            gT = [k.sb(es, f"cv_gT{j}", [128, 8, 512], BF16) for j in range(2)]
            ctmp = [k.sb(es, f"cv_ct{j}", [128, 512]) for j in range(2)]
            it = 0
            for b in range(NBC):
                for (bb, t0, n, r) in tiles_of(b, need_ctx):
                    isctx = (r == 2)
                    s1 = (self.SC1c if isctx else self.SC1)
                    s2 = (self.SC2c if isctx else self.SC2)
                    c0 = t0 if isctx else t0 - CTX
                    nt = n // 128
                    sg, sxx, gbf, g = stg[it % 2], sx[it % 2], gbuf[it % 2], gT[it % 2]
                    k.dma("sp", sg[:, 0:nt, :], src[b, t0:t0 + n, :].rearrange("(t p) d -> p t d", p=128), r=[(_key(src), b, t0)])
                    k.dma("sp", sxx[:, :, 0:n + 2], s1[b, :, c0:c0 + n + 2].rearrange("(k p) t -> p k t", p=128), r=[("SC1all",)])
                    k.dma("sp", gbf[:, :, 0:n], s2[b, :, c0:c0 + n].rearrange("(k p) t -> p k t", p=128), r=[("SC2all",)])
                    for kk in range(8):
                        c = ctmp[kk % 2]
                        k.act(c[:, 0:n], sxx[:, kk, 1:n + 1], AF.Identity, scale=cw[:, 1, kk:kk + 1], bias=cw[:, 3, kk:kk + 1])
                        k.stt(c[:, 0:n], sxx[:, kk, 0:n], cw[:, 0, kk:kk + 1], c[:, 0:n], ALU.mult, ALU.add)
                        k.stt(c[:, 0:n], sxx[:, kk, 2:n + 2], cw[:, 2, kk:kk + 1], c[:, 0:n], ALU.mult, ALU.add)
                        k.tt("dve", g[:, kk, 0:n], c[:, 0:n], gbf[:, kk, 0:n], ALU.mult)
                    for t in range(nt):
                        for half in range(2):
                            ps = PS[(t * 2 + half) % 8]
                            for kk in range(8):
                                k.mm(ps[:], g[:, kk, t * 128:(t + 1) * 128], wout[:, kk, half * 512:(half + 1) * 512], kk == 0, kk == 7)
                    self.ln_epilogue(lambda t, half: PS[(t * 2 + half) % 8][:], sg, nt, r, 0, 0, zb, st, mv, rs,
                                     dst[b, t0:t0 + n, :], (_key(dst), b, t0))
                    it += 1
            k.S.barrier()

    def ret_layer(self, i, src, dst):
        k = self.k
        I = self.I
        PS = self.PS
        need_ctx = i < DEPTH - 1
        j = i // 3
        NCH = TT // 128
        with contextlib.ExitStack() as tes:
            lg = k.sb(tes, "rt_lg", [128, 8])
            GL = k.sb(tes, "rt_GL", [128, 8])
            MT = k.sb(tes, "rt_MT", [128, 4, 128])
            DF = k.sb(tes, "rt_DF", [128, 4, 128])
            DB = k.sb(tes, "rt_DB", [128, 4, 128])
            KFd = k.sb(tes, "rt_KFd", [128, 4])
            KBd = k.sb(tes, "rt_KBd", [128, 4])
            with contextlib.ExitStack() as es:
                Dm = k.sb(es, "rt_D", [128, 128])
                Dp = k.sb(es, "rt_Dp", [128, 128])
                Dn = k.sb(es, "rt_Dn", [128, 128])
                mf = k.sb(es, "rt_mf", [128, 128])
                mb = k.sb(es, "rt_mb", [128, 128])
                Ef = k.sb(es, "rt_Ef", [128, 128])
                Eb = k.sb(es, "rt_Eb", [128, 128])
                I1 = k.sb(es, "rt_I1", [128, 128])
                I2 = k.sb(es, "rt_I2", [128, 128])
                P1 = k.sb(es, "rt_P1", [128, 2])
                k.dma("sp", lg[:], I["ret_decay"][j].partition_broadcast(128))
                k.act(lg[:], lg[:], AF.Exp, scale=-1.0)
                k.act(lg[:], lg[:], AF.Ln, bias=1.0)
                k.ts("dve", lg[:], lg[:], -1.0, None, ALU.mult)
                k.act(GL[:], lg[:], AF.Exp, scale=128.0)
                k.fn("pool", lambda e: e.iota(Dm[:], pattern=[[1, 128]], base=0, channel_multiplier=-1,
                                              allow_small_or_imprecise_dtypes=True), [], [Dm])
                k.fn("pool", lambda e: e.iota(I1[:], pattern=[[1, 128]], base=1, channel_multiplier=0,
                                              allow_small_or_imprecise_dtypes=True), [], [I1])
                k.fn("pool", lambda e: e.iota(I2[:], pattern=[[-1, 128]], base=128, channel_multiplier=0,
                                              allow_small_or_imprecise_dtypes=True), [], [I2])
                k.fn("pool", lambda e: e.iota(P1[:, 0:1], pattern=[[0, 1]], base=127, channel_multiplier=-1,
                                              allow_small_or_imprecise_dtypes=True), [], [P1])
                k.fn("pool", lambda e: e.iota(P1[:, 1:2], pattern=[[0, 1]], base=0, channel_multiplier=1,
                                              allow_small_or_imprecise_dtypes=True), [P1], [P1])
                k.ts("dve", Dp[:], Dm[:], 0.0, None, ALU.max)
                k.ts("dve", Dn[:], Dm[:], -1.0, 0.0, ALU.mult, ALU.max)
                k.ts("dve", mf[:], Dm[:], 0.0, None, ALU.is_ge)
                k.ts("dve", mb[:], Dm[:], 0.0, None, ALU.is_le)
                for h in range(4):
                    k.act(Ef[:], Dp[:], AF.Exp, scale=lg[:, h:h + 1])
                    k.tt("dve", Ef[:], Ef[:], mf[:], ALU.mult)
                    k.act(Eb[:], Dn[:], AF.Exp, scale=lg[:, 4 + h:5 + h])
                    k.tt("dve", Eb[:], Eb[:], mb[:], ALU.mult)
                    k.tt("dve", Ef[:], Ef[:], Eb[:], ALU.add)
                    k.ts("dve", MT[:, h, :], Ef[:], 0.0625, None, ALU.mult)
                    k.act(DF[:, h, :], I1[:], AF.Exp, scale=lg[:, h:h + 1])
                    k.act(DB[:, h, :], I2[:], AF.Exp, scale=lg[:, 4 + h:5 + h])
                    k.act(KFd[:, h:h + 1], P1[:, 0:1], AF.Exp, scale=lg[:, h:h + 1])
                    k.act(KBd[:, h:h + 1], P1[:, 1:2], AF.Exp, scale=lg[:, 4 + h:5 + h])
                k.ts("dve", KFd[:], KFd[:], 0.0625, None, ALU.mult)
                k.ts("dve", KBd[:], KBd[:], 0.0625, None, ALU.mult)
                k.S.barrier()
            for part in range(2):
              with contextlib.ExitStack() as es:
                wqk = k.sb(es, "r1_wqk", [128, 8, D], BF16)
                k.dma("pool", wqk[:], I["ret_w_in"][j][:, part * D:(part + 1) * D].rearrange("(k p) n -> p k n", p=128))
                cosT = k.sb(es, "r1_cos", [128, SEQ])
                sinT = k.sb(es, "r1_sin", [128, SEQ])
                k.dma("sp", cosT[:], I["ret_cos"])
                k.dma("sp", sinT[:], I["ret_sin"])
                stg = k.sb(es, "r1_stg", [128, 4, D])
                uT = [k.sb(es, f"r1_uT{x}", [128, 8, 512], BF16) for x in range(2)]
                tm = [k.sb(es, f"r1_tm{x}", [128, 512]) for x in range(4)]
                if part == 0:
                    qT = k.sb(es, "r1_qT", [128, 8, 512], BF16)
                    qf = k.sb(es, "r1_qf", [128, 8, 512], BF16)
                    qb = k.sb(es, "r1_qb", [128, 8, 512], BF16)
                    q32 = [k.sb(es, f"r1_q32{x}", [128, 512]) for x in range(2)]
                else:
                    kT = k.sb(es, "r1_kT", [128, 8, 512], BF16)
                    k32 = k.sb(es, "r1_k32", [128, 8, 512])
                    kfb = k.sb(es, "r1_kf", [128, 4, D], BF16)
                    kbb = k.sb(es, "r1_kb", [128, 4, D], BF16)
                it = 0
                for b in range(NBC):
                    for (bb, t0, n, r) in tiles_of(b, True):
                        isctx = (r == 2)
                        c0 = t0 - CTX
                        nt = n // 128
                        u = uT[it % 2]
                        it += 1
                        self.prologue(src, b, t0, n, r, 0, stg, u, [PS[6], PS[7]])
                        for hh in range(part * 4, part * 4 + 4):
                            isq = hh < 4
                            h = hh % 4
                            c1, c2 = 2 * h, 2 * h + 1
                            x1, x2 = PS[(2 * hh) % 4], PS[(2 * hh + 1) % 4]
                            for kk in range(8):
                                k.mm(x1[:, 0:n], wqk[:, kk, c1 * 128:(c1 + 1) * 128], u[:, kk, 0:n], kk == 0, kk == 7)
                            for kk in range(8):
                                k.mm(x2[:, 0:n], wqk[:, kk, c2 * 128:(c2 + 1) * 128], u[:, kk, 0:n], kk == 0, kk == 7)
                            d1, d2 = 2 * h, 2 * h + 1
                            if isq:
                                o1, o2 = q32[0][:, 0:n], q32[1][:, 0:n]
                            else:
                                o1, o2 = k32[:, d1, 0:n], k32[:, d2, 0:n]
                            if isctx:
                                k.copy("act", o1, x1[:, 0:n])
                                k.copy("act", o2, x2[:, 0:n])
                            else:
                                cs, sn = cosT[:, c0:c0 + n], sinT[:, c0:c0 + n]
                                k.tt("dve", tm[0][:, 0:n], x1[:, 0:n], cs, ALU.mult)
                                k.tt("dve", tm[1][:, 0:n], x2[:, 0:n], sn, ALU.mult)
                                k.tt("dve", tm[2][:, 0:n], x1[:, 0:n], sn, ALU.mult)
                                k.tt("dve", tm[3][:, 0:n], x2[:, 0:n], cs, ALU.mult)
                                k.tt("pool", o1, tm[0][:, 0:n], tm[1][:, 0:n], ALU.subtract)
                                k.tt("pool", o2, tm[2][:, 0:n], tm[3][:, 0:n], ALU.add)
                            for (o, d) in ((o1, d1), (o2, d2)):
                                if isq:
                                    k.copy("act", qT[:, d, 0:n], o)
                                    o3 = o.rearrange("p (s i) -> p s i", i=128)
                                    k.tt("dve", qf[:, d, 0:n].rearrange("p (s i) -> p s i", i=128), o3,
                                         DF[:, h:h + 1, :].broadcast_to([128, nt, 128]), ALU.mult)
                                    k.tt("dve", qb[:, d, 0:n].rearrange("p (s i) -> p s i", i=128), o3,
                                         DB[:, h:h + 1, :].broadcast_to([128, nt, 128]), ALU.mult)
                                else:
                                    k.copy("act", kT[:, d, 0:n], o)
                        for s_ in range(nt if part == 1 else 0):
                            pa, pb = PS[4], PS[5]
                            for c in range(8):
                                pp = pa if c < 4 else pb
                                k.tr(pp[:, (c % 4) * 128:(c % 4 + 1) * 128], k32[:, c, s_ * 128:(s_ + 1) * 128], self.ident[:])
                            for h in range(4):
                                pp = pa if h < 2 else pb
                                sl = pp[:, (h % 2) * 256:(h % 2 + 1) * 256]
                                k.act(kfb[:, s_, h * 256:(h + 1) * 256], sl, AF.Identity, scale=KFd[:, h:h + 1])
                                k.ts("dve", kbb[:, s_, h * 256:(h + 1) * 256], sl, KBd[:, h:h + 1], None, ALU.mult)
                        for (buf, dr) in (((qT, self.QT), (qf, self.QTF), (qb, self.QTB)) if part == 0 else ((kT, self.KT),)):
                            k.dma("sp", dr[b, :, t0:t0 + n].rearrange("(k p) t -> p k t", p=128), buf[:, :, 0:n], store=True,
                                  w=[(_key(dr), b, t0)])
                        for (buf, dr) in (((kfb, self.KF), (kbb, self.KB)) if part == 1 else ()):
                            k.dma("sp", dr[b, t0:t0 + n, :].rearrange("(s p) d -> p s d", p=128), buf[:, 0:nt, :], store=True,
                                  w=[(_key(dr), b, t0)])
                k.S.barrier()
            with contextlib.ExitStack() as es:
                wvg = k.sb(es, "r1_wvg", [128, 8, 4 * D], BF16)
                k.dma("pool", wvg[:], I["ret_w_in"][j][:, 2 * D:6 * D].rearrange("(k p) n -> p k n", p=128))
                stg = k.sb(es, "r1b_stg", [128, 4, D])
                uT = [k.sb(es, f"r1b_uT{x}", [128, 8, 512], BF16) for x in range(2)]
                vt = [k.sb(es, "r1b_vt0", [128, 4, 2 * D], BF16)] * 2
                gt = [k.sb(es, "r1b_gt0", [128, 4, 2 * D], BF16)] * 2
                it = 0
                for b in range(NBC):
                    for (bb, t0, n, r) in tiles_of(b, True):
                        nt = n // 128
                        u, vv, gg = uT[it % 2], vt[it % 2], gt[it % 2]
                        it += 1
                        self.prologue(src, b, t0, n, r, 0, stg, u, [PS[6], PS[7]])
                        pi = 0
                        for s_ in range(nt):
                            for nb in range(8):
                                ps = PS[pi % 6]
                                pi += 1
                                for kk in range(8):
                                    k.mm(ps[:], u[:, kk, s_ * 128:(s_ + 1) * 128], wvg[:, kk, nb * 512:(nb + 1) * 512], kk == 0, kk == 7)
                                if nb < 4:
                                    k.copy("act", vv[:, s_, nb * 512:(nb + 1) * 512], ps[:])
                                else:
                                    k.act(gg[:, s_, (nb - 4) * 512:(nb - 3) * 512], ps[:], AF.Silu)
                        k.dma("sp", self.RV[b, t0:t0 + n, :].rearrange("(s p) d -> p s d", p=128), vv[:, 0:nt, :], store=True, w=[("RV", b, t0)])
                        k.dma("sp", self.RG[b, t0:t0 + n, :].rearrange("(s p) d -> p s d", p=128), gg[:, 0:nt, :], store=True, w=[("RG", b, t0)])
                k.S.barrier()
            with contextlib.ExitStack() as es:
                Sb = k.sb(es, "r2_S", [128, 8, 512])
                sbf = [k.sb(es, f"r2_sbf{x}", [128, 8 * 512], BF16) for x in range(2)]
                kbc = [k.sb(es, f"r2_kb{x}", [128, D], BF16) for x in range(2)]
                vc = [k.sb(es, f"r2_v{x}", [128, 2 * D], BF16) for x in range(2)]
                it = 0
                for b in range(NBC):
                    k.memset("dve", Sb[:], 0.0)
                    order = [1, 0] + list(range(NCH - 1, 1, -1))
                    for oi, g in enumerate(order):
                        sf, kb_, v_ = sbf[it % 2], kbc[it % 2], vc[it % 2]
                        it += 1
                        k.copy("act", sf[:], Sb[:].rearrange("p a c -> p (a c)"))
                        k.dma("sp", self.SBD[b, g], sf[:], store=True, w=[("SBD", b, g)])
                        if oi == len(order) - 1:
                            break
                        k.dma("sp", kb_[:], self.KB[b, g * 128:(g + 1) * 128, :], r=[("KBall",)])
                        k.dma("sp", v_[:], self.RV[b, g * 128:(g + 1) * 128, :], r=[("RVall",)])
                        for h in range(4):
                            for a in range(2):
                                ps = PS[(h * 2 + a) % 8]
                                k.mm(ps[:], kb_[:, h * 256 + a * 128:h * 256 + (a + 1) * 128], v_[:, h * 512:(h + 1) * 512], True, True)
                                k.stt(Sb[:, h * 2 + a, :], Sb[:, h * 2 + a, :], GL[:, 4 + h:5 + h], ps[:], ALU.mult, ALU.add)
                k.S.barrier()
            with contextlib.ExitStack() as es:
                wout = k.sb(es, "r3_wout", [128, 16, D], BF16)
                k.dma("pool", wout[:], I["ret_w_out"][j].rearrange("(k p) n -> p k n", p=128))
                gng = k.sb(es, "r3_gng", [128, 2 * D])
                k.dma("sp", gng[:], I["ret_gn_g"][j].partition_broadcast(128))
                Sf = k.sb(es, "r3_Sf", [128, 8, 512])
                Sfb = k.sb(es, "r3_Sfb", [128, 8, 512], BF16)
                L = []
                for x in range(2):
                    L.append(dict(
                        qT=k.sb(es, f"r3_qT{x}", [128, 8, 128], BF16), kT=k.sb(es, f"r3_kT{x}", [128, 8, 128], BF16),
                        qf=k.sb(es, f"r3_qf{x}", [128, 8, 128], BF16), qb=k.sb(es, f"r3_qb{x}", [128, 8, 128], BF16),
                        kf=k.sb(es, f"r3_kf{x}", [128, D], BF16), v=k.sb(es, f"r3_v{x}", [128, 2 * D], BF16),
                        g=(k.sb(es, f"r3_g{x}", [128, 2 * D], BF16) if x == 0 else None),
                        sb=(k.sb(es, f"r3_sb{x}", [128, 8, 512], BF16) if x == 0 else None),
                        h=k.sb(es, f"r3_h{x}", [128, 1, D])))
                L[1]["sb"] = L[0]["sb"]
                L[1]["g"] = L[0]["g"]
                z32 = k.sb(es, "r3_z32", [128, 2 * D])
                zT = k.sb(es, "r3_zT", [128, 16, 128], BF16)
                on = [k.sb(es, f"r3_on{x}", [128, 512]) for x in range(2)]
                Pm = [k.sb(es, f"r3_P{x}", [128, 128], BF16) for x in range(2)]
                gst = k.sb(es, "r3_gst", [128, 4, 12])
                gmv = k.sb(es, "r3_gmv", [128, 4, 2])
                grs = k.sb(es, "r3_grs", [128, 4])
                zb = k.sb(es, "r3_zb", [128, 1, D])
                st = k.sb(es, "r3_st", [128, 1, 12])
                mv = k.sb(es, "r3_mv", [128, 1, 2])
                rs = k.sb(es, "r3_rs", [128, 1])
                it = 0
                for b in range(NBC):
                    k.memset("dve", Sf[:], 0.0)
                    k.memset("pool", Sfb[:], 0.0)
                    for g in range(NCH):
                        B_ = L[it % 2]
                        it += 1
                        isctx = g < 2
                        want_out = (not isctx) or need_ctx
                        r = 2 if isctx else b
                        cs = slice(g * 128, (g + 1) * 128)
                        k.dma("sp", B_["kf"][:], self.KF[b, cs, :], r=[("KFall",)])
                        k.dma("sp", B_["v"][:], self.RV[b, cs, :], r=[("RVall",)])
                        if want_out:
                            for nm, dr in (("qT", self.QT), ("kT", self.KT), ("qf", self.QTF), ("qb", self.QTB)):
                                k.dma("sp", B_[nm][:], dr[b, :, cs].rearrange("(k p) t -> p k t", p=128), r=[(nm + "all",)])
                            k.dma("sp", B_["g"][:], self.RG[b, cs, :], r=[("RGall",)])
                            k.dma("sp", B_["sb"][:].rearrange("p a c -> p (a c)"), self.SBD[b, g], r=[("SBDall",)])
                            k.dma("sp", B_["h"][:, 0, :], src[b, cs, :], r=[(_key(src), b, (g * 128 // 512) * 512 if not isctx else 0)])
                        for h in range(4):
                            vh = B_["v"][:, h * 512:(h + 1) * 512]
                            if want_out:
                                sc = PS[0]
                                for a in range(2):
                                    k.mm(sc[:, 0:128], B_["kT"][:, 2 * h + a, :], B_["qT"][:, 2 * h + a, :], a == 0, a == 1)
                            for a in range(2):
                                k.mm(PS[3 + a][:], B_["kf"][:, h * 256 + a * 128:h * 256 + (a + 1) * 128], vh, True, True)
                            if want_out:
                                P_ = Pm[h % 2]
                                k.tt("dve", P_[:], sc[:, 0:128], MT[:, h, :], ALU.mult)
                                O = PS[1 + h % 2]
                                k.mm(O[:], P_[:], vh, True, False)
                                for a in range(2):
                                    k.mm(O[:], B_["qf"][:, 2 * h + a, :], Sfb[:, 2 * h + a, :], False, False)
                                for a in range(2):
                                    k.mm(O[:], B_["qb"][:, 2 * h + a, :], B_["sb"][:, 2 * h + a, :], False, a == 1)
                            for a in range(2):
                                ps = PS[3 + a]
                                k.stt(Sf[:, 2 * h + a, :], Sf[:, 2 * h + a, :], GL[:, h:h + 1], ps[:], ALU.mult, ALU.add)
                                k.copy("act", Sfb[:, 2 * h + a, :], Sf[:, 2 * h + a, :])
                            if want_out:
                                k.fn("dve", (lambda e, h=h, O=O: e.bn_stats(out=gst[:, h, 0:6], in_=O[:])), [O], [gst])
                                k.fn("dve", (lambda e, h=h: e.bn_aggr(out=gmv[:, h, :], in_=gst[:, h, 0:6])), [gst], [gmv])
                                k.ts("pool", grs[:, h:h + 1], gmv[:, h, 1:2], LN_EPS, None, ALU.add)
                                k.tt("pool", grs[:, h:h + 1], grs[:, h:h + 1], self.negh[:, 0:1], ALU.pow)
                                o_ = on[h % 2]
                                k.ts("dve", o_[:], O[:], gmv[:, h, 0:1], grs[:, h:h + 1], ALU.subtract, ALU.mult)
                                k.tt("pool", o_[:], o_[:], gng[:, h * 512:(h + 1) * 512], ALU.mult)
                                k.tt("pool", z32[:, h * 512:(h + 1) * 512], o_[:], B_["g"][:, h * 512:(h + 1) * 512], ALU.mult,
                                     w=[(_key(z32), h)])
                        if want_out:
                            for c in range(16):
                                pp = PS[5]
                                k.tr(pp[:, (c % 4) * 128:(c % 4 + 1) * 128], z32[:, c * 128:(c + 1) * 128], self.ident[:],
                                     r=[(_key(z32), c // 4), self.ident])
                                if c % 4 == 3:
                                    k.copy("act", zT[:, c - 3:c + 1, :], pp[:].rearrange("p (c i) -> p c i", i=128))
                            for half in range(2):
                                y = PS[6 + half]
                                for c in range(16):
                                    k.mm(y[:], zT[:, c, :], wout[:, c, half * 512:(half + 1) * 512], c == 0, c == 15)
                            t0 = 0 if isctx else (g * 128 // 512) * 512
                            self.ln_epilogue(lambda t, half: PS[6 + half][:], B_["h"], 1, r, 0, 0, zb, st, mv, rs,
                                             dst[b, cs, :], (_key(dst), b, t0, g))
                k.S.barrier()

    def mla_layer(self, i, src, dst):
        k = self.k
        I = self.I
        PS = self.PS
        need_ctx = i < DEPTH - 1
        NKT = TT // 128
        with contextlib.ExitStack() as es:
            wd = k.sb(es, "m1_wd", [128, 8, 768], BF16)
            wuq = k.sb(es, "m1_wuq", [128, 3, 2048], BF16)
            wukv = k.sb(es, "m1_wukv", [128, 2, 2048], BF16)
            k.dma("pool", wd[:], I["mla_wd"].rearrange("(k p) n -> p k n", p=128))
            k.dma("pool", wuq[:], I["mla_wuq"].rearrange("(k p) n -> p k n", p=128))
            k.dma("pool", wukv[:], I["mla_wukv"].rearrange("(k p) n -> p k n", p=128))
            C1 = k.sb(es, "m1_c1", [128, SEQ])
            C2 = k.sb(es, "m1_c2", [128, SEQ])
            k.dma("sp", C1[:], I["mla_c1"])
            k.dma("sp", C2[:], I["mla_c2"])
            nraw = k.sb(es, "m1_nraw", [5, 128])
            npp = k.sb(es, "m1_npp", [128, 5])
            k.dma("sp", nraw[:], I["mla_norms"])
            k.tr(PS[7][:, 0:5], nraw[:, :], self.ident[0:5, 0:5])
            k.copy("dve", npp[:], PS[7][:, 0:5])
            stg = k.sb(es, "m1_stg", [128, 4, D])
            u = k.sb(es, "m1_uT", [128, 8, 512], BF16)
            d32 = k.sb(es, "m1_d32", [128, 5, 512])
            sq = [k.sb(es, f"m1_sq{x}", [128, 512]) for x in range(2)]
            rr = k.sb(es, "m1_rr", [128, 2, 512])
            dn = k.sb(es, "m1_dn", [128, 5, 512], BF16)
            qnb = k.sb(es, "m1_qnb", [128, 8, 512], BF16)
            qrb = k.sb(es, "m1_qrb", [128, 4, 512], BF16)
            knb = k.sb(es, "m1_knb", [128, 8, 512], BF16)
            vb = k.sb(es, "m1_vb", [128, 4, D], BF16)
            krb = k.sb(es, "m1_krb", [64, 512], BF16)
            tm = [k.sb(es, f"m1_tm{x}", [128, 512]) for x in range(2)]
            for b in range(NBC):
                for (bb, t0, n, r) in tiles_of(b, True):
                    isctx = (r == 2)
                    c0 = t0 - CTX
                    nt = n // 128
                    self.prologue(src, b, t0, n, r, 0, stg, u, [PS[6], PS[7]])
                    for c in range(5):
                        ps = PS[c]
                        for kk in range(8):
                            k.mm(ps[:, 0:n], wd[:, kk, c * 128:(c + 1) * 128], u[:, kk, 0:n], kk == 0, kk == 7)
                        k.copy("act", d32[:, c, 0:n], ps[:, 0:n])
                    for (x, ps) in ((0, PS[5]), (1, PS[6])):
                        for kk in range(8):
                            k.mm(ps[0:64, 0:n], wd[:, kk, 640 + 64 * x:704 + 64 * x], u[:, kk, 0:n], kk == 0, kk == 7)
                    if isctx:
                        k.copy("act", krb[:, 0:n], PS[5][0:64, 0:n])
                    else:
                        k.tt("dve", tm[0][0:64, 0:n], PS[5][0:64, 0:n], C1[0:64, c0:c0 + n], ALU.mult)
                        k.tt("dve", tm[1][0:64, 0:n], PS[6][0:64, 0:n], C2[0:64, c0:c0 + n], ALU.mult)
                        k.tt("pool", krb[:, 0:n], tm[0][0:64, 0:n], tm[1][0:64, 0:n], ALU.add)
                    k.dma("sp", self.KR[b, :, t0:t0 + n], krb[:, 0:n], store=True, w=[("KR", b, t0)])
                    for (gi_, cl, dim) in ((0, (0, 1, 2), 384.0), (1, (3, 4), 256.0)):
                        ps = PS[7]
                        for ci, c in enumerate(cl):
                            sqq = sq[ci % 2]
                            k.tt("dve", sqq[:, 0:n], d32[:, c, 0:n], d32[:, c, 0:n], ALU.mult)
                            k.mm(ps[:, 0:n], self.onesf[:], sqq[:, 0:n], ci == 0, ci == len(cl) - 1)
                        k.ts("dve", rr[:, gi_, 0:n], ps[:, 0:n], 1.0 / dim, RMS_EPS, ALU.mult, ALU.add)
                        k.tt("pool", rr[:, gi_, 0:n], rr[:, gi_, 0:n], self.negh[:, 0:n], ALU.pow)
                        for c in cl:
                            k.stt(dn[:, c, 0:n], d32[:, c, 0:n], npp[:, c:c + 1], rr[:, gi_, 0:n], ALU.mult, ALU.mult)
                    for h in range(8):
                        ps = PS[h % 4]
                        for c in range(3):
                            k.mm(ps[:, 0:n], wuq[:, c, h * 128:(h + 1) * 128], dn[:, c, 0:n], c == 0, c == 2)
                        k.copy("act", qnb[:, h, 0:n], ps[:, 0:n])
                    k.dma("sp", self.QN[b, :, :, t0:t0 + n].rearrange("h p t -> p h t"), qnb[:, :, 0:n], store=True, w=[("QN", b, t0)])
                    for hp in range(4):
                        pa, pb = PS[4 + (2 * hp) % 2], PS[4 + (2 * hp + 1) % 2]
                        for c in range(3):
                            k.mm(pa[:, 0:n], wuq[:, c, 1024 + hp * 128:1024 + (hp + 1) * 128], dn[:, c, 0:n], c == 0, c == 2)
                        if isctx:
                            k.copy("act", qrb[:, hp, 0:n], pa[:, 0:n])
                        else:
                            for c in range(3):
                                k.mm(pb[:, 0:n], wuq[:, c, 1536 + hp * 128:1536 + (hp + 1) * 128], dn[:, c, 0:n], c == 0, c == 2)
                            k.tt("dve", tm[0][:, 0:n], pa[:, 0:n], C1[:, c0:c0 + n], ALU.mult)
                            k.tt("dve", tm[1][:, 0:n], pb[:, 0:n], C2[:, c0:c0 + n], ALU.mult)
                            k.tt("pool", qrb[:, hp, 0:n], tm[0][:, 0:n], tm[1][:, 0:n], ALU.add)
                    k.dma("sp", self.QR[b, :, :, t0:t0 + n].rearrange("h p t -> p h t"), qrb[:, :, 0:n], store=True, w=[("QR", b, t0)])
                    for h in range(8):
                        ps = PS[h % 4]
                        for c in range(2):
                            k.mm(ps[:, 0:n], wukv[:, c, h * 128:(h + 1) * 128], dn[:, 3 + c, 0:n], c == 0, c == 1)
                        k.copy("act", knb[:, h, 0:n], ps[:, 0:n])
                    k.dma("sp", self.KN[b, :, :, t0:t0 + n].rearrange("h p t -> p h t"), knb[:, :, 0:n], store=True, w=[("KN", b, t0)])
                    for s_ in range(nt):
                        for half in range(2):
                            ps = PS[4 + half]
                            for c in range(2):
                                k.mm(ps[:], dn[:, 3 + c, s_ * 128:(s_ + 1) * 128], wukv[:, c, 1024 + half * 512:1024 + (half + 1) * 512], c == 0, c == 1)
                            k.copy("act", vb[:, s_, half * 512:(half + 1) * 512], ps[:])
                    k.dma("sp", self.MV[b, t0:t0 + n, :].rearrange("(s p) d -> p s d", p=128), vb[:, 0:nt, :], store=True, w=[("MV", b, t0)])
            k.S.barrier()
        with contextlib.ExitStack() as es:
            kra = k.sb(es, "m2_kr", [64, TT], BF16)
            knh = [k.sb(es, f"m2_kn{x}", [128, TT], BF16) for x in range(2)]
            vh = [k.sb(es, f"m2_v{x}", [128, NKT, 128], BF16) for x in range(2)]
            qn = [k.sb(es, f"m2_qn{x}", [128, 512], BF16) for x in range(2)]
            qr = [k.sb(es, f"m2_qr{x}", [64, 512], BF16) for x in range(2)]
            PT = [k.sb(es, f"m2_PT{x}", [128, 512], BF16) for x in range(3)]
            rec = [k.sb(es, f"m2_rec{x}", [128, 512]) for x in range(2)]
            otb = [k.sb(es, f"m2_ot{x}", [128, 512], BF16) for x in range(2)]
            scale = float(192.0 ** -0.5)
            it = 0
            ih = 0
            for b in range(NBC):
                k.dma("sp", kra[:], self.KR[b], r=[("KRall",)])
                for h in range(8):
                    kn_, v_ = knh[ih % 2], vh[ih % 2]
                    ih += 1
                    k.dma("sp", kn_[:], self.KN[b, h], r=[("KNall",)])
                    k.dma("sp", v_[:], self.MV[b, :, h * 128:(h + 1) * 128].rearrange("(t p) d -> p t d", p=128), r=[("MVall",)])
                    for (bb, t0, n, r) in tiles_of(b, need_ctx):
                        isctx = (r == 2)
                        kts = [0, 1] if isctx else list(range(NKT))
                        q_, r_ = qn[it % 2], qr[it % 2]
                        O, DEN = PS[4 + it % 2], PS[6 + it % 2]
                        rc, ot = rec[it % 2], otb[it % 2]
                        it += 1
                        k.dma("sp", q_[:, 0:n], self.QN[b, h, :, t0:t0 + n], r=[("QNall",)])
                        k.dma("sp", r_[:, 0:n], self.QR[b, h // 2, (h % 2) * 64:(h % 2) * 64 + 64, t0:t0 + n], r=[("QRall",)])

                        def scores(idx):
                            kt = kts[idx]
                            sc = PS[idx % 4]
                            k.mm(sc[:, 0:n], kn_[:, kt * 128:(kt + 1) * 128], q_[:, 0:n], True, False)
                            k.mm(sc[:, 0:n], kra[:, kt * 128:(kt + 1) * 128], r_[:, 0:n], False, True)
                        scores(0)
                        for idx, kt in enumerate(kts):
                            if idx + 1 < len(kts):
                                scores(idx + 1)
                            p_ = PT[idx % 3]
                            k.act(p_[:, 0:n], PS[idx % 4][:, 0:n], AF.Exp, scale=scale)
                            k.mm(O[:, 0:n], v_[:, kt, :], p_[:, 0:n], idx == 0, idx == len(kts) - 1)
                            k.mm(DEN[:, 0:n], self.onesb[:], p_[:, 0:n], idx == 0, idx == len(kts) - 1)
                        k.fn("dve", (lambda e, rc=rc, DEN=DEN, n=n: e.reciprocal(out=rc[:, 0:n], in_=DEN[:, 0:n])), [DEN], [rc])
                        k.tt("dve", ot[:, 0:n], O[:, 0:n], rc[:, 0:n], ALU.mult)
                        k.dma("sp", self.OT[b, h * 128:(h + 1) * 128, t0:t0 + n], ot[:, 0:n], store=True, w=[("OT", b, h, t0)])
            k.S.barrier()
        with contextlib.ExitStack() as es:
            wout = k.sb(es, "m3_wout", [128, 8, D], BF16)
            k.dma("pool", wout[:], I["mla_w_out"].rearrange("(k p) n -> p k n", p=128))
            stg = [k.sb(es, f"m3_stg{x}", [128, 4, D]) for x in range(2)]
            oT = [k.sb(es, f"m3_oT{x}", [128, 8, 512], BF16) for x in range(2)]
            zb = k.sb(es, "m3_zb", [128, 4, D])
            st = k.sb(es, "m3_st", [128, 4, 12])
            mv = k.sb(es, "m3_mv", [128, 4, 2])
            rs = k.sb(es, "m3_rs", [128, 4])
            it = 0
            for b in range(NBC):
                for (bb, t0, n, r) in tiles_of(b, need_ctx):
                    nt = n // 128
                    sg, o_ = stg[it % 2], oT[it % 2]
                    it += 1
                    k.dma("sp", sg[:, 0:nt, :], src[b, t0:t0 + n, :].rearrange("(t p) d -> p t d", p=128), r=[(_key(src), b, t0)])
                    k.dma("sp", o_[:, :, 0:n], self.OT[b, :, t0:t0 + n].rearrange("(k p) t -> p k t", p=128), r=[("OTall",)])
                    for t in range(nt):
                        for half in range(2):
                            ps = PS[(t * 2 + half) % 8]
                            for kk in range(8):
                                k.mm(ps[:], o_[:, kk, t * 128:(t + 1) * 128], wout[:, kk, half * 512:(half + 1) * 512], kk == 0, kk == 7)
                    self.ln_epilogue(lambda t, half: PS[(t * 2 + half) % 8][:], sg, nt, r, 0, 0, zb, st, mv, rs,
                                     dst[b, t0:t0 + n, :], (_key(dst), b, t0))
            k.S.barrier()

    def moe_layer(self, i, src, dst, last, dbg_ns=None, dbg_ne=NE):
        k = self.k
        I = self.I
        PS = self.PS
        need_ctx = not last
        i = self.li[i]
        alltiles = []
        for b in range(NBC):
            alltiles += tiles_of(b, need_ctx)
        NS = 2
        with contextlib.ExitStack() as es:
            wr = k.sb(es, "mo_wr", [128, 8, 64])
            rbg = k.sb(es, "mo_rbg", [128, 4])
            rbe = k.sb(es, "mo_rbe", [128, 32])
            k.dma("sp", wr[:], I["moe_wr"][i].rearrange("(k p) n -> p k n", p=128))
            k.dma("sp", rbg[:], I["moe_bg"][i].partition_broadcast(128))
            k.dma("sp", rbe[:], I["moe_be"][i].partition_broadcast(128))
            vT = k.sb(es, "mo_vT", [128, 8, NS * 512], BF16)
            acc = [[k.sb(es, f"mo_acc{s}_{h}", [128, 512]) for h in range(2)] for s in range(NS * 4)]
            G = k.sb(es, "mo_G", [128, NS * 4, 32])
            stg = k.sb(es, "mo_stg", [128, 4, D])
            w1b = [k.sb(es, f"mo_w1_{j}", [128, 8, 512], BF16) for j in range(2)]
            w3b = [k.sb(es, f"mo_w3_{j}", [128, 8, 512], BF16) for j in range(2)]
            w2b = [k.sb(es, f"mo_w2_{j}", [128, 4, D], BF16) for j in range(2)]
            sil = [k.sb(es, f"mo_sil{j}", [128, 512]) for j in range(2)]
            hdn = [k.sb(es, f"mo_hdn{j}", [128, 4, 512], BF16) for j in range(2)]
            rt = k.sb(es, "mo_rt", [128, 64])
            r8 = k.sb(es, "mo_r8", [128, 8])
            rsm = k.sb(es, "mo_rsm", [128, 16])
            zb = k.sb(es, "mo_zb", [128, 4, D])
            v32 = zb[:].rearrange("p a (b c) -> p (a b) c", c=512)
            st = k.sb(es, "mo_st", [128, 4, 12])
            mv = k.sb(es, "mo_mv", [128, 4, 2])
            rs = k.sb(es, "mo_rs", [128, 4])
            wi = 0
            for s0 in range(0, len(alltiles) if dbg_ns is None else dbg_ns * NS, NS):
                tl = alltiles[s0:s0 + NS]
                subs = []
                for ti, (b, t0, n, r) in enumerate(tl):
                    self.prologue(src, b, t0, n, r, 2, stg, vT[:, :, ti * 512:(ti + 1) * 512], [PS[6], PS[7]], v32=v32)
                    for t in range(n // 128):
                        si = ti * 4 + t
                        subs.append((ti, t, ti * 512 + t * 128, si))
                        lg = PS[5]
                        for kk in range(8):
                            k.mm(lg[:, 0:64], v32[:, kk, t * 128:(t + 1) * 128], wr[:, kk, :], kk == 0, kk == 7)
                        self.route(lg, rbg, rbe, rt, r8, rsm, G[:, si, :])
                for e in range(dbg_ne):
                    wb1, wb3, wb2 = w1b[wi % 2], w3b[wi % 2], w2b[wi % 2]
                    wi += 1
                    k.dma("pool", wb1[:], I["moe_w1"][i, e].rearrange("(k p) n -> p k n", p=128))
                    k.dma("pool", wb3[:], I["moe_w3"][i, e].rearrange("(k p) n -> p k n", p=128))
                    k.dma("pool", wb2[:], I["moe_w2"][i, e].rearrange("(k p) n -> p k n", p=128))
                    for ti, (b, t0, n, r) in enumerate(tl):
                        hb = hdn[ti % 2]
                        cs = ti * 512
                        for j in range(4):
                            pa, pb = PS[(2 * j) % 4], PS[(2 * j + 1) % 4]
                            for kk in range(8):
                                k.mm(pa[:, 0:n], wb1[:, kk, j * 128:(j + 1) * 128], vT[:, kk, cs:cs + n], kk == 0, kk == 7)
                            for kk in range(8):
                                k.mm(pb[:, 0:n], wb3[:, kk, j * 128:(j + 1) * 128], vT[:, kk, cs:cs + n], kk == 0, kk == 7)
                            sl = sil[j % 2]
                            k.act(sl[:, 0:n], pa[:, 0:n], AF.Silu)
                            k.tt("dve", hb[:, j, 0:n], sl[:, 0:n], pb[:, 0:n], ALU.mult, w=[(_key(hb), j)])
                        for t in range(n // 128):
                            si = ti * 4 + t
                            for half in range(2):
                                po = PS[4 + (t * 2 + half) % 2]
                                for j in range(4):
                                    k.mm(po[:], hb[:, j, t * 128:(t + 1) * 128], wb2[:, j, half * 512:(half + 1) * 512], j == 0, j == 3,
                                         r=[(_key(hb), j), wb2])
                                a = acc[si][half][:]
                                if e == 0:
                                    k.ts("dve", a, po[:], G[:, si, e:e + 1], None, ALU.mult)
                                else:
                                    k.stt(a, po[:], G[:, si, e:e + 1], a, ALU.mult, ALU.add)
                for ti, (b, t0, n, r) in enumerate(tl):
                    nt = n // 128
                    k.dma("sp", stg[:, 0:nt, :], src[b, t0:t0 + n, :].rearrange("(t p) d -> p t d", p=128), r=[(_key(src), b, t0)])
                    if last:
                        drows = self.out[b, t0 - CTX:t0 - CTX + n, :]
                        dkey = ("out", b, t0)
                    else:
                        drows = dst[b, t0:t0 + n, :]
                        dkey = (_key(dst), b, t0)
                    self.ln_epilogue(lambda t, half, ti=ti: acc[ti * 4 + t][half][:], stg, nt, r, 1, 1,
                                     zb, st, mv, rs, drows, dkey)
            k.S.barrier()

    def route(self, lg, rbg, rbe, rt, r8, rsm, Gout):
        k = self.k
        k.tt("dve", rt[:, 0:4], lg[:, 0:4], rbg[:], ALU.add)
        k.tt("dve", rt[:, 8:40], lg[:, 4:36], rbe[:], ALU.add)
        k.fn("dve", lambda e: e.tensor_reduce(out=rsm[:, 0:1], in_=rt[:, 0:4], axis=mybir.AxisListType.X, op=ALU.max), [rt], [rsm])
        k.ts("dve", rt[:, 4:8], rt[:, 0:4], rsm[:, 0:1], None, ALU.subtract)
        k.act(rt[:, 40:44], rt[:, 4:8], AF.Exp)
        k.fn("dve", lambda e: e.tensor_reduce(out=rsm[:, 1:2], in_=rt[:, 40:44], axis=mybir.AxisListType.X, op=ALU.add), [rt], [rsm])
        k.fn("dve", lambda e: e.reciprocal(out=rsm[:, 2:3], in_=rsm[:, 1:2]), [rsm], [rsm])
        k.ts("dve", rt[:, 44:48], rt[:, 4:8], 0.0, -1e30, ALU.is_lt, ALU.mult)
        k.tt("dve", rt[:, 8:40].rearrange("p (g e) -> p g e", e=8), rt[:, 8:40].rearrange("p (g e) -> p g e", e=8),
             rt[:, 44:48].unsqueeze(2).broadcast_to([128, 4, 8]), ALU.add)
        k.fn("dve", lambda e: e.max(out=r8[:], in_=rt[:, 8:40]), [rt], [r8])
        k.tt("dve", rsm[:, 3:4], r8[:, 1:2], r8[:, 0:1], ALU.subtract)
        k.act(rsm[:, 4:5], rsm[:, 3:4], AF.Exp)
        k.ts("dve", rsm[:, 5:6], rsm[:, 4:5], 1.0, None, ALU.add)
        k.fn("dve", lambda e: e.reciprocal(out=rsm[:, 6:7], in_=rsm[:, 5:6]), [rsm], [rsm])
        k.tt("dve", rsm[:, 7:8], rsm[:, 6:7], rsm[:, 2:3], ALU.mult)
        k.tt("dve", rsm[:, 8:9], rsm[:, 7:8], rsm[:, 4:5], ALU.mult)
        k.ts("dve", Gout, rt[:, 8:40], r8[:, 0:1], rsm[:, 7:8], ALU.is_equal, ALU.mult, r=[rt, r8, rsm], w=[Gout])
        k.ts("dve", rt[:, 8:40], rt[:, 8:40], r8[:, 1:2], rsm[:, 8:9], ALU.is_equal, ALU.mult)
        k.tt("dve", Gout, Gout, rt[:, 8:40], ALU.add, r=[Gout, rt], w=[Gout])


_CACHE = {}


def _host_inputs(inputs, core):
    b0 = core * NBC
    f = np.float32
    hin = np.concatenate([inputs["ctx"][b0:b0 + NBC], inputs["x"][b0:b0 + NBC]], axis=1)
    cc = np.concatenate([inputs["c"][b0:b0 + NBC], inputs["c_ctx"][None, :]], axis=0)
    return {"hin": np.ascontiguousarray(hin, dtype=f), "cc": np.ascontiguousarray(cc, dtype=f)}


def _shared_inputs(inputs):
    f = np.float32
    wr = np.zeros((DEPTH, D, 64), f)
    wr[:, :, 0:4] = inputs["moe_w_group"]
    wr[:, :, 4:36] = inputs["moe_w_expert"]
    conv_wb = np.concatenate([inputs["conv_w"], inputs["conv_b"][:, None, :]], axis=1)
    rows = np.repeat(np.arange(SEQ // 64, dtype=f), 64)
    cols = np.tile(np.arange(64, dtype=f), SEQ // 64)

    def rope_tabs(dim):
        q = dim // 4
        inv = (np.float32(10000.0) ** (-np.arange(q, dtype=f) / np.float32(q))).astype(f)
        ang = np.concatenate([rows[:, None] * inv[None, :], cols[:, None] * inv[None, :]], axis=-1).astype(f)
        return np.ascontiguousarray(np.cos(ang).T.astype(f)), np.ascontiguousarray(np.sin(ang).T.astype(f))
    rc, rs_ = rope_tabs(256)
    mc, ms = rope_tabs(64)
    c1 = np.ascontiguousarray(np.concatenate([mc, mc, mc, mc], axis=0))
    c2 = np.ascontiguousarray(np.concatenate([-ms, ms, -ms, ms], axis=0))
    wdn = inputs["mla_w_down"][0]
    kr = wdn[:, 640:704]
    mla_wd = np.ascontiguousarray(np.concatenate([wdn[:, 0:640], kr, kr[:, 32:64], kr[:, 0:32]], axis=1))
    wuq = inputs["mla_w_uq"][0].reshape(384, 8, 192)
    rp = wuq[:, :, 128:192]
    mla_wuq = np.ascontiguousarray(np.concatenate([
        wuq[:, :, 0:128].reshape(384, 1024), rp.reshape(384, 512),
        np.concatenate([rp[:, :, 32:64], rp[:, :, 0:32]], axis=2).reshape(384, 512)], axis=1))
    wukv = inputs["mla_w_ukv"][0].reshape(256, 8, 256)
    mla_wukv = np.ascontiguousarray(np.concatenate([wukv[:, :, 0:128].reshape(256, 1024), wukv[:, :, 128:256].reshape(256, 1024)], axis=1))
    norms = np.ascontiguousarray(np.concatenate([inputs["mla_q_norm"][0].reshape(3, 128), inputs["mla_kv_norm"][0].reshape(2, 128)], axis=0))
    return {
        "mla_wd": mla_wd, "mla_norms": norms, "mla_wuq": mla_wuq, "mla_wukv": mla_wukv,
        "mla_w_out": np.ascontiguousarray(inputs["mla_w_out"][0]), "mla_c1": c1, "mla_c2": c2,
        "ret_w_in": inputs["ret_w_in"], "ret_decay": np.ascontiguousarray(inputs["ret_decay"].reshape(2, 8)),
        "ret_gn_g": inputs["ret_gn_g"], "ret_w_out": inputs["ret_w_out"], "ret_cos": rc, "ret_sin": rs_,
        "ada_w": inputs["ada_w"], "ada_b": np.ascontiguousarray(inputs["ada_b"][:, None, :]),
        "ln_g": inputs["ln_g"], "ln_b": inputs["ln_b"],
        "conv_w_in": inputs["conv_w_in"], "conv_wb": np.ascontiguousarray(conv_wb), "conv_w_out": inputs["conv_w_out"],
        "moe_wr": wr, "moe_bg": inputs["moe_b_group"], "moe_be": inputs["moe_b_expert"],
        "moe_w1": inputs["moe_w1"], "moe_w3": inputs["moe_w3"], "moe_w2": inputs["moe_w2"],
    }


def kernel(**inputs):
    inputs = {k_: np.asarray(v) for k_, v in inputs.items()}
    if "prog" not in _CACHE:
        _CACHE["prog"] = Prog()
    prog = _CACHE["prog"]
    shared = _shared_inputs(inputs)
    in_maps = []
    for core in range(8):
        m = dict(shared)
        m.update(_host_inputs(inputs, core))
        in_maps.append({k_: np.ascontiguousarray(v, dtype=np.float32) for k_, v in m.items() if k_ in prog.I})
    res = run_bass_kernel_spmd(prog.nc, in_maps, core_ids=list(range(8)))
    return np.concatenate([r["out"] for r in res.results], axis=0).astype(np.float32)
```

```python
import contextlib
import numpy as np
import concourse.bass as bass
import concourse.mybir as mybir
from concourse.bass_utils import run_bass_kernel_spmd

F32 = mybir.dt.float32
BF16 = mybir.dt.bfloat16
AF = mybir.ActivationFunctionType
ALU = mybir.AluOpType

EPOCH_D = 1500
EPOCH_C = 30000
PH = "__phase__"

D = 1024
NBC = 2
SEQ = 4096
CTX = 256
TT = SEQ + CTX
DEPTH = 4
ALPHA = (2.0 * DEPTH) ** 0.25
LN_EPS = 1e-5
RMS_EPS = 1e-6
NE = 32


def _key(x):
    if isinstance(x, (tuple, str)):
        return x
    t = getattr(x, "tensor", None)
    if t is not None:
        return t.name
    return x.name


class Sched:
    def __init__(self, nc):
        self.nc = nc
        self.ops = []

    def op(self, eng, fn, r=(), w=(), dma=False):
        rk = [_key(k) for k in r]
        wk = [_key(k) for k in w]
        for kk in rk:
            if isinstance(kk, str) and kk.startswith("ps") and kk not in wk:
                wk.append(kk)
        rk.append(PH)
        self.ops.append([eng, fn, tuple(rk), tuple(wk), dma])

    def dma(self, eng, out, in_, r=None, w=None, store=False, skey=None, **kw):
        r = [in_] if r is None else r
        w = [out] if w is None else w
        if skey is None:
            skey = _key(in_) if store else _key(out)
        self.op(eng, lambda e, out=out, in_=in_, kw=kw: e.dma_start(out=out, in_=in_, **kw), r, w, dma=skey)

    def barrier(self):
        self.ops.append(["sp", lambda e: e.nop(), (), (PH,), False])

    def finish(self):
        nc = self.nc
        ops = self.ops
        n = len(ops)
        last_w = {}
        rd_eng = {}
        rd_dma = {}
        deps_all = [None] * n
        for i, (eng, fn, r, w, dma) in enumerate(ops):
            deps = set()
            for k in r:
                j = last_w.get(k)
                if j is not None:
                    deps.add(j)
            for k in w:
                j = last_w.get(k)
                if j is not None:
                    deps.add(j)
                d = rd_eng.get(k)
                if d:
                    deps.update(d.values())
                d = rd_dma.get(k)
                if d:
                    deps.update(d)
            for k in r:
                if dma:
                    rd_dma.setdefault(k, []).append(i)
                else:
                    rd_eng.setdefault(k, {})[eng] = i
            for k in w:
                last_w[k] = i
                rd_eng[k] = {}
                rd_dma[k] = []
            deps.discard(i)
            deps_all[i] = deps
        need = [False] * n
        for i, (eng, fn, r, w, dma) in enumerate(ops):
            keep = set()
            for j in deps_all[i]:
                je, _, _, _, jd = ops[j]
                if jd:
                    keep.add(j)
                elif je != eng or eng != "pe":
                    keep.add(j)
            deps_all[i] = keep
            for j in keep:
                need[j] = True
        pos = [None] * n
        cnt = {}
        for i, (eng, fn, r, w, dma) in enumerate(ops):
            if dma:
                stream = ("dma", dma)
            elif need[i]:
                stream = ("eng", eng)
            else:
                continue
            c = cnt.get(stream, 0)
            pos[i] = (stream, c)
            cnt[stream] = c + 1
        phase = [0] * n
        ph = 0
        for i, o in enumerate(ops):
            if o[3] == (PH,):
                ph += 1
            phase[i] = ph
        seg = {}
        for i in range(n):
            if pos[i] is None:
                continue
            stream, c = pos[i]
            EP = EPOCH_D if stream[0] == "dma" else EPOCH_C
            sk = (stream, c // EP)
            g = seg.get(sk)
            if g is None:
                seg[sk] = [phase[i], phase[i], 1]
            else:
                g[1] = phase[i]
                g[2] += 1
        es = contextlib.ExitStack()
        hw = []
        base = {}
        semof = {}
        for sk in sorted(seg, key=lambda q: seg[q][0]):
            pf, pl, c = seg[sk]
            inc = 16 if sk[0][0] == "dma" else 1
            pick = None
            for hwi in hw:
                if hwi[2] < pf and hwi[1] + c * inc <= 30000:
                    pick = hwi
                    break
            if pick is None:
                pick = [es.enter_context(nc.semaphore(f"s{len(hw)}")), 0, -1]
                hw.append(pick)
            base[sk] = pick[1]
            semof[sk] = pick[0]
            pick[1] += c * inc
            pick[2] = pl
        self.nsem = len(hw)
        by_eng = {}
        for i, o in enumerate(ops):
            by_eng.setdefault(o[0], []).append(i)

        def emit(e, eng):
            waited = {}
            for i in by_eng.get(eng, []):
                _, fn, r, w, dma = ops[i]
                reqs = {}
                for j in deps_all[i]:
                    stream, c = pos[j]
                    isd = stream[0] == "dma"
                    EP = EPOCH_D if isd else EPOCH_C
                    sk = (stream, c // EP)
                    v = base[sk] + (c % EP + 1) * (16 if isd else 1)
                    sm = semof[sk]
                    if reqs.get(id(sm), (0, None))[0] < v:
                        reqs[id(sm)] = (v, sm)
                for sid, (v, sm) in reqs.items():
                    if waited.get(sid, 0) < v:
                        e.wait_ge(sm, v)
                        waited[sid] = v
                ins = fn(e)
                if pos[i] is not None:
                    stream, c = pos[i]
                    isd = stream[0] == "dma"
                    EP = EPOCH_D if isd else EPOCH_C
                    ins.then_inc(semof[(stream, c // EP)], 16 if isd else 1)

        with nc.Block() as block:
            @block.tensor
            def _(e): emit(e, "pe")

            @block.scalar
            def _(e): emit(e, "act")

            @block.vector
            def _(e): emit(e, "dve")

            @block.gpsimd
            def _(e): emit(e, "pool")

            @block.sync
            def _(e): emit(e, "sp")
        es.close()


class KB:
    def __init__(self, nc):
        self.nc = nc
        self.S = Sched(nc)
        self.gs = contextlib.ExitStack()

    def sb(self, es, name, shape, dt=F32):
        self.uid = getattr(self, "uid", 0) + 1
        return es.enter_context(self.nc.sbuf_tensor(f"{name}_u{self.uid}", list(shape), dt))

    def dram(self, name, shape, dt, kind):
        return self.nc.dram_tensor(name, list(shape), dt, kind=kind).ap()

    @staticmethod
    def _rw(out, ins, r, w):
        if r is None:
            r = [a for a in ins if not isinstance(a, (int, float)) and a is not None]
        if w is None:
            w = [out]
        return r, w

    def mm(self, out, lhsT, rhs, start, stop, r=None, w=None):
        r, w = self._rw(out, [lhsT, rhs], r, w)
        self.S.op("pe", lambda e: e.matmul(out, lhsT=lhsT, rhs=rhs, start=start, stop=stop), r, w)

    def tr(self, out, in_, ident, r=None, w=None):
        r, w = self._rw(out, [in_, ident], r, w)
        self.S.op("pe", lambda e: e.transpose(out, in_, ident), r, w)

    def act(self, out, in_, func, scale=None, bias=None, r=None, w=None):
        r, w = self._rw(out, [in_, scale, bias], r, w)
        kw = {}
        if scale is not None:
            kw["scale"] = scale
        if bias is not None:
            kw["bias"] = bias
        self.S.op("act", lambda e: e.activation(out=out, in_=in_, func=func, **kw), r, w)

    def tt(self, eng, out, in0, in1, op, r=None, w=None):
        r, w = self._rw(out, [in0, in1], r, w)
        self.S.op(eng, lambda e: e.tensor_tensor(out=out, in0=in0, in1=in1, op=op), r, w)

    def ts(self, eng, out, in0, s1, s2, op0, op1=None, r=None, w=None):
        r, w = self._rw(out, [in0, s1, s2], r, w)
        if op1 is None:
            self.S.op(eng, lambda e: e.tensor_scalar(out=out, in0=in0, scalar1=s1, scalar2=None, op0=op0), r, w)
        else:
            self.S.op(eng, lambda e: e.tensor_scalar(out=out, in0=in0, scalar1=s1, scalar2=s2, op0=op0, op1=op1), r, w)

    def stt(self, out, in0, scalar, in1, op0, op1, r=None, w=None):
        r, w = self._rw(out, [in0, scalar, in1], r, w)
        self.S.op("dve", lambda e: e.scalar_tensor_tensor(out=out, in0=in0, scalar=scalar, in1=in1, op0=op0, op1=op1), r, w)

    def copy(self, eng, out, in_, r=None, w=None):
        r, w = self._rw(out, [in_], r, w)
        if eng == "act":
            self.S.op("act", lambda e: e.copy(out=out, in_=in_), r, w)
        else:
            self.S.op(eng, lambda e: e.tensor_copy(out=out, in_=in_), r, w)

    def memset(self, eng, ap, val, w=None):
        self.S.op(eng, lambda e: e.memset(ap, val), (), [ap] if w is None else w)

    def fn(self, eng, f, r, w):
        self.S.op(eng, f, r, w)

    def dma(self, eng, out, in_, **kw):
        self.S.dma(eng, out, in_, **kw)


def tiles_of(b, with_ctx=True):
    t = []
    if with_ctx:
        t.append((b, 0, CTX, 2))
    for j in range(SEQ // 512):
        t.append((b, CTX + 512 * j, 512, b))
    return t


class Prog:
    def __init__(self, layers=(0, 1, 2, 3), h_from_input=True, dbg=None):
        self.layers = list(layers)
        self.dbg = dbg
        NL = len(self.layers)
        self.li = {l: j for j, l in enumerate(self.layers)}
        nc = bass.Bass("TRN2", target_bir_lowering=False)
        self.nc = nc
        k = KB(nc)
        self.k = k
        NEd = dbg[2] if isinstance(dbg, tuple) else NE
        shapes = {
            "hin": [NBC, TT, D], "cc": [3, D],
            "ada_w": [NL, D, 6 * D], "ada_b": [NL, 1, 6 * D], "ln_g": [NL, 2, D], "ln_b": [NL, 2, D],
            "ret_w_in": [2, D, 6 * D], "ret_decay": [2, 8], "ret_gn_g": [2, 2 * D], "ret_w_out": [2, 2 * D, D],
            "ret_cos": [128, SEQ], "ret_sin": [128, SEQ],
            "mla_wd": [D, 768], "mla_norms": [5, 128], "mla_wuq": [384, 2048], "mla_wukv": [256, 2048],
            "mla_w_out": [D, D], "mla_c1": [128, SEQ], "mla_c2": [128, SEQ],
            "conv_w_in": [1, D, 3 * D], "conv_wb": [1, 4, D], "conv_w_out": [1, D, D],
            "moe_wr": [NL, D, 64], "moe_bg": [NL, 4], "moe_be": [NL, 32],
            "moe_w1": [NL, NE, 128, 8, 512], "moe_w3": [NL, NE, 128, 8, 512], "moe_w2": [NL, NE, 128, 4, D],
        }

        class Lazy(dict):
            def __missing__(d, name):
                d[name] = k.dram(name, shapes[name], F32, "ExternalInput")
                return d[name]
        I = Lazy()
        self.I = I
        self.out = k.dram("out", [NBC, SEQ, D], F32, "ExternalOutput")
        self.HA = k.dram("HA", [NBC, TT, D], F32, "ExternalOutput" if dbg else "Internal")
        self.HB = k.dram("HB", [NBC, TT, D], F32, "ExternalOutput" if dbg else "Internal")
        self.SC1 = k.dram("SC1", [NBC, D, SEQ + 2], F32, "Internal")
        self.SC2 = k.dram("SC2", [NBC, D, SEQ], F32, "Internal")
        self.SC1c = k.dram("SC1c", [NBC, D, CTX + 2], F32, "Internal")
        self.SC2c = k.dram("SC2c", [NBC, D, CTX], F32, "Internal")
        if any(l % 3 == 0 for l in self.layers):
            for nm in ("QT", "KT", "QTF", "QTB"):
                setattr(self, nm, k.dram(nm, [NBC, D, TT], BF16, "Internal"))
            for nm in ("KF", "KB"):
                setattr(self, nm, k.dram(nm, [NBC, TT, D], BF16, "Internal"))
            for nm in ("RV", "RG"):
                setattr(self, nm, k.dram(nm, [NBC, TT, 2 * D], BF16, "Internal"))
            self.SBD = k.dram("SBD", [NBC, TT // 128, 128, 8 * 512], BF16, "Internal")
        if any(l % 3 == 1 for l in self.layers):
            self.QN = k.dram("QN", [NBC, 8, 128, TT], BF16, "Internal")
            self.QR = k.dram("QR", [NBC, 4, 128, TT], BF16, "Internal")
            self.KN = k.dram("KN", [NBC, 8, 128, TT], BF16, "Internal")
            self.KR = k.dram("KR", [NBC, 64, TT], BF16, "Internal")
            self.MV = k.dram("MV", [NBC, TT, D], BF16, "Internal")
            self.OT = k.dram("OT", [NBC, D, TT], BF16, "Internal")
        NROWS = ((2 * (TT // 128) * NBC * 128 + 511) // 512 + NE) * 512
        self.XS = k.dram("XS", [NROWS, D], F32, "Internal")
        self.YS = k.dram("YS", [NROWS, D], F32, "Internal")
        gs = k.gs
        self.PS = [gs.enter_context(nc.psum_tensor(f"ps{i}", [128, 512], F32)) for i in range(8)]
        self.ident = k.sb(gs, "ident", [128, 128])
        self.actT = k.sb(gs, "actT", [128, 8, 4])
        self.ones = k.sb(gs, "ones", [1, 512])
        self.negh = k.sb(gs, "negh", [128, 512])
        self.onesf = k.sb(gs, "onesf", [128, 128])
        self.onesb = k.sb(gs, "onesb", [128, 128], BF16)
        self.MP = k.sb(gs, "MP", [128, 4, 8, 4])
        self.GB = k.sb(gs, "GB", [128, 3, 2, 1024])
        self.LNP = k.sb(gs, "LNP", [128, 4, 1024])
        self.setup()
        first = True
        for i in self.layers:
            src = I["hin"] if (first and h_from_input) else self.HA
            first = False
            self.mod_params(i)
            if isinstance(dbg, tuple) and dbg[0] == "moe":
                self.moe_layer(i, src, self.HA, False, dbg_ns=dbg[1], dbg_ne=dbg[2])
                break
            if dbg == "mod":
                d1 = k.dram("dbg_MP", [128, 4 * 8 * 4], F32, "ExternalOutput")
                d2 = k.dram("dbg_GB", [128, 3 * 2 * 1024], F32, "ExternalOutput")
                d3 = k.dram("dbg_LNP", [128, 4 * 1024], F32, "ExternalOutput")
                k.dma("sp", d1, self.MP[:].rearrange("p a b c -> p (a b c)"), store=True)
                k.dma("sp", d2, self.GB[:].rearrange("p a b c -> p (a b c)"), store=True)
                k.dma("sp", d3, self.LNP[:].rearrange("p a c -> p (a c)"), store=True)
                break
            kind = i % 3
            if kind == 2:
                self.conv_layer(i, src, self.HB)
            elif kind == 0:
                self.ret_layer(i, src, self.HB)
            elif kind == 1:
                self.mla_layer(i, src, self.HB)
            else:
                raise NotImplementedError
            if dbg == "mix":
                break
            last = (i == DEPTH - 1)
            self.moe_layer(i, self.HB, self.HA, last)
        import os
        mo = int(os.environ.get("MAXOPS", "0"))
        if mo:
            for o in k.S.ops[mo:mo + 3]:
                print("TRUNC next ops:", o[0], o[2][:3], o[3])
            del k.S.ops[mo:]
        k.S.barrier()
        k.S.op("sp", lambda e: e.nop(), (), ())
        k.S.finish()
        gs.close()

    def setup(self):
        k = self.k
        nc = self.nc
        ident = self.ident
        k.memset("pool", ident[:], 0.0)
        k.fn("pool", lambda e: e.affine_select(out=ident[:], in_=ident[:], pattern=[[-1, 128]], compare_op=ALU.not_equal,
                                               fill=1.0, base=0, channel_multiplier=1), [ident], [ident])
        k.memset("dve", self.ones[:], 1.0)
        k.memset("dve", self.negh[:], -0.5)
        k.memset("dve", self.onesf[:], 1.0)
        k.memset("dve", self.onesb[:], 1.0)
        with contextlib.ExitStack() as es:
            craw = k.sb(es, "craw", [4, D])
            k.memset("dve", craw[:], 0.0)
            k.dma("sp", craw[0:3, :], self.I["cc"][:, :])
            k.act(craw[:], craw[:], AF.Silu)
            ps = self.PS[0]
            for kk in range(8):
                k.tr(ps[:, kk * 4:(kk + 1) * 4], craw[0:4, kk * 128:(kk + 1) * 128], ident[0:4, 0:4])
            k.copy("dve", self.actT[:], ps[:, 0:32].rearrange("p (k f) -> p k f", f=4))
            k.S.barrier()

    def mod_params(self, i):
        k = self.k
        I = self.I
        i = self.li[i]
        with contextlib.ExitStack() as es:
            wblk = [k.sb(es, f"mp_w{j}", [128, 8, 1024]) for j in range(2)]
            brow = k.sb(es, "mp_brow", [1, 6 * D])
            REP = k.sb(es, "mp_REP", [128, 3, 8, 128])
            for r in range(3):
                for kk in range(8):
                    k.copy("dve", REP[:, r, kk, :], self.actT[:, kk, r:r + 1].broadcast_to([128, 128]))
            k.dma("sp", brow[:], I["ada_b"][i])
            for j in range(4):
                k.dma("sp", self.LNP[:, j, :], (I["ln_g"] if j % 2 == 0 else I["ln_b"])[i, j // 2].partition_broadcast(128))
            pi = 0
            for wi in range(6):
                buf = wblk[wi % 2]
                k.dma("sp", buf[:], I["ada_w"][i][:, wi * 1024:(wi + 1) * 1024].rearrange("(k p) n -> p k n", p=128))
                if wi in (0, 1, 3, 4):
                    slot = {0: 0, 1: 1, 3: 2, 4: 3}[wi]
                    ps = self.PS[pi % 8]
                    pi += 1
                    for ko in range(8):
                        o = ps[:, ko * 4:ko * 4 + 3]
                        for ki in range(8):
                            k.mm(o, buf[:, ki, ko * 128:(ko + 1) * 128], self.actT[:, ki, 0:3], ki == 0, False)
                        k.mm(o, brow[0:1, wi * 1024 + ko * 128: wi * 1024 + (ko + 1) * 128], self.ones[0:1, 0:3], False, True)
                    src = ps[:, 0:32].rearrange("p (k f) -> p k f", f=4)[:, :, 0:3]
                    if wi in (1, 4):
                        k.ts("dve", self.MP[:, slot, :, 0:3], src, 1.0, None, ALU.add)
                    else:
                        k.copy("dve", self.MP[:, slot, :, 0:3], src)
                else:
                    gi = 0 if wi == 2 else 1
                    for r in range(3):
                        for half in range(2):
                            ps = self.PS[pi % 8]
                            pi += 1
                            for ki in range(8):
                                k.mm(ps[:], REP[:, r, ki, :], buf[:, ki, half * 512:(half + 1) * 512], ki == 0, False)
                            k.mm(ps[:], self.ones[0:1, 0:128], brow[0:1, wi * 1024 + half * 512: wi * 1024 + (half + 1) * 512], False, True)
                            k.copy("act", self.GB[:, r, gi, half * 512:(half + 1) * 512], ps[:])
            k.S.barrier()

    def prologue(self, src, b, t0, n, r, slot_sh, stg, uT, psb, v32=None):
        k = self.k
        nt = n // 128
        k.dma("sp", stg[:, 0:nt, :], src[b, t0:t0 + n, :].rearrange("(t p) d -> p t d", p=128),
              r=[(_key(src), b, t0)])
        for kk in range(8):
            ps = psb[kk % len(psb)]
            for t in range(nt):
                k.tr(ps[:, t * 128:(t + 1) * 128], stg[:, t, kk * 128:(kk + 1) * 128], self.ident[:])
            k.act(uT[:, kk, 0:n], ps[:, 0:n], AF.Identity, scale=self.MP[:, slot_sh + 1, kk, r:r + 1],
                  bias=self.MP[:, slot_sh, kk, r:r + 1])
            if v32 is not None:
                k.ts("dve", v32[:, kk, 0:n], ps[:, 0:n], self.MP[:, slot_sh + 1, kk, r:r + 1],
                     self.MP[:, slot_sh, kk, r:r + 1], ALU.mult, ALU.add)

    def ln_epilogue(self, ysrc, stg, nt, r, gi, li, zb, st, mv, rs, dst_rows, dst_key):
        k = self.k
        for t in range(nt):
            z = zb[:, t, :]
            for half in range(2):
                k.tt("dve", zb[:, t, half * 512:(half + 1) * 512], ysrc(t, half), self.GB[:, r, gi, half * 512:(half + 1) * 512], ALU.mult)
            k.stt(z, stg[:, t, :], ALPHA, z, ALU.mult, ALU.add)
            for c in range(2):
                k.fn("dve", (lambda e, t=t, c=c: e.bn_stats(out=st[:, t, c * 6:(c + 1) * 6], in_=zb[:, t, c * 512:(c + 1) * 512])), [zb], [st])
            k.fn("dve", (lambda e, t=t: e.bn_aggr(out=mv[:, t, :], in_=st[:, t, :])), [st], [mv])
        k.ts("pool", rs[:, 0:nt], mv[:, 0:nt, 1], LN_EPS, None, ALU.add)
        k.tt("pool", rs[:, 0:nt], rs[:, 0:nt], self.negh[:, 0:nt], ALU.pow)
        for t in range(nt):
            z = zb[:, t, :]
            k.ts("dve", z, z, mv[:, t, 0:1], rs[:, t:t + 1], ALU.subtract, ALU.mult)
            k.tt("pool", z, z, self.LNP[:, 2 * li, :], ALU.mult)
            k.tt("pool", z, z, self.LNP[:, 2 * li + 1, :], ALU.add)
        k.dma("sp", dst_rows.rearrange("(t p) d -> p t d", p=128), zb[:, 0:nt, :], store=True, w=[dst_key])

    def conv_layer(self, i, src, dst):
        k = self.k
        I = self.I
        PS = self.PS
        need_ctx = i < DEPTH - 1
        with contextlib.ExitStack() as es:
            win = k.sb(es, "cv_win", [128, 8, 3 * D], BF16)
            k.dma("pool", win[:], I["conv_w_in"][0].rearrange("(k p) n -> p k n", p=128))
            stg = [k.sb(es, "cv_stg0", [128, 4, D])]
            uT = [k.sb(es, f"cv_uT{j}", [128, 8, 512], BF16) for j in range(2)]
            sbuf = [k.sb(es, f"cv_s{j}", [128, 8, 512]) for j in range(2)]
            gbuf = [k.sb(es, f"cv_g{j}", [128, 8, 512]) for j in range(2)]
            gct = [k.sb(es, f"cv_gc{j}", [128, 512]) for j in range(2)]
            zero = k.sb(es, "cv_zero", [128, 8, 1])
            k.memset("dve", zero[:], 0.0)
            it = 0
            for b in range(NBC):
                for (bb, t0, n, r) in tiles_of(b, need_ctx):
                    isctx = (r == 2)
                    s1 = (self.SC1c if isctx else self.SC1)
                    s2 = (self.SC2c if isctx else self.SC2)
                    c0 = t0 if isctx else t0 - CTX
                    sg, u, sbf, gbf = stg[0], uT[it % 2], sbuf[it % 2], gbuf[it % 2]
                    self.prologue(src, b, t0, n, r, 0, sg, u, [PS[6], PS[7]])
                    for j in range(8):
                        pb, pc, ph = PS[(3 * j) % 6], PS[(3 * j + 1) % 6], PS[(3 * j + 2) % 6]
                        for (pp, col) in ((pb, j), (pc, 8 + j), (ph, 16 + j)):
                            for kk in range(8):
                                k.mm(pp[:, 0:n], win[:, kk, col * 128:(col + 1) * 128], u[:, kk, 0:n], kk == 0, kk == 7)
                        g = gct[j % 2]
                        k.copy("act", gbf[:, j, 0:n], pb[:, 0:n], w=[(_key(gbf), j)])
                        k.copy("act", g[:, 0:n], pc[:, 0:n])
                        k.tt("dve", sbf[:, j, 0:n], g[:, 0:n], ph[:, 0:n], ALU.mult, w=[(_key(sbf), j)])
                    k.dma("sp", s1[b, :, 1 + c0:1 + c0 + n].rearrange("(k p) t -> p k t", p=128), sbf[:, :, 0:n], store=True,
                          r=[(_key(sbf), j) for j in range(8)], w=[("SC1", b, isctx, c0)])
                    k.dma("sp", s2[b, :, c0:c0 + n].rearrange("(k p) t -> p k t", p=128), gbf[:, :, 0:n], store=True,
                          r=[(_key(gbf), j) for j in range(8)], w=[("SC2", b, isctx, c0)])
                    it += 1
                for (s1, L) in (((self.SC1c, CTX), (self.SC1, SEQ)) if need_ctx else ((self.SC1, SEQ),)):
                    for col in (0, L + 1):
                        k.dma("sp", s1[b, :, col:col + 1].rearrange("(k p) t -> p k t", p=128), zero[:], store=True,
                              w=[("SC1h", b, L, col)], allow_slow_non_contiguous=True)
            k.S.barrier()
        with contextlib.ExitStack() as es:
            wout = k.sb(es, "cv_wout", [128, 8, D], BF16)
            cw = k.sb(es, "cv_cw", [128, 4, 8])
            craw = k.sb(es, "cv_craw", [32, 128])
            k.dma("pool", wout[:], I["conv_w_out"][0].rearrange("(k p) n -> p k n", p=128))
            k.dma("sp", craw[:], I["conv_wb"][0].rearrange("j (k p) -> (j k) p", p=128))
            k.tr(PS[7][:, 0:32], craw[:, :], self.ident[0:32, 0:32])
            k.copy("dve", cw[:], PS[7][:, 0:32].rearrange("p (j k) -> p j k", k=8))
            stg = [k.sb(es, f"cv_stg{j}", [128, 4, D]) for j in range(2)]
            gbuf = [k.sb(es, f"cv_g{j}", [128, 8, 512]) for j in range(2)]
            zb = k.sb(es, "cv_zb", [128, 4, D])
            st = k.sb(es, "cv_st", [128, 4, 12])
            mv = k.sb(es, "cv_mv", [128, 4, 2])
            rs = k.sb(es, "cv_rs", [128, 4])
            sx = [k.sb(es, f"cv_sx{j}", [128, 8, 514]) for j in range(2)]
            gT = [k.sb(es, f"cv_gT{j}", [128, 8, 512], BF16) for j in range(2)]
            ctmp = [k.sb(es, f"cv_ct{j}", [128, 512]) for j in range(2)]
            it = 0
            for b in range(NBC):
                for (bb, t0, n, r) in tiles_of(b, need_ctx):
                    isctx = (r == 2)
                    s1 = (self.SC1c if isctx else self.SC1)
                    s2 = (self.SC2c if isctx else self.SC2)
                    c0 = t0 if isctx else t0 - CTX
                    nt = n // 128
                    sg, sxx, gbf, g = stg[it % 2], sx[it % 2], gbuf[it % 2], gT[it % 2]
                    k.dma("sp", sg[:, 0:nt, :], src[b, t0:t0 + n, :].rearrange("(t p) d -> p t d", p=128), r=[(_key(src), b, t0)])
                    k.dma("sp", sxx[:, :, 0:n + 2], s1[b, :, c0:c0 + n + 2].rearrange("(k p) t -> p k t", p=128), r=[("SC1all",)])
                    k.dma("sp", gbf[:, :, 0:n], s2[b, :, c0:c0 + n].rearrange("(k p) t -> p k t", p=128), r=[("SC2all",)])
                    for kk in range(8):
                        c = ctmp[kk % 2]
                        k.act(c[:, 0:n], sxx[:, kk, 1:n + 1], AF.Identity, scale=cw[:, 1, kk:kk + 1], bias=cw[:, 3, kk:kk + 1])
                        k.stt(c[:, 0:n], sxx[:, kk, 0:n], cw[:, 0, kk:kk + 1], c[:, 0:n], ALU.mult, ALU.add)
                        k.stt(c[:, 0:n], sxx[:, kk, 2:n + 2], cw[:, 2, kk:kk + 1], c[:, 0:n], ALU.mult, ALU.add)
                        k.tt("dve", g[:, kk, 0:n], c[:, 0:n], gbf[:, kk, 0:n], ALU.mult)
                    for t in range(nt):
                        for half in range(2):
                            ps = PS[(t * 2 + half) % 8]
                            for kk in range(8):
                                k.mm(ps[:], g[:, kk, t * 128:(t + 1) * 128], wout[:, kk, half * 512:(half + 1) * 512], kk == 0, kk == 7)
                    self.ln_epilogue(lambda t, half: PS[(t * 2 + half) % 8][:], sg, nt, r, 0, 0, zb, st, mv, rs,
                                     dst[b, t0:t0 + n, :], (_key(dst), b, t0))
                    it += 1
            k.S.barrier()

    def ret_layer(self, i, src, dst):
        k = self.k
        I = self.I
        PS = self.PS
        need_ctx = i < DEPTH - 1
        j = i // 3
        NCH = TT // 128
        with contextlib.ExitStack() as tes:
            lg = k.sb(tes, "rt_lg", [128, 8])
            GL = k.sb(tes, "rt_GL", [128, 8])
            MT = k.sb(tes, "rt_MT", [128, 4, 128])
            DF = k.sb(tes, "rt_DF", [128, 4, 128])
            DB = k.sb(tes, "rt_DB", [128, 4, 128])
            KFd = k.sb(tes, "rt_KFd", [128, 4])
            KBd = k.sb(tes, "rt_KBd", [128, 4])
            with contextlib.ExitStack() as es:
                Dm = k.sb(es, "rt_D", [128, 128])
                Dp = k.sb(es, "rt_Dp", [128, 128])
                Dn = k.sb(es, "rt_Dn", [128, 128])
                mf = k.sb(es, "rt_mf", [128, 128])
                mb = k.sb(es, "rt_mb", [128, 128])
                Ef = k.sb(es, "rt_Ef", [128, 128])
                Eb = k.sb(es, "rt_Eb", [128, 128])
                I1 = k.sb(es, "rt_I1", [128, 128])
                I2 = k.sb(es, "rt_I2", [128, 128])
                P1 = k.sb(es, "rt_P1", [128, 2])
                k.dma("sp", lg[:], I["ret_decay"][j].partition_broadcast(128))
                k.act(lg[:], lg[:], AF.Exp, scale=-1.0)
                k.act(lg[:], lg[:], AF.Ln, bias=1.0)
                k.ts("dve", lg[:], lg[:], -1.0, None, ALU.mult)
                k.act(GL[:], lg[:], AF.Exp, scale=128.0)
                k.fn("pool", lambda e: e.iota(Dm[:], pattern=[[1, 128]], base=0, channel_multiplier=-1,
                                              allow_small_or_imprecise_dtypes=True), [], [Dm])
                k.fn("pool", lambda e: e.iota(I1[:], pattern=[[1, 128]], base=1, channel_multiplier=0,
                                              allow_small_or_imprecise_dtypes=True), [], [I1])
                k.fn("pool", lambda e: e.iota(I2[:], pattern=[[-1, 128]], base=128, channel_multiplier=0,
                                              allow_small_or_imprecise_dtypes=True), [], [I2])
                k.fn("pool", lambda e: e.iota(P1[:, 0:1], pattern=[[0, 1]], base=127, channel_multiplier=-1,
                                              allow_small_or_imprecise_dtypes=True), [], [P1])
                k.fn("pool", lambda e: e.iota(P1[:, 1:2], pattern=[[0, 1]], base=0, channel_multiplier=1,
                                              allow_small_or_imprecise_dtypes=True), [P1], [P1])
                k.ts("dve", Dp[:], Dm[:], 0.0, None, ALU.max)
                k.ts("dve", Dn[:], Dm[:], -1.0, 0.0, ALU.mult, ALU.max)
                k.ts("dve", mf[:], Dm[:], 0.0, None, ALU.is_ge)
                k.ts("dve", mb[:], Dm[:], 0.0, None, ALU.is_le)
                for h in range(4):
                    k.act(Ef[:], Dp[:], AF.Exp, scale=lg[:, h:h + 1])
                    k.tt("dve", Ef[:], Ef[:], mf[:], ALU.mult)
                    k.act(Eb[:], Dn[:], AF.Exp, scale=lg[:, 4 + h:5 + h])
                    k.tt("dve", Eb[:], Eb[:], mb[:], ALU.mult)
                    k.tt("dve", Ef[:], Ef[:], Eb[:], ALU.add)
                    k.ts("dve", MT[:, h, :], Ef[:], 0.0625, None, ALU.mult)
                    k.act(DF[:, h, :], I1[:], AF.Exp, scale=lg[:, h:h + 1])
                    k.act(DB[:, h, :], I2[:], AF.Exp, scale=lg[:, 4 + h:5 + h])
                    k.act(KFd[:, h:h + 1], P1[:, 0:1], AF.Exp, scale=lg[:, h:h + 1])
                    k.act(KBd[:, h:h + 1], P1[:, 1:2], AF.Exp, scale=lg[:, 4 + h:5 + h])
                k.ts("dve", KFd[:], KFd[:], 0.0625, None, ALU.mult)
                k.ts("dve", KBd[:], KBd[:], 0.0625, None, ALU.mult)
                k.S.barrier()
            for part in range(2):
              with contextlib.ExitStack() as es:
                wqk = k.sb(es, "r1_wqk", [128, 8, D], BF16)
                k.dma("pool", wqk[:], I["ret_w_in"][j][:, part * D:(part + 1) * D].rearrange("(k p) n -> p k n", p=128))
                cosT = k.sb(es, "r1_cos", [128, SEQ])
                sinT = k.sb(es, "r1_sin", [128, SEQ])
                k.dma("sp", cosT[:], I["ret_cos"])
                k.dma("sp", sinT[:], I["ret_sin"])
                stg = k.sb(es, "r1_stg", [128, 4, D])
                uT = [k.sb(es, f"r1_uT{x}", [128, 8, 512], BF16) for x in range(2)]
                tm = [k.sb(es, f"r1_tm{x}", [128, 512]) for x in range(4)]
                if part == 0:
                    qT = k.sb(es, "r1_qT", [128, 8, 512], BF16)
                    qf = k.sb(es, "r1_qf", [128, 8, 512], BF16)
                    qb = k.sb(es, "r1_qb", [128, 8, 512], BF16)
                    q32 = [k.sb(es, f"r1_q32{x}", [128, 512]) for x in range(2)]
                else:
                    kT = k.sb(es, "r1_kT", [128, 8, 512], BF16)
                    k32 = k.sb(es, "r1_k32", [128, 8, 512])
                    kfb = k.sb(es, "r1_kf", [128, 4, D], BF16)
                    kbb = k.sb(es, "r1_kb", [128, 4, D], BF16)
                it = 0
                for b in range(NBC):
                    for (bb, t0, n, r) in tiles_of(b, True):
                        isctx = (r == 2)
                        c0 = t0 - CTX
                        nt = n // 128
                        u = uT[it % 2]
                        it += 1
                        self.prologue(src, b, t0, n, r, 0, stg, u, [PS[6], PS[7]])
                        for hh in range(part * 4, part * 4 + 4):
                            isq = hh < 4
                            h = hh % 4
                            c1, c2 = 2 * h, 2 * h + 1
                            x1, x2 = PS[(2 * hh) % 4], PS[(2 * hh + 1) % 4]
                            for kk in range(8):
                                k.mm(x1[:, 0:n], wqk[:, kk, c1 * 128:(c1 + 1) * 128], u[:, kk, 0:n], kk == 0, kk == 7)
                            for kk in range(8):
                                k.mm(x2[:, 0:n], wqk[:, kk, c2 * 128:(c2 + 1) * 128], u[:, kk, 0:n], kk == 0, kk == 7)
                            d1, d2 = 2 * h, 2 * h + 1
                            if isq:
                                o1, o2 = q32[0][:, 0:n], q32[1][:, 0:n]
                            else:
                                o1, o2 = k32[:, d1, 0:n], k32[:, d2, 0:n]
                            if isctx:
                                k.copy("act", o1, x1[:, 0:n])
                                k.copy("act", o2, x2[:, 0:n])
                            else:
                                cs, sn = cosT[:, c0:c0 + n], sinT[:, c0:c0 + n]
                                k.tt("dve", tm[0][:, 0:n], x1[:, 0:n], cs, ALU.mult)
                                k.tt("dve", tm[1][:, 0:n], x2[:, 0:n], sn, ALU.mult)
                                k.tt("dve", tm[2][:, 0:n], x1[:, 0:n], sn, ALU.mult)
                                k.tt("dve", tm[3][:, 0:n], x2[:, 0:n], cs, ALU.mult)
                                k.tt("pool", o1, tm[0][:, 0:n], tm[1][:, 0:n], ALU.subtract)
                                k.tt("pool", o2, tm[2][:, 0:n], tm[3][:, 0:n], ALU.add)
                            for (o, d) in ((o1, d1), (o2, d2)):
                                if isq:
                                    k.copy("act", qT[:, d, 0:n], o)
                                    o3 = o.rearrange("p (s i) -> p s i", i=128)
                                    k.tt("dve", qf[:, d, 0:n].rearrange("p (s i) -> p s i", i=128), o3,
                                         DF[:, h:h + 1, :].broadcast_to([128, nt, 128]), ALU.mult)
                                    k.tt("dve", qb[:, d, 0:n].rearrange("p (s i) -> p s i", i=128), o3,
                                         DB[:, h:h + 1, :].broadcast_to([128, nt, 128]), ALU.mult)
                                else:
                                    k.copy("act", kT[:, d, 0:n], o)
                        for s_ in range(nt if part == 1 else 0):
                            pa, pb = PS[4], PS[5]
                            for c in range(8):
                                pp = pa if c < 4 else pb
                                k.tr(pp[:, (c % 4) * 128:(c % 4 + 1) * 128], k32[:, c, s_ * 128:(s_ + 1) * 128], self.ident[:])
                            for h in range(4):
                                pp = pa if h < 2 else pb
                                sl = pp[:, (h % 2) * 256:(h % 2 + 1) * 256]
                                k.act(kfb[:, s_, h * 256:(h + 1) * 256], sl, AF.Identity, scale=KFd[:, h:h + 1])
                                k.ts("dve", kbb[:, s_, h * 256:(h + 1) * 256], sl, KBd[:, h:h + 1], None, ALU.mult)
                        for (buf, dr) in (((qT, self.QT), (qf, self.QTF), (qb, self.QTB)) if part == 0 else ((kT, self.KT),)):
                            k.dma("sp", dr[b, :, t0:t0 + n].rearrange("(k p) t -> p k t", p=128), buf[:, :, 0:n], store=True,
                                  w=[(_key(dr), b, t0)])
                        for (buf, dr) in (((kfb, self.KF), (kbb, self.KB)) if part == 1 else ()):
                            k.dma("sp", dr[b, t0:t0 + n, :].rearrange("(s p) d -> p s d", p=128), buf[:, 0:nt, :], store=True,
                                  w=[(_key(dr), b, t0)])
                k.S.barrier()
            with contextlib.ExitStack() as es:
                wvg = k.sb(es, "r1_wvg", [128, 8, 4 * D], BF16)
                k.dma("pool", wvg[:], I["ret_w_in"][j][:, 2 * D:6 * D].rearrange("(k p) n -> p k n", p=128))
                stg = k.sb(es, "r1b_stg", [128, 4, D])
                uT = [k.sb(es, f"r1b_uT{x}", [128, 8, 512], BF16) for x in range(2)]
                vt = [k.sb(es, "r1b_vt0", [128, 4, 2 * D], BF16)] * 2
                gt = [k.sb(es, "r1b_gt0", [128, 4, 2 * D], BF16)] * 2
                it = 0
                for b in range(NBC):
                    for (bb, t0, n, r) in tiles_of(b, True):
                        nt = n // 128
                        u, vv, gg = uT[it % 2], vt[it % 2], gt[it % 2]
                        it += 1
                        self.prologue(src, b, t0, n, r, 0, stg, u, [PS[6], PS[7]])
                        pi = 0
                        for s_ in range(nt):
                            for nb in range(8):
                                ps = PS[pi % 6]
                                pi += 1
                                for kk in range(8):
                                    k.mm(ps[:], u[:, kk, s_ * 128:(s_ + 1) * 128], wvg[:, kk, nb * 512:(nb + 1) * 512], kk == 0, kk == 7)
                                if nb < 4:
                                    k.copy("act", vv[:, s_, nb * 512:(nb + 1) * 512], ps[:])
                                else:
                                    k.act(gg[:, s_, (nb - 4) * 512:(nb - 3) * 512], ps[:], AF.Silu)
                        k.dma("sp", self.RV[b, t0:t0 + n, :].rearrange("(s p) d -> p s d", p=128), vv[:, 0:nt, :], store=True, w=[("RV", b, t0)])
                        k.dma("sp", self.RG[b, t0:t0 + n, :].rearrange("(s p) d -> p s d", p=128), gg[:, 0:nt, :], store=True, w=[("RG", b, t0)])
                k.S.barrier()
            with contextlib.ExitStack() as es:
                Sb = k.sb(es, "r2_S", [128, 8, 512])
                sbf = [k.sb(es, f"r2_sbf{x}", [128, 8 * 512], BF16) for x in range(2)]
                kbc = [k.sb(es, f"r2_kb{x}", [128, D], BF16) for x in range(2)]
                vc = [k.sb(es, f"r2_v{x}", [128, 2 * D], BF16) for x in range(2)]
                it = 0
                for b in range(NBC):
                    k.memset("dve", Sb[:], 0.0)
                    order = [1, 0] + list(range(NCH - 1, 1, -1))
                    for oi, g in enumerate(order):
                        sf, kb_, v_ = sbf[it % 2], kbc[it % 2], vc[it % 2]
                        it += 1
                        k.copy("act", sf[:], Sb[:].rearrange("p a c -> p (a c)"))
                        k.dma("sp", self.SBD[b, g], sf[:], store=True, w=[("SBD", b, g)])
                        if oi == len(order) - 1:
                            break
                        k.dma("sp", kb_[:], self.KB[b, g * 128:(g + 1) * 128, :], r=[("KBall",)])
                        k.dma("sp", v_[:], self.RV[b, g * 128:(g + 1) * 128, :], r=[("RVall",)])
                        for h in range(4):
                            for a in range(2):
                                ps = PS[(h * 2 + a) % 8]
                                k.mm(ps[:], kb_[:, h * 256 + a * 128:h * 256 + (a + 1) * 128], v_[:, h * 512:(h + 1) * 512], True, True)
                                k.stt(Sb[:, h * 2 + a, :], Sb[:, h * 2 + a, :], GL[:, 4 + h:5 + h], ps[:], ALU.mult, ALU.add)
                k.S.barrier()
            with contextlib.ExitStack() as es:
                wout = k.sb(es, "r3_wout", [128, 16, D], BF16)
                k.dma("pool", wout[:], I["ret_w_out"][j].rearrange("(k p) n -> p k n", p=128))
                gng = k.sb(es, "r3_gng", [128, 2 * D])
                k.dma("sp", gng[:], I["ret_gn_g"][j].partition_broadcast(128))
                Sf = k.sb(es, "r3_Sf", [128, 8, 512])
                Sfb = k.sb(es, "r3_Sfb", [128, 8, 512], BF16)
                L = []
                for x in range(2):
                    L.append(dict(
                        qT=k.sb(es, f"r3_qT{x}", [128, 8, 128], BF16), kT=k.sb(es, f"r3_kT{x}", [128, 8, 128], BF16),
                        qf=k.sb(es, f"r3_qf{x}", [128, 8, 128], BF16), qb=k.sb(es, f"r3_qb{x}", [128, 8, 128], BF16),
                        kf=k.sb(es, f"r3_kf{x}", [128, D], BF16), v=k.sb(es, f"r3_v{x}", [128, 2 * D], BF16),
                        g=(k.sb(es, f"r3_g{x}", [128, 2 * D], BF16) if x == 0 else None),
                        sb=(k.sb(es, f"r3_sb{x}", [128, 8, 512], BF16) if x == 0 else None),
                        h=k.sb(es, f"r3_h{x}", [128, 1, D])))
                L[1]["sb"] = L[0]["sb"]
                L[1]["g"] = L[0]["g"]
                z32 = k.sb(es, "r3_z32", [128, 2 * D])
                zT = k.sb(es, "r3_zT", [128, 16, 128], BF16)
                on = [k.sb(es, f"r3_on{x}", [128, 512]) for x in range(2)]
                Pm = [k.sb(es, f"r3_P{x}", [128, 128], BF16) for x in range(2)]
                gst = k.sb(es, "r3_gst", [128, 4, 12])
                gmv = k.sb(es, "r3_gmv", [128, 4, 2])
                grs = k.sb(es, "r3_grs", [128, 4])
                zb = k.sb(es, "r3_zb", [128, 1, D])
                st = k.sb(es, "r3_st", [128, 1, 12])
                mv = k.sb(es, "r3_mv", [128, 1, 2])
                rs = k.sb(es, "r3_rs", [128, 1])
                it = 0
                for b in range(NBC):
                    k.memset("dve", Sf[:], 0.0)
                    k.memset("pool", Sfb[:], 0.0)
                    for g in range(NCH):
                        B_ = L[it % 2]
                        it += 1
                        isctx = g < 2
                        want_out = (not isctx) or need_ctx
                        r = 2 if isctx else b
                        cs = slice(g * 128, (g + 1) * 128)
                        k.dma("sp", B_["kf"][:], self.KF[b, cs, :], r=[("KFall",)])
                        k.dma("sp", B_["v"][:], self.RV[b, cs, :], r=[("RVall",)])
                        if want_out:
                            for nm, dr in (("qT", self.QT), ("kT", self.KT), ("qf", self.QTF), ("qb", self.QTB)):
                                k.dma("sp", B_[nm][:], dr[b, :, cs].rearrange("(k p) t -> p k t", p=128), r=[(nm + "all",)])
                            k.dma("sp", B_["g"][:], self.RG[b, cs, :], r=[("RGall",)])
                            k.dma("sp", B_["sb"][:].rearrange("p a c -> p (a c)"), self.SBD[b, g], r=[("SBDall",)])
                            k.dma("sp", B_["h"][:, 0, :], src[b, cs, :], r=[(_key(src), b, (g * 128 // 512) * 512 if not isctx else 0)])
                        for h in range(4):
                            vh = B_["v"][:, h * 512:(h + 1) * 512]
                            if want_out:
                                sc = PS[0]
                                for a in range(2):
                                    k.mm(sc[:, 0:128], B_["kT"][:, 2 * h + a, :], B_["qT"][:, 2 * h + a, :], a == 0, a == 1)
                            for a in range(2):
                                k.mm(PS[3 + a][:], B_["kf"][:, h * 256 + a * 128:h * 256 + (a + 1) * 128], vh, True, True)
                            if want_out:
                                P_ = Pm[h % 2]
                                k.tt("dve", P_[:], sc[:, 0:128], MT[:, h, :], ALU.mult)
                                O = PS[1 + h % 2]
                                k.mm(O[:], P_[:], vh, True, False)
                                for a in range(2):
                                    k.mm(O[:], B_["qf"][:, 2 * h + a, :], Sfb[:, 2 * h + a, :], False, False)
                                for a in range(2):
                                    k.mm(O[:], B_["qb"][:, 2 * h + a, :], B_["sb"][:, 2 * h + a, :], False, a == 1)
                            for a in range(2):
                                ps = PS[3 + a]
                                k.stt(Sf[:, 2 * h + a, :], Sf[:, 2 * h + a, :], GL[:, h:h + 1], ps[:], ALU.mult, ALU.add)
                                k.copy("act", Sfb[:, 2 * h + a, :], Sf[:, 2 * h + a, :])
                            if want_out:
                                k.fn("dve", (lambda e, h=h, O=O: e.bn_stats(out=gst[:, h, 0:6], in_=O[:])), [O], [gst])
                                k.fn("dve", (lambda e, h=h: e.bn_aggr(out=gmv[:, h, :], in_=gst[:, h, 0:6])), [gst], [gmv])
                                k.ts("pool", grs[:, h:h + 1], gmv[:, h, 1:2], LN_EPS, None, ALU.add)
                                k.tt("pool", grs[:, h:h + 1], grs[:, h:h + 1], self.negh[:, 0:1], ALU.pow)
                                o_ = on[h % 2]
                                k.ts("dve", o_[:], O[:], gmv[:, h, 0:1], grs[:, h:h + 1], ALU.subtract, ALU.mult)
                                k.tt("pool", o_[:], o_[:], gng[:, h * 512:(h + 1) * 512], ALU.mult)
                                k.tt("pool", z32[:, h * 512:(h + 1) * 512], o_[:], B_["g"][:, h * 512:(h + 1) * 512], ALU.mult,
                                     w=[(_key(z32), h)])
                        if want_out:
                            for c in range(16):
                                pp = PS[5]
                                k.tr(pp[:, (c % 4) * 128:(c % 4 + 1) * 128], z32[:, c * 128:(c + 1) * 128], self.ident[:],
                                     r=[(_key(z32), c // 4), self.ident])
                                if c % 4 == 3:
                                    k.copy("act", zT[:, c - 3:c + 1, :], pp[:].rearrange("p (c i) -> p c i", i=128))
                            for half in range(2):
                                y = PS[6 + half]
                                for c in range(16):
                                    k.mm(y[:], zT[:, c, :], wout[:, c, half * 512:(half + 1) * 512], c == 0, c == 15)
                            t0 = 0 if isctx else (g * 128 // 512) * 512
                            self.ln_epilogue(lambda t, half: PS[6 + half][:], B_["h"], 1, r, 0, 0, zb, st, mv, rs,
                                             dst[b, cs, :], (_key(dst), b, t0, g))
                k.S.barrier()

    def mla_layer(self, i, src, dst):
        k = self.k
        I = self.I
        PS = self.PS
        need_ctx = i < DEPTH - 1
        NKT = TT // 128
        with contextlib.ExitStack() as es:
            wd = k.sb(es, "m1_wd", [128, 8, 768], BF16)
            wuq = k.sb(es, "m1_wuq", [128, 3, 2048], BF16)
            wukv = k.sb(es, "m1_wukv", [128, 2, 2048], BF16)
            k.dma("pool", wd[:], I["mla_wd"].rearrange("(k p) n -> p k n", p=128))
            k.dma("pool", wuq[:], I["mla_wuq"].rearrange("(k p) n -> p k n", p=128))
            k.dma("pool", wukv[:], I["mla_wukv"].rearrange("(k p) n -> p k n", p=128))
            C1 = k.sb(es, "m1_c1", [128, SEQ])
            C2 = k.sb(es, "m1_c2", [128, SEQ])
            k.dma("sp", C1[:], I["mla_c1"])
            k.dma("sp", C2[:], I["mla_c2"])
            nraw = k.sb(es, "m1_nraw", [5, 128])
            npp = k.sb(es, "m1_npp", [128, 5])
            k.dma("sp", nraw[:], I["mla_norms"])
            k.tr(PS[7][:, 0:5], nraw[:, :], self.ident[0:5, 0:5])
            k.copy("dve", npp[:], PS[7][:, 0:5])
            stg = k.sb(es, "m1_stg", [128, 4, D])
            u = k.sb(es, "m1_uT", [128, 8, 512], BF16)
            d32 = k.sb(es, "m1_d32", [128, 5, 512])
            sq = [k.sb(es, f"m1_sq{x}", [128, 512]) for x in range(2)]
            rr = k.sb(es, "m1_rr", [128, 2, 512])
            dn = k.sb(es, "m1_dn", [128, 5, 512], BF16)
            qnb = k.sb(es, "m1_qnb", [128, 8, 512], BF16)
            qrb = k.sb(es, "m1_qrb", [128, 4, 512], BF16)
            knb = k.sb(es, "m1_knb", [128, 8, 512], BF16)
            vb = k.sb(es, "m1_vb", [128, 4, D], BF16)
            krb = k.sb(es, "m1_krb", [64, 512], BF16)
            tm = [k.sb(es, f"m1_tm{x}", [128, 512]) for x in range(2)]
            for b in range(NBC):
                for (bb, t0, n, r) in tiles_of(b, True):
                    isctx = (r == 2)
                    c0 = t0 - CTX
                    nt = n // 128
                    self.prologue(src, b, t0, n, r, 0, stg, u, [PS[6], PS[7]])
                    for c in range(5):
                        ps = PS[c]
                        for kk in range(8):
                            k.mm(ps[:, 0:n], wd[:, kk, c * 128:(c + 1) * 128], u[:, kk, 0:n], kk == 0, kk == 7)
                        k.copy("act", d32[:, c, 0:n], ps[:, 0:n])
                    for (x, ps) in ((0, PS[5]), (1, PS[6])):
                        for kk in range(8):
                            k.mm(ps[0:64, 0:n], wd[:, kk, 640 + 64 * x:704 + 64 * x], u[:, kk, 0:n], kk == 0, kk == 7)
                    if isctx:
                        k.copy("act", krb[:, 0:n], PS[5][0:64, 0:n])
                    else:
                        k.tt("dve", tm[0][0:64, 0:n], PS[5][0:64, 0:n], C1[0:64, c0:c0 + n], ALU.mult)
                        k.tt("dve", tm[1][0:64, 0:n], PS[6][0:64, 0:n], C2[0:64, c0:c0 + n], ALU.mult)
                        k.tt("pool", krb[:, 0:n], tm[0][0:64, 0:n], tm[1][0:64, 0:n], ALU.add)
                    k.dma("sp", self.KR[b, :, t0:t0 + n], krb[:, 0:n], store=True, w=[("KR", b, t0)])
                    for (gi_, cl, dim) in ((0, (0, 1, 2), 384.0), (1, (3, 4), 256.0)):
                        ps = PS[7]
                        for ci, c in enumerate(cl):
                            sqq = sq[ci % 2]
                            k.tt("dve", sqq[:, 0:n], d32[:, c, 0:n], d32[:, c, 0:n], ALU.mult)
                            k.mm(ps[:, 0:n], self.onesf[:], sqq[:, 0:n], ci == 0, ci == len(cl) - 1)
                        k.ts("dve", rr[:, gi_, 0:n], ps[:, 0:n], 1.0 / dim, RMS_EPS, ALU.mult, ALU.add)
                        k.tt("pool", rr[:, gi_, 0:n], rr[:, gi_, 0:n], self.negh[:, 0:n], ALU.pow)
                        for c in cl:
                            k.stt(dn[:, c, 0:n], d32[:, c, 0:n], npp[:, c:c + 1], rr[:, gi_, 0:n], ALU.mult, ALU.mult)
                    for h in range(8):
                        ps = PS[h % 4]
                        for c in range(3):
                            k.mm(ps[:, 0:n], wuq[:, c, h * 128:(h + 1) * 128], dn[:, c, 0:n], c == 0, c == 2)
                        k.copy("act", qnb[:, h, 0:n], ps[:, 0:n])
                    k.dma("sp", self.QN[b, :, :, t0:t0 + n].rearrange("h p t -> p h t"), qnb[:, :, 0:n], store=True, w=[("QN", b, t0)])
                    for hp in range(4):
                        pa, pb = PS[4 + (2 * hp) % 2], PS[4 + (2 * hp + 1) % 2]
                        for c in range(3):
                            k.mm(pa[:, 0:n], wuq[:, c, 1024 + hp * 128:1024 + (hp + 1) * 128], dn[:, c, 0:n], c == 0, c == 2)
                        if isctx:
                            k.copy("act", qrb[:, hp, 0:n], pa[:, 0:n])
                        else:
                            for c in range(3):
                                k.mm(pb[:, 0:n], wuq[:, c, 1536 + hp * 128:1536 + (hp + 1) * 128], dn[:, c, 0:n], c == 0, c == 2)
                            k.tt("dve", tm[0][:, 0:n], pa[:, 0:n], C1[:, c0:c0 + n], ALU.mult)
                            k.tt("dve", tm[1][:, 0:n], pb[:, 0:n], C2[:, c0:c0 + n], ALU.mult)
                            k.tt("pool", qrb[:, hp, 0:n], tm[0][:, 0:n], tm[1][:, 0:n], ALU.add)
                    k.dma("sp", self.QR[b, :, :, t0:t0 + n].rearrange("h p t -> p h t"), qrb[:, :, 0:n], store=True, w=[("QR", b, t0)])
                    for h in range(8):
                        ps = PS[h % 4]
                        for c in range(2):
                            k.mm(ps[:, 0:n], wukv[:, c, h * 128:(h + 1) * 128], dn[:, 3 + c, 0:n], c == 0, c == 1)
                        k.copy("act", knb[:, h, 0:n], ps[:, 0:n])
                    k.dma("sp", self.KN[b, :, :, t0:t0 + n].rearrange("h p t -> p h t"), knb[:, :, 0:n], store=True, w=[("KN", b, t0)])
                    for s_ in range(nt):
                        for half in range(2):
                            ps = PS[4 + half]
                            for c in range(2):
                                k.mm(ps[:], dn[:, 3 + c, s_ * 128:(s_ + 1) * 128], wukv[:, c, 1024 + half * 512:1024 + (half + 1) * 512], c == 0, c == 1)
                            k.copy("act", vb[:, s_, half * 512:(half + 1) * 512], ps[:])
                    k.dma("sp", self.MV[b, t0:t0 + n, :].rearrange("(s p) d -> p s d", p=128), vb[:, 0:nt, :], store=True, w=[("MV", b, t0)])
            k.S.barrier()
        with contextlib.ExitStack() as es:
            kra = k.sb(es, "m2_kr", [64, TT], BF16)
            knh = [k.sb(es, f"m2_kn{x}", [128, TT], BF16) for x in range(2)]
            vh = [k.sb(es, f"m2_v{x}", [128, NKT, 128], BF16) for x in range(2)]
            qn = [k.sb(es, f"m2_qn{x}", [128, 512], BF16) for x in range(2)]
            qr = [k.sb(es, f"m2_qr{x}", [64, 512], BF16) for x in range(2)]
            PT = [k.sb(es, f"m2_PT{x}", [128, 512], BF16) for x in range(3)]
            rec = [k.sb(es, f"m2_rec{x}", [128, 512]) for x in range(2)]
            otb = [k.sb(es, f"m2_ot{x}", [128, 512], BF16) for x in range(2)]
            scale = float(192.0 ** -0.5)
            it = 0
            ih = 0
            for b in range(NBC):
                k.dma("sp", kra[:], self.KR[b], r=[("KRall",)])
                for h in range(8):
                    kn_, v_ = knh[ih % 2], vh[ih % 2]
                    ih += 1
                    k.dma("sp", kn_[:], self.KN[b, h], r=[("KNall",)])
                    k.dma("sp", v_[:], self.MV[b, :, h * 128:(h + 1) * 128].rearrange("(t p) d -> p t d", p=128), r=[("MVall",)])
                    for (bb, t0, n, r) in tiles_of(b, need_ctx):
                        isctx = (r == 2)
                        kts = [0, 1] if isctx else list(range(NKT))
                        q_, r_ = qn[it % 2], qr[it % 2]
                        O, DEN = PS[4 + it % 2], PS[6 + it % 2]
                        rc, ot = rec[it % 2], otb[it % 2]
                        it += 1
                        k.dma("sp", q_[:, 0:n], self.QN[b, h, :, t0:t0 + n], r=[("QNall",)])
                        k.dma("sp", r_[:, 0:n], self.QR[b, h // 2, (h % 2) * 64:(h % 2) * 64 + 64, t0:t0 + n], r=[("QRall",)])

                        def scores(idx):
                            kt = kts[idx]
                            sc = PS[idx % 4]
                            k.mm(sc[:, 0:n], kn_[:, kt * 128:(kt + 1) * 128], q_[:, 0:n], True, False)
                            k.mm(sc[:, 0:n], kra[:, kt * 128:(kt + 1) * 128], r_[:, 0:n], False, True)
                        scores(0)
                        for idx, kt in enumerate(kts):
                            if idx + 1 < len(kts):
                                scores(idx + 1)
                            p_ = PT[idx % 3]
                            k.act(p_[:, 0:n], PS[idx % 4][:, 0:n], AF.Exp, scale=scale)
                            k.mm(O[:, 0:n], v_[:, kt, :], p_[:, 0:n], idx == 0, idx == len(kts) - 1)
                            k.mm(DEN[:, 0:n], self.onesb[:], p_[:, 0:n], idx == 0, idx == len(kts) - 1)
                        k.fn("dve", (lambda e, rc=rc, DEN=DEN, n=n: e.reciprocal(out=rc[:, 0:n], in_=DEN[:, 0:n])), [DEN], [rc])
                        k.tt("dve", ot[:, 0:n], O[:, 0:n], rc[:, 0:n], ALU.mult)
                        k.dma("sp", self.OT[b, h * 128:(h + 1) * 128, t0:t0 + n], ot[:, 0:n], store=True, w=[("OT", b, h, t0)])
            k.S.barrier()
        with contextlib.ExitStack() as es:
            wout = k.sb(es, "m3_wout", [128, 8, D], BF16)
            k.dma("pool", wout[:], I["mla_w_out"].rearrange("(k p) n -> p k n", p=128))
            stg = [k.sb(es, f"m3_stg{x}", [128, 4, D]) for x in range(2)]
            oT = [k.sb(es, f"m3_oT{x}", [128, 8, 512], BF16) for x in range(2)]
            zb = k.sb(es, "m3_zb", [128, 4, D])
            st = k.sb(es, "m3_st", [128, 4, 12])
            mv = k.sb(es, "m3_mv", [128, 4, 2])
            rs = k.sb(es, "m3_rs", [128, 4])
            it = 0
            for b in range(NBC):
                for (bb, t0, n, r) in tiles_of(b, need_ctx):
                    nt = n // 128
                    sg, o_ = stg[it % 2], oT[it % 2]
                    it += 1
                    k.dma("sp", sg[:, 0:nt, :], src[b, t0:t0 + n, :].rearrange("(t p) d -> p t d", p=128), r=[(_key(src), b, t0)])
                    k.dma("sp", o_[:, :, 0:n], self.OT[b, :, t0:t0 + n].rearrange("(k p) t -> p k t", p=128), r=[("OTall",)])
                    for t in range(nt):
                        for half in range(2):
                            ps = PS[(t * 2 + half) % 8]
                            for kk in range(8):
                                k.mm(ps[:], o_[:, kk, t * 128:(t + 1) * 128], wout[:, kk, half * 512:(half + 1) * 512], kk == 0, kk == 7)
                    self.ln_epilogue(lambda t, half: PS[(t * 2 + half) % 8][:], sg, nt, r, 0, 0, zb, st, mv, rs,
                                     dst[b, t0:t0 + n, :], (_key(dst), b, t0))
            k.S.barrier()

    def moe_layer_dense(self, i, src, dst, last, dbg_ns=None, dbg_ne=NE):
        k = self.k
        I = self.I
        PS = self.PS
        need_ctx = not last
        i = self.li[i]
        alltiles = []
        for b in range(NBC):
            alltiles += tiles_of(b, need_ctx)
        NS = 2
        with contextlib.ExitStack() as es:
            wr = k.sb(es, "mo_wr", [128, 8, 64])
            rbg = k.sb(es, "mo_rbg", [128, 4])
            rbe = k.sb(es, "mo_rbe", [128, 32])
            k.dma("sp", wr[:], I["moe_wr"][i].rearrange("(k p) n -> p k n", p=128))
            k.dma("sp", rbg[:], I["moe_bg"][i].partition_broadcast(128))
            k.dma("sp", rbe[:], I["moe_be"][i].partition_broadcast(128))
            vT = k.sb(es, "mo_vT", [128, 8, NS * 512], BF16)
            acc = [[k.sb(es, f"mo_acc{s}_{h}", [128, 512]) for h in range(2)] for s in range(NS * 4)]
            G = k.sb(es, "mo_G", [128, NS * 4, 32])
            stg = k.sb(es, "mo_stg", [128, 4, D])
            w1b = [k.sb(es, f"mo_w1_{j}", [128, 8, 512], BF16) for j in range(2)]
            w3b = [k.sb(es, f"mo_w3_{j}", [128, 8, 512], BF16) for j in range(2)]
            w2b = [k.sb(es, f"mo_w2_{j}", [128, 4, D], BF16) for j in range(2)]
            sil = [k.sb(es, f"mo_sil{j}", [128, 512]) for j in range(2)]
            hdn = [k.sb(es, f"mo_hdn{j}", [128, 4, 512], BF16) for j in range(2)]
            rt = k.sb(es, "mo_rt", [128, 64])
            r8 = k.sb(es, "mo_r8", [128, 8])
            rsm = k.sb(es, "mo_rsm", [128, 16])
            zb = k.sb(es, "mo_zb", [128, 4, D])
            v32 = zb[:].rearrange("p a (b c) -> p (a b) c", c=512)
            st = k.sb(es, "mo_st", [128, 4, 12])
            mv = k.sb(es, "mo_mv", [128, 4, 2])
            rs = k.sb(es, "mo_rs", [128, 4])
            wi = 0
            for s0 in range(0, len(alltiles) if dbg_ns is None else dbg_ns * NS, NS):
                tl = alltiles[s0:s0 + NS]
                subs = []
                for ti, (b, t0, n, r) in enumerate(tl):
                    self.prologue(src, b, t0, n, r, 2, stg, vT[:, :, ti * 512:(ti + 1) * 512], [PS[6], PS[7]], v32=v32)
                    for t in range(n // 128):
                        si = ti * 4 + t
                        subs.append((ti, t, ti * 512 + t * 128, si))
                        lg = PS[5]
                        for kk in range(8):
                            k.mm(lg[:, 0:64], v32[:, kk, t * 128:(t + 1) * 128], wr[:, kk, :], kk == 0, kk == 7)
                        self.route(lg, rbg, rbe, rt, r8, rsm, G[:, si, :])
                for e in range(dbg_ne):
                    wb1, wb3, wb2 = w1b[wi % 2], w3b[wi % 2], w2b[wi % 2]
                    wi += 1
                    k.dma("pool", wb1[:], I["moe_w1"][i, e].rearrange("(k p) n -> p k n", p=128))
                    k.dma("pool", wb3[:], I["moe_w3"][i, e].rearrange("(k p) n -> p k n", p=128))
                    k.dma("pool", wb2[:], I["moe_w2"][i, e].rearrange("(k p) n -> p k n", p=128))
                    for ti, (b, t0, n, r) in enumerate(tl):
                        hb = hdn[ti % 2]
                        cs = ti * 512
                        for j in range(4):
                            pa, pb = PS[(2 * j) % 4], PS[(2 * j + 1) % 4]
                            for kk in range(8):
                                k.mm(pa[:, 0:n], wb1[:, kk, j * 128:(j + 1) * 128], vT[:, kk, cs:cs + n], kk == 0, kk == 7)
                            for kk in range(8):
                                k.mm(pb[:, 0:n], wb3[:, kk, j * 128:(j + 1) * 128], vT[:, kk, cs:cs + n], kk == 0, kk == 7)
                            sl = sil[j % 2]
                            k.act(sl[:, 0:n], pa[:, 0:n], AF.Silu)
                            k.tt("dve", hb[:, j, 0:n], sl[:, 0:n], pb[:, 0:n], ALU.mult, w=[(_key(hb), j)])
                        for t in range(n // 128):
                            si = ti * 4 + t
                            for half in range(2):
                                po = PS[4 + (t * 2 + half) % 2]
                                for j in range(4):
                                    k.mm(po[:], hb[:, j, t * 128:(t + 1) * 128], wb2[:, j, half * 512:(half + 1) * 512], j == 0, j == 3,
                                         r=[(_key(hb), j), wb2])
                                a = acc[si][half][:]
                                if e == 0:
                                    k.ts("dve", a, po[:], G[:, si, e:e + 1], None, ALU.mult)
                                else:
                                    k.stt(a, po[:], G[:, si, e:e + 1], a, ALU.mult, ALU.add)
                for ti, (b, t0, n, r) in enumerate(tl):
                    nt = n // 128
                    k.dma("sp", stg[:, 0:nt, :], src[b, t0:t0 + n, :].rearrange("(t p) d -> p t d", p=128), r=[(_key(src), b, t0)])
                    if last:
                        drows = self.out[b, t0 - CTX:t0 - CTX + n, :]
                        dkey = ("out", b, t0)
                    else:
                        drows = dst[b, t0:t0 + n, :]
                        dkey = (_key(dst), b, t0)
                    self.ln_epilogue(lambda t, half, ti=ti: acc[ti * 4 + t][half][:], stg, nt, r, 1, 1,
                                     zb, st, mv, rs, drows, dkey)
            k.S.barrier()

    def moe_layer(self, i, src, dst, last, dbg_ns=None, dbg_ne=NE):
        k = self.k
        I = self.I
        PS = self.PS
        need_ctx = not last
        li = self.li[i]
        alltiles = []
        for b in range(NBC):
            alltiles += tiles_of(b, need_ctx)
        nsub_all = sum(n // 128 for (_, _, n, _) in alltiles)
        NBLK = (2 * nsub_all * 128 + 511) // 512 + NE
        NSUB = nsub_all
        XS, YS = self.XS, self.YS
        I32 = mybir.dt.int32
        with contextlib.ExitStack() as mes:
            GT = k.sb(mes, "ms_GT", [128, NSUB, 2])
            D0 = k.sb(mes, "ms_D0", [128, NSUB], I32)
            D1 = k.sb(mes, "ms_D1", [128, NSUB], I32)
            WI = k.sb(mes, "ms_WI", [128, NBLK], I32)
            ves = contextlib.ExitStack()
            VB = k.sb(ves, "ms_VB", [128, 3, 2, D])
            with contextlib.ExitStack() as es:
                wblk = [k.sb(es, f"mv_w{x}", [128, 8, 1024]) for x in range(2)]
                brow = k.sb(es, "mv_brow", [1, 6 * D])
                REP = k.sb(es, "mv_REP", [128, 3, 8, 128])
                for r in range(3):
                    for kk in range(8):
                        k.copy("dve", REP[:, r, kk, :], self.actT[:, kk, r:r + 1].broadcast_to([128, 128]))
                k.dma("sp", brow[:], I["ada_b"][li])
                pi = 0
                for wi in (3, 4):
                    buf = wblk[wi % 2]
                    k.dma("sp", buf[:], I["ada_w"][li][:, wi * 1024:(wi + 1) * 1024].rearrange("(k p) n -> p k n", p=128))
                    for r in range(3):
                        for half in range(2):
                            ps = PS[pi % 8]
                            pi += 1
                            for ki in range(8):
                                k.mm(ps[:], REP[:, r, ki, :], buf[:, ki, half * 512:(half + 1) * 512], ki == 0, False)
                            k.mm(ps[:], self.ones[0:1, 0:128], brow[0:1, wi * 1024 + half * 512: wi * 1024 + (half + 1) * 512], False, True)
                            if wi == 3:
                                k.copy("act", VB[:, r, 0, half * 512:(half + 1) * 512], ps[:])
                            else:
                                k.act(VB[:, r, 1, half * 512:(half + 1) * 512], ps[:], AF.Identity, bias=1.0)
                k.S.barrier()
            with contextlib.ExitStack() as es:
                wr = k.sb(es, "ma_wr", [128, 8, 64])
                rbg = k.sb(es, "ma_rbg", [128, 4])
                rbe = k.sb(es, "ma_rbe", [128, 32])
                k.dma("sp", wr[:], I["moe_wr"][li].rearrange("(k p) n -> p k n", p=128))
                k.dma("sp", rbg[:], I["moe_bg"][li].partition_broadcast(128))
                k.dma("sp", rbe[:], I["moe_be"][li].partition_broadcast(128))
                U = k.sb(es, "ma_U", [128, 128], BF16)
                k.memset("pool", U[:], 1.0)
                k.fn("pool", lambda e: e.affine_select(out=U[:], in_=U[:], pattern=[[1, 128]], compare_op=ALU.is_gt,
                                                       fill=0.0, base=0, channel_multiplier=-1), [U], [U])
                OH1 = k.sb(es, "ms_OH1", [128, NSUB, 32])
                OH2 = k.sb(es, "ms_OH2", [128, NSUB, 32])
                RK = k.sb(es, "ms_RK", [128, NSUB, 32])
                cnt = k.sb(es, "ma_cnt", [128, 32])
                k.memset("dve", cnt[:], 0.0)
                stg = [k.sb(es, f"ma_stg{x}", [128, 4, D]) for x in range(2)]
                vtk = k.sb(es, "ma_vtk", [128, 4, D])
                v32 = k.sb(es, "ma_v32", [128, 8, 512])
                ind = [k.sb(es, f"ma_ind{x}", [128, 32], BF16) for x in range(2)]
                rt = k.sb(es, "ma_rt", [128, 64])
                r8 = k.sb(es, "ma_r8", [128, 8])
                rsm = k.sb(es, "ma_rsm", [128, 16])
                si = 0
                for ti, (b, t0, n, r) in enumerate(alltiles):
                    nt = n // 128
                    sg = stg[ti % 2]
                    k.dma("sp", sg[:, 0:nt, :], src[b, t0:t0 + n, :].rearrange("(t p) d -> p t d", p=128), r=[(_key(src), b, t0)])
                    for t in range(nt):
                        k.tt("dve", vtk[:, t, :], sg[:, t, :], VB[:, r, 1, :], ALU.mult, w=[(_key(vtk), t)])
                        k.tt("pool", vtk[:, t, :], vtk[:, t, :], VB[:, r, 0, :], ALU.add, r=[(_key(vtk), t), VB], w=[(_key(vtk), t)])
                    for kk in range(8):
                        ps = PS[6 + kk % 2]
                        for t in range(nt):
                            k.tr(ps[:, t * 128:(t + 1) * 128], vtk[:, t, kk * 128:(kk + 1) * 128], self.ident[:],
                                 r=[(_key(vtk), t), self.ident])
                        k.copy("act", v32[:, kk, 0:n], ps[:, 0:n], w=[(_key(v32), kk)])
                    for t in range(nt):
                        lg = PS[5]
                        for kk in range(8):
                            k.mm(lg[:, 0:64], v32[:, kk, t * 128:(t + 1) * 128], wr[:, kk, :], kk == 0, kk == 7,
                                 r=[(_key(v32), kk), wr])
                        self.route(lg, rbg, rbe, rt, r8, rsm, None, oh1=OH1[:, si, :], oh2=OH2[:, si, :], gts=GT[:, si, :])
                        id_ = ind[si % 2]
                        k.tt("dve", id_[:], OH1[:, si, :], OH2[:, si, :], ALU.add)
                        pr = PS[4]
                        k.mm(pr[:, 0:32], U[:], id_[:], True, True)
                        k.mm(pr[:, 32:64], self.onesb[:], id_[:], True, True)
                        k.tt("dve", RK[:, si, :], pr[:, 0:32], cnt[:], ALU.add)
                        k.tt("dve", cnt[:], pr[:, 32:64], cnt[:], ALU.add)
                        si += 1
                sm = k.sb(es, "ma_sm", [128, 6, 32])
                k.ts("dve", sm[:, 0, :], cnt[:], 511.0, 1.0 / 512.0, ALU.add, ALU.mult)
                k.ts("dve", sm[:, 0, :], sm[:, 0, :], -0.499, 8388608.0, ALU.add, ALU.add)
                k.ts("dve", sm[:, 0, :], sm[:, 0, :], -8388608.0, 512.0, ALU.add, ALU.mult)
                k.memset("dve", sm[:, 1, :], 1.0)
                k.fn("dve", lambda e: e.tensor_tensor_scan(out=sm[:, 2, :], data0=sm[:, 1, :], data1=sm[:, 0, :], initial=0.0,
                                                          op0=ALU.mult, op1=ALU.add), [sm], [sm])
                k.tt("dve", sm[:, 3, :], sm[:, 2, :], sm[:, 0, :], ALU.subtract)
                jv = k.sb(es, "ma_jv", [128, NBLK, 32])
                k.fn("pool", lambda e: e.iota(jv[:], pattern=[[512, NBLK], [0, 32]], base=0, channel_multiplier=0,
                                              allow_small_or_imprecise_dtypes=True), [], [jv])
                k.tt("dve", jv[:], jv[:], sm[:, 2:3, :].broadcast_to([128, NBLK, 32]), ALU.is_ge)
                eb = k.sb(es, "ma_eb", [128, NBLK])
                k.fn("dve", lambda e: e.tensor_reduce(out=eb[:], in_=jv[:], axis=mybir.AxisListType.X, op=ALU.add), [jv], [eb])
                pid = k.sb(es, "ma_pid", [128, 1])
                k.fn("pool", lambda e: e.iota(pid[:], pattern=[[0, 1]], base=0, channel_multiplier=1,
                                              allow_small_or_imprecise_dtypes=True), [], [pid])
                k.ts("dve", eb[:], eb[:], 31.0, 128.0, ALU.min, ALU.mult)
                k.ts("dve", eb[:], eb[:], pid[:, 0:1], float(li * NE * 128), ALU.add, ALU.add)
                k.copy("dve", WI[:], eb[:])
                k.tt("dve", RK[:], RK[:], sm[:, 3:4, :].broadcast_to([128, NSUB, 32]), ALU.add)
                dd = k.sb(es, "ma_dd", [128, NSUB])
                for (OH, Dd) in ((OH1, D0), (OH2, D1)):
                    k.tt("dve", OH[:], OH[:], RK[:], ALU.mult)
                    k.fn("dve", (lambda e, OH=OH: e.tensor_reduce(out=dd[:], in_=OH[:], axis=mybir.AxisListType.X, op=ALU.add)), [OH], [dd])
                    k.copy("dve", Dd[:], dd[:])
                k.S.barrier()
            with contextlib.ExitStack() as es:
                stg = [k.sb(es, f"mb_stg{x}", [128, 4, D]) for x in range(2)]
                vtk = [k.sb(es, f"mb_vtk{x}", [128, 4, D]) for x in range(2)]
                si = 0
                for ti, (b, t0, n, r) in enumerate(alltiles):
                    nt = n // 128
                    sg, vt = stg[ti % 2], vtk[ti % 2]
                    k.dma("sp", sg[:, 0:nt, :], src[b, t0:t0 + n, :].rearrange("(t p) d -> p t d", p=128), r=[(_key(src), b, t0)])
                    for t in range(nt):
                        k.tt("dve", vt[:, t, :], sg[:, t, :], VB[:, r, 1, :], ALU.mult)
                        k.tt("dve", vt[:, t, :], vt[:, t, :], VB[:, r, 0, :], ALU.add)
                        for Dd in (D0, D1):
                            k.S.op("pool", (lambda e, vt=vt, t=t, Dd=Dd, si=si: e.indirect_dma_start(
                                out=XS, out_offset=bass.IndirectOffsetOnAxis(ap=Dd[:, si:si + 1], axis=0),
                                in_=vt[:, t, :], in_offset=None)), [vt, Dd], [("XS", si, id(Dd))], dma=_key(vt))
                        si += 1
                k.S.barrier()
            ves.close()
            with contextlib.ExitStack() as es:
                wst = [k.sb(es, f"mc_wst{x}", [128, 4096]) for x in range(3)]
                wb = [[k.sb(es, f"mc_wb{x}_{y}", [128, 4096], BF16) for x in range(3)] for y in range(2)]
                stg = k.sb(es, "mc_stg", [128, 4, D])
                xT = [k.sb(es, f"mc_xT{x}", [128, 8, 512], BF16) for x in range(2)]
                sil = [k.sb(es, f"mc_sil{x}", [128, 512]) for x in range(2)]
                hdn = [k.sb(es, f"mc_hdn{x}", [128, 4, 512], BF16) for x in range(2)]
                ysb = k.sb(es, "mc_ysb", [128, 4, D])
                wsrc = [I["moe_w1"].rearrange("l e p k n -> (l e p) (k n)"), I["moe_w3"].rearrange("l e p k n -> (l e p) (k n)"),
                        I["moe_w2"].rearrange("l e p k n -> (l e p) (k n)")]
                for j in range(NBLK if dbg_ns is None else dbg_ns):
                    wbj = wb[j % 2]
                    for x in range(3):
                        k.S.op("pool", (lambda e, x=x, j=j: e.indirect_dma_start(
                            out=wst[x][:], out_offset=None, in_=wsrc[x],
                            in_offset=bass.IndirectOffsetOnAxis(ap=WI[:, j:j + 1], axis=0))), [WI], [wst[x]], dma=_key(wst[x]))
                    k.copy("act", wbj[0][:], wst[0][:])
                    k.copy("pool", wbj[1][:], wst[1][:])
                    k.copy("dve", wbj[2][:], wst[2][:])
                    w1v = wbj[0][:].rearrange("p (k n) -> p k n", n=512)
                    w3v = wbj[1][:].rearrange("p (k n) -> p k n", n=512)
                    w2v = wbj[2][:].rearrange("p (k n) -> p k n", n=1024)
                    x_ = xT[j % 2]
                    hb = hdn[j % 2]
                    k.dma("sp", stg[:], XS[j * 512:(j + 1) * 512, :].rearrange("(t p) d -> p t d", p=128), r=[("XSall",)])
                    for kk in range(8):
                        ps = PS[6 + kk % 2]
                        for t in range(4):
                            k.tr(ps[:, t * 128:(t + 1) * 128], stg[:, t, kk * 128:(kk + 1) * 128], self.ident[:])
                        k.copy("act" if kk % 2 == 0 else "dve", x_[:, kk, :], ps[:])
                    for jj in range(4):
                        pa, pb = PS[(2 * jj) % 4], PS[(2 * jj + 1) % 4]
                        for kk in range(8):
                            k.mm(pa[:], w1v[:, kk, jj * 128:(jj + 1) * 128], x_[:, kk, :], kk == 0, kk == 7)
                        for kk in range(8):
                            k.mm(pb[:], w3v[:, kk, jj * 128:(jj + 1) * 128], x_[:, kk, :], kk == 0, kk == 7)
                        sl = sil[jj % 2]
                        k.act(sl[:], pa[:], AF.Silu)
                        k.tt("dve", hb[:, jj, :], sl[:], pb[:], ALU.mult, w=[(_key(hb), jj)])
                    for t in range(4):
                        for half in range(2):
                            po = PS[4 + half]
                            for jj in range(4):
                                k.mm(po[:], hb[:, jj, t * 128:(t + 1) * 128], w2v[:, jj, half * 512:(half + 1) * 512], jj == 0, jj == 3,
                                     r=[(_key(hb), jj), wbj[2]])
                            k.copy("act" if half == 0 else "dve", ysb[:, t, half * 512:(half + 1) * 512], po[:])
                    k.dma("sp", YS[j * 512:(j + 1) * 512, :].rearrange("(t p) d -> p t d", p=128), ysb[:], store=True, w=[("YS", j)])
                k.S.barrier()
            with contextlib.ExitStack() as es:
                stg = [k.sb(es, f"md_stg{x}", [128, 4, D]) for x in range(2)]
                y0 = [k.sb(es, f"md_y0{x}", [128, D]) for x in range(2)]
                y1 = [k.sb(es, f"md_y1{x}", [128, D]) for x in range(2)]
                fb = [k.sb(es, f"md_f{x}", [128, 4, D]) for x in range(2)]
                zb = k.sb(es, "md_zb", [128, 4, D])
                st = k.sb(es, "md_st", [128, 4, 12])
                mv = k.sb(es, "md_mv", [128, 4, 2])
                rs = k.sb(es, "md_rs", [128, 4])
                si = 0
                for ti, (b, t0, n, r) in enumerate(alltiles):
                    nt = n // 128
                    sg, f_ = stg[ti % 2], fb[ti % 2]
                    k.dma("sp", sg[:, 0:nt, :], src[b, t0:t0 + n, :].rearrange("(t p) d -> p t d", p=128), r=[(_key(src), b, t0)])
                    for t in range(nt):
                        a0, a1 = y0[si % 2], y1[si % 2]
                        for (yy, Dd) in ((a0, D0), (a1, D1)):
                            k.S.op("pool", (lambda e, yy=yy, Dd=Dd, si=si: e.indirect_dma_start(
                                out=yy[:], out_offset=None, in_=YS,
                                in_offset=bass.IndirectOffsetOnAxis(ap=Dd[:, si:si + 1], axis=0))), [Dd], [yy], dma=_key(yy))
                        k.ts("dve", f_[:, t, :], a0[:], GT[:, si, 0:1], None, ALU.mult)
                        k.stt(f_[:, t, :], a1[:], GT[:, si, 1:2], f_[:, t, :], ALU.mult, ALU.add)
                        si += 1
                    if dbg_ns is not None and ti >= 1:
                        break
                    if last:
                        drows = self.out[b, t0 - CTX:t0 - CTX + n, :]
                        dkey = ("out", b, t0)
                    else:
                        drows = dst[b, t0:t0 + n, :]
                        dkey = (_key(dst), b, t0)
                    self.ln_epilogue(lambda t, half, f_=f_: f_[:, t, half * 512:(half + 1) * 512], sg, nt, r, 1, 1,
                                     zb, st, mv, rs, drows, dkey)
                k.S.barrier()

    def route(self, lg, rbg, rbe, rt, r8, rsm, Gout, oh1=None, oh2=None, gts=None):
        k = self.k
        k.tt("dve", rt[:, 0:4], lg[:, 0:4], rbg[:], ALU.add)
        k.tt("dve", rt[:, 8:40], lg[:, 4:36], rbe[:], ALU.add)
        k.fn("dve", lambda e: e.tensor_reduce(out=rsm[:, 0:1], in_=rt[:, 0:4], axis=mybir.AxisListType.X, op=ALU.max), [rt], [rsm])
        k.ts("dve", rt[:, 4:8], rt[:, 0:4], rsm[:, 0:1], None, ALU.subtract)
        k.act(rt[:, 40:44], rt[:, 4:8], AF.Exp)
        k.fn("dve", lambda e: e.tensor_reduce(out=rsm[:, 1:2], in_=rt[:, 40:44], axis=mybir.AxisListType.X, op=ALU.add), [rt], [rsm])
        k.fn("dve", lambda e: e.reciprocal(out=rsm[:, 2:3], in_=rsm[:, 1:2]), [rsm], [rsm])
        k.ts("dve", rt[:, 44:48], rt[:, 4:8], 0.0, -1e30, ALU.is_lt, ALU.mult)
        k.tt("dve", rt[:, 8:40].rearrange("p (g e) -> p g e", e=8), rt[:, 8:40].rearrange("p (g e) -> p g e", e=8),
             rt[:, 44:48].unsqueeze(2).broadcast_to([128, 4, 8]), ALU.add)
        k.fn("dve", lambda e: e.max(out=r8[:], in_=rt[:, 8:40]), [rt], [r8])
        k.tt("dve", rsm[:, 3:4], r8[:, 1:2], r8[:, 0:1], ALU.subtract)
        k.act(rsm[:, 4:5], rsm[:, 3:4], AF.Exp)
        k.ts("dve", rsm[:, 5:6], rsm[:, 4:5], 1.0, None, ALU.add)
        k.fn("dve", lambda e: e.reciprocal(out=rsm[:, 6:7], in_=rsm[:, 5:6]), [rsm], [rsm])
        k.tt("dve", rsm[:, 7:8], rsm[:, 6:7], rsm[:, 2:3], ALU.mult)
        k.tt("dve", rsm[:, 8:9], rsm[:, 7:8], rsm[:, 4:5], ALU.mult)
        if oh1 is not None:
            k.ts("dve", oh1, rt[:, 8:40], r8[:, 0:1], None, ALU.is_equal)
            k.ts("dve", oh2, rt[:, 8:40], r8[:, 1:2], None, ALU.is_equal)
            k.copy("dve", gts, rsm[:, 7:9])
            return
        k.ts("dve", Gout, rt[:, 8:40], r8[:, 0:1], rsm[:, 7:8], ALU.is_equal, ALU.mult, r=[rt, r8, rsm], w=[Gout])
        k.ts("dve", rt[:, 8:40], rt[:, 8:40], r8[:, 1:2], rsm[:, 8:9], ALU.is_equal, ALU.mult)
        k.tt("dve", Gout, Gout, rt[:, 8:40], ALU.add, r=[Gout, rt], w=[Gout])


_CACHE = {}


def _host_inputs(inputs, core):
    b0 = core * NBC
    f = np.float32
    hin = np.concatenate([inputs["ctx"][b0:b0 + NBC], inputs["x"][b0:b0 + NBC]], axis=1)
    cc = np.concatenate([inputs["c"][b0:b0 + NBC], inputs["c_ctx"][None, :]], axis=0)
    return {"hin": np.ascontiguousarray(hin, dtype=f), "cc": np.ascontiguousarray(cc, dtype=f)}


def _shared_inputs(inputs):
    f = np.float32
    wr = np.zeros((DEPTH, D, 64), f)
    wr[:, :, 0:4] = inputs["moe_w_group"]
    wr[:, :, 4:36] = inputs["moe_w_expert"]
    conv_wb = np.concatenate([inputs["conv_w"], inputs["conv_b"][:, None, :]], axis=1)
    rows = np.repeat(np.arange(SEQ // 64, dtype=f), 64)
    cols = np.tile(np.arange(64, dtype=f), SEQ // 64)

    def rope_tabs(dim):
        q = dim // 4
        inv = (np.float32(10000.0) ** (-np.arange(q, dtype=f) / np.float32(q))).astype(f)
        ang = np.concatenate([rows[:, None] * inv[None, :], cols[:, None] * inv[None, :]], axis=-1).astype(f)
        return np.ascontiguousarray(np.cos(ang).T.astype(f)), np.ascontiguousarray(np.sin(ang).T.astype(f))
    rc, rs_ = rope_tabs(256)
    mc, ms = rope_tabs(64)
    c1 = np.ascontiguousarray(np.concatenate([mc, mc, mc, mc], axis=0))
    c2 = np.ascontiguousarray(np.concatenate([-ms, ms, -ms, ms], axis=0))
    wdn = inputs["mla_w_down"][0]
    kr = wdn[:, 640:704]
    mla_wd = np.ascontiguousarray(np.concatenate([wdn[:, 0:640], kr, kr[:, 32:64], kr[:, 0:32]], axis=1))
    wuq = inputs["mla_w_uq"][0].reshape(384, 8, 192)
    rp = wuq[:, :, 128:192]
    mla_wuq = np.ascontiguousarray(np.concatenate([
        wuq[:, :, 0:128].reshape(384, 1024), rp.reshape(384, 512),
        np.concatenate([rp[:, :, 32:64], rp[:, :, 0:32]], axis=2).reshape(384, 512)], axis=1))
    wukv = inputs["mla_w_ukv"][0].reshape(256, 8, 256)
    mla_wukv = np.ascontiguousarray(np.concatenate([wukv[:, :, 0:128].reshape(256, 1024), wukv[:, :, 128:256].reshape(256, 1024)], axis=1))
    norms = np.ascontiguousarray(np.concatenate([inputs["mla_q_norm"][0].reshape(3, 128), inputs["mla_kv_norm"][0].reshape(2, 128)], axis=0))
    return {
        "mla_wd": mla_wd, "mla_norms": norms, "mla_wuq": mla_wuq, "mla_wukv": mla_wukv,
        "mla_w_out": np.ascontiguousarray(inputs["mla_w_out"][0]), "mla_c1": c1, "mla_c2": c2,
        "ret_w_in": inputs["ret_w_in"], "ret_decay": np.ascontiguousarray(inputs["ret_decay"].reshape(2, 8)),
        "ret_gn_g": inputs["ret_gn_g"], "ret_w_out": inputs["ret_w_out"], "ret_cos": rc, "ret_sin": rs_,
        "ada_w": inputs["ada_w"], "ada_b": np.ascontiguousarray(inputs["ada_b"][:, None, :]),
        "ln_g": inputs["ln_g"], "ln_b": inputs["ln_b"],
        "conv_w_in": inputs["conv_w_in"], "conv_wb": np.ascontiguousarray(conv_wb), "conv_w_out": inputs["conv_w_out"],
        "moe_wr": wr, "moe_bg": inputs["moe_b_group"], "moe_be": inputs["moe_b_expert"],
        "moe_w1": np.ascontiguousarray(inputs["moe_w1"].reshape(DEPTH, NE, 8, 128, 512).transpose(0, 1, 3, 2, 4)),
        "moe_w3": np.ascontiguousarray(inputs["moe_w3"].reshape(DEPTH, NE, 8, 128, 512).transpose(0, 1, 3, 2, 4)),
        "moe_w2": np.ascontiguousarray(inputs["moe_w2"].reshape(DEPTH, NE, 4, 128, D).transpose(0, 1, 3, 2, 4)),
    }


def kernel(**inputs):
    inputs = {k_: np.asarray(v) for k_, v in inputs.items()}
    if "prog" not in _CACHE:
        _CACHE["prog"] = Prog()
    prog = _CACHE["prog"]
    shared = _shared_inputs(inputs)
    in_maps = []
    for core in range(8):
        m = dict(shared)
        m.update(_host_inputs(inputs, core))
        in_maps.append({k_: np.ascontiguousarray(v, dtype=np.float32) for k_, v in m.items() if k_ in prog.I})
    res = run_bass_kernel_spmd(prog.nc, in_maps, core_ids=list(range(8)))
    return np.concatenate([r["out"] for r in res.results], axis=0).astype(np.float32)
```

```python
import contextlib
import numpy as np
import concourse.bass as bass
import concourse.mybir as mybir
from concourse.bass_utils import run_bass_kernel_spmd

F32 = mybir.dt.float32
BF16 = mybir.dt.bfloat16
AF = mybir.ActivationFunctionType
ALU = mybir.AluOpType

EPOCH_D = 1500
EPOCH_C = 30000
PH = "__phase__"

D = 1024
NBC = 2
SEQ = 4096
CTX = 256
TT = SEQ + CTX
DEPTH = 4
ALPHA = (2.0 * DEPTH) ** 0.25
LN_EPS = 1e-5
RMS_EPS = 1e-6
NE = 32


def _key(x):
    if isinstance(x, (tuple, str)):
        return x
    t = getattr(x, "tensor", None)
    if t is not None:
        return t.name
    return x.name


class Sched:
    def __init__(self, nc):
        self.nc = nc
        self.ops = []

    def op(self, eng, fn, r=(), w=(), dma=False):
        rk = [_key(k) for k in r]
        wk = [_key(k) for k in w]
        for kk in rk:
            if isinstance(kk, str) and kk.startswith("ps") and kk not in wk:
                wk.append(kk)
        rk.append(PH)
        self.ops.append([eng, fn, tuple(rk), tuple(wk), dma])

    def dma(self, eng, out, in_, r=None, w=None, store=False, skey=None, **kw):
        r = [in_] if r is None else r
        w = [out] if w is None else w
        if skey is None:
            skey = _key(in_) if store else _key(out)
        self.op(eng, lambda e, out=out, in_=in_, kw=kw: e.dma_start(out=out, in_=in_, **kw), r, w, dma=skey)

    def barrier(self):
        self.ops.append(["sp", lambda e: e.nop(), (), (PH,), False])

    def finish(self):
        nc = self.nc
        ops = self.ops
        n = len(ops)
        last_w = {}
        rd_eng = {}
        rd_dma = {}
        deps_all = [None] * n
        for i, (eng, fn, r, w, dma) in enumerate(ops):
            deps = set()
            for k in r:
                j = last_w.get(k)
                if j is not None:
                    deps.add(j)
            for k in w:
                j = last_w.get(k)
                if j is not None:
                    deps.add(j)
                d = rd_eng.get(k)
                if d:
                    deps.update(d.values())
                d = rd_dma.get(k)
                if d:
                    deps.update(d)
            for k in r:
                if dma:
                    rd_dma.setdefault(k, []).append(i)
                else:
                    rd_eng.setdefault(k, {})[eng] = i
            for k in w:
                last_w[k] = i
                rd_eng[k] = {}
                rd_dma[k] = []
            deps.discard(i)
            deps_all[i] = deps
        need = [False] * n
        for i, (eng, fn, r, w, dma) in enumerate(ops):
            keep = set()
            for j in deps_all[i]:
                je, _, _, _, jd = ops[j]
                if jd:
                    keep.add(j)
                elif je != eng or eng != "pe":
                    keep.add(j)
            deps_all[i] = keep
            for j in keep:
                need[j] = True
        pos = [None] * n
        cnt = {}
        for i, (eng, fn, r, w, dma) in enumerate(ops):
            if dma:
                stream = ("dma", dma)
            elif need[i]:
                stream = ("eng", eng)
            else:
                continue
            c = cnt.get(stream, 0)
            pos[i] = (stream, c)
            cnt[stream] = c + 1
        phase = [0] * n
        ph = 0
        for i, o in enumerate(ops):
            if o[3] == (PH,):
                ph += 1
            phase[i] = ph
        seg = {}
        for i in range(n):
            if pos[i] is None:
                continue
            stream, c = pos[i]
            EP = EPOCH_D if stream[0] == "dma" else EPOCH_C
            sk = (stream, c // EP)
            g = seg.get(sk)
            if g is None:
                seg[sk] = [phase[i], phase[i], 1]
            else:
                g[1] = phase[i]
                g[2] += 1
        es = contextlib.ExitStack()
        hw = []
        base = {}
        semof = {}
        for sk in sorted(seg, key=lambda q: seg[q][0]):
            pf, pl, c = seg[sk]
            inc = 16 if sk[0][0] == "dma" else 1
            pick = None
            for hwi in hw:
                if hwi[2] < pf and hwi[1] + c * inc <= 30000:
                    pick = hwi
                    break
            if pick is None:
                pick = [es.enter_context(nc.semaphore(f"s{len(hw)}")), 0, -1]
                hw.append(pick)
            base[sk] = pick[1]
            semof[sk] = pick[0]
            pick[1] += c * inc
            pick[2] = pl
        self.nsem = len(hw)
        by_eng = {}
        for i, o in enumerate(ops):
            by_eng.setdefault(o[0], []).append(i)

        def emit(e, eng):
            waited = {}
            for i in by_eng.get(eng, []):
                _, fn, r, w, dma = ops[i]
                reqs = {}
                for j in deps_all[i]:
                    stream, c = pos[j]
                    isd = stream[0] == "dma"
                    EP = EPOCH_D if isd else EPOCH_C
                    sk = (stream, c // EP)
                    v = base[sk] + (c % EP + 1) * (16 if isd else 1)
                    sm = semof[sk]
                    if reqs.get(id(sm), (0, None))[0] < v:
                        reqs[id(sm)] = (v, sm)
                for sid, (v, sm) in reqs.items():
                    if waited.get(sid, 0) < v:
                        e.wait_ge(sm, v)
                        waited[sid] = v
                ins = fn(e)
                if pos[i] is not None:
                    stream, c = pos[i]
                    isd = stream[0] == "dma"
                    EP = EPOCH_D if isd else EPOCH_C
                    ins.then_inc(semof[(stream, c // EP)], 16 if isd else 1)

        with nc.Block() as block:
            @block.tensor
            def _(e): emit(e, "pe")

            @block.scalar
            def _(e): emit(e, "act")

            @block.vector
            def _(e): emit(e, "dve")

            @block.gpsimd
            def _(e): emit(e, "pool")

            @block.sync
            def _(e): emit(e, "sp")
        es.close()


class KB:
    def __init__(self, nc):
        self.nc = nc
        self.S = Sched(nc)
        self.gs = contextlib.ExitStack()

    def sb(self, es, name, shape, dt=F32):
        self.uid = getattr(self, "uid", 0) + 1
        return es.enter_context(self.nc.sbuf_tensor(f"{name}_u{self.uid}", list(shape), dt))

    def dram(self, name, shape, dt, kind):
        return self.nc.dram_tensor(name, list(shape), dt, kind=kind).ap()

    @staticmethod
    def _rw(out, ins, r, w):
        if r is None:
            r = [a for a in ins if not isinstance(a, (int, float)) and a is not None]
        if w is None:
            w = [out]
        return r, w

    def mm(self, out, lhsT, rhs, start, stop, r=None, w=None):
        r, w = self._rw(out, [lhsT, rhs], r, w)
        self.S.op("pe", lambda e: e.matmul(out, lhsT=lhsT, rhs=rhs, start=start, stop=stop), r, w)

    def tr(self, out, in_, ident, r=None, w=None):
        r, w = self._rw(out, [in_, ident], r, w)
        self.S.op("pe", lambda e: e.transpose(out, in_, ident), r, w)

    def act(self, out, in_, func, scale=None, bias=None, r=None, w=None):
        r, w = self._rw(out, [in_, scale, bias], r, w)
        kw = {}
        if scale is not None:
            kw["scale"] = scale
        if bias is not None:
            kw["bias"] = bias
        self.S.op("act", lambda e: e.activation(out=out, in_=in_, func=func, **kw), r, w)

    def tt(self, eng, out, in0, in1, op, r=None, w=None):
        r, w = self._rw(out, [in0, in1], r, w)
        self.S.op(eng, lambda e: e.tensor_tensor(out=out, in0=in0, in1=in1, op=op), r, w)

    def ts(self, eng, out, in0, s1, s2, op0, op1=None, r=None, w=None):
        r, w = self._rw(out, [in0, s1, s2], r, w)
        if op1 is None:
            self.S.op(eng, lambda e: e.tensor_scalar(out=out, in0=in0, scalar1=s1, scalar2=None, op0=op0), r, w)
        else:
            self.S.op(eng, lambda e: e.tensor_scalar(out=out, in0=in0, scalar1=s1, scalar2=s2, op0=op0, op1=op1), r, w)

    def stt(self, out, in0, scalar, in1, op0, op1, r=None, w=None):
        r, w = self._rw(out, [in0, scalar, in1], r, w)
        self.S.op("dve", lambda e: e.scalar_tensor_tensor(out=out, in0=in0, scalar=scalar, in1=in1, op0=op0, op1=op1), r, w)

    def copy(self, eng, out, in_, r=None, w=None):
        r, w = self._rw(out, [in_], r, w)
        if eng == "act":
            self.S.op("act", lambda e: e.copy(out=out, in_=in_), r, w)
        else:
            self.S.op(eng, lambda e: e.tensor_copy(out=out, in_=in_), r, w)

    def memset(self, eng, ap, val, w=None):
        self.S.op(eng, lambda e: e.memset(ap, val), (), [ap] if w is None else w)

    def fn(self, eng, f, r, w):
        self.S.op(eng, f, r, w)

    def dma(self, eng, out, in_, **kw):
        self.S.dma(eng, out, in_, **kw)


def tiles_of(b, with_ctx=True):
    t = []
    if with_ctx:
        t.append((b, 0, CTX, 2))
    for j in range(SEQ // 512):
        t.append((b, CTX + 512 * j, 512, b))
    return t


class Prog:
    def __init__(self, layers=(0, 1, 2, 3), h_from_input=True, dbg=None):
        self.layers = list(layers)
        self.dbg = dbg
        NL = len(self.layers)
        self.li = {l: j for j, l in enumerate(self.layers)}
        nc = bass.Bass("TRN2", target_bir_lowering=False)
        self.nc = nc
        k = KB(nc)
        self.k = k
        NEd = dbg[2] if isinstance(dbg, tuple) else NE
        shapes = {
            "hin": [NBC, TT, D], "cc": [3, D],
            "ada_w": [NL, D, 6 * D], "ada_b": [NL, 1, 6 * D], "ln_g": [NL, 2, D], "ln_b": [NL, 2, D],
            "ret_w_in": [2, D, 6 * D], "ret_decay": [2, 8], "ret_gn_g": [2, 2 * D], "ret_w_out": [2, 2 * D, D],
            "ret_cos": [128, SEQ], "ret_sin": [128, SEQ],
            "mla_wd": [D, 768], "mla_norms": [5, 128], "mla_wuq": [384, 2048], "mla_wukv": [256, 2048],
            "mla_w_out": [D, D], "mla_c1": [128, SEQ], "mla_c2": [128, SEQ],
            "conv_w_in": [1, D, 3 * D], "conv_wb": [1, 4, D], "conv_w_out": [1, D, D],
            "moe_wr": [NL, D, 64], "moe_bg": [NL, 4], "moe_be": [NL, 32],
            "moe_w1": [NL, NE, 128, 8, 512], "moe_w3": [NL, NE, 128, 8, 512], "moe_w2": [NL, NE, 128, 4, D],
        }

        class Lazy(dict):
            def __missing__(d, name):
                d[name] = k.dram(name, shapes[name], F32, "ExternalInput")
                return d[name]
        I = Lazy()
        self.I = I
        self.out = k.dram("out", [NBC, SEQ, D], F32, "ExternalOutput")
        self.HA = k.dram("HA", [NBC, TT, D], F32, "ExternalOutput" if dbg else "Internal")
        self.HB = k.dram("HB", [NBC, TT, D], F32, "ExternalOutput" if dbg else "Internal")
        self.SC1 = k.dram("SC1", [NBC, D, SEQ + 2], F32, "Internal")
        self.SC2 = k.dram("SC2", [NBC, D, SEQ], F32, "Internal")
        self.SC1c = k.dram("SC1c", [NBC, D, CTX + 2], F32, "Internal")
        self.SC2c = k.dram("SC2c", [NBC, D, CTX], F32, "Internal")
        if any(l % 3 == 0 for l in self.layers):
            for nm in ("QT", "KT", "QTF", "QTB"):
                setattr(self, nm, k.dram(nm, [NBC, D, TT], BF16, "Internal"))
            for nm in ("KF", "KB"):
                setattr(self, nm, k.dram(nm, [NBC, TT, D], BF16, "Internal"))
            for nm in ("RV", "RG"):
                setattr(self, nm, k.dram(nm, [NBC, TT, 2 * D], BF16, "Internal"))
            self.SBD = k.dram("SBD", [NBC, TT // 128, 128, 8 * 512], BF16, "Internal")
        if any(l % 3 == 1 for l in self.layers):
            self.QN = k.dram("QN", [NBC, 8, 128, TT], BF16, "Internal")
            self.QR = k.dram("QR", [NBC, 4, 128, TT], BF16, "Internal")
            self.KN = k.dram("KN", [NBC, 8, 128, TT], BF16, "Internal")
            self.KR = k.dram("KR", [NBC, 64, TT], BF16, "Internal")
            self.MV = k.dram("MV", [NBC, TT, D], BF16, "Internal")
            self.OT = k.dram("OT", [NBC, D, TT], BF16, "Internal")
        NROWS = ((2 * (TT // 128) * NBC * 128 + 511) // 512 + NE) * 512
        self.XS = k.dram("XS", [NROWS, D], F32, "Internal")
        self.YS = k.dram("YS", [NROWS, D], F32, "Internal")
        gs = k.gs
        self.PS = [gs.enter_context(nc.psum_tensor(f"ps{i}", [128, 512], F32)) for i in range(8)]
        self.ident = k.sb(gs, "ident", [128, 128])
        self.actT = k.sb(gs, "actT", [128, 8, 4])
        self.ones = k.sb(gs, "ones", [1, 512])
        self.negh = k.sb(gs, "negh", [128, 512])
        self.onesf = k.sb(gs, "onesf", [128, 128])
        self.onesb = k.sb(gs, "onesb", [128, 128], BF16)
        self.MP = k.sb(gs, "MP", [128, 4, 8, 4])
        self.GB = k.sb(gs, "GB", [128, 3, 2, 1024])
        self.LNP = k.sb(gs, "LNP", [128, 4, 1024])
        self.setup()
        first = True
        for i in self.layers:
            src = I["hin"] if (first and h_from_input) else self.HA
            first = False
            self.mod_params(i)
            if isinstance(dbg, tuple) and dbg[0] == "moe":
                self.moe_layer(i, src, self.HA, False, dbg_ns=dbg[1], dbg_ne=dbg[2])
                break
            if dbg == "mod":
                d1 = k.dram("dbg_MP", [128, 4 * 8 * 4], F32, "ExternalOutput")
                d2 = k.dram("dbg_GB", [128, 3 * 2 * 1024], F32, "ExternalOutput")
                d3 = k.dram("dbg_LNP", [128, 4 * 1024], F32, "ExternalOutput")
                k.dma("sp", d1, self.MP[:].rearrange("p a b c -> p (a b c)"), store=True)
                k.dma("sp", d2, self.GB[:].rearrange("p a b c -> p (a b c)"), store=True)
                k.dma("sp", d3, self.LNP[:].rearrange("p a c -> p (a c)"), store=True)
                break
            kind = i % 3
            if kind == 2:
                self.conv_layer(i, src, self.HB)
            elif kind == 0:
                self.ret_layer(i, src, self.HB)
            elif kind == 1:
                self.mla_layer(i, src, self.HB)
            else:
                raise NotImplementedError
            if dbg == "mix":
                break
            last = (i == DEPTH - 1)
            self.moe_layer(i, self.HB, self.HA, last)
        import os
        mo = int(os.environ.get("MAXOPS", "0"))
        if mo:
            for o in k.S.ops[mo:mo + 3]:
                print("TRUNC next ops:", o[0], o[2][:3], o[3])
            del k.S.ops[mo:]
        k.S.barrier()
        k.S.op("sp", lambda e: e.nop(), (), ())
        k.S.finish()
        gs.close()

    def setup(self):
        k = self.k
        nc = self.nc
        ident = self.ident
        k.memset("pool", ident[:], 0.0)
        k.fn("pool", lambda e: e.affine_select(out=ident[:], in_=ident[:], pattern=[[-1, 128]], compare_op=ALU.not_equal,
                                               fill=1.0, base=0, channel_multiplier=1), [ident], [ident])
        k.memset("dve", self.ones[:], 1.0)
        k.memset("dve", self.negh[:], -0.5)
        k.memset("dve", self.onesf[:], 1.0)
        k.memset("dve", self.onesb[:], 1.0)
        with contextlib.ExitStack() as es:
            craw = k.sb(es, "craw", [4, D])
            k.memset("dve", craw[:], 0.0)
            k.dma("sp", craw[0:3, :], self.I["cc"][:, :])
            k.act(craw[:], craw[:], AF.Silu)
            ps = self.PS[0]
            for kk in range(8):
                k.tr(ps[:, kk * 4:(kk + 1) * 4], craw[0:4, kk * 128:(kk + 1) * 128], ident[0:4, 0:4])
            k.copy("dve", self.actT[:], ps[:, 0:32].rearrange("p (k f) -> p k f", f=4))
            k.S.barrier()

    def mod_params(self, i):
        k = self.k
        I = self.I
        i = self.li[i]
        with contextlib.ExitStack() as es:
            wblk = [k.sb(es, f"mp_w{j}", [128, 8, 1024]) for j in range(2)]
            brow = k.sb(es, "mp_brow", [1, 6 * D])
            REP = k.sb(es, "mp_REP", [128, 3, 8, 128])
            for r in range(3):
                for kk in range(8):
                    k.copy("dve", REP[:, r, kk, :], self.actT[:, kk, r:r + 1].broadcast_to([128, 128]))
            k.dma("sp", brow[:], I["ada_b"][i])
            for j in range(4):
                k.dma("sp", self.LNP[:, j, :], (I["ln_g"] if j % 2 == 0 else I["ln_b"])[i, j // 2].partition_broadcast(128))
            pi = 0
            for wi in range(6):
                buf = wblk[wi % 2]
                k.dma("sp", buf[:], I["ada_w"][i][:, wi * 1024:(wi + 1) * 1024].rearrange("(k p) n -> p k n", p=128))
                if wi in (0, 1, 3, 4):
                    slot = {0: 0, 1: 1, 3: 2, 4: 3}[wi]
                    ps = self.PS[pi % 8]
                    pi += 1
                    for ko in range(8):
                        o = ps[:, ko * 4:ko * 4 + 3]
                        for ki in range(8):
                            k.mm(o, buf[:, ki, ko * 128:(ko + 1) * 128], self.actT[:, ki, 0:3], ki == 0, False)
                        k.mm(o, brow[0:1, wi * 1024 + ko * 128: wi * 1024 + (ko + 1) * 128], self.ones[0:1, 0:3], False, True)
                    src = ps[:, 0:32].rearrange("p (k f) -> p k f", f=4)[:, :, 0:3]
                    if wi in (1, 4):
                        k.ts("dve", self.MP[:, slot, :, 0:3], src, 1.0, None, ALU.add)
                    else:
                        k.copy("dve", self.MP[:, slot, :, 0:3], src)
                else:
                    gi = 0 if wi == 2 else 1
                    for r in range(3):
                        for half in range(2):
                            ps = self.PS[pi % 8]
                            pi += 1
                            for ki in range(8):
                                k.mm(ps[:], REP[:, r, ki, :], buf[:, ki, half * 512:(half + 1) * 512], ki == 0, False)
                            k.mm(ps[:], self.ones[0:1, 0:128], brow[0:1, wi * 1024 + half * 512: wi * 1024 + (half + 1) * 512], False, True)
                            k.copy("act", self.GB[:, r, gi, half * 512:(half + 1) * 512], ps[:])
            k.S.barrier()

    def prologue(self, src, b, t0, n, r, slot_sh, stg, uT, psb, v32=None):
        k = self.k
        nt = n // 128
        k.dma("sp", stg[:, 0:nt, :], src[b, t0:t0 + n, :].rearrange("(t p) d -> p t d", p=128),
              r=[(_key(src), b, t0)])
        for kk in range(8):
            ps = psb[kk % len(psb)]
            for t in range(nt):
                k.tr(ps[:, t * 128:(t + 1) * 128], stg[:, t, kk * 128:(kk + 1) * 128], self.ident[:])
            k.act(uT[:, kk, 0:n], ps[:, 0:n], AF.Identity, scale=self.MP[:, slot_sh + 1, kk, r:r + 1],
                  bias=self.MP[:, slot_sh, kk, r:r + 1])
            if v32 is not None:
                k.ts("dve", v32[:, kk, 0:n], ps[:, 0:n], self.MP[:, slot_sh + 1, kk, r:r + 1],
                     self.MP[:, slot_sh, kk, r:r + 1], ALU.mult, ALU.add)

    def ln_epilogue(self, ysrc, stg, nt, r, gi, li, zb, st, mv, rs, dst_rows, dst_key):
        k = self.k
        if isinstance(zb, list):
            self._lnc = getattr(self, "_lnc", 0) + 1
            x = self._lnc % len(zb)
            zb, st, mv, rs = zb[x], st[x], mv[x], rs[x]
        zk = lambda t: (_key(zb), t)
        for t in range(nt):
            z = zb[:, t, :]
            for half in range(2):
                y = ysrc(t, half)
                k.tt("dve", zb[:, t, half * 512:(half + 1) * 512], y, self.GB[:, r, gi, half * 512:(half + 1) * 512], ALU.mult,
                     r=[y, self.GB], w=[zk(t)])
            k.stt(z, stg[:, t, :], ALPHA, z, ALU.mult, ALU.add, r=[stg, zk(t)], w=[zk(t)])
            for c in range(2):
                k.fn("dve", (lambda e, t=t, c=c: e.bn_stats(out=st[:, t, c * 6:(c + 1) * 6], in_=zb[:, t, c * 512:(c + 1) * 512])),
                     [zk(t)], [(_key(st), t, c)])
            k.fn("dve", (lambda e, t=t: e.bn_aggr(out=mv[:, t, :], in_=st[:, t, :])), [(_key(st), t, 0), (_key(st), t, 1)], [(_key(mv), t)])
        k.ts("pool", rs[:, 0:nt], mv[:, 0:nt, 1], LN_EPS, None, ALU.add, r=[(_key(mv), t) for t in range(nt)], w=[rs])
        k.tt("pool", rs[:, 0:nt], rs[:, 0:nt], self.negh[:, 0:nt], ALU.pow)
        k.tt("pool", rs[:, 4:4 + nt], mv[:, 0:nt, 0], rs[:, 0:nt], ALU.mult, r=[(_key(mv), t) for t in range(nt)] + [rs], w=[rs])
        k.ts("pool", rs[:, 4:4 + nt], rs[:, 4:4 + nt], -1.0, None, ALU.mult)
        for t in range(nt):
            z = zb[:, t, :]
            k.act(z, z, AF.Identity, scale=rs[:, t:t + 1], bias=rs[:, 4 + t:5 + t], r=[zk(t), rs], w=[zk(t)])
            k.tt("dve", z, z, self.LNP[:, 2 * li, :], ALU.mult, r=[zk(t), self.LNP], w=[zk(t)])
            k.tt("pool", z, z, self.LNP[:, 2 * li + 1, :], ALU.add, r=[zk(t), self.LNP], w=[zk(t)])
        k.dma("pool", dst_rows.rearrange("(t p) d -> p t d", p=128), zb[:, 0:nt, :], store=True,
              r=[zk(t) for t in range(nt)], w=[dst_key])

    def conv_layer(self, i, src, dst):
        k = self.k
        I = self.I
        PS = self.PS
        need_ctx = i < DEPTH - 1
        with contextlib.ExitStack() as es:
            win = k.sb(es, "cv_win", [128, 8, 3 * D], BF16)
            k.dma("pool", win[:], I["conv_w_in"][0].rearrange("(k p) n -> p k n", p=128))
            stg = [k.sb(es, "cv_stg0", [128, 4, D])]
            uT = [k.sb(es, f"cv_uT{j}", [128, 8, 512], BF16) for j in range(2)]
            sbuf = [k.sb(es, f"cv_s{j}", [128, 8, 512]) for j in range(2)]
            gbuf = [k.sb(es, f"cv_g{j}", [128, 8, 512]) for j in range(2)]
            gct = [k.sb(es, f"cv_gc{j}", [128, 512]) for j in range(2)]
            zero = k.sb(es, "cv_zero", [128, 8, 1])
            k.memset("dve", zero[:], 0.0)
            it = 0
            for b in range(NBC):
                for (bb, t0, n, r) in tiles_of(b, need_ctx):
                    isctx = (r == 2)
                    s1 = (self.SC1c if isctx else self.SC1)
                    s2 = (self.SC2c if isctx else self.SC2)
                    c0 = t0 if isctx else t0 - CTX
                    sg, u, sbf, gbf = stg[0], uT[it % 2], sbuf[it % 2], gbuf[it % 2]
                    self.prologue(src, b, t0, n, r, 0, sg, u, [PS[6], PS[7]])
                    for j in range(8):
                        pb, pc, ph = PS[(3 * j) % 6], PS[(3 * j + 1) % 6], PS[(3 * j + 2) % 6]
                        for (pp, col) in ((pb, j), (pc, 8 + j), (ph, 16 + j)):
                            for kk in range(8):
                                k.mm(pp[:, 0:n], win[:, kk, col * 128:(col + 1) * 128], u[:, kk, 0:n], kk == 0, kk == 7)
                        g = gct[j % 2]
                        k.copy("act", gbf[:, j, 0:n], pb[:, 0:n], w=[(_key(gbf), j)])
                        k.copy("act", g[:, 0:n], pc[:, 0:n])
                        k.tt("dve", sbf[:, j, 0:n], g[:, 0:n], ph[:, 0:n], ALU.mult, w=[(_key(sbf), j)])
                    k.dma("pool", s1[b, :, 1 + c0:1 + c0 + n].rearrange("(k p) t -> p k t", p=128), sbf[:, :, 0:n], store=True,
                          r=[(_key(sbf), j) for j in range(8)], w=[("SC1", b, isctx, c0)])
                    k.dma("act", s2[b, :, c0:c0 + n].rearrange("(k p) t -> p k t", p=128), gbf[:, :, 0:n], store=True,
                          r=[(_key(gbf), j) for j in range(8)], w=[("SC2", b, isctx, c0)])
                    it += 1
                for (s1, L) in (((self.SC1c, CTX), (self.SC1, SEQ)) if need_ctx else ((self.SC1, SEQ),)):
                    for col in (0, L + 1):
                        k.dma("sp", s1[b, :, col:col + 1].rearrange("(k p) t -> p k t", p=128), zero[:], store=True,
                              w=[("SC1h", b, L, col)], allow_slow_non_contiguous=True)
            k.S.barrier()
        with contextlib.ExitStack() as es:
            wout = k.sb(es, "cv_wout", [128, 8, D], BF16)
            cw = k.sb(es, "cv_cw", [128, 4, 8])
            craw = k.sb(es, "cv_craw", [32, 128])
            k.dma("pool", wout[:], I["conv_w_out"][0].rearrange("(k p) n -> p k n", p=128))
            k.dma("sp", craw[:], I["conv_wb"][0].rearrange("j (k p) -> (j k) p", p=128))
            k.tr(PS[7][:, 0:32], craw[:, :], self.ident[0:32, 0:32])
            k.copy("dve", cw[:], PS[7][:, 0:32].rearrange("p (j k) -> p j k", k=8))
            stg = [k.sb(es, f"cv_stg{j}", [128, 4, D]) for j in range(2)]
            gbuf = [k.sb(es, f"cv_g{j}", [128, 8, 512]) for j in range(2)]
            zb = [k.sb(es, f"cv_zb{x}", [128, 4, D]) for x in range(1)]
            st = [k.sb(es, f"cv_st{x}", [128, 4, 12]) for x in range(1)]
            mv = [k.sb(es, f"cv_mv{x}", [128, 4, 2]) for x in range(1)]
            rs = [k.sb(es, f"cv_rs{x}", [128, 8]) for x in range(1)]
            sx = [k.sb(es, f"cv_sx{j}", [128, 8, 514]) for j in range(2)]
            gT = [k.sb(es, f"cv_gT{j}", [128, 8, 512], BF16) for j in range(2)]
            ctmp = [k.sb(es, f"cv_ct{j}", [128, 512]) for j in range(2)]
            it = 0
            for b in range(NBC):
                for (bb, t0, n, r) in tiles_of(b, need_ctx):
                    isctx = (r == 2)
                    s1 = (self.SC1c if isctx else self.SC1)
                    s2 = (self.SC2c if isctx else self.SC2)
                    c0 = t0 if isctx else t0 - CTX
                    nt = n // 128
                    sg, sxx, gbf, g = stg[it % 2], sx[it % 2], gbuf[it % 2], gT[it % 2]
                    k.dma("sp", sg[:, 0:nt, :], src[b, t0:t0 + n, :].rearrange("(t p) d -> p t d", p=128), r=[(_key(src), b, t0)])
                    k.dma("sp", sxx[:, :, 0:n + 2], s1[b, :, c0:c0 + n + 2].rearrange("(k p) t -> p k t", p=128), r=[("SC1all",)])
                    k.dma("sp", gbf[:, :, 0:n], s2[b, :, c0:c0 + n].rearrange("(k p) t -> p k t", p=128), r=[("SC2all",)])
                    for kk in range(8):
                        c = ctmp[kk % 2]
                        k.act(c[:, 0:n], sxx[:, kk, 1:n + 1], AF.Identity, scale=cw[:, 1, kk:kk + 1], bias=cw[:, 3, kk:kk + 1])
                        k.stt(c[:, 0:n], sxx[:, kk, 0:n], cw[:, 0, kk:kk + 1], c[:, 0:n], ALU.mult, ALU.add)
                        k.stt(c[:, 0:n], sxx[:, kk, 2:n + 2], cw[:, 2, kk:kk + 1], c[:, 0:n], ALU.mult, ALU.add)
                        k.tt("dve", g[:, kk, 0:n], c[:, 0:n], gbf[:, kk, 0:n], ALU.mult)
                    for t in range(nt):
                        for half in range(2):
                            ps = PS[(t * 2 + half) % 8]
                            for kk in range(8):
                                k.mm(ps[:], g[:, kk, t * 128:(t + 1) * 128], wout[:, kk, half * 512:(half + 1) * 512], kk == 0, kk == 7)
                    self.ln_epilogue(lambda t, half: PS[(t * 2 + half) % 8][:], sg, nt, r, 0, 0, zb, st, mv, rs,
                                     dst[b, t0:t0 + n, :], (_key(dst), b, t0))
                    it += 1
            k.S.barrier()

    def ret_layer(self, i, src, dst):
        k = self.k
        I = self.I
        PS = self.PS
        need_ctx = i < DEPTH - 1
        j = i // 3
        NCH = TT // 128
        with contextlib.ExitStack() as tes:
            lg = k.sb(tes, "rt_lg", [128, 8])
            GL = k.sb(tes, "rt_GL", [128, 8])
            MT = k.sb(tes, "rt_MT", [128, 4, 128])
            DF = k.sb(tes, "rt_DF", [128, 4, 128])
            DB = k.sb(tes, "rt_DB", [128, 4, 128])
            KFd = k.sb(tes, "rt_KFd", [128, 4])
            KBd = k.sb(tes, "rt_KBd", [128, 4])
            with contextlib.ExitStack() as es:
                Dm = k.sb(es, "rt_D", [128, 128])
                Dp = k.sb(es, "rt_Dp", [128, 128])
                Dn = k.sb(es, "rt_Dn", [128, 128])
                mf = k.sb(es, "rt_mf", [128, 128])
                mb = k.sb(es, "rt_mb", [128, 128])
                Ef = k.sb(es, "rt_Ef", [128, 128])
                Eb = k.sb(es, "rt_Eb", [128, 128])
                I1 = k.sb(es, "rt_I1", [128, 128])
                I2 = k.sb(es, "rt_I2", [128, 128])
                P1 = k.sb(es, "rt_P1", [128, 2])
                k.dma("sp", lg[:], I["ret_decay"][j].partition_broadcast(128))
                k.act(lg[:], lg[:], AF.Exp, scale=-1.0)
                k.act(lg[:], lg[:], AF.Ln, bias=1.0)
                k.ts("dve", lg[:], lg[:], -1.0, None, ALU.mult)
                k.act(GL[:], lg[:], AF.Exp, scale=128.0)
                k.fn("pool", lambda e: e.iota(Dm[:], pattern=[[1, 128]], base=0, channel_multiplier=-1,
                                              allow_small_or_imprecise_dtypes=True), [], [Dm])
                k.fn("pool", lambda e: e.iota(I1[:], pattern=[[1, 128]], base=1, channel_multiplier=0,
                                              allow_small_or_imprecise_dtypes=True), [], [I1])
                k.fn("pool", lambda e: e.iota(I2[:], pattern=[[-1, 128]], base=128, channel_multiplier=0,
                                              allow_small_or_imprecise_dtypes=True), [], [I2])
                k.fn("pool", lambda e: e.iota(P1[:, 0:1], pattern=[[0, 1]], base=127, channel_multiplier=-1,
                                              allow_small_or_imprecise_dtypes=True), [], [P1])
                k.fn("pool", lambda e: e.iota(P1[:, 1:2], pattern=[[0, 1]], base=0, channel_multiplier=1,
                                              allow_small_or_imprecise_dtypes=True), [P1], [P1])
                k.ts("dve", Dp[:], Dm[:], 0.0, None, ALU.max)
                k.ts("dve", Dn[:], Dm[:], -1.0, 0.0, ALU.mult, ALU.max)
                k.ts("dve", mf[:], Dm[:], 0.0, None, ALU.is_ge)
                k.ts("dve", mb[:], Dm[:], 0.0, None, ALU.is_le)
                for h in range(4):
                    k.act(Ef[:], Dp[:], AF.Exp, scale=lg[:, h:h + 1])
                    k.tt("dve", Ef[:], Ef[:], mf[:], ALU.mult)
                    k.act(Eb[:], Dn[:], AF.Exp, scale=lg[:, 4 + h:5 + h])
                    k.tt("dve", Eb[:], Eb[:], mb[:], ALU.mult)
                    k.tt("dve", Ef[:], Ef[:], Eb[:], ALU.add)
                    k.ts("dve", MT[:, h, :], Ef[:], 0.0625, None, ALU.mult)
                    k.act(DF[:, h, :], I1[:], AF.Exp, scale=lg[:, h:h + 1])
                    k.act(DB[:, h, :], I2[:], AF.Exp, scale=lg[:, 4 + h:5 + h])
                    k.act(KFd[:, h:h + 1], P1[:, 0:1], AF.Exp, scale=lg[:, h:h + 1])
                    k.act(KBd[:, h:h + 1], P1[:, 1:2], AF.Exp, scale=lg[:, 4 + h:5 + h])
                k.ts("dve", KFd[:], KFd[:], 0.0625, None, ALU.mult)
                k.ts("dve", KBd[:], KBd[:], 0.0625, None, ALU.mult)
                k.S.barrier()
            for part in range(2):
              with contextlib.ExitStack() as es:
                wqk = k.sb(es, "r1_wqk", [128, 8, D], BF16)
                k.dma("pool", wqk[:], I["ret_w_in"][j][:, part * D:(part + 1) * D].rearrange("(k p) n -> p k n", p=128))
                cosT = k.sb(es, "r1_cos", [128, SEQ])
                sinT = k.sb(es, "r1_sin", [128, SEQ])
                k.dma("sp", cosT[:], I["ret_cos"])
                k.dma("sp", sinT[:], I["ret_sin"])
                stg = k.sb(es, "r1_stg", [128, 4, D])
                uT = [k.sb(es, f"r1_uT{x}", [128, 8, 512], BF16) for x in range(2)]
                tm = [k.sb(es, f"r1_tm{x}", [128, 512]) for x in range(4)]
                if part == 0:
                    qT = k.sb(es, "r1_qT", [128, 8, 512], BF16)
                    qf = k.sb(es, "r1_qf", [128, 8, 512], BF16)
                    qb = k.sb(es, "r1_qb", [128, 8, 512], BF16)
                    q32 = [k.sb(es, f"r1_q32{x}", [128, 512]) for x in range(2)]
                else:
                    kT = k.sb(es, "r1_kT", [128, 8, 512], BF16)
                    k32 = k.sb(es, "r1_k32", [128, 8, 512])
                    kfb = k.sb(es, "r1_kf", [128, 4, D], BF16)
                    kbb = k.sb(es, "r1_kb", [128, 4, D], BF16)
                it = 0
                for b in range(NBC):
                    for (bb, t0, n, r) in tiles_of(b, True):
                        isctx = (r == 2)
                        c0 = t0 - CTX
                        nt = n // 128
                        u = uT[it % 2]
                        it += 1
                        self.prologue(src, b, t0, n, r, 0, stg, u, [PS[6], PS[7]])
                        for hh in range(part * 4, part * 4 + 4):
                            isq = hh < 4
                            h = hh % 4
                            c1, c2 = 2 * h, 2 * h + 1
                            x1, x2 = PS[(2 * hh) % 4], PS[(2 * hh + 1) % 4]
                            for kk in range(8):
                                k.mm(x1[:, 0:n], wqk[:, kk, c1 * 128:(c1 + 1) * 128], u[:, kk, 0:n], kk == 0, kk == 7)
                            for kk in range(8):
                                k.mm(x2[:, 0:n], wqk[:, kk, c2 * 128:(c2 + 1) * 128], u[:, kk, 0:n], kk == 0, kk == 7)
                            d1, d2 = 2 * h, 2 * h + 1
                            if isq:
                                o1, o2 = q32[0][:, 0:n], q32[1][:, 0:n]
                            else:
                                o1, o2 = k32[:, d1, 0:n], k32[:, d2, 0:n]
                            if isctx:
                                k.copy("act", o1, x1[:, 0:n])
                                k.copy("act", o2, x2[:, 0:n])
                            else:
                                cs, sn = cosT[:, c0:c0 + n], sinT[:, c0:c0 + n]
                                k.tt("dve", tm[0][:, 0:n], x1[:, 0:n], cs, ALU.mult)
                                k.tt("dve", tm[1][:, 0:n], x2[:, 0:n], sn, ALU.mult)
                                k.tt("dve", tm[2][:, 0:n], x1[:, 0:n], sn, ALU.mult)
                                k.tt("dve", tm[3][:, 0:n], x2[:, 0:n], cs, ALU.mult)
                                k.tt("pool", o1, tm[0][:, 0:n], tm[1][:, 0:n], ALU.subtract)
                                k.tt("pool", o2, tm[2][:, 0:n], tm[3][:, 0:n], ALU.add)
                            for (o, d) in ((o1, d1), (o2, d2)):
                                if isq:
                                    k.copy("act", qT[:, d, 0:n], o)
                                    o3 = o.rearrange("p (s i) -> p s i", i=128)
                                    k.tt("dve", qf[:, d, 0:n].rearrange("p (s i) -> p s i", i=128), o3,
                                         DF[:, h:h + 1, :].broadcast_to([128, nt, 128]), ALU.mult)
                                    k.tt("dve", qb[:, d, 0:n].rearrange("p (s i) -> p s i", i=128), o3,
                                         DB[:, h:h + 1, :].broadcast_to([128, nt, 128]), ALU.mult)
                                else:
                                    k.copy("act", kT[:, d, 0:n], o)
                        for s_ in range(nt if part == 1 else 0):
                            pa, pb = PS[4], PS[5]
                            for c in range(8):
                                pp = pa if c < 4 else pb
                                k.tr(pp[:, (c % 4) * 128:(c % 4 + 1) * 128], k32[:, c, s_ * 128:(s_ + 1) * 128], self.ident[:])
                            for h in range(4):
                                pp = pa if h < 2 else pb
                                sl = pp[:, (h % 2) * 256:(h % 2 + 1) * 256]
                                k.act(kfb[:, s_, h * 256:(h + 1) * 256], sl, AF.Identity, scale=KFd[:, h:h + 1])
                                k.ts("dve", kbb[:, s_, h * 256:(h + 1) * 256], sl, KBd[:, h:h + 1], None, ALU.mult)
                        for (buf, dr, qe) in (((qT, self.QT, "act"), (qf, self.QTF, "pool"), (qb, self.QTB, "pool")) if part == 0 else ((kT, self.KT, "act"),)):
                            k.dma(qe, dr[b, :, t0:t0 + n].rearrange("(k p) t -> p k t", p=128), buf[:, :, 0:n], store=True,
                                  w=[(_key(dr), b, t0)])
                        for (buf, dr, qe) in (((kfb, self.KF, "act"), (kbb, self.KB, "pool")) if part == 1 else ()):
                            k.dma(qe, dr[b, t0:t0 + n, :].rearrange("(s p) d -> p s d", p=128), buf[:, 0:nt, :], store=True,
                                  w=[(_key(dr), b, t0)])
                k.S.barrier()
            with contextlib.ExitStack() as es:
                wvg = k.sb(es, "r1_wvg", [128, 8, 4 * D], BF16)
                k.dma("pool", wvg[:], I["ret_w_in"][j][:, 2 * D:6 * D].rearrange("(k p) n -> p k n", p=128))
                stg = k.sb(es, "r1b_stg", [128, 4, D])
                uT = [k.sb(es, f"r1b_uT{x}", [128, 8, 512], BF16) for x in range(2)]
                vt = [k.sb(es, "r1b_vt0", [128, 4, 2 * D], BF16)] * 2
                gt = [k.sb(es, "r1b_gt0", [128, 4, 2 * D], BF16)] * 2
                it = 0
                for b in range(NBC):
                    for (bb, t0, n, r) in tiles_of(b, True):
                        nt = n // 128
                        u, vv, gg = uT[it % 2], vt[it % 2], gt[it % 2]
                        it += 1
                        self.prologue(src, b, t0, n, r, 0, stg, u, [PS[6], PS[7]])
                        pi = 0
                        for s_ in range(nt):
                            for nb in range(8):
                                ps = PS[pi % 6]
                                pi += 1
                                for kk in range(8):
                                    k.mm(ps[:], u[:, kk, s_ * 128:(s_ + 1) * 128], wvg[:, kk, nb * 512:(nb + 1) * 512], kk == 0, kk == 7)
                                if nb < 4:
                                    k.copy("act", vv[:, s_, nb * 512:(nb + 1) * 512], ps[:])
                                else:
                                    k.act(gg[:, s_, (nb - 4) * 512:(nb - 3) * 512], ps[:], AF.Silu)
                        k.dma("act", self.RV[b, t0:t0 + n, :].rearrange("(s p) d -> p s d", p=128), vv[:, 0:nt, :], store=True, w=[("RV", b, t0)])
                        k.dma("act", self.RG[b, t0:t0 + n, :].rearrange("(s p) d -> p s d", p=128), gg[:, 0:nt, :], store=True, w=[("RG", b, t0)])
                k.S.barrier()
            with contextlib.ExitStack() as es:
                Sb = k.sb(es, "r2_S", [128, 8, 512])
                sbf = [k.sb(es, f"r2_sbf{x}", [128, 8 * 512], BF16) for x in range(2)]
                kbc = [k.sb(es, f"r2_kb{x}", [128, D], BF16) for x in range(2)]
                vc = [k.sb(es, f"r2_v{x}", [128, 2 * D], BF16) for x in range(2)]
                it = 0
                for b in range(NBC):
                    k.memset("dve", Sb[:], 0.0)
                    order = [1, 0] + list(range(NCH - 1, 1, -1))
                    for oi, g in enumerate(order):
                        sf, kb_, v_ = sbf[it % 2], kbc[it % 2], vc[it % 2]
                        it += 1
                        k.copy("act", sf[:], Sb[:].rearrange("p a c -> p (a c)"))
                        k.dma("act", self.SBD[b, g], sf[:], store=True, w=[("SBD", b, g)])
                        if oi == len(order) - 1:
                            break
                        k.dma("sp", kb_[:], self.KB[b, g * 128:(g + 1) * 128, :], r=[("KBall",)])
                        k.dma("sp", v_[:], self.RV[b, g * 128:(g + 1) * 128, :], r=[("RVall",)])
                        for h in range(4):
                            for a in range(2):
                                ps = PS[(h * 2 + a) % 8]
                                k.mm(ps[:], kb_[:, h * 256 + a * 128:h * 256 + (a + 1) * 128], v_[:, h * 512:(h + 1) * 512], True, True)
                                k.stt(Sb[:, h * 2 + a, :], Sb[:, h * 2 + a, :], GL[:, 4 + h:5 + h], ps[:], ALU.mult, ALU.add)
                k.S.barrier()
            with contextlib.ExitStack() as es:
                wout = k.sb(es, "r3_wout", [128, 16, D], BF16)
                k.dma("pool", wout[:], I["ret_w_out"][j].rearrange("(k p) n -> p k n", p=128))
                gng = k.sb(es, "r3_gng", [128, 2 * D])
                k.dma("sp", gng[:], I["ret_gn_g"][j].partition_broadcast(128))
                Sf = k.sb(es, "r3_Sf", [128, 8, 512])
                Sfb = k.sb(es, "r3_Sfb", [128, 8, 512], BF16)
                L = []
                for x in range(2):
                    L.append(dict(
                        qT=k.sb(es, f"r3_qT{x}", [128, 8, 128], BF16), kT=k.sb(es, f"r3_kT{x}", [128, 8, 128], BF16),
                        qf=k.sb(es, f"r3_qf{x}", [128, 8, 128], BF16), qb=k.sb(es, f"r3_qb{x}", [128, 8, 128], BF16),
                        kf=k.sb(es, f"r3_kf{x}", [128, D], BF16), v=k.sb(es, f"r3_v{x}", [128, 2 * D], BF16),
                        g=(k.sb(es, f"r3_g{x}", [128, 2 * D], BF16) if x == 0 else None),
                        sb=(k.sb(es, f"r3_sb{x}", [128, 8, 512], BF16) if x == 0 else None),
                        h=k.sb(es, f"r3_h{x}", [128, 1, D])))
                L[1]["sb"] = L[0]["sb"]
                L[1]["g"] = L[0]["g"]
                z32 = k.sb(es, "r3_z32", [128, 2 * D])
                zT = k.sb(es, "r3_zT", [128, 16, 128], BF16)
                on = [k.sb(es, f"r3_on{x}", [128, 512]) for x in range(2)]
                Pm = [k.sb(es, f"r3_P{x}", [128, 128], BF16) for x in range(2)]
                gst = k.sb(es, "r3_gst", [128, 4, 12])
                gmv = k.sb(es, "r3_gmv", [128, 4, 2])
                grs = k.sb(es, "r3_grs", [128, 4])
                zb = [k.sb(es, f"r3_zb{x}", [128, 1, D]) for x in range(2)]
                st = [k.sb(es, f"r3_st{x}", [128, 1, 12]) for x in range(2)]
                mv = [k.sb(es, f"r3_mv{x}", [128, 1, 2]) for x in range(2)]
                rs = [k.sb(es, f"r3_rs{x}", [128, 8]) for x in range(2)]
                z32s = [z32, k.sb(es, "r3_z32b", [128, 2 * D])]
                seq = [(b, g) for b in range(NBC) for g in range(NCH)]

                def loads(ci):
                    b, g = seq[ci]
                    B_ = L[ci % 2]
                    isctx = g < 2
                    want_out = (not isctx) or need_ctx
                    cs = slice(g * 128, (g + 1) * 128)
                    k.dma("sp", B_["kf"][:], self.KF[b, cs, :], r=[("KFall",)])
                    k.dma("sp", B_["v"][:], self.RV[b, cs, :], r=[("RVall",)])
                    if want_out:
                        for nm, dr in (("qT", self.QT), ("kT", self.KT), ("qf", self.QTF), ("qb", self.QTB)):
                            k.dma("sp", B_[nm][:], dr[b, :, cs].rearrange("(k p) t -> p k t", p=128), r=[(nm + "all",)])
                        k.dma("sp", B_["h"][:, 0, :], src[b, cs, :], r=[(_key(src), b, (g * 128 // 512) * 512 if not isctx else 0)])
                        k.dma("sp", B_["g"][:], self.RG[b, cs, :], r=[("RGall",)])
                        k.dma("sp", B_["sb"][:].rearrange("p a c -> p (a c)"), self.SBD[b, g], r=[("SBDall",)])

                def front(ci):
                    b, g = seq[ci]
                    B_ = L[ci % 2]
                    zz = z32s[ci % 2]
                    isctx = g < 2
                    want_out = (not isctx) or need_ctx
                    if g == 0:
                        k.memset("dve", Sf[:], 0.0)
                        k.memset("pool", Sfb[:], 0.0)
                    for h in range(4):
                        vh = B_["v"][:, h * 512:(h + 1) * 512]
                        if want_out:
                            sc = PS[0]
                            for a in range(2):
                                k.mm(sc[:, 0:128], B_["kT"][:, 2 * h + a, :], B_["qT"][:, 2 * h + a, :], a == 0, a == 1)
                        for a in range(2):
                            k.mm(PS[3 + a][:], B_["kf"][:, h * 256 + a * 128:h * 256 + (a + 1) * 128], vh, True, True)
                        if want_out:
                            P_ = Pm[h % 2]
                            k.tt("dve", P_[:], sc[:, 0:128], MT[:, h, :], ALU.mult)
                            O = PS[1 + h % 2]
                            k.mm(O[:], P_[:], vh, True, False)
                            for a in range(2):
                                k.mm(O[:], B_["qf"][:, 2 * h + a, :], Sfb[:, 2 * h + a, :], False, False)
                            for a in range(2):
                                k.mm(O[:], B_["qb"][:, 2 * h + a, :], B_["sb"][:, 2 * h + a, :], False, a == 1)
                        for a in range(2):
                            ps = PS[3 + a]
                            k.stt(Sf[:, 2 * h + a, :], Sf[:, 2 * h + a, :], GL[:, h:h + 1], ps[:], ALU.mult, ALU.add)
                            k.copy("act", Sfb[:, 2 * h + a, :], Sf[:, 2 * h + a, :])
                        if want_out:
                            k.fn("dve", (lambda e, h=h, O=O: e.bn_stats(out=gst[:, h, 0:6], in_=O[:])), [O], [gst])
                            k.fn("dve", (lambda e, h=h: e.bn_aggr(out=gmv[:, h, :], in_=gst[:, h, 0:6])), [gst], [gmv])
                            k.ts("pool", grs[:, h:h + 1], gmv[:, h, 1:2], LN_EPS, None, ALU.add)
                            k.tt("pool", grs[:, h:h + 1], grs[:, h:h + 1], self.negh[:, 0:1], ALU.pow)
                            o_ = on[h % 2]
                            k.ts("dve", o_[:], O[:], gmv[:, h, 0:1], grs[:, h:h + 1], ALU.subtract, ALU.mult)
                            k.tt("pool", o_[:], o_[:], gng[:, h * 512:(h + 1) * 512], ALU.mult)
                            k.tt("pool", zz[:, h * 512:(h + 1) * 512], o_[:], B_["g"][:, h * 512:(h + 1) * 512], ALU.mult,
                                 w=[(_key(zz), h)])

                def back(ci):
                    b, g = seq[ci]
                    B_ = L[ci % 2]
                    zz = z32s[ci % 2]
                    isctx = g < 2
                    want_out = (not isctx) or need_ctx
                    if not want_out:
                        return
                    r = 2 if isctx else b
                    cs = slice(g * 128, (g + 1) * 128)
                    for c in range(16):
                        pp = PS[5]
                        k.tr(pp[:, (c % 4) * 128:(c % 4 + 1) * 128], zz[:, c * 128:(c + 1) * 128], self.ident[:],
                             r=[(_key(zz), c // 4), self.ident])
                        if c % 4 == 3:
                            k.copy("act", zT[:, c - 3:c + 1, :], pp[:].rearrange("p (c i) -> p c i", i=128))
                    for half in range(2):
                        y = PS[6 + half]
                        for c in range(16):
                            k.mm(y[:], zT[:, c, :], wout[:, c, half * 512:(half + 1) * 512], c == 0, c == 15)
                    t0 = 0 if isctx else (g * 128 // 512) * 512
                    self.ln_epilogue(lambda t, half: PS[6 + half][:], B_["h"], 1, r, 0, 0, zb, st, mv, rs,
                                     dst[b, cs, :], (_key(dst), b, t0, g))
                loads(0)
                front(0)
                for ci in range(len(seq)):
                    if ci + 1 < len(seq):
                        loads(ci + 1)
                        front(ci + 1)
                    back(ci)
                k.S.barrier()

    def mla_layer(self, i, src, dst):
        k = self.k
        I = self.I
        PS = self.PS
        need_ctx = i < DEPTH - 1
        NKT = TT // 128
        with contextlib.ExitStack() as es:
            wd = k.sb(es, "m1_wd", [128, 8, 768], BF16)
            wuq = k.sb(es, "m1_wuq", [128, 3, 2048], BF16)
            wukv = k.sb(es, "m1_wukv", [128, 2, 2048], BF16)
            k.dma("pool", wd[:], I["mla_wd"].rearrange("(k p) n -> p k n", p=128))
            k.dma("pool", wuq[:], I["mla_wuq"].rearrange("(k p) n -> p k n", p=128))
            k.dma("pool", wukv[:], I["mla_wukv"].rearrange("(k p) n -> p k n", p=128))
            C1 = k.sb(es, "m1_c1", [128, SEQ])
            C2 = k.sb(es, "m1_c2", [128, SEQ])
            k.dma("sp", C1[:], I["mla_c1"])
            k.dma("sp", C2[:], I["mla_c2"])
            nraw = k.sb(es, "m1_nraw", [5, 128])
            npp = k.sb(es, "m1_npp", [128, 5])
            k.dma("sp", nraw[:], I["mla_norms"])
            k.tr(PS[7][:, 0:5], nraw[:, :], self.ident[0:5, 0:5])
            k.copy("dve", npp[:], PS[7][:, 0:5])
            stg = k.sb(es, "m1_stg", [128, 4, D])
            u = k.sb(es, "m1_uT", [128, 8, 512], BF16)
            d32 = k.sb(es, "m1_d32", [128, 5, 512])
            sq = [k.sb(es, f"m1_sq{x}", [128, 512]) for x in range(2)]
            rr = k.sb(es, "m1_rr", [128, 2, 512])
            dn = k.sb(es, "m1_dn", [128, 5, 512], BF16)
            qnb = k.sb(es, "m1_qnb", [128, 8, 512], BF16)
            qrb = k.sb(es, "m1_qrb", [128, 4, 512], BF16)
            knb = k.sb(es, "m1_knb", [128, 8, 512], BF16)
            vb = k.sb(es, "m1_vb", [128, 4, D], BF16)
            krb = k.sb(es, "m1_krb", [64, 512], BF16)
            tm = [k.sb(es, f"m1_tm{x}", [128, 512]) for x in range(2)]
            for b in range(NBC):
                for (bb, t0, n, r) in tiles_of(b, True):
                    isctx = (r == 2)
                    c0 = t0 - CTX
                    nt = n // 128
                    self.prologue(src, b, t0, n, r, 0, stg, u, [PS[6], PS[7]])
                    for c in range(5):
                        ps = PS[c]
                        for kk in range(8):
                            k.mm(ps[:, 0:n], wd[:, kk, c * 128:(c + 1) * 128], u[:, kk, 0:n], kk == 0, kk == 7)
                        k.copy("act", d32[:, c, 0:n], ps[:, 0:n])
                    for (x, ps) in ((0, PS[5]), (1, PS[6])):
                        for kk in range(8):
                            k.mm(ps[0:64, 0:n], wd[:, kk, 640 + 64 * x:704 + 64 * x], u[:, kk, 0:n], kk == 0, kk == 7)
                    if isctx:
                        k.copy("act", krb[:, 0:n], PS[5][0:64, 0:n])
                    else:
                        k.tt("dve", tm[0][0:64, 0:n], PS[5][0:64, 0:n], C1[0:64, c0:c0 + n], ALU.mult)
                        k.tt("dve", tm[1][0:64, 0:n], PS[6][0:64, 0:n], C2[0:64, c0:c0 + n], ALU.mult)
                        k.tt("pool", krb[:, 0:n], tm[0][0:64, 0:n], tm[1][0:64, 0:n], ALU.add)
                    k.dma("pool", self.KR[b, :, t0:t0 + n], krb[:, 0:n], store=True, w=[("KR", b, t0)])
                    for (gi_, cl, dim) in ((0, (0, 1, 2), 384.0), (1, (3, 4), 256.0)):
                        ps = PS[7]
                        for ci, c in enumerate(cl):
                            sqq = sq[ci % 2]
                            k.tt("dve", sqq[:, 0:n], d32[:, c, 0:n], d32[:, c, 0:n], ALU.mult)
                            k.mm(ps[:, 0:n], self.onesf[:], sqq[:, 0:n], ci == 0, ci == len(cl) - 1)
                        k.ts("dve", rr[:, gi_, 0:n], ps[:, 0:n], 1.0 / dim, RMS_EPS, ALU.mult, ALU.add)
                        k.act(rr[:, gi_, 0:n], rr[:, gi_, 0:n], AF.Ln)
                        k.act(rr[:, gi_, 0:n], rr[:, gi_, 0:n], AF.Exp, scale=-0.5)
                        for c in cl:
                            k.stt(dn[:, c, 0:n], d32[:, c, 0:n], npp[:, c:c + 1], rr[:, gi_, 0:n], ALU.mult, ALU.mult)
                    for h in range(8):
                        ps = PS[h % 4]
                        for c in range(3):
                            k.mm(ps[:, 0:n], wuq[:, c, h * 128:(h + 1) * 128], dn[:, c, 0:n], c == 0, c == 2)
                        k.copy("act", qnb[:, h, 0:n], ps[:, 0:n])
                    k.dma("act", self.QN[b, :, :, t0:t0 + n].rearrange("h p t -> p h t"), qnb[:, :, 0:n], store=True, w=[("QN", b, t0)])
                    for hp in range(4):
                        pa, pb = PS[4 + (2 * hp) % 2], PS[4 + (2 * hp + 1) % 2]
                        for c in range(3):
                            k.mm(pa[:, 0:n], wuq[:, c, 1024 + hp * 128:1024 + (hp + 1) * 128], dn[:, c, 0:n], c == 0, c == 2)
                        if isctx:
                            k.copy("act", qrb[:, hp, 0:n], pa[:, 0:n])
                        else:
                            for c in range(3):
                                k.mm(pb[:, 0:n], wuq[:, c, 1536 + hp * 128:1536 + (hp + 1) * 128], dn[:, c, 0:n], c == 0, c == 2)
                            k.tt("dve", tm[0][:, 0:n], pa[:, 0:n], C1[:, c0:c0 + n], ALU.mult)
                            k.tt("dve", tm[1][:, 0:n], pb[:, 0:n], C2[:, c0:c0 + n], ALU.mult)
                            k.tt("pool", qrb[:, hp, 0:n], tm[0][:, 0:n], tm[1][:, 0:n], ALU.add)
                    k.dma("pool", self.QR[b, :, :, t0:t0 + n].rearrange("h p t -> p h t"), qrb[:, :, 0:n], store=True, w=[("QR", b, t0)])
                    for h in range(8):
                        ps = PS[h % 4]
                        for c in range(2):
                            k.mm(ps[:, 0:n], wukv[:, c, h * 128:(h + 1) * 128], dn[:, 3 + c, 0:n], c == 0, c == 1)
                        k.copy("act", knb[:, h, 0:n], ps[:, 0:n])
                    k.dma("act", self.KN[b, :, :, t0:t0 + n].rearrange("h p t -> p h t"), knb[:, :, 0:n], store=True, w=[("KN", b, t0)])
                    for s_ in range(nt):
                        for half in range(2):
                            ps = PS[4 + half]
                            for c in range(2):
                                k.mm(ps[:], dn[:, 3 + c, s_ * 128:(s_ + 1) * 128], wukv[:, c, 1024 + half * 512:1024 + (half + 1) * 512], c == 0, c == 1)
                            k.copy("act", vb[:, s_, half * 512:(half + 1) * 512], ps[:])
                    k.dma("act", self.MV[b, t0:t0 + n, :].rearrange("(s p) d -> p s d", p=128), vb[:, 0:nt, :], store=True, w=[("MV", b, t0)])
            k.S.barrier()
        with contextlib.ExitStack() as es:
            kra = k.sb(es, "m2_kr", [64, TT], BF16)
            knh = [k.sb(es, f"m2_kn{x}", [128, TT], BF16) for x in range(2)]
            vh = [k.sb(es, f"m2_v{x}", [128, NKT, 128], BF16) for x in range(2)]
            qn = [k.sb(es, f"m2_qn{x}", [128, 512], BF16) for x in range(2)]
            qr = [k.sb(es, f"m2_qr{x}", [64, 512], BF16) for x in range(2)]
            PT = [k.sb(es, f"m2_PT{x}", [128, 512], BF16) for x in range(3)]
            rec = [k.sb(es, f"m2_rec{x}", [128, 512]) for x in range(2)]
            otb = [k.sb(es, f"m2_ot{x}", [128, 512], BF16) for x in range(2)]
            dac = [k.sb(es, f"m2_dac{x}", [128, 512]) for x in range(2)]
            scale = float(192.0 ** -0.5)
            it = 0
            ih = 0
            for b in range(NBC):
                k.dma("sp", kra[:], self.KR[b], r=[("KRall",)])
                for h in range(8):
                    kn_, v_ = knh[ih % 2], vh[ih % 2]
                    ih += 1
                    k.dma("sp", kn_[:], self.KN[b, h], r=[("KNall",)])
                    k.dma("sp", v_[:], self.MV[b, :, h * 128:(h + 1) * 128].rearrange("(t p) d -> p t d", p=128), r=[("MVall",)])
                    for (bb, t0, n, r) in tiles_of(b, need_ctx):
                        isctx = (r == 2)
                        kts = [0, 1] if isctx else list(range(NKT))
                        q_, r_ = qn[it % 2], qr[it % 2]
                        O, DEN = PS[4 + it % 2], PS[6 + it % 2]
                        rc, ot = rec[it % 2], otb[it % 2]
                        dacc = dac[it % 2]
                        it += 1
                        k.dma("sp", q_[:, 0:n], self.QN[b, h, :, t0:t0 + n], r=[("QNall",)])
                        k.dma("sp", r_[:, 0:n], self.QR[b, h // 2, (h % 2) * 64:(h % 2) * 64 + 64, t0:t0 + n], r=[("QRall",)])

                        def scores(idx):
                            kt = kts[idx]
                            sc = PS[idx % 4]
                            k.mm(sc[:, 0:n], kn_[:, kt * 128:(kt + 1) * 128], q_[:, 0:n], True, False)
                            k.mm(sc[:, 0:n], kra[:, kt * 128:(kt + 1) * 128], r_[:, 0:n], False, True)
                        scores(0)
                        for idx, kt in enumerate(kts):
                            if idx + 1 < len(kts):
                                scores(idx + 1)
                            p_ = PT[idx % 3]
                            k.act(p_[:, 0:n], PS[idx % 4][:, 0:n], AF.Exp, scale=scale)
                            k.mm(O[:, 0:n], v_[:, kt, :], p_[:, 0:n], idx == 0, idx == len(kts) - 1)
                            k.mm(DEN[:, 0:n], self.onesb[:], p_[:, 0:n], idx == 0, idx == len(kts) - 1)
                        k.fn("dve", (lambda e, rc=rc, DEN=DEN, n=n: e.reciprocal(out=rc[:, 0:n], in_=DEN[:, 0:n])), [DEN], [rc])
                        k.tt("dve", ot[:, 0:n], O[:, 0:n], rc[:, 0:n], ALU.mult)
                        k.dma("pool", self.OT[b, h * 128:(h + 1) * 128, t0:t0 + n], ot[:, 0:n], store=True, w=[("OT", b, h, t0)])
            k.S.barrier()
        with contextlib.ExitStack() as es:
            wout = k.sb(es, "m3_wout", [128, 8, D], BF16)
            k.dma("pool", wout[:], I["mla_w_out"].rearrange("(k p) n -> p k n", p=128))
            stg = [k.sb(es, f"m3_stg{x}", [128, 4, D]) for x in range(2)]
            oT = [k.sb(es, f"m3_oT{x}", [128, 8, 512], BF16) for x in range(2)]
            zb = [k.sb(es, f"m3_zb{x}", [128, 4, D]) for x in range(2)]
            st = [k.sb(es, f"m3_st{x}", [128, 4, 12]) for x in range(2)]
            mv = [k.sb(es, f"m3_mv{x}", [128, 4, 2]) for x in range(2)]
            rs = [k.sb(es, f"m3_rs{x}", [128, 8]) for x in range(2)]
            it = 0
            for b in range(NBC):
                for (bb, t0, n, r) in tiles_of(b, need_ctx):
                    nt = n // 128
                    sg, o_ = stg[it % 2], oT[it % 2]
                    it += 1
                    k.dma("sp", sg[:, 0:nt, :], src[b, t0:t0 + n, :].rearrange("(t p) d -> p t d", p=128), r=[(_key(src), b, t0)])
                    k.dma("sp", o_[:, :, 0:n], self.OT[b, :, t0:t0 + n].rearrange("(k p) t -> p k t", p=128), r=[("OTall",)])
                    for t in range(nt):
                        for half in range(2):
                            ps = PS[(t * 2 + half) % 8]
                            for kk in range(8):
                                k.mm(ps[:], o_[:, kk, t * 128:(t + 1) * 128], wout[:, kk, half * 512:(half + 1) * 512], kk == 0, kk == 7)
                    self.ln_epilogue(lambda t, half: PS[(t * 2 + half) % 8][:], sg, nt, r, 0, 0, zb, st, mv, rs,
                                     dst[b, t0:t0 + n, :], (_key(dst), b, t0))
            k.S.barrier()

    def moe_layer_dense(self, i, src, dst, last, dbg_ns=None, dbg_ne=NE):
        k = self.k
        I = self.I
        PS = self.PS
        need_ctx = not last
        i = self.li[i]
        alltiles = []
        for b in range(NBC):
            alltiles += tiles_of(b, need_ctx)
        NS = 2
        with contextlib.ExitStack() as es:
            wr = k.sb(es, "mo_wr", [128, 8, 64])
            rbg = k.sb(es, "mo_rbg", [128, 4])
            rbe = k.sb(es, "mo_rbe", [128, 32])
            k.dma("sp", wr[:], I["moe_wr"][i].rearrange("(k p) n -> p k n", p=128))
            k.dma("sp", rbg[:], I["moe_bg"][i].partition_broadcast(128))
            k.dma("sp", rbe[:], I["moe_be"][i].partition_broadcast(128))
            vT = k.sb(es, "mo_vT", [128, 8, NS * 512], BF16)
            acc = [[k.sb(es, f"mo_acc{s}_{h}", [128, 512]) for h in range(2)] for s in range(NS * 4)]
            G = k.sb(es, "mo_G", [128, NS * 4, 32])
            stg = k.sb(es, "mo_stg", [128, 4, D])
            w1b = [k.sb(es, f"mo_w1_{j}", [128, 8, 512], BF16) for j in range(2)]
            w3b = [k.sb(es, f"mo_w3_{j}", [128, 8, 512], BF16) for j in range(2)]
            w2b = [k.sb(es, f"mo_w2_{j}", [128, 4, D], BF16) for j in range(2)]
            sil = [k.sb(es, f"mo_sil{j}", [128, 512]) for j in range(2)]
            hdn = [k.sb(es, f"mo_hdn{j}", [128, 4, 512], BF16) for j in range(2)]
            rt = k.sb(es, "mo_rt", [128, 64])
            r8 = k.sb(es, "mo_r8", [128, 8])
            rsm = k.sb(es, "mo_rsm", [128, 16])
            zb = k.sb(es, "mo_zb", [128, 4, D])
            v32 = zb[:].rearrange("p a (b c) -> p (a b) c", c=512)
            st = k.sb(es, "mo_st", [128, 4, 12])
            mv = k.sb(es, "mo_mv", [128, 4, 2])
            rs = k.sb(es, "mo_rs", [128, 8])
            wi = 0
            for s0 in range(0, len(alltiles) if dbg_ns is None else dbg_ns * NS, NS):
                tl = alltiles[s0:s0 + NS]
                subs = []
                for ti, (b, t0, n, r) in enumerate(tl):
                    self.prologue(src, b, t0, n, r, 2, stg, vT[:, :, ti * 512:(ti + 1) * 512], [PS[6], PS[7]], v32=v32)
                    for t in range(n // 128):
                        si = ti * 4 + t
                        subs.append((ti, t, ti * 512 + t * 128, si))
                        lg = PS[5]
                        for kk in range(8):
                            k.mm(lg[:, 0:64], v32[:, kk, t * 128:(t + 1) * 128], wr[:, kk, :], kk == 0, kk == 7)
                        self.route(lg, rbg, rbe, rt, r8, rsm, G[:, si, :])
                for e in range(dbg_ne):
                    wb1, wb3, wb2 = w1b[wi % 2], w3b[wi % 2], w2b[wi % 2]
                    wi += 1
                    k.dma("pool", wb1[:], I["moe_w1"][i, e].rearrange("(k p) n -> p k n", p=128))
                    k.dma("pool", wb3[:], I["moe_w3"][i, e].rearrange("(k p) n -> p k n", p=128))
                    k.dma("pool", wb2[:], I["moe_w2"][i, e].rearrange("(k p) n -> p k n", p=128))
                    for ti, (b, t0, n, r) in enumerate(tl):
                        hb = hdn[ti % 2]
                        cs = ti * 512
                        for j in range(4):
                            pa, pb = PS[(2 * j) % 4], PS[(2 * j + 1) % 4]
                            for kk in range(8):
                                k.mm(pa[:, 0:n], wb1[:, kk, j * 128:(j + 1) * 128], vT[:, kk, cs:cs + n], kk == 0, kk == 7)
                            for kk in range(8):
                                k.mm(pb[:, 0:n], wb3[:, kk, j * 128:(j + 1) * 128], vT[:, kk, cs:cs + n], kk == 0, kk == 7)
                            sl = sil[j % 2]
                            k.act(sl[:, 0:n], pa[:, 0:n], AF.Silu)
                            k.tt("dve", hb[:, j, 0:n], sl[:, 0:n], pb[:, 0:n], ALU.mult, w=[(_key(hb), j)])
                        for t in range(n // 128):
                            si = ti * 4 + t
                            for half in range(2):
                                po = PS[4 + (t * 2 + half) % 2]
                                for j in range(4):
                                    k.mm(po[:], hb[:, j, t * 128:(t + 1) * 128], wb2[:, j, half * 512:(half + 1) * 512], j == 0, j == 3,
                                         r=[(_key(hb), j), wb2])
                                a = acc[si][half][:]
                                if e == 0:
                                    k.ts("dve", a, po[:], G[:, si, e:e + 1], None, ALU.mult)
                                else:
                                    k.stt(a, po[:], G[:, si, e:e + 1], a, ALU.mult, ALU.add)
                for ti, (b, t0, n, r) in enumerate(tl):
                    nt = n // 128
                    k.dma("sp", stg[:, 0:nt, :], src[b, t0:t0 + n, :].rearrange("(t p) d -> p t d", p=128), r=[(_key(src), b, t0)])
                    if last:
                        drows = self.out[b, t0 - CTX:t0 - CTX + n, :]
                        dkey = ("out", b, t0)
                    else:
                        drows = dst[b, t0:t0 + n, :]
                        dkey = (_key(dst), b, t0)
                    self.ln_epilogue(lambda t, half, ti=ti: acc[ti * 4 + t][half][:], stg, nt, r, 1, 1,
                                     zb, st, mv, rs, drows, dkey)
            k.S.barrier()

    def moe_layer(self, i, src, dst, last, dbg_ns=None, dbg_ne=NE):
        k = self.k
        I = self.I
        PS = self.PS
        need_ctx = not last
        li = self.li[i]
        alltiles = []
        for b in range(NBC):
            alltiles += tiles_of(b, need_ctx)
        nsub_all = sum(n // 128 for (_, _, n, _) in alltiles)
        NBLK = (2 * nsub_all * 128 + 511) // 512 + NE
        NSUB = nsub_all
        XS, YS = self.XS, self.YS
        I32 = mybir.dt.int32
        with contextlib.ExitStack() as mes:
            GT = k.sb(mes, "ms_GT", [128, NSUB, 2])
            D0 = k.sb(mes, "ms_D0", [128, NSUB], I32)
            D1 = k.sb(mes, "ms_D1", [128, NSUB], I32)
            WI = k.sb(mes, "ms_WI", [128, NBLK], I32)
            ves = contextlib.ExitStack()
            VB = k.sb(ves, "ms_VB", [128, 3, 2, D])
            with contextlib.ExitStack() as es:
                wblk = [k.sb(es, f"mv_w{x}", [128, 8, 1024]) for x in range(2)]
                brow = k.sb(es, "mv_brow", [1, 6 * D])
                REP = k.sb(es, "mv_REP", [128, 3, 8, 128])
                for r in range(3):
                    for kk in range(8):
                        k.copy("dve", REP[:, r, kk, :], self.actT[:, kk, r:r + 1].broadcast_to([128, 128]))
                k.dma("sp", brow[:], I["ada_b"][li])
                pi = 0
                for wi in (3, 4):
                    buf = wblk[wi % 2]
                    k.dma("sp", buf[:], I["ada_w"][li][:, wi * 1024:(wi + 1) * 1024].rearrange("(k p) n -> p k n", p=128))
                    for r in range(3):
                        for half in range(2):
                            ps = PS[pi % 8]
                            pi += 1
                            for ki in range(8):
                                k.mm(ps[:], REP[:, r, ki, :], buf[:, ki, half * 512:(half + 1) * 512], ki == 0, False)
                            k.mm(ps[:], self.ones[0:1, 0:128], brow[0:1, wi * 1024 + half * 512: wi * 1024 + (half + 1) * 512], False, True)
                            if wi == 3:
                                k.copy("act", VB[:, r, 0, half * 512:(half + 1) * 512], ps[:])
                            else:
                                k.act(VB[:, r, 1, half * 512:(half + 1) * 512], ps[:], AF.Identity, bias=1.0)
                k.S.barrier()
            with contextlib.ExitStack() as es:
                wr = k.sb(es, "ma_wr", [128, 8, 64])
                rbg = k.sb(es, "ma_rbg", [128, 4])
                rbe = k.sb(es, "ma_rbe", [128, 32])
                k.dma("sp", wr[:], I["moe_wr"][li].rearrange("(k p) n -> p k n", p=128))
                k.dma("sp", rbg[:], I["moe_bg"][li].partition_broadcast(128))
                k.dma("sp", rbe[:], I["moe_be"][li].partition_broadcast(128))
                U = k.sb(es, "ma_U", [128, 128], BF16)
                k.memset("pool", U[:], 1.0)
                k.fn("pool", lambda e: e.affine_select(out=U[:], in_=U[:], pattern=[[1, 128]], compare_op=ALU.is_gt,
                                                       fill=0.0, base=0, channel_multiplier=-1), [U], [U])
                OH1 = k.sb(es, "ms_OH1", [128, NSUB, 32])
                OH2 = k.sb(es, "ms_OH2", [128, NSUB, 32])
                RK = k.sb(es, "ms_RK", [128, NSUB, 32])
                cnt = k.sb(es, "ma_cnt", [128, 32])
                k.memset("dve", cnt[:], 0.0)
                stg = [k.sb(es, f"ma_stg{x}", [128, 4, D]) for x in range(2)]
                vtk = k.sb(es, "ma_vtk", [128, 4, D])
                v32 = k.sb(es, "ma_v32", [128, 8, 512])
                ind = [k.sb(es, f"ma_ind{x}", [128, 32], BF16) for x in range(2)]
                rt = k.sb(es, "ma_rt", [128, 64])
                r8 = k.sb(es, "ma_r8", [128, 8])
                rsm = k.sb(es, "ma_rsm", [128, 16])
                si = 0
                for ti, (b, t0, n, r) in enumerate(alltiles):
                    nt = n // 128
                    sg = stg[ti % 2]
                    k.dma("sp", sg[:, 0:nt, :], src[b, t0:t0 + n, :].rearrange("(t p) d -> p t d", p=128), r=[(_key(src), b, t0)])
                    for t in range(nt):
                        k.tt("dve", vtk[:, t, :], sg[:, t, :], VB[:, r, 1, :], ALU.mult, w=[(_key(vtk), t)])
                        k.tt("pool", vtk[:, t, :], vtk[:, t, :], VB[:, r, 0, :], ALU.add, r=[(_key(vtk), t), VB], w=[(_key(vtk), t)])
                    for kk in range(8):
                        ps = PS[6 + kk % 2]
                        for t in range(nt):
                            k.tr(ps[:, t * 128:(t + 1) * 128], vtk[:, t, kk * 128:(kk + 1) * 128], self.ident[:],
                                 r=[(_key(vtk), t), self.ident])
                        k.copy("act", v32[:, kk, 0:n], ps[:, 0:n], w=[(_key(v32), kk)])
                    for t in range(nt):
                        lg = PS[5]
                        for kk in range(8):
                            k.mm(lg[:, 0:64], v32[:, kk, t * 128:(t + 1) * 128], wr[:, kk, :], kk == 0, kk == 7,
                                 r=[(_key(v32), kk), wr])
                        self.route(lg, rbg, rbe, rt, r8, rsm, None, oh1=OH1[:, si, :], oh2=OH2[:, si, :], gts=GT[:, si, :])
                        id_ = ind[si % 2]
                        k.tt("dve", id_[:], OH1[:, si, :], OH2[:, si, :], ALU.add)
                        pr = PS[4]
                        k.mm(pr[:, 0:32], U[:], id_[:], True, True)
                        k.mm(pr[:, 32:64], self.onesb[:], id_[:], True, True)
                        k.tt("dve", RK[:, si, :], pr[:, 0:32], cnt[:], ALU.add)
                        k.tt("dve", cnt[:], pr[:, 32:64], cnt[:], ALU.add)
                        si += 1
                sm = k.sb(es, "ma_sm", [128, 6, 32])
                k.ts("dve", sm[:, 0, :], cnt[:], 511.0, 1.0 / 512.0, ALU.add, ALU.mult)
                k.ts("dve", sm[:, 0, :], sm[:, 0, :], -0.499, 8388608.0, ALU.add, ALU.add)
                k.ts("dve", sm[:, 0, :], sm[:, 0, :], -8388608.0, 512.0, ALU.add, ALU.mult)
                k.memset("dve", sm[:, 1, :], 1.0)
                k.fn("dve", lambda e: e.tensor_tensor_scan(out=sm[:, 2, :], data0=sm[:, 1, :], data1=sm[:, 0, :], initial=0.0,
                                                          op0=ALU.mult, op1=ALU.add), [sm], [sm])
                k.tt("dve", sm[:, 3, :], sm[:, 2, :], sm[:, 0, :], ALU.subtract)
                jv = k.sb(es, "ma_jv", [128, NBLK, 32])
                k.fn("pool", lambda e: e.iota(jv[:], pattern=[[512, NBLK], [0, 32]], base=0, channel_multiplier=0,
                                              allow_small_or_imprecise_dtypes=True), [], [jv])
                k.tt("dve", jv[:], jv[:], sm[:, 2:3, :].broadcast_to([128, NBLK, 32]), ALU.is_ge)
                eb = k.sb(es, "ma_eb", [128, NBLK])
                k.fn("dve", lambda e: e.tensor_reduce(out=eb[:], in_=jv[:], axis=mybir.AxisListType.X, op=ALU.add), [jv], [eb])
                pid = k.sb(es, "ma_pid", [128, 1])
                k.fn("pool", lambda e: e.iota(pid[:], pattern=[[0, 1]], base=0, channel_multiplier=1,
                                              allow_small_or_imprecise_dtypes=True), [], [pid])
                k.ts("dve", eb[:], eb[:], 31.0, 128.0, ALU.min, ALU.mult)
                k.ts("dve", eb[:], eb[:], pid[:, 0:1], float(li * NE * 128), ALU.add, ALU.add)
                k.copy("dve", WI[:], eb[:])
                k.tt("dve", RK[:], RK[:], sm[:, 3:4, :].broadcast_to([128, NSUB, 32]), ALU.add)
                dd = k.sb(es, "ma_dd", [128, NSUB])
                for (OH, Dd) in ((OH1, D0), (OH2, D1)):
                    k.tt("dve", OH[:], OH[:], RK[:], ALU.mult)
                    k.fn("dve", (lambda e, OH=OH: e.tensor_reduce(out=dd[:], in_=OH[:], axis=mybir.AxisListType.X, op=ALU.add)), [OH], [dd])
                    k.copy("dve", Dd[:], dd[:])
                k.S.barrier()
            with contextlib.ExitStack() as es:
                stg = [k.sb(es, f"mb_stg{x}", [128, 4, D]) for x in range(2)]
                vtk = [k.sb(es, f"mb_vtk{x}", [128, 4, D]) for x in range(2)]
                si = 0
                for ti, (b, t0, n, r) in enumerate(alltiles):
                    nt = n // 128
                    sg, vt = stg[ti % 2], vtk[ti % 2]
                    k.dma("sp", sg[:, 0:nt, :], src[b, t0:t0 + n, :].rearrange("(t p) d -> p t d", p=128), r=[(_key(src), b, t0)])
                    for t in range(nt):
                        k.tt("dve", vt[:, t, :], sg[:, t, :], VB[:, r, 1, :], ALU.mult)
                        k.tt("dve", vt[:, t, :], vt[:, t, :], VB[:, r, 0, :], ALU.add)
                        for Dd in (D0, D1):
                            k.S.op("pool", (lambda e, vt=vt, t=t, Dd=Dd, si=si: e.indirect_dma_start(
                                out=XS, out_offset=bass.IndirectOffsetOnAxis(ap=Dd[:, si:si + 1], axis=0),
                                in_=vt[:, t, :], in_offset=None)), [vt, Dd], [("XS", si, id(Dd))], dma=_key(vt))
                        si += 1
                k.S.barrier()
            ves.close()
            with contextlib.ExitStack() as es:
                wst = [k.sb(es, f"mc_wst{x}", [128, 4096]) for x in range(3)]
                wb = [[k.sb(es, f"mc_wb{x}_{y}", [128, 4096], BF16) for x in range(3)] for y in range(2)]
                stg = k.sb(es, "mc_stg", [128, 4, D])
                xT = [k.sb(es, f"mc_xT{x}", [128, 8, 512], BF16) for x in range(2)]
                sil = [k.sb(es, f"mc_sil{x}", [128, 512]) for x in range(2)]
                hdn = [k.sb(es, f"mc_hdn{x}", [128, 4, 512], BF16) for x in range(2)]
                ysb = k.sb(es, "mc_ysb", [128, 4, D])
                wsrc = [I["moe_w1"].rearrange("l e p k n -> (l e p) (k n)"), I["moe_w3"].rearrange("l e p k n -> (l e p) (k n)"),
                        I["moe_w2"].rearrange("l e p k n -> (l e p) (k n)")]
                NB_ = NBLK if dbg_ns is None else dbg_ns

                def gather_w(j):
                    for x in range(3):
                        k.S.op("pool", (lambda e, x=x, j=j: e.indirect_dma_start(
                            out=wst[x][:], out_offset=None, in_=wsrc[x],
                            in_offset=bass.IndirectOffsetOnAxis(ap=WI[:, j:j + 1], axis=0))), [WI], [wst[x]], dma=_key(wst[x]))

                def cast_w(j):
                    wbj = wb[j % 2]
                    k.copy("act", wbj[0][:], wst[0][:])
                    k.copy("pool", wbj[1][:], wst[1][:])
                    k.copy("dve", wbj[2][:], wst[2][:])

                def load_x(j):
                    k.dma("sp", stg[:], XS[j * 512:(j + 1) * 512, :].rearrange("(t p) d -> p t d", p=128), r=[("XSall",)])
                gather_w(0)
                cast_w(0)
                load_x(0)
                for j in range(NB_):
                    wbj = wb[j % 2]
                    w1v = wbj[0][:].rearrange("p (k n) -> p k n", n=512)
                    w3v = wbj[1][:].rearrange("p (k n) -> p k n", n=512)
                    w2v = wbj[2][:].rearrange("p (k n) -> p k n", n=1024)
                    x_ = xT[j % 2]
                    hb = hdn[j % 2]
                    for kk in range(8):
                        ps = PS[6 + kk % 2]
                        for t in range(4):
                            k.tr(ps[:, t * 128:(t + 1) * 128], stg[:, t, kk * 128:(kk + 1) * 128], self.ident[:])
                        k.copy("act" if kk % 2 == 0 else "dve", x_[:, kk, :], ps[:])
                    if j + 1 < NB_:
                        load_x(j + 1)
                        gather_w(j + 1)
                    for jj in range(4):
                        pa, pb = PS[(2 * jj) % 4], PS[(2 * jj + 1) % 4]
                        for kk in range(8):
                            k.mm(pa[:], w1v[:, kk, jj * 128:(jj + 1) * 128], x_[:, kk, :], kk == 0, kk == 7)
                        for kk in range(8):
                            k.mm(pb[:], w3v[:, kk, jj * 128:(jj + 1) * 128], x_[:, kk, :], kk == 0, kk == 7)
                        sl = sil[jj % 2]
                        k.act(sl[:], pa[:], AF.Silu)
                        k.tt("dve", hb[:, jj, :], sl[:], pb[:], ALU.mult, w=[(_key(hb), jj)])
                    if j + 1 < NB_:
                        cast_w(j + 1)
                    for t in range(4):
                        for half in range(2):
                            po = PS[4 + half]
                            for jj in range(4):
                                k.mm(po[:], hb[:, jj, t * 128:(t + 1) * 128], w2v[:, jj, half * 512:(half + 1) * 512], jj == 0, jj == 3,
                                     r=[(_key(hb), jj), wbj[2]])
                            k.copy("act" if half == 0 else "dve", ysb[:, t, half * 512:(half + 1) * 512], po[:])
                    k.dma("act", YS[j * 512:(j + 1) * 512, :].rearrange("(t p) d -> p t d", p=128), ysb[:], store=True, w=[("YS", j)])
                k.S.barrier()
            with contextlib.ExitStack() as es:
                stg = [k.sb(es, f"md_stg{x}", [128, 4, D]) for x in range(2)]
                y0 = [k.sb(es, f"md_y0{x}", [128, D]) for x in range(2)]
                y1 = [k.sb(es, f"md_y1{x}", [128, D]) for x in range(2)]
                fb = [k.sb(es, f"md_f{x}", [128, 4, D]) for x in range(2)]
                zb = [k.sb(es, f"md_zb{x}", [128, 4, D]) for x in range(2)]
                st = [k.sb(es, f"md_st{x}", [128, 4, 12]) for x in range(2)]
                mv = [k.sb(es, f"md_mv{x}", [128, 4, 2]) for x in range(2)]
                rs = [k.sb(es, f"md_rs{x}", [128, 8]) for x in range(2)]
                si = 0
                for ti, (b, t0, n, r) in enumerate(alltiles):
                    nt = n // 128
                    sg, f_ = stg[ti % 2], fb[ti % 2]
                    k.dma("sp", sg[:, 0:nt, :], src[b, t0:t0 + n, :].rearrange("(t p) d -> p t d", p=128), r=[(_key(src), b, t0)])
                    for t in range(nt):
                        a0, a1 = y0[si % 2], y1[si % 2]
                        for (yy, Dd) in ((a0, D0), (a1, D1)):
                            k.S.op("pool", (lambda e, yy=yy, Dd=Dd, si=si: e.indirect_dma_start(
                                out=yy[:], out_offset=None, in_=YS,
                                in_offset=bass.IndirectOffsetOnAxis(ap=Dd[:, si:si + 1], axis=0))), [Dd], [yy], dma=_key(yy))
                        k.ts("dve", f_[:, t, :], a0[:], GT[:, si, 0:1], None, ALU.mult)
                        k.stt(f_[:, t, :], a1[:], GT[:, si, 1:2], f_[:, t, :], ALU.mult, ALU.add)
                        si += 1
                    if dbg_ns is not None and ti >= 1:
                        break
                    if last:
                        drows = self.out[b, t0 - CTX:t0 - CTX + n, :]
                        dkey = ("out", b, t0)
                    else:
                        drows = dst[b, t0:t0 + n, :]
                        dkey = (_key(dst), b, t0)
                    self.ln_epilogue(lambda t, half, f_=f_: f_[:, t, half * 512:(half + 1) * 512], sg, nt, r, 1, 1,
                                     zb, st, mv, rs, drows, dkey)
                k.S.barrier()

    def route(self, lg, rbg, rbe, rt, r8, rsm, Gout, oh1=None, oh2=None, gts=None):
        k = self.k
        k.tt("dve", rt[:, 0:4], lg[:, 0:4], rbg[:], ALU.add)
        k.tt("dve", rt[:, 8:40], lg[:, 4:36], rbe[:], ALU.add)
        k.fn("dve", lambda e: e.tensor_reduce(out=rsm[:, 0:1], in_=rt[:, 0:4], axis=mybir.AxisListType.X, op=ALU.max), [rt], [rsm])
        k.ts("dve", rt[:, 4:8], rt[:, 0:4], rsm[:, 0:1], None, ALU.subtract)
        k.act(rt[:, 40:44], rt[:, 4:8], AF.Exp)
        k.fn("dve", lambda e: e.tensor_reduce(out=rsm[:, 1:2], in_=rt[:, 40:44], axis=mybir.AxisListType.X, op=ALU.add), [rt], [rsm])
        k.fn("dve", lambda e: e.reciprocal(out=rsm[:, 2:3], in_=rsm[:, 1:2]), [rsm], [rsm])
        k.ts("dve", rt[:, 44:48], rt[:, 4:8], 0.0, -1e30, ALU.is_lt, ALU.mult)
        k.tt("dve", rt[:, 8:40].rearrange("p (g e) -> p g e", e=8), rt[:, 8:40].rearrange("p (g e) -> p g e", e=8),
             rt[:, 44:48].unsqueeze(2).broadcast_to([128, 4, 8]), ALU.add)
        k.fn("dve", lambda e: e.max(out=r8[:], in_=rt[:, 8:40]), [rt], [r8])
        k.tt("dve", rsm[:, 3:4], r8[:, 1:2], r8[:, 0:1], ALU.subtract)
        k.act(rsm[:, 4:5], rsm[:, 3:4], AF.Exp)
        k.ts("dve", rsm[:, 5:6], rsm[:, 4:5], 1.0, None, ALU.add)
        k.fn("dve", lambda e: e.reciprocal(out=rsm[:, 6:7], in_=rsm[:, 5:6]), [rsm], [rsm])
        k.tt("dve", rsm[:, 7:8], rsm[:, 6:7], rsm[:, 2:3], ALU.mult)
        k.tt("dve", rsm[:, 8:9], rsm[:, 7:8], rsm[:, 4:5], ALU.mult)
        if oh1 is not None:
            k.ts("dve", oh1, rt[:, 8:40], r8[:, 0:1], None, ALU.is_equal)
            k.ts("dve", oh2, rt[:, 8:40], r8[:, 1:2], None, ALU.is_equal)
            k.copy("dve", gts, rsm[:, 7:9])
            return
        k.ts("dve", Gout, rt[:, 8:40], r8[:, 0:1], rsm[:, 7:8], ALU.is_equal, ALU.mult, r=[rt, r8, rsm], w=[Gout])
        k.ts("dve", rt[:, 8:40], rt[:, 8:40], r8[:, 1:2], rsm[:, 8:9], ALU.is_equal, ALU.mult)
        k.tt("dve", Gout, Gout, rt[:, 8:40], ALU.add, r=[Gout, rt], w=[Gout])


_CACHE = {}


def _host_inputs(inputs, core):
    b0 = core * NBC
    f = np.float32
    hin = np.concatenate([inputs["ctx"][b0:b0 + NBC], inputs["x"][b0:b0 + NBC]], axis=1)
    cc = np.concatenate([inputs["c"][b0:b0 + NBC], inputs["c_ctx"][None, :]], axis=0)
    return {"hin": np.ascontiguousarray(hin, dtype=f), "cc": np.ascontiguousarray(cc, dtype=f)}


def _shared_inputs(inputs):
    f = np.float32
    wr = np.zeros((DEPTH, D, 64), f)
    wr[:, :, 0:4] = inputs["moe_w_group"]
    wr[:, :, 4:36] = inputs["moe_w_expert"]
    conv_wb = np.concatenate([inputs["conv_w"], inputs["conv_b"][:, None, :]], axis=1)
    rows = np.repeat(np.arange(SEQ // 64, dtype=f), 64)
    cols = np.tile(np.arange(64, dtype=f), SEQ // 64)

    def rope_tabs(dim):
        q = dim // 4
        inv = (np.float32(10000.0) ** (-np.arange(q, dtype=f) / np.float32(q))).astype(f)
        ang = np.concatenate([rows[:, None] * inv[None, :], cols[:, None] * inv[None, :]], axis=-1).astype(f)
        return np.ascontiguousarray(np.cos(ang).T.astype(f)), np.ascontiguousarray(np.sin(ang).T.astype(f))
    rc, rs_ = rope_tabs(256)
    mc, ms = rope_tabs(64)
    c1 = np.ascontiguousarray(np.concatenate([mc, mc, mc, mc], axis=0))
    c2 = np.ascontiguousarray(np.concatenate([-ms, ms, -ms, ms], axis=0))
    wdn = inputs["mla_w_down"][0]
    kr = wdn[:, 640:704]
    mla_wd = np.ascontiguousarray(np.concatenate([wdn[:, 0:640], kr, kr[:, 32:64], kr[:, 0:32]], axis=1))
    wuq = inputs["mla_w_uq"][0].reshape(384, 8, 192)
    rp = wuq[:, :, 128:192]
    mla_wuq = np.ascontiguousarray(np.concatenate([
        wuq[:, :, 0:128].reshape(384, 1024), rp.reshape(384, 512),
        np.concatenate([rp[:, :, 32:64], rp[:, :, 0:32]], axis=2).reshape(384, 512)], axis=1))
    wukv = inputs["mla_w_ukv"][0].reshape(256, 8, 256)
    mla_wukv = np.ascontiguousarray(np.concatenate([wukv[:, :, 0:128].reshape(256, 1024), wukv[:, :, 128:256].reshape(256, 1024)], axis=1))
    norms = np.ascontiguousarray(np.concatenate([inputs["mla_q_norm"][0].reshape(3, 128), inputs["mla_kv_norm"][0].reshape(2, 128)], axis=0))
    return {
        "mla_wd": mla_wd, "mla_norms": norms, "mla_wuq": mla_wuq, "mla_wukv": mla_wukv,
        "mla_w_out": np.ascontiguousarray(inputs["mla_w_out"][0]), "mla_c1": c1, "mla_c2": c2,
        "ret_w_in": inputs["ret_w_in"], "ret_decay": np.ascontiguousarray(inputs["ret_decay"].reshape(2, 8)),
        "ret_gn_g": inputs["ret_gn_g"], "ret_w_out": inputs["ret_w_out"], "ret_cos": rc, "ret_sin": rs_,
        "ada_w": inputs["ada_w"], "ada_b": np.ascontiguousarray(inputs["ada_b"][:, None, :]),
        "ln_g": inputs["ln_g"], "ln_b": inputs["ln_b"],
        "conv_w_in": inputs["conv_w_in"], "conv_wb": np.ascontiguousarray(conv_wb), "conv_w_out": inputs["conv_w_out"],
        "moe_wr": wr, "moe_bg": inputs["moe_b_group"], "moe_be": inputs["moe_b_expert"],
        "moe_w1": np.ascontiguousarray(inputs["moe_w1"].reshape(DEPTH, NE, 8, 128, 512).transpose(0, 1, 3, 2, 4)),
        "moe_w3": np.ascontiguousarray(inputs["moe_w3"].reshape(DEPTH, NE, 8, 128, 512).transpose(0, 1, 3, 2, 4)),
        "moe_w2": np.ascontiguousarray(inputs["moe_w2"].reshape(DEPTH, NE, 4, 128, D).transpose(0, 1, 3, 2, 4)),
    }


def kernel(**inputs):
    inputs = {k_: np.asarray(v) for k_, v in inputs.items()}
    if "prog" not in _CACHE:
        _CACHE["prog"] = Prog()
    prog = _CACHE["prog"]
    shared = _shared_inputs(inputs)
    in_maps = []
    for core in range(8):
        m = dict(shared)
        m.update(_host_inputs(inputs, core))
        in_maps.append({k_: np.ascontiguousarray(v, dtype=np.float32) for k_, v in m.items() if k_ in prog.I})
    res = run_bass_kernel_spmd(prog.nc, in_maps, core_ids=list(range(8)))
    return np.concatenate([r["out"] for r in res.results], axis=0).astype(np.float32)
```

```python
import contextlib
import numpy as np
import concourse.bass as bass
import concourse.mybir as mybir
from concourse.bass_utils import run_bass_kernel_spmd

F32 = mybir.dt.float32
BF16 = mybir.dt.bfloat16
AF = mybir.ActivationFunctionType
ALU = mybir.AluOpType

EPOCH_D = 1500
EPOCH_C = 30000
PH = "__phase__"

D = 1024
NBC = 2
SEQ = 4096
CTX = 256
TT = SEQ + CTX
DEPTH = 4
ALPHA = (2.0 * DEPTH) ** 0.25
LN_EPS = 1e-5
RMS_EPS = 1e-6
NE = 32


def _key(x):
    if isinstance(x, (tuple, str)):
        return x
    t = getattr(x, "tensor", None)
    if t is not None:
        return t.name
    return x.name


class Sched:
    def __init__(self, nc):
        self.nc = nc
        self.ops = []

    def op(self, eng, fn, r=(), w=(), dma=False):
        rk = [_key(k) for k in r]
        wk = [_key(k) for k in w]
        for kk in rk:
            if isinstance(kk, str) and kk.startswith("ps") and kk not in wk:
                wk.append(kk)
        rk.append(PH)
        self.ops.append([eng, fn, tuple(rk), tuple(wk), dma])

    def dma(self, eng, out, in_, r=None, w=None, store=False, skey=None, **kw):
        r = [in_] if r is None else r
        w = [out] if w is None else w
        if skey is None:
            skey = _key(in_) if store else _key(out)
        self.op(eng, lambda e, out=out, in_=in_, kw=kw: e.dma_start(out=out, in_=in_, **kw), r, w, dma=skey)

    def barrier(self):
        self.ops.append(["sp", lambda e: e.nop(), (), (PH,), False])

    def finish(self):
        nc = self.nc
        ops = self.ops
        n = len(ops)
        last_w = {}
        rd_eng = {}
        rd_dma = {}
        deps_all = [None] * n
        for i, (eng, fn, r, w, dma) in enumerate(ops):
            deps = set()
            for k in r:
                j = last_w.get(k)
                if j is not None:
                    deps.add(j)
            for k in w:
                j = last_w.get(k)
                if j is not None:
                    deps.add(j)
                d = rd_eng.get(k)
                if d:
                    deps.update(d.values())
                d = rd_dma.get(k)
                if d:
                    deps.update(d)
            for k in r:
                if dma:
                    rd_dma.setdefault(k, []).append(i)
                else:
                    rd_eng.setdefault(k, {})[eng] = i
            for k in w:
                last_w[k] = i
                rd_eng[k] = {}
                rd_dma[k] = []
            deps.discard(i)
            deps_all[i] = deps
        need = [False] * n
        for i, (eng, fn, r, w, dma) in enumerate(ops):
            keep = set()
            for j in deps_all[i]:
                je, _, _, _, jd = ops[j]
                if jd:
                    keep.add(j)
                elif je != eng or eng != "pe":
                    keep.add(j)
            deps_all[i] = keep
            for j in keep:
                need[j] = True
        pos = [None] * n
        cnt = {}
        for i, (eng, fn, r, w, dma) in enumerate(ops):
            if dma:
                stream = ("dma", dma)
            elif need[i]:
                stream = ("eng", eng)
            else:
                continue
            c = cnt.get(stream, 0)
            pos[i] = (stream, c)
            cnt[stream] = c + 1
        phase = [0] * n
        ph = 0
        for i, o in enumerate(ops):
            if o[3] == (PH,):
                ph += 1
            phase[i] = ph
        seg = {}
        for i in range(n):
            if pos[i] is None:
                continue
            stream, c = pos[i]
            EP = EPOCH_D if stream[0] == "dma" else EPOCH_C
            sk = (stream, c // EP)
            g = seg.get(sk)
            if g is None:
                seg[sk] = [phase[i], phase[i], 1]
            else:
                g[1] = phase[i]
                g[2] += 1
        es = contextlib.ExitStack()
        hw = []
        base = {}
        semof = {}
        for sk in sorted(seg, key=lambda q: seg[q][0]):
            pf, pl, c = seg[sk]
            inc = 16 if sk[0][0] == "dma" else 1
            pick = None
            for hwi in hw:
                if hwi[2] < pf and hwi[1] + c * inc <= 30000:
                    pick = hwi
                    break
            if pick is None:
                pick = [es.enter_context(nc.semaphore(f"s{len(hw)}")), 0, -1]
                hw.append(pick)
            base[sk] = pick[1]
            semof[sk] = pick[0]
            pick[1] += c * inc
            pick[2] = pl
        self.nsem = len(hw)
        by_eng = {}
        for i, o in enumerate(ops):
            by_eng.setdefault(o[0], []).append(i)

        def emit(e, eng):
            waited = {}
            for i in by_eng.get(eng, []):
                _, fn, r, w, dma = ops[i]
                reqs = {}
                for j in deps_all[i]:
                    stream, c = pos[j]
                    isd = stream[0] == "dma"
                    EP = EPOCH_D if isd else EPOCH_C
                    sk = (stream, c // EP)
                    v = base[sk] + (c % EP + 1) * (16 if isd else 1)
                    sm = semof[sk]
                    if reqs.get(id(sm), (0, None))[0] < v:
                        reqs[id(sm)] = (v, sm)
                for sid, (v, sm) in reqs.items():
                    if waited.get(sid, 0) < v:
                        e.wait_ge(sm, v)
                        waited[sid] = v
                ins = fn(e)
                if pos[i] is not None:
                    stream, c = pos[i]
                    isd = stream[0] == "dma"
                    EP = EPOCH_D if isd else EPOCH_C
                    ins.then_inc(semof[(stream, c // EP)], 16 if isd else 1)

        with nc.Block() as block:
            @block.tensor
            def _(e): emit(e, "pe")

            @block.scalar
            def _(e): emit(e, "act")

            @block.vector
            def _(e): emit(e, "dve")

            @block.gpsimd
            def _(e): emit(e, "pool")

            @block.sync
            def _(e): emit(e, "sp")
        es.close()


class KB:
    def __init__(self, nc):
        self.nc = nc
        self.S = Sched(nc)
        self.gs = contextlib.ExitStack()

    def sb(self, es, name, shape, dt=F32):
        self.uid = getattr(self, "uid", 0) + 1
        return es.enter_context(self.nc.sbuf_tensor(f"{name}_u{self.uid}", list(shape), dt))

    def dram(self, name, shape, dt, kind):
        return self.nc.dram_tensor(name, list(shape), dt, kind=kind).ap()

    @staticmethod
    def _rw(out, ins, r, w):
        if r is None:
            r = [a for a in ins if not isinstance(a, (int, float)) and a is not None]
        if w is None:
            w = [out]
        return r, w

    def mm(self, out, lhsT, rhs, start, stop, r=None, w=None):
        r, w = self._rw(out, [lhsT, rhs], r, w)
        self.S.op("pe", lambda e: e.matmul(out, lhsT=lhsT, rhs=rhs, start=start, stop=stop), r, w)

    def tr(self, out, in_, ident, r=None, w=None):
        r, w = self._rw(out, [in_, ident], r, w)
        self.S.op("pe", lambda e: e.transpose(out, in_, ident), r, w)

    def act(self, out, in_, func, scale=None, bias=None, r=None, w=None):
        r, w = self._rw(out, [in_, scale, bias], r, w)
        kw = {}
        if scale is not None:
            kw["scale"] = scale
        if bias is not None:
            kw["bias"] = bias
        self.S.op("act", lambda e: e.activation(out=out, in_=in_, func=func, **kw), r, w)

    def tt(self, eng, out, in0, in1, op, r=None, w=None):
        r, w = self._rw(out, [in0, in1], r, w)
        self.S.op(eng, lambda e: e.tensor_tensor(out=out, in0=in0, in1=in1, op=op), r, w)

    def ts(self, eng, out, in0, s1, s2, op0, op1=None, r=None, w=None):
        r, w = self._rw(out, [in0, s1, s2], r, w)
        if op1 is None:
            self.S.op(eng, lambda e: e.tensor_scalar(out=out, in0=in0, scalar1=s1, scalar2=None, op0=op0), r, w)
        else:
            self.S.op(eng, lambda e: e.tensor_scalar(out=out, in0=in0, scalar1=s1, scalar2=s2, op0=op0, op1=op1), r, w)

    def stt(self, out, in0, scalar, in1, op0, op1, r=None, w=None):
        r, w = self._rw(out, [in0, scalar, in1], r, w)
        self.S.op("dve", lambda e: e.scalar_tensor_tensor(out=out, in0=in0, scalar=scalar, in1=in1, op0=op0, op1=op1), r, w)

    def copy(self, eng, out, in_, r=None, w=None):
        r, w = self._rw(out, [in_], r, w)
        if eng == "act":
            self.S.op("act", lambda e: e.copy(out=out, in_=in_), r, w)
        else:
            self.S.op(eng, lambda e: e.tensor_copy(out=out, in_=in_), r, w)

    def memset(self, eng, ap, val, w=None):
        self.S.op(eng, lambda e: e.memset(ap, val), (), [ap] if w is None else w)

    def fn(self, eng, f, r, w):
        self.S.op(eng, f, r, w)

    def dma(self, eng, out, in_, **kw):
        self.S.dma(eng, out, in_, **kw)


def tiles_of(b, with_ctx=True):
    t = []
    if with_ctx:
        t.append((b, 0, CTX, 2))
    for j in range(SEQ // 512):
        t.append((b, CTX + 512 * j, 512, b))
    return t


class Prog:
    def __init__(self, layers=(0, 1, 2, 3), h_from_input=True, dbg=None):
        self.layers = list(layers)
        self.dbg = dbg
        NL = len(self.layers)
        self.li = {l: j for j, l in enumerate(self.layers)}
        nc = bass.Bass("TRN2", target_bir_lowering=False)
        self.nc = nc
        k = KB(nc)
        self.k = k
        NEd = dbg[2] if isinstance(dbg, tuple) else NE
        shapes = {
            "hin": [NBC, TT, D], "cc": [3, D],
            "ada_w": [NL, D, 6 * D], "ada_b": [NL, 1, 6 * D], "ln_g": [NL, 2, D], "ln_b": [NL, 2, D],
            "ret_w_in": [2, D, 6 * D], "ret_decay": [2, 8], "ret_gn_g": [2, 2 * D], "ret_w_out": [2, 2 * D, D],
            "ret_cos": [128, SEQ], "ret_sin": [128, SEQ],
            "mla_wd": [D, 768], "mla_norms": [5, 128], "mla_wuq": [384, 2048], "mla_wukv": [256, 2048],
            "mla_w_out": [D, D], "mla_c1": [128, SEQ], "mla_c2": [128, SEQ],
            "conv_w_in": [1, D, 3 * D], "conv_wb": [1, 4, D], "conv_w_out": [1, D, D],
            "moe_wr": [NL, D, 64], "moe_bg": [NL, 4], "moe_be": [NL, 32],
            "moe_w1": [NL, NE, 128, 8, 512], "moe_w3": [NL, NE, 128, 8, 512], "moe_w2": [NL, NE, 128, 4, D],
        }

        class Lazy(dict):
            def __missing__(d, name):
                d[name] = k.dram(name, shapes[name], F32, "ExternalInput")
                return d[name]
        I = Lazy()
        self.I = I
        self.out = k.dram("out", [NBC, SEQ, D], F32, "ExternalOutput")
        self.HA = k.dram("HA", [NBC, TT, D], F32, "ExternalOutput" if dbg else "Internal")
        self.HB = k.dram("HB", [NBC, TT, D], F32, "ExternalOutput" if dbg else "Internal")
        self.SC1 = k.dram("SC1", [NBC, D, SEQ + 2], F32, "Internal")
        self.SC2 = k.dram("SC2", [NBC, D, SEQ], F32, "Internal")
        self.SC1c = k.dram("SC1c", [NBC, D, CTX + 2], F32, "Internal")
        self.SC2c = k.dram("SC2c", [NBC, D, CTX], F32, "Internal")
        if any(l % 3 == 0 for l in self.layers):
            for nm in ("QT", "KT", "QTF", "QTB"):
                setattr(self, nm, k.dram(nm, [NBC, D, TT], BF16, "Internal"))
            for nm in ("KF", "KB"):
                setattr(self, nm, k.dram(nm, [NBC, TT, D], BF16, "Internal"))
            for nm in ("RV", "RG"):
                setattr(self, nm, k.dram(nm, [NBC, TT, 2 * D], BF16, "Internal"))
            self.SBD = k.dram("SBD", [NBC, TT // 128, 128, 8 * 512], BF16, "Internal")
        if any(l % 3 == 1 for l in self.layers):
            self.QN = k.dram("QN", [NBC, 8, 128, TT], BF16, "Internal")
            self.QR = k.dram("QR", [NBC, 4, 128, TT], BF16, "Internal")
            self.KN = k.dram("KN", [NBC, 8, 128, TT], BF16, "Internal")
            self.KR = k.dram("KR", [NBC, 64, TT], BF16, "Internal")
            self.MV = k.dram("MV", [NBC, TT, D], BF16, "Internal")
            self.OT = k.dram("OT", [NBC, D, TT], BF16, "Internal")
        NROWS = ((2 * (TT // 128) * NBC * 128 + 511) // 512 + NE) * 512
        self.XS = k.dram("XS", [NROWS, D], BF16, "Internal")
        self.VTB = k.dram("VTB", [NBC, TT, D], BF16, "Internal")
        self.YS = k.dram("YS", [NROWS, D], F32, "Internal")
        gs = k.gs
        self.PS = [gs.enter_context(nc.psum_tensor(f"ps{i}", [128, 512], F32)) for i in range(8)]
        self.ident = k.sb(gs, "ident", [128, 128])
        self.actT = k.sb(gs, "actT", [128, 8, 4])
        self.ones = k.sb(gs, "ones", [1, 512])
        self.negh = k.sb(gs, "negh", [128, 512])
        self.onesf = k.sb(gs, "onesf", [128, 128])
        self.onesb = k.sb(gs, "onesb", [128, 128], BF16)
        self.identb = k.sb(gs, "identb", [128, 128], BF16)
        self.MP = k.sb(gs, "MP", [128, 4, 8, 4])
        self.GB = k.sb(gs, "GB", [128, 3, 2, 1024])
        self.LNP = k.sb(gs, "LNP", [128, 4, 1024])
        self.setup()
        first = True
        for i in self.layers:
            src = I["hin"] if (first and h_from_input) else self.HA
            first = False
            self.mod_params(i)
            if isinstance(dbg, tuple) and dbg[0] == "moe":
                self.moe_layer(i, src, self.HA, False, dbg_ns=dbg[1], dbg_ne=dbg[2])
                break
            if dbg == "mod":
                d1 = k.dram("dbg_MP", [128, 4 * 8 * 4], F32, "ExternalOutput")
                d2 = k.dram("dbg_GB", [128, 3 * 2 * 1024], F32, "ExternalOutput")
                d3 = k.dram("dbg_LNP", [128, 4 * 1024], F32, "ExternalOutput")
                k.dma("sp", d1, self.MP[:].rearrange("p a b c -> p (a b c)"), store=True)
                k.dma("sp", d2, self.GB[:].rearrange("p a b c -> p (a b c)"), store=True)
                k.dma("sp", d3, self.LNP[:].rearrange("p a c -> p (a c)"), store=True)
                break
            kind = i % 3
            if kind == 2:
                self.conv_layer(i, src, self.HB)
            elif kind == 0:
                self.ret_layer(i, src, self.HB)
            elif kind == 1:
                self.mla_layer(i, src, self.HB)
            else:
                raise NotImplementedError
            if dbg == "mix":
                break
            last = (i == DEPTH - 1)
            self.moe_layer(i, self.HB, self.HA, last)
        import os
        mo = int(os.environ.get("MAXOPS", "0"))
        if mo:
            for o in k.S.ops[mo:mo + 3]:
                print("TRUNC next ops:", o[0], o[2][:3], o[3])
            del k.S.ops[mo:]
        k.S.barrier()
        k.S.op("sp", lambda e: e.nop(), (), ())
        k.S.finish()
        gs.close()

    def setup(self):
        k = self.k
        nc = self.nc
        ident = self.ident
        k.memset("pool", ident[:], 0.0)
        k.fn("pool", lambda e: e.affine_select(out=ident[:], in_=ident[:], pattern=[[-1, 128]], compare_op=ALU.not_equal,
                                               fill=1.0, base=0, channel_multiplier=1), [ident], [ident])
        k.copy("dve", self.identb[:], ident[:])
        k.memset("dve", self.ones[:], 1.0)
        k.memset("dve", self.negh[:], -0.5)
        k.memset("dve", self.onesf[:], 1.0)
        k.memset("dve", self.onesb[:], 1.0)
        with contextlib.ExitStack() as es:
            craw = k.sb(es, "craw", [4, D])
            k.memset("dve", craw[:], 0.0)
            k.dma("sp", craw[0:3, :], self.I["cc"][:, :])
            k.act(craw[:], craw[:], AF.Silu)
            ps = self.PS[0]
            for kk in range(8):
                k.tr(ps[:, kk * 4:(kk + 1) * 4], craw[0:4, kk * 128:(kk + 1) * 128], ident[0:4, 0:4])
            k.copy("dve", self.actT[:], ps[:, 0:32].rearrange("p (k f) -> p k f", f=4))
            k.S.barrier()

    def mod_params(self, i):
        k = self.k
        I = self.I
        i = self.li[i]
        with contextlib.ExitStack() as es:
            wblk = [k.sb(es, f"mp_w{j}", [128, 8, 1024]) for j in range(2)]
            brow = k.sb(es, "mp_brow", [1, 6 * D])
            REP = k.sb(es, "mp_REP", [128, 3, 8, 128])
            for r in range(3):
                for kk in range(8):
                    k.copy("dve", REP[:, r, kk, :], self.actT[:, kk, r:r + 1].broadcast_to([128, 128]))
            k.dma("sp", brow[:], I["ada_b"][i])
            for j in range(4):
                k.dma("sp", self.LNP[:, j, :], (I["ln_g"] if j % 2 == 0 else I["ln_b"])[i, j // 2].partition_broadcast(128))
            pi = 0
            for wi in range(6):
                buf = wblk[wi % 2]
                k.dma("sp", buf[:], I["ada_w"][i][:, wi * 1024:(wi + 1) * 1024].rearrange("(k p) n -> p k n", p=128))
                if wi in (0, 1, 3, 4):
                    slot = {0: 0, 1: 1, 3: 2, 4: 3}[wi]
                    ps = self.PS[pi % 8]
                    pi += 1
                    for ko in range(8):
                        o = ps[:, ko * 4:ko * 4 + 3]
                        for ki in range(8):
                            k.mm(o, buf[:, ki, ko * 128:(ko + 1) * 128], self.actT[:, ki, 0:3], ki == 0, False)
                        k.mm(o, brow[0:1, wi * 1024 + ko * 128: wi * 1024 + (ko + 1) * 128], self.ones[0:1, 0:3], False, True)
                    src = ps[:, 0:32].rearrange("p (k f) -> p k f", f=4)[:, :, 0:3]
                    if wi in (1, 4):
                        k.ts("dve", self.MP[:, slot, :, 0:3], src, 1.0, None, ALU.add)
                    else:
                        k.copy("dve", self.MP[:, slot, :, 0:3], src)
                else:
                    gi = 0 if wi == 2 else 1
                    for r in range(3):
                        for half in range(2):
                            ps = self.PS[pi % 8]
                            pi += 1
                            for ki in range(8):
                                k.mm(ps[:], REP[:, r, ki, :], buf[:, ki, half * 512:(half + 1) * 512], ki == 0, False)
                            k.mm(ps[:], self.ones[0:1, 0:128], brow[0:1, wi * 1024 + half * 512: wi * 1024 + (half + 1) * 512], False, True)
                            k.copy("act", self.GB[:, r, gi, half * 512:(half + 1) * 512], ps[:])
            k.S.barrier()

    def prologue(self, src, b, t0, n, r, slot_sh, stg, uT, psb, v32=None):
        k = self.k
        nt = n // 128
        k.dma("sp", stg[:, 0:nt, :], src[b, t0:t0 + n, :].rearrange("(t p) d -> p t d", p=128),
              r=[(_key(src), b, t0)])
        for kk in range(8):
            ps = psb[kk % len(psb)]
            for t in range(nt):
                k.tr(ps[:, t * 128:(t + 1) * 128], stg[:, t, kk * 128:(kk + 1) * 128], self.ident[:])
            k.act(uT[:, kk, 0:n], ps[:, 0:n], AF.Identity, scale=self.MP[:, slot_sh + 1, kk, r:r + 1],
                  bias=self.MP[:, slot_sh, kk, r:r + 1])
            if v32 is not None:
                k.ts("dve", v32[:, kk, 0:n], ps[:, 0:n], self.MP[:, slot_sh + 1, kk, r:r + 1],
                     self.MP[:, slot_sh, kk, r:r + 1], ALU.mult, ALU.add)

    def ln_epilogue(self, ysrc, stg, nt, r, gi, li, zb, st, mv, rs, dst_rows, dst_key):
        k = self.k
        if isinstance(zb, list):
            self._lnc = getattr(self, "_lnc", 0) + 1
            x = self._lnc % len(zb)
            zb, st, mv, rs = zb[x], st[x], mv[x], rs[x]
        zk = lambda t: (_key(zb), t)
        for t in range(nt):
            z = zb[:, t, :]
            for half in range(2):
                y = ysrc(t, half)
                k.tt("dve", zb[:, t, half * 512:(half + 1) * 512], y, self.GB[:, r, gi, half * 512:(half + 1) * 512], ALU.mult,
                     r=[y, self.GB], w=[zk(t)])
            k.stt(z, stg[:, t, :], ALPHA, z, ALU.mult, ALU.add, r=[stg, zk(t)], w=[zk(t)])
            for c in range(2):
                k.fn("dve", (lambda e, t=t, c=c: e.bn_stats(out=st[:, t, c * 6:(c + 1) * 6], in_=zb[:, t, c * 512:(c + 1) * 512])),
                     [zk(t)], [(_key(st), t, c)])
            k.fn("dve", (lambda e, t=t: e.bn_aggr(out=mv[:, t, :], in_=st[:, t, :])), [(_key(st), t, 0), (_key(st), t, 1)], [(_key(mv), t)])
        k.ts("pool", rs[:, 0:nt], mv[:, 0:nt, 1], LN_EPS, None, ALU.add, r=[(_key(mv), t) for t in range(nt)], w=[rs])
        k.tt("pool", rs[:, 0:nt], rs[:, 0:nt], self.negh[:, 0:nt], ALU.pow)
        k.tt("pool", rs[:, 4:4 + nt], mv[:, 0:nt, 0], rs[:, 0:nt], ALU.mult, r=[(_key(mv), t) for t in range(nt)] + [rs], w=[rs])
        k.ts("pool", rs[:, 4:4 + nt], rs[:, 4:4 + nt], -1.0, None, ALU.mult)
        for t in range(nt):
            z = zb[:, t, :]
            k.act(z, z, AF.Identity, scale=rs[:, t:t + 1], bias=rs[:, 4 + t:5 + t], r=[zk(t), rs], w=[zk(t)])
            k.tt("dve", z, z, self.LNP[:, 2 * li, :], ALU.mult, r=[zk(t), self.LNP], w=[zk(t)])
            k.tt("pool", z, z, self.LNP[:, 2 * li + 1, :], ALU.add, r=[zk(t), self.LNP], w=[zk(t)])
        k.dma("pool", dst_rows.rearrange("(t p) d -> p t d", p=128), zb[:, 0:nt, :], store=True,
              r=[zk(t) for t in range(nt)], w=[dst_key])

    def conv_layer(self, i, src, dst):
        k = self.k
        I = self.I
        PS = self.PS
        need_ctx = i < DEPTH - 1
        with contextlib.ExitStack() as es:
            win = k.sb(es, "cv_win", [128, 8, 3 * D], BF16)
            k.dma("pool", win[:], I["conv_w_in"][0].rearrange("(k p) n -> p k n", p=128))
            stg = [k.sb(es, "cv_stg0", [128, 4, D])]
            uT = [k.sb(es, f"cv_uT{j}", [128, 8, 512], BF16) for j in range(2)]
            sbuf = [k.sb(es, f"cv_s{j}", [128, 8, 512]) for j in range(2)]
            gbuf = [k.sb(es, f"cv_g{j}", [128, 8, 512]) for j in range(2)]
            gct = [k.sb(es, f"cv_gc{j}", [128, 512]) for j in range(2)]
            zero = k.sb(es, "cv_zero", [128, 8, 1])
            k.memset("dve", zero[:], 0.0)
            it = 0
            for b in range(NBC):
                for (bb, t0, n, r) in tiles_of(b, need_ctx):
                    isctx = (r == 2)
                    s1 = (self.SC1c if isctx else self.SC1)
                    s2 = (self.SC2c if isctx else self.SC2)
                    c0 = t0 if isctx else t0 - CTX
                    sg, u, sbf, gbf = stg[0], uT[it % 2], sbuf[it % 2], gbuf[it % 2]
                    self.prologue(src, b, t0, n, r, 0, sg, u, [PS[6], PS[7]])
                    for j in range(8):
                        pb, pc, ph = PS[(3 * j) % 6], PS[(3 * j + 1) % 6], PS[(3 * j + 2) % 6]
                        for (pp, col) in ((pb, j), (pc, 8 + j), (ph, 16 + j)):
                            for kk in range(8):
                                k.mm(pp[:, 0:n], win[:, kk, col * 128:(col + 1) * 128], u[:, kk, 0:n], kk == 0, kk == 7)
                        g = gct[j % 2]
                        k.copy("act", gbf[:, j, 0:n], pb[:, 0:n], w=[(_key(gbf), j)])
                        k.copy("act", g[:, 0:n], pc[:, 0:n])
                        k.tt("dve", sbf[:, j, 0:n], g[:, 0:n], ph[:, 0:n], ALU.mult, w=[(_key(sbf), j)])
                    k.dma("pool", s1[b, :, 1 + c0:1 + c0 + n].rearrange("(k p) t -> p k t", p=128), sbf[:, :, 0:n], store=True,
                          r=[(_key(sbf), j) for j in range(8)], w=[("SC1", b, isctx, c0)])
                    k.dma("act", s2[b, :, c0:c0 + n].rearrange("(k p) t -> p k t", p=128), gbf[:, :, 0:n], store=True,
                          r=[(_key(gbf), j) for j in range(8)], w=[("SC2", b, isctx, c0)])
                    it += 1
                for (s1, L) in (((self.SC1c, CTX), (self.SC1, SEQ)) if need_ctx else ((self.SC1, SEQ),)):
                    for col in (0, L + 1):
                        k.dma("sp", s1[b, :, col:col + 1].rearrange("(k p) t -> p k t", p=128), zero[:], store=True,
                              w=[("SC1h", b, L, col)], allow_slow_non_contiguous=True)
            k.S.barrier()
        with contextlib.ExitStack() as es:
            wout = k.sb(es, "cv_wout", [128, 8, D], BF16)
            cw = k.sb(es, "cv_cw", [128, 4, 8])
            craw = k.sb(es, "cv_craw", [32, 128])
            k.dma("pool", wout[:], I["conv_w_out"][0].rearrange("(k p) n -> p k n", p=128))
            k.dma("sp", craw[:], I["conv_wb"][0].rearrange("j (k p) -> (j k) p", p=128))
            k.tr(PS[7][:, 0:32], craw[:, :], self.ident[0:32, 0:32])
            k.copy("dve", cw[:], PS[7][:, 0:32].rearrange("p (j k) -> p j k", k=8))
            stg = [k.sb(es, f"cv_stg{j}", [128, 4, D]) for j in range(2)]
            gbuf = [k.sb(es, f"cv_g{j}", [128, 8, 512]) for j in range(2)]
            zb = [k.sb(es, f"cv_zb{x}", [128, 4, D]) for x in range(1)]
            st = [k.sb(es, f"cv_st{x}", [128, 4, 12]) for x in range(1)]
            mv = [k.sb(es, f"cv_mv{x}", [128, 4, 2]) for x in range(1)]
            rs = [k.sb(es, f"cv_rs{x}", [128, 8]) for x in range(1)]
            sx = [k.sb(es, f"cv_sx{j}", [128, 8, 514]) for j in range(2)]
            gT = [k.sb(es, f"cv_gT{j}", [128, 8, 512], BF16) for j in range(2)]
            ctmp = [k.sb(es, f"cv_ct{j}", [128, 512]) for j in range(2)]
            it = 0
            for b in range(NBC):
                for (bb, t0, n, r) in tiles_of(b, need_ctx):
                    isctx = (r == 2)
                    s1 = (self.SC1c if isctx else self.SC1)
                    s2 = (self.SC2c if isctx else self.SC2)
                    c0 = t0 if isctx else t0 - CTX
                    nt = n // 128
                    sg, sxx, gbf, g = stg[it % 2], sx[it % 2], gbuf[it % 2], gT[it % 2]
                    k.dma("sp", sg[:, 0:nt, :], src[b, t0:t0 + n, :].rearrange("(t p) d -> p t d", p=128), r=[(_key(src), b, t0)])
                    k.dma("sp", sxx[:, :, 0:n + 2], s1[b, :, c0:c0 + n + 2].rearrange("(k p) t -> p k t", p=128), r=[("SC1all",)])
                    k.dma("sp", gbf[:, :, 0:n], s2[b, :, c0:c0 + n].rearrange("(k p) t -> p k t", p=128), r=[("SC2all",)])
                    for kk in range(8):
                        c = ctmp[kk % 2]
                        k.act(c[:, 0:n], sxx[:, kk, 1:n + 1], AF.Identity, scale=cw[:, 1, kk:kk + 1], bias=cw[:, 3, kk:kk + 1])
                        k.stt(c[:, 0:n], sxx[:, kk, 0:n], cw[:, 0, kk:kk + 1], c[:, 0:n], ALU.mult, ALU.add)
                        k.stt(c[:, 0:n], sxx[:, kk, 2:n + 2], cw[:, 2, kk:kk + 1], c[:, 0:n], ALU.mult, ALU.add)
                        k.tt("dve", g[:, kk, 0:n], c[:, 0:n], gbf[:, kk, 0:n], ALU.mult)
                    for t in range(nt):
                        for half in range(2):
                            ps = PS[(t * 2 + half) % 8]
                            for kk in range(8):
                                k.mm(ps[:], g[:, kk, t * 128:(t + 1) * 128], wout[:, kk, half * 512:(half + 1) * 512], kk == 0, kk == 7)
                    self.ln_epilogue(lambda t, half: PS[(t * 2 + half) % 8][:], sg, nt, r, 0, 0, zb, st, mv, rs,
                                     dst[b, t0:t0 + n, :], (_key(dst), b, t0))
                    it += 1
            k.S.barrier()

    def ret_layer(self, i, src, dst):
        k = self.k
        I = self.I
        PS = self.PS
        need_ctx = i < DEPTH - 1
        j = i // 3
        NCH = TT // 128
        with contextlib.ExitStack() as tes:
            lg = k.sb(tes, "rt_lg", [128, 8])
            GL = k.sb(tes, "rt_GL", [128, 8])
            MT = k.sb(tes, "rt_MT", [128, 4, 128])
            DF = k.sb(tes, "rt_DF", [128, 4, 128])
            DB = k.sb(tes, "rt_DB", [128, 4, 128])
            KFd = k.sb(tes, "rt_KFd", [128, 4])
            KBd = k.sb(tes, "rt_KBd", [128, 4])
            with contextlib.ExitStack() as es:
                Dm = k.sb(es, "rt_D", [128, 128])
                Dp = k.sb(es, "rt_Dp", [128, 128])
                Dn = k.sb(es, "rt_Dn", [128, 128])
                mf = k.sb(es, "rt_mf", [128, 128])
                mb = k.sb(es, "rt_mb", [128, 128])
                Ef = k.sb(es, "rt_Ef", [128, 128])
                Eb = k.sb(es, "rt_Eb", [128, 128])
                I1 = k.sb(es, "rt_I1", [128, 128])
                I2 = k.sb(es, "rt_I2", [128, 128])
                P1 = k.sb(es, "rt_P1", [128, 2])
                k.dma("sp", lg[:], I["ret_decay"][j].partition_broadcast(128))
                k.act(lg[:], lg[:], AF.Exp, scale=-1.0)
                k.act(lg[:], lg[:], AF.Ln, bias=1.0)
                k.ts("dve", lg[:], lg[:], -1.0, None, ALU.mult)
                k.act(GL[:], lg[:], AF.Exp, scale=128.0)
                k.fn("pool", lambda e: e.iota(Dm[:], pattern=[[1, 128]], base=0, channel_multiplier=-1,
                                              allow_small_or_imprecise_dtypes=True), [], [Dm])
                k.fn("pool", lambda e: e.iota(I1[:], pattern=[[1, 128]], base=1, channel_multiplier=0,
                                              allow_small_or_imprecise_dtypes=True), [], [I1])
                k.fn("pool", lambda e: e.iota(I2[:], pattern=[[-1, 128]], base=128, channel_multiplier=0,
                                              allow_small_or_imprecise_dtypes=True), [], [I2])
                k.fn("pool", lambda e: e.iota(P1[:, 0:1], pattern=[[0, 1]], base=127, channel_multiplier=-1,
                                              allow_small_or_imprecise_dtypes=True), [], [P1])
                k.fn("pool", lambda e: e.iota(P1[:, 1:2], pattern=[[0, 1]], base=0, channel_multiplier=1,
                                              allow_small_or_imprecise_dtypes=True), [P1], [P1])
                k.ts("dve", Dp[:], Dm[:], 0.0, None, ALU.max)
                k.ts("dve", Dn[:], Dm[:], -1.0, 0.0, ALU.mult, ALU.max)
                k.ts("dve", mf[:], Dm[:], 0.0, None, ALU.is_ge)
                k.ts("dve", mb[:], Dm[:], 0.0, None, ALU.is_le)
                for h in range(4):
                    k.act(Ef[:], Dp[:], AF.Exp, scale=lg[:, h:h + 1])
                    k.tt("dve", Ef[:], Ef[:], mf[:], ALU.mult)
                    k.act(Eb[:], Dn[:], AF.Exp, scale=lg[:, 4 + h:5 + h])
                    k.tt("dve", Eb[:], Eb[:], mb[:], ALU.mult)
                    k.tt("dve", Ef[:], Ef[:], Eb[:], ALU.add)
                    k.ts("dve", MT[:, h, :], Ef[:], 0.0625, None, ALU.mult)
                    k.act(DF[:, h, :], I1[:], AF.Exp, scale=lg[:, h:h + 1])
                    k.act(DB[:, h, :], I2[:], AF.Exp, scale=lg[:, 4 + h:5 + h])
                    k.act(KFd[:, h:h + 1], P1[:, 0:1], AF.Exp, scale=lg[:, h:h + 1])
                    k.act(KBd[:, h:h + 1], P1[:, 1:2], AF.Exp, scale=lg[:, 4 + h:5 + h])
                k.ts("dve", KFd[:], KFd[:], 0.0625, None, ALU.mult)
                k.ts("dve", KBd[:], KBd[:], 0.0625, None, ALU.mult)
                k.S.barrier()
            for part in range(2):
              with contextlib.ExitStack() as es:
                wqk = k.sb(es, "r1_wqk", [128, 8, D], BF16)
                k.dma("pool", wqk[:], I["ret_w_in"][j][:, part * D:(part + 1) * D].rearrange("(k p) n -> p k n", p=128))
                cosT = k.sb(es, "r1_cos", [128, SEQ])
                sinT = k.sb(es, "r1_sin", [128, SEQ])
                k.dma("sp", cosT[:], I["ret_cos"])
                k.dma("sp", sinT[:], I["ret_sin"])
                stg = k.sb(es, "r1_stg", [128, 4, D])
                uT = [k.sb(es, f"r1_uT{x}", [128, 8, 512], BF16) for x in range(2)]
                tm = [k.sb(es, f"r1_tm{x}", [128, 512]) for x in range(4)]
                if part == 0:
                    qT = k.sb(es, "r1_qT", [128, 8, 512], BF16)
                    qf = k.sb(es, "r1_qf", [128, 8, 512], BF16)
                    qb = k.sb(es, "r1_qb", [128, 8, 512], BF16)
                    q32 = [k.sb(es, f"r1_q32{x}", [128, 512]) for x in range(2)]
                else:
                    kT = k.sb(es, "r1_kT", [128, 8, 512], BF16)
                    k32 = k.sb(es, "r1_k32", [128, 8, 512])
                    kfb = k.sb(es, "r1_kf", [128, 4, D], BF16)
                    kbb = k.sb(es, "r1_kb", [128, 4, D], BF16)
                it = 0
                for b in range(NBC):
                    for (bb, t0, n, r) in tiles_of(b, True):
                        isctx = (r == 2)
                        c0 = t0 - CTX
                        nt = n // 128
                        u = uT[it % 2]
                        it += 1
                        self.prologue(src, b, t0, n, r, 0, stg, u, [PS[6], PS[7]])
                        for hh in range(part * 4, part * 4 + 4):
                            isq = hh < 4
                            h = hh % 4
                            c1, c2 = 2 * h, 2 * h + 1
                            x1, x2 = PS[(2 * hh) % 4], PS[(2 * hh + 1) % 4]
                            for kk in range(8):
                                k.mm(x1[:, 0:n], wqk[:, kk, c1 * 128:(c1 + 1) * 128], u[:, kk, 0:n], kk == 0, kk == 7)
                            for kk in range(8):
                                k.mm(x2[:, 0:n], wqk[:, kk, c2 * 128:(c2 + 1) * 128], u[:, kk, 0:n], kk == 0, kk == 7)
                            d1, d2 = 2 * h, 2 * h + 1
                            if isq:
                                o1, o2 = q32[0][:, 0:n], q32[1][:, 0:n]
                            else:
                                o1, o2 = k32[:, d1, 0:n], k32[:, d2, 0:n]
                            if isctx:
                                k.copy("act", o1, x1[:, 0:n])
                                k.copy("act", o2, x2[:, 0:n])
                            else:
                                cs, sn = cosT[:, c0:c0 + n], sinT[:, c0:c0 + n]
                                k.tt("dve", tm[0][:, 0:n], x1[:, 0:n], cs, ALU.mult)
                                k.tt("dve", tm[1][:, 0:n], x2[:, 0:n], sn, ALU.mult)
                                k.tt("dve", tm[2][:, 0:n], x1[:, 0:n], sn, ALU.mult)
                                k.tt("dve", tm[3][:, 0:n], x2[:, 0:n], cs, ALU.mult)
                                k.tt("pool", o1, tm[0][:, 0:n], tm[1][:, 0:n], ALU.subtract)
                                k.tt("pool", o2, tm[2][:, 0:n], tm[3][:, 0:n], ALU.add)
                            for (o, d) in ((o1, d1), (o2, d2)):
                                if isq:
                                    k.copy("act", qT[:, d, 0:n], o)
                                    o3 = o.rearrange("p (s i) -> p s i", i=128)
                                    k.tt("dve", qf[:, d, 0:n].rearrange("p (s i) -> p s i", i=128), o3,
                                         DF[:, h:h + 1, :].broadcast_to([128, nt, 128]), ALU.mult)
                                    k.tt("dve", qb[:, d, 0:n].rearrange("p (s i) -> p s i", i=128), o3,
                                         DB[:, h:h + 1, :].broadcast_to([128, nt, 128]), ALU.mult)
                                else:
                                    k.copy("act", kT[:, d, 0:n], o)
                        for s_ in range(nt if part == 1 else 0):
                            pa, pb = PS[4], PS[5]
                            for c in range(8):
                                pp = pa if c < 4 else pb
                                k.tr(pp[:, (c % 4) * 128:(c % 4 + 1) * 128], k32[:, c, s_ * 128:(s_ + 1) * 128], self.ident[:])
                            for h in range(4):
                                pp = pa if h < 2 else pb
                                sl = pp[:, (h % 2) * 256:(h % 2 + 1) * 256]
                                k.act(kfb[:, s_, h * 256:(h + 1) * 256], sl, AF.Identity, scale=KFd[:, h:h + 1])
                                k.ts("dve", kbb[:, s_, h * 256:(h + 1) * 256], sl, KBd[:, h:h + 1], None, ALU.mult)
                        for (buf, dr, qe) in (((qT, self.QT, "act"), (qf, self.QTF, "pool"), (qb, self.QTB, "pool")) if part == 0 else ((kT, self.KT, "act"),)):
                            k.dma(qe, dr[b, :, t0:t0 + n].rearrange("(k p) t -> p k t", p=128), buf[:, :, 0:n], store=True,
                                  w=[(_key(dr), b, t0)])
                        for (buf, dr, qe) in (((kfb, self.KF, "act"), (kbb, self.KB, "pool")) if part == 1 else ()):
                            k.dma(qe, dr[b, t0:t0 + n, :].rearrange("(s p) d -> p s d", p=128), buf[:, 0:nt, :], store=True,
                                  w=[(_key(dr), b, t0)])
                k.S.barrier()
            with contextlib.ExitStack() as es:
                wvg = k.sb(es, "r1_wvg", [128, 8, 4 * D], BF16)
                k.dma("pool", wvg[:], I["ret_w_in"][j][:, 2 * D:6 * D].rearrange("(k p) n -> p k n", p=128))
                stg = k.sb(es, "r1b_stg", [128, 4, D])
                uT = [k.sb(es, f"r1b_uT{x}", [128, 8, 512], BF16) for x in range(2)]
                vt = [k.sb(es, "r1b_vt0", [128, 4, 2 * D], BF16)] * 2
                gt = [k.sb(es, "r1b_gt0", [128, 4, 2 * D], BF16)] * 2
                it = 0
                for b in range(NBC):
                    for (bb, t0, n, r) in tiles_of(b, True):
                        nt = n // 128
                        u, vv, gg = uT[it % 2], vt[it % 2], gt[it % 2]
                        it += 1
                        self.prologue(src, b, t0, n, r, 0, stg, u, [PS[6], PS[7]])
                        pi = 0
                        for s_ in range(nt):
                            for nb in range(8):
                                ps = PS[pi % 6]
                                pi += 1
                                for kk in range(8):
                                    k.mm(ps[:], u[:, kk, s_ * 128:(s_ + 1) * 128], wvg[:, kk, nb * 512:(nb + 1) * 512], kk == 0, kk == 7)
                                if nb < 4:
                                    k.copy("act", vv[:, s_, nb * 512:(nb + 1) * 512], ps[:])
                                else:
                                    k.act(gg[:, s_, (nb - 4) * 512:(nb - 3) * 512], ps[:], AF.Silu)
                        k.dma("act", self.RV[b, t0:t0 + n, :].rearrange("(s p) d -> p s d", p=128), vv[:, 0:nt, :], store=True, w=[("RV", b, t0)])
                        k.dma("act", self.RG[b, t0:t0 + n, :].rearrange("(s p) d -> p s d", p=128), gg[:, 0:nt, :], store=True, w=[("RG", b, t0)])
                k.S.barrier()
            with contextlib.ExitStack() as es:
                Sb = k.sb(es, "r2_S", [128, 8, 512])
                sbf = [k.sb(es, f"r2_sbf{x}", [128, 8 * 512], BF16) for x in range(2)]
                kbc = [k.sb(es, f"r2_kb{x}", [128, D], BF16) for x in range(2)]
                vc = [k.sb(es, f"r2_v{x}", [128, 2 * D], BF16) for x in range(2)]
                it = 0
                for b in range(NBC):
                    for h in range(4):
                        for a in range(2):
                            k.memset("dve", Sb[:, h * 2 + a, :], 0.0, w=[(_key(Sb), h, a)])
                    order = [1, 0] + list(range(NCH - 1, 1, -1))
                    k.memset("pool", sbf[it % 2][:], 0.0)
                    for oi, g in enumerate(order):
                        sf, kb_, v_ = sbf[it % 2], kbc[it % 2], vc[it % 2]
                        sfn = sbf[(it + 1) % 2]
                        it += 1
                        k.dma("act", self.SBD[b, g], sf[:], store=True, w=[("SBD", b, g)])
                        if oi == len(order) - 1:
                            break
                        k.dma("sp", kb_[:], self.KB[b, g * 128:(g + 1) * 128, :], r=[("KBall",)])
                        k.dma("sp", v_[:], self.RV[b, g * 128:(g + 1) * 128, :], r=[("RVall",)])
                        for h in range(4):
                            for a in range(2):
                                ps = PS[(h * 2 + a) % 8]
                                k.mm(ps[:], kb_[:, h * 256 + a * 128:h * 256 + (a + 1) * 128], v_[:, h * 512:(h + 1) * 512], True, True)
                                k.stt(Sb[:, h * 2 + a, :], Sb[:, h * 2 + a, :], GL[:, 4 + h:5 + h], ps[:], ALU.mult, ALU.add,
                                      r=[(_key(Sb), h, a), GL, ps], w=[(_key(Sb), h, a)])
                                k.copy("act", sfn[:, (h * 2 + a) * 512:(h * 2 + a + 1) * 512], Sb[:, h * 2 + a, :],
                                       r=[(_key(Sb), h, a)], w=[sfn])
                k.S.barrier()
            with contextlib.ExitStack() as es:
                wout = k.sb(es, "r3_wout", [128, 16, D], BF16)
                k.dma("pool", wout[:], I["ret_w_out"][j].rearrange("(k p) n -> p k n", p=128))
                gng = k.sb(es, "r3_gng", [128, 2 * D])
                k.dma("sp", gng[:], I["ret_gn_g"][j].partition_broadcast(128))
                Sf = k.sb(es, "r3_Sf", [128, 8, 512])
                Sfb = k.sb(es, "r3_Sfb", [128, 8, 512], BF16)
                L = []
                for x in range(2):
                    L.append(dict(
                        qT=k.sb(es, f"r3_qT{x}", [128, 8, 128], BF16), kT=k.sb(es, f"r3_kT{x}", [128, 8, 128], BF16),
                        qf=k.sb(es, f"r3_qf{x}", [128, 8, 128], BF16), qb=k.sb(es, f"r3_qb{x}", [128, 8, 128], BF16),
                        kf=k.sb(es, f"r3_kf{x}", [128, D], BF16), v=k.sb(es, f"r3_v{x}", [128, 2 * D], BF16),
                        g=(k.sb(es, f"r3_g{x}", [128, 2 * D], BF16) if x == 0 else None),
                        sb=(k.sb(es, f"r3_sb{x}", [128, 8, 512], BF16) if x == 0 else None),
                        h=k.sb(es, f"r3_h{x}", [128, 1, D])))
                L[1]["sb"] = L[0]["sb"]
                L[1]["g"] = L[0]["g"]
                z32 = k.sb(es, "r3_z32", [128, 2 * D])
                zT = k.sb(es, "r3_zT", [128, 16, 128], BF16)
                on = [k.sb(es, f"r3_on{x}", [128, 512]) for x in range(2)]
                Pm = [k.sb(es, f"r3_P{x}", [128, 128], BF16) for x in range(2)]
                gst = k.sb(es, "r3_gst", [128, 4, 12])
                gmv = k.sb(es, "r3_gmv", [128, 4, 2])
                grs = k.sb(es, "r3_grs", [128, 8])
                zb = [k.sb(es, f"r3_zb{x}", [128, 1, D]) for x in range(2)]
                st = [k.sb(es, f"r3_st{x}", [128, 1, 12]) for x in range(2)]
                mv = [k.sb(es, f"r3_mv{x}", [128, 1, 2]) for x in range(2)]
                rs = [k.sb(es, f"r3_rs{x}", [128, 8]) for x in range(2)]
                z32s = [z32, k.sb(es, "r3_z32b", [128, 2 * D])]
                seq = [(b, g) for b in range(NBC) for g in range(NCH)]

                def loads(ci):
                    b, g = seq[ci]
                    B_ = L[ci % 2]
                    isctx = g < 2
                    want_out = (not isctx) or need_ctx
                    cs = slice(g * 128, (g + 1) * 128)
                    k.dma("sp", B_["kf"][:], self.KF[b, cs, :], r=[("KFall",)])
                    k.dma("sp", B_["v"][:], self.RV[b, cs, :], r=[("RVall",)])
                    if want_out:
                        for nm, dr in (("qT", self.QT), ("kT", self.KT), ("qf", self.QTF), ("qb", self.QTB)):
                            k.dma("sp", B_[nm][:], dr[b, :, cs].rearrange("(k p) t -> p k t", p=128), r=[(nm + "all",)])
                        k.dma("sp", B_["h"][:, 0, :], src[b, cs, :], r=[(_key(src), b, (g * 128 // 512) * 512 if not isctx else 0)])
                        k.dma("sp", B_["g"][:], self.RG[b, cs, :], r=[("RGall",)])
                        k.dma("sp", B_["sb"][:].rearrange("p a c -> p (a c)"), self.SBD[b, g], r=[("SBDall",)])

                def front(ci):
                    b, g = seq[ci]
                    B_ = L[ci % 2]
                    zz = z32s[ci % 2]
                    isctx = g < 2
                    want_out = (not isctx) or need_ctx
                    if g == 0:
                        k.memset("dve", Sf[:], 0.0)
                        k.memset("pool", Sfb[:], 0.0)
                    for h in range(4):
                        vh = B_["v"][:, h * 512:(h + 1) * 512]
                        if want_out:
                            sc = PS[0]
                            for a in range(2):
                                k.mm(sc[:, 0:128], B_["kT"][:, 2 * h + a, :], B_["qT"][:, 2 * h + a, :], a == 0, a == 1)
                        for a in range(2):
                            k.mm(PS[3 + a][:], B_["kf"][:, h * 256 + a * 128:h * 256 + (a + 1) * 128], vh, True, True)
                        if want_out:
                            P_ = Pm[h % 2]
                            k.tt("dve", P_[:], sc[:, 0:128], MT[:, h, :], ALU.mult)
                            O = PS[1 + h % 2]
                            k.mm(O[:], P_[:], vh, True, False)
                            for a in range(2):
                                k.mm(O[:], B_["qf"][:, 2 * h + a, :], Sfb[:, 2 * h + a, :], False, False)
                            for a in range(2):
                                k.mm(O[:], B_["qb"][:, 2 * h + a, :], B_["sb"][:, 2 * h + a, :], False, a == 1)
                        for a in range(2):
                            ps = PS[3 + a]
                            k.stt(Sf[:, 2 * h + a, :], Sf[:, 2 * h + a, :], GL[:, h:h + 1], ps[:], ALU.mult, ALU.add)
                            k.copy("act", Sfb[:, 2 * h + a, :], Sf[:, 2 * h + a, :])
                        if want_out:
                            k.fn("dve", (lambda e, h=h, O=O: e.bn_stats(out=gst[:, h, 0:6], in_=O[:])), [O], [gst])
                            k.fn("dve", (lambda e, h=h: e.bn_aggr(out=gmv[:, h, :], in_=gst[:, h, 0:6])), [gst], [gmv])
                            k.ts("pool", grs[:, h:h + 1], gmv[:, h, 1:2], LN_EPS, None, ALU.add)
                            k.tt("pool", grs[:, h:h + 1], grs[:, h:h + 1], self.negh[:, 0:1], ALU.pow)
                            o_ = on[h % 2]
                            k.ts("dve", o_[:], O[:], gmv[:, h, 0:1], grs[:, h:h + 1], ALU.subtract, ALU.mult)
                            k.tt("pool", o_[:], o_[:], gng[:, h * 512:(h + 1) * 512], ALU.mult)
                            k.tt("pool", zz[:, h * 512:(h + 1) * 512], o_[:], B_["g"][:, h * 512:(h + 1) * 512], ALU.mult,
                                 w=[(_key(zz), h)])

                def back(ci):
                    b, g = seq[ci]
                    B_ = L[ci % 2]
                    zz = z32s[ci % 2]
                    isctx = g < 2
                    want_out = (not isctx) or need_ctx
                    if not want_out:
                        return
                    r = 2 if isctx else b
                    cs = slice(g * 128, (g + 1) * 128)
                    for c in range(16):
                        pp = PS[5]
                        k.tr(pp[:, (c % 4) * 128:(c % 4 + 1) * 128], zz[:, c * 128:(c + 1) * 128], self.ident[:],
                             r=[(_key(zz), c // 4), self.ident])
                        if c % 4 == 3:
                            k.copy("act", zT[:, c - 3:c + 1, :], pp[:].rearrange("p (c i) -> p c i", i=128))
                    for half in range(2):
                        y = PS[6 + half]
                        for c in range(16):
                            k.mm(y[:], zT[:, c, :], wout[:, c, half * 512:(half + 1) * 512], c == 0, c == 15)
                    t0 = 0 if isctx else (g * 128 // 512) * 512
                    self.ln_epilogue(lambda t, half: PS[6 + half][:], B_["h"], 1, r, 0, 0, zb, st, mv, rs,
                                     dst[b, cs, :], (_key(dst), b, t0, g))
                loads(0)
                front(0)
                for ci in range(len(seq)):
                    if ci + 1 < len(seq):
                        loads(ci + 1)
                        front(ci + 1)
                    back(ci)
                k.S.barrier()

    def mla_layer(self, i, src, dst):
        k = self.k
        I = self.I
        PS = self.PS
        need_ctx = i < DEPTH - 1
        NKT = TT // 128
        with contextlib.ExitStack() as es:
            wd = k.sb(es, "m1_wd", [128, 8, 768], BF16)
            wuq = k.sb(es, "m1_wuq", [128, 3, 2048], BF16)
            wukv = k.sb(es, "m1_wukv", [128, 2, 2048], BF16)
            k.dma("pool", wd[:], I["mla_wd"].rearrange("(k p) n -> p k n", p=128))
            k.dma("pool", wuq[:], I["mla_wuq"].rearrange("(k p) n -> p k n", p=128))
            k.dma("pool", wukv[:], I["mla_wukv"].rearrange("(k p) n -> p k n", p=128))
            C1 = k.sb(es, "m1_c1", [128, SEQ])
            C2 = k.sb(es, "m1_c2", [128, SEQ])
            k.dma("sp", C1[:], I["mla_c1"])
            k.dma("sp", C2[:], I["mla_c2"])
            nraw = k.sb(es, "m1_nraw", [5, 128])
            npp = k.sb(es, "m1_npp", [128, 5])
            k.dma("sp", nraw[:], I["mla_norms"])
            k.tr(PS[7][:, 0:5], nraw[:, :], self.ident[0:5, 0:5])
            k.copy("dve", npp[:], PS[7][:, 0:5])
            stg = k.sb(es, "m1_stg", [128, 4, D])
            u = k.sb(es, "m1_uT", [128, 8, 512], BF16)
            d32 = k.sb(es, "m1_d32", [128, 5, 512])
            sq = [k.sb(es, f"m1_sq{x}", [128, 512]) for x in range(2)]
            rr = k.sb(es, "m1_rr", [128, 2, 512])
            dn = k.sb(es, "m1_dn", [128, 5, 512], BF16)
            qnb = k.sb(es, "m1_qnb", [128, 8, 512], BF16)
            qrb = k.sb(es, "m1_qrb", [128, 4, 512], BF16)
            knb = k.sb(es, "m1_knb", [128, 8, 512], BF16)
            vb = k.sb(es, "m1_vb", [128, 4, D], BF16)
            krb = k.sb(es, "m1_krb", [64, 512], BF16)
            tm = [k.sb(es, f"m1_tm{x}", [128, 512]) for x in range(2)]
            for b in range(NBC):
                for (bb, t0, n, r) in tiles_of(b, True):
                    isctx = (r == 2)
                    c0 = t0 - CTX
                    nt = n // 128
                    self.prologue(src, b, t0, n, r, 0, stg, u, [PS[6], PS[7]])
                    for c in range(5):
                        ps = PS[c]
                        for kk in range(8):
                            k.mm(ps[:, 0:n], wd[:, kk, c * 128:(c + 1) * 128], u[:, kk, 0:n], kk == 0, kk == 7)
                        k.copy("act", d32[:, c, 0:n], ps[:, 0:n])
                    for (x, ps) in ((0, PS[5]), (1, PS[6])):
                        for kk in range(8):
                            k.mm(ps[0:64, 0:n], wd[:, kk, 640 + 64 * x:704 + 64 * x], u[:, kk, 0:n], kk == 0, kk == 7)
                    if isctx:
                        k.copy("act", krb[:, 0:n], PS[5][0:64, 0:n])
                    else:
                        k.tt("dve", tm[0][0:64, 0:n], PS[5][0:64, 0:n], C1[0:64, c0:c0 + n], ALU.mult)
                        k.tt("dve", tm[1][0:64, 0:n], PS[6][0:64, 0:n], C2[0:64, c0:c0 + n], ALU.mult)
                        k.tt("pool", krb[:, 0:n], tm[0][0:64, 0:n], tm[1][0:64, 0:n], ALU.add)
                    k.dma("pool", self.KR[b, :, t0:t0 + n], krb[:, 0:n], store=True, w=[("KR", b, t0)])
                    for (gi_, cl, dim) in ((0, (0, 1, 2), 384.0), (1, (3, 4), 256.0)):
                        ps = PS[7]
                        for ci, c in enumerate(cl):
                            sqq = sq[ci % 2]
                            k.tt("dve", sqq[:, 0:n], d32[:, c, 0:n], d32[:, c, 0:n], ALU.mult)
                            k.mm(ps[:, 0:n], self.onesf[:], sqq[:, 0:n], ci == 0, ci == len(cl) - 1)
                        k.ts("dve", rr[:, gi_, 0:n], ps[:, 0:n], 1.0 / dim, RMS_EPS, ALU.mult, ALU.add)
                        k.act(rr[:, gi_, 0:n], rr[:, gi_, 0:n], AF.Ln)
                        k.act(rr[:, gi_, 0:n], rr[:, gi_, 0:n], AF.Exp, scale=-0.5)
                        for c in cl:
                            k.stt(dn[:, c, 0:n], d32[:, c, 0:n], npp[:, c:c + 1], rr[:, gi_, 0:n], ALU.mult, ALU.mult)
                    for h in range(8):
                        ps = PS[h % 4]
                        for c in range(3):
                            k.mm(ps[:, 0:n], wuq[:, c, h * 128:(h + 1) * 128], dn[:, c, 0:n], c == 0, c == 2)
                        k.copy("act", qnb[:, h, 0:n], ps[:, 0:n])
                    k.dma("act", self.QN[b, :, :, t0:t0 + n].rearrange("h p t -> p h t"), qnb[:, :, 0:n], store=True, w=[("QN", b, t0)])
                    for hp in range(4):
                        pa, pb = PS[4 + (2 * hp) % 2], PS[4 + (2 * hp + 1) % 2]
                        for c in range(3):
                            k.mm(pa[:, 0:n], wuq[:, c, 1024 + hp * 128:1024 + (hp + 1) * 128], dn[:, c, 0:n], c == 0, c == 2)
                        if isctx:
                            k.copy("act", qrb[:, hp, 0:n], pa[:, 0:n])
                        else:
                            for c in range(3):
                                k.mm(pb[:, 0:n], wuq[:, c, 1536 + hp * 128:1536 + (hp + 1) * 128], dn[:, c, 0:n], c == 0, c == 2)
                            k.tt("dve", tm[0][:, 0:n], pa[:, 0:n], C1[:, c0:c0 + n], ALU.mult)
                            k.tt("dve", tm[1][:, 0:n], pb[:, 0:n], C2[:, c0:c0 + n], ALU.mult)
                            k.tt("pool", qrb[:, hp, 0:n], tm[0][:, 0:n], tm[1][:, 0:n], ALU.add)
                    k.dma("pool", self.QR[b, :, :, t0:t0 + n].rearrange("h p t -> p h t"), qrb[:, :, 0:n], store=True, w=[("QR", b, t0)])
                    for h in range(8):
                        ps = PS[h % 4]
                        for c in range(2):
                            k.mm(ps[:, 0:n], wukv[:, c, h * 128:(h + 1) * 128], dn[:, 3 + c, 0:n], c == 0, c == 1)
                        k.copy("act", knb[:, h, 0:n], ps[:, 0:n])
                    k.dma("act", self.KN[b, :, :, t0:t0 + n].rearrange("h p t -> p h t"), knb[:, :, 0:n], store=True, w=[("KN", b, t0)])
                    for s_ in range(nt):
                        for half in range(2):
                            ps = PS[4 + half]
                            for c in range(2):
                                k.mm(ps[:], dn[:, 3 + c, s_ * 128:(s_ + 1) * 128], wukv[:, c, 1024 + half * 512:1024 + (half + 1) * 512], c == 0, c == 1)
                            k.copy("act", vb[:, s_, half * 512:(half + 1) * 512], ps[:])
                    k.dma("act", self.MV[b, t0:t0 + n, :].rearrange("(s p) d -> p s d", p=128), vb[:, 0:nt, :], store=True, w=[("MV", b, t0)])
            k.S.barrier()
        with contextlib.ExitStack() as es:
            kra = k.sb(es, "m2_kr", [64, TT], BF16)
            knh = [k.sb(es, f"m2_kn{x}", [128, TT], BF16) for x in range(2)]
            vh = [k.sb(es, f"m2_v{x}", [128, NKT, 128], BF16) for x in range(2)]
            qn = [k.sb(es, f"m2_qn{x}", [128, 512], BF16) for x in range(2)]
            qr = [k.sb(es, f"m2_qr{x}", [64, 512], BF16) for x in range(2)]
            PT = [k.sb(es, f"m2_PT{x}", [128, 512], BF16) for x in range(3)]
            rec = [k.sb(es, f"m2_rec{x}", [128, 512]) for x in range(2)]
            otb = [k.sb(es, f"m2_ot{x}", [128, 512], BF16) for x in range(2)]
            dac = [k.sb(es, f"m2_dac{x}", [128, 512]) for x in range(2)]
            scale = float(192.0 ** -0.5)
            it = 0
            ih = 0
            for b in range(NBC):
                k.dma("sp", kra[:], self.KR[b], r=[("KRall",)])
                for h in range(8):
                    kn_, v_ = knh[ih % 2], vh[ih % 2]
                    ih += 1
                    k.dma("sp", kn_[:], self.KN[b, h], r=[("KNall",)])
                    k.dma("sp", v_[:], self.MV[b, :, h * 128:(h + 1) * 128].rearrange("(t p) d -> p t d", p=128), r=[("MVall",)])
                    for (bb, t0, n, r) in tiles_of(b, need_ctx):
                        isctx = (r == 2)
                        kts = [0, 1] if isctx else list(range(NKT))
                        q_, r_ = qn[it % 2], qr[it % 2]
                        O, DEN = PS[4 + it % 2], PS[6 + it % 2]
                        rc, ot = rec[it % 2], otb[it % 2]
                        dacc = dac[it % 2]
                        it += 1
                        k.dma("sp", q_[:, 0:n], self.QN[b, h, :, t0:t0 + n], r=[("QNall",)])
                        k.dma("sp", r_[:, 0:n], self.QR[b, h // 2, (h % 2) * 64:(h % 2) * 64 + 64, t0:t0 + n], r=[("QRall",)])

                        def scores(idx):
                            kt = kts[idx]
                            sc = PS[idx % 4]
                            k.mm(sc[:, 0:n], kn_[:, kt * 128:(kt + 1) * 128], q_[:, 0:n], True, False)
                            k.mm(sc[:, 0:n], kra[:, kt * 128:(kt + 1) * 128], r_[:, 0:n], False, True)
                        scores(0)
                        for idx, kt in enumerate(kts):
                            if idx + 1 < len(kts):
                                scores(idx + 1)
                            p_ = PT[idx % 3]
                            k.act(p_[:, 0:n], PS[idx % 4][:, 0:n], AF.Exp, scale=scale)
                            k.mm(O[:, 0:n], v_[:, kt, :], p_[:, 0:n], idx == 0, idx == len(kts) - 1)
                            k.mm(DEN[:, 0:n], self.onesb[:], p_[:, 0:n], idx == 0, idx == len(kts) - 1)
                        k.fn("dve", (lambda e, rc=rc, DEN=DEN, n=n: e.reciprocal(out=rc[:, 0:n], in_=DEN[:, 0:n])), [DEN], [rc])
                        k.tt("dve", ot[:, 0:n], O[:, 0:n], rc[:, 0:n], ALU.mult)
                        k.dma("pool", self.OT[b, h * 128:(h + 1) * 128, t0:t0 + n], ot[:, 0:n], store=True, w=[("OT", b, h, t0)])
            k.S.barrier()
        with contextlib.ExitStack() as es:
            wout = k.sb(es, "m3_wout", [128, 8, D], BF16)
            k.dma("pool", wout[:], I["mla_w_out"].rearrange("(k p) n -> p k n", p=128))
            stg = [k.sb(es, f"m3_stg{x}", [128, 4, D]) for x in range(2)]
            oT = [k.sb(es, f"m3_oT{x}", [128, 8, 512], BF16) for x in range(2)]
            zb = [k.sb(es, f"m3_zb{x}", [128, 4, D]) for x in range(2)]
            st = [k.sb(es, f"m3_st{x}", [128, 4, 12]) for x in range(2)]
            mv = [k.sb(es, f"m3_mv{x}", [128, 4, 2]) for x in range(2)]
            rs = [k.sb(es, f"m3_rs{x}", [128, 8]) for x in range(2)]
            it = 0
            for b in range(NBC):
                for (bb, t0, n, r) in tiles_of(b, need_ctx):
                    nt = n // 128
                    sg, o_ = stg[it % 2], oT[it % 2]
                    it += 1
                    k.dma("sp", sg[:, 0:nt, :], src[b, t0:t0 + n, :].rearrange("(t p) d -> p t d", p=128), r=[(_key(src), b, t0)])
                    k.dma("sp", o_[:, :, 0:n], self.OT[b, :, t0:t0 + n].rearrange("(k p) t -> p k t", p=128), r=[("OTall",)])
                    for t in range(nt):
                        for half in range(2):
                            ps = PS[(t * 2 + half) % 8]
                            for kk in range(8):
                                k.mm(ps[:], o_[:, kk, t * 128:(t + 1) * 128], wout[:, kk, half * 512:(half + 1) * 512], kk == 0, kk == 7)
                    self.ln_epilogue(lambda t, half: PS[(t * 2 + half) % 8][:], sg, nt, r, 0, 0, zb, st, mv, rs,
                                     dst[b, t0:t0 + n, :], (_key(dst), b, t0))
            k.S.barrier()

    def moe_layer_dense(self, i, src, dst, last, dbg_ns=None, dbg_ne=NE):
        k = self.k
        I = self.I
        PS = self.PS
        need_ctx = not last
        i = self.li[i]
        alltiles = []
        for b in range(NBC):
            alltiles += tiles_of(b, need_ctx)
        NS = 2
        with contextlib.ExitStack() as es:
            wr = k.sb(es, "mo_wr", [128, 8, 64])
            rbg = k.sb(es, "mo_rbg", [128, 4])
            rbe = k.sb(es, "mo_rbe", [128, 32])
            k.dma("sp", wr[:], I["moe_wr"][i].rearrange("(k p) n -> p k n", p=128))
            k.dma("sp", rbg[:], I["moe_bg"][i].partition_broadcast(128))
            k.dma("sp", rbe[:], I["moe_be"][i].partition_broadcast(128))
            vT = k.sb(es, "mo_vT", [128, 8, NS * 512], BF16)
            acc = [[k.sb(es, f"mo_acc{s}_{h}", [128, 512]) for h in range(2)] for s in range(NS * 4)]
            G = k.sb(es, "mo_G", [128, NS * 4, 32])
            stg = k.sb(es, "mo_stg", [128, 4, D])
            w1b = [k.sb(es, f"mo_w1_{j}", [128, 8, 512], BF16) for j in range(2)]
            w3b = [k.sb(es, f"mo_w3_{j}", [128, 8, 512], BF16) for j in range(2)]
            w2b = [k.sb(es, f"mo_w2_{j}", [128, 4, D], BF16) for j in range(2)]
            sil = [k.sb(es, f"mo_sil{j}", [128, 512]) for j in range(2)]
            hdn = [k.sb(es, f"mo_hdn{j}", [128, 4, 512], BF16) for j in range(2)]
            rt = k.sb(es, "mo_rt", [128, 64])
            r8 = k.sb(es, "mo_r8", [128, 8])
            rsm = k.sb(es, "mo_rsm", [128, 16])
            zb = k.sb(es, "mo_zb", [128, 4, D])
            v32 = zb[:].rearrange("p a (b c) -> p (a b) c", c=512)
            st = k.sb(es, "mo_st", [128, 4, 12])
            mv = k.sb(es, "mo_mv", [128, 4, 2])
            rs = k.sb(es, "mo_rs", [128, 8])
            wi = 0
            for s0 in range(0, len(alltiles) if dbg_ns is None else dbg_ns * NS, NS):
                tl = alltiles[s0:s0 + NS]
                subs = []
                for ti, (b, t0, n, r) in enumerate(tl):
                    self.prologue(src, b, t0, n, r, 2, stg, vT[:, :, ti * 512:(ti + 1) * 512], [PS[6], PS[7]], v32=v32)
                    for t in range(n // 128):
                        si = ti * 4 + t
                        subs.append((ti, t, ti * 512 + t * 128, si))
                        lg = PS[5]
                        for kk in range(8):
                            k.mm(lg[:, 0:64], v32[:, kk, t * 128:(t + 1) * 128], wr[:, kk, :], kk == 0, kk == 7)
                        self.route(lg, rbg, rbe, rt, r8, rsm, G[:, si, :])
                for e in range(dbg_ne):
                    wb1, wb3, wb2 = w1b[wi % 2], w3b[wi % 2], w2b[wi % 2]
                    wi += 1
                    k.dma("pool", wb1[:], I["moe_w1"][i, e].rearrange("(k p) n -> p k n", p=128))
                    k.dma("pool", wb3[:], I["moe_w3"][i, e].rearrange("(k p) n -> p k n", p=128))
                    k.dma("pool", wb2[:], I["moe_w2"][i, e].rearrange("(k p) n -> p k n", p=128))
                    for ti, (b, t0, n, r) in enumerate(tl):
                        hb = hdn[ti % 2]
                        cs = ti * 512
                        for j in range(4):
                            pa, pb = PS[(2 * j) % 4], PS[(2 * j + 1) % 4]
                            for kk in range(8):
                                k.mm(pa[:, 0:n], wb1[:, kk, j * 128:(j + 1) * 128], vT[:, kk, cs:cs + n], kk == 0, kk == 7)
                            for kk in range(8):
                                k.mm(pb[:, 0:n], wb3[:, kk, j * 128:(j + 1) * 128], vT[:, kk, cs:cs + n], kk == 0, kk == 7)
                            sl = sil[j % 2]
                            k.act(sl[:, 0:n], pa[:, 0:n], AF.Silu)
                            k.tt("dve", hb[:, j, 0:n], sl[:, 0:n], pb[:, 0:n], ALU.mult, w=[(_key(hb), j)])
                        for t in range(n // 128):
                            si = ti * 4 + t
                            for half in range(2):
                                po = PS[4 + (t * 2 + half) % 2]
                                for j in range(4):
                                    k.mm(po[:], hb[:, j, t * 128:(t + 1) * 128], wb2[:, j, half * 512:(half + 1) * 512], j == 0, j == 3,
                                         r=[(_key(hb), j), wb2])
                                a = acc[si][half][:]
                                if e == 0:
                                    k.ts("dve", a, po[:], G[:, si, e:e + 1], None, ALU.mult)
                                else:
                                    k.stt(a, po[:], G[:, si, e:e + 1], a, ALU.mult, ALU.add)
                for ti, (b, t0, n, r) in enumerate(tl):
                    nt = n // 128
                    k.dma("sp", stg[:, 0:nt, :], src[b, t0:t0 + n, :].rearrange("(t p) d -> p t d", p=128), r=[(_key(src), b, t0)])
                    if last:
                        drows = self.out[b, t0 - CTX:t0 - CTX + n, :]
                        dkey = ("out", b, t0)
                    else:
                        drows = dst[b, t0:t0 + n, :]
                        dkey = (_key(dst), b, t0)
                    self.ln_epilogue(lambda t, half, ti=ti: acc[ti * 4 + t][half][:], stg, nt, r, 1, 1,
                                     zb, st, mv, rs, drows, dkey)
            k.S.barrier()

    def moe_layer(self, i, src, dst, last, dbg_ns=None, dbg_ne=NE):
        k = self.k
        I = self.I
        PS = self.PS
        need_ctx = not last
        li = self.li[i]
        alltiles = []
        for b in range(NBC):
            alltiles += tiles_of(b, need_ctx)
        nsub_all = sum(n // 128 for (_, _, n, _) in alltiles)
        NBLK = (2 * nsub_all * 128 + 511) // 512 + NE
        NSUB = nsub_all
        XS, YS = self.XS, self.YS
        I32 = mybir.dt.int32
        with contextlib.ExitStack() as mes:
            GT = k.sb(mes, "ms_GT", [128, NSUB, 2])
            D0 = k.sb(mes, "ms_D0", [128, NSUB], I32)
            D1 = k.sb(mes, "ms_D1", [128, NSUB], I32)
            WI = k.sb(mes, "ms_WI", [128, NBLK], I32)
            ves = contextlib.ExitStack()
            VB = k.sb(ves, "ms_VB", [128, 3, 2, D])
            with contextlib.ExitStack() as es:
                wblk = [k.sb(es, f"mv_w{x}", [128, 8, 1024]) for x in range(2)]
                brow = k.sb(es, "mv_brow", [1, 6 * D])
                REP = k.sb(es, "mv_REP", [128, 3, 8, 128])
                for r in range(3):
                    for kk in range(8):
                        k.copy("dve", REP[:, r, kk, :], self.actT[:, kk, r:r + 1].broadcast_to([128, 128]))
                k.dma("sp", brow[:], I["ada_b"][li])
                pi = 0
                for wi in (3, 4):
                    buf = wblk[wi % 2]
                    k.dma("sp", buf[:], I["ada_w"][li][:, wi * 1024:(wi + 1) * 1024].rearrange("(k p) n -> p k n", p=128))
                    for r in range(3):
                        for half in range(2):
                            ps = PS[pi % 8]
                            pi += 1
                            for ki in range(8):
                                k.mm(ps[:], REP[:, r, ki, :], buf[:, ki, half * 512:(half + 1) * 512], ki == 0, False)
                            k.mm(ps[:], self.ones[0:1, 0:128], brow[0:1, wi * 1024 + half * 512: wi * 1024 + (half + 1) * 512], False, True)
                            if wi == 3:
                                k.copy("act", VB[:, r, 0, half * 512:(half + 1) * 512], ps[:])
                            else:
                                k.act(VB[:, r, 1, half * 512:(half + 1) * 512], ps[:], AF.Identity, bias=1.0)
                k.S.barrier()
            with contextlib.ExitStack() as es:
                wr = k.sb(es, "ma_wr", [128, 8, 64])
                rbg = k.sb(es, "ma_rbg", [128, 4])
                rbe = k.sb(es, "ma_rbe", [128, 32])
                k.dma("sp", wr[:], I["moe_wr"][li].rearrange("(k p) n -> p k n", p=128))
                k.dma("sp", rbg[:], I["moe_bg"][li].partition_broadcast(128))
                k.dma("sp", rbe[:], I["moe_be"][li].partition_broadcast(128))
                U = k.sb(es, "ma_U", [128, 128], BF16)
                k.memset("pool", U[:], 1.0)
                k.fn("pool", lambda e: e.affine_select(out=U[:], in_=U[:], pattern=[[1, 128]], compare_op=ALU.is_gt,
                                                       fill=0.0, base=0, channel_multiplier=-1), [U], [U])
                OH1 = k.sb(es, "ms_OH1", [128, NSUB, 32])
                OH2 = k.sb(es, "ms_OH2", [128, NSUB, 32])
                RK = k.sb(es, "ms_RK", [128, NSUB, 32])
                cnt = k.sb(es, "ma_cnt", [128, 32])
                k.memset("dve", cnt[:], 0.0)
                stg = [k.sb(es, f"ma_stg{x}", [128, 4, D]) for x in range(2)]
                vtk = k.sb(es, "ma_vtk", [128, 4, D])
                vtb = [k.sb(es, f"ma_vtb{x}", [128, 4, D], BF16) for x in range(2)]
                v32 = k.sb(es, "ma_v32", [128, 8, 512])
                ind = [k.sb(es, f"ma_ind{x}", [128, 32], BF16) for x in range(2)]
                rt = k.sb(es, "ma_rt", [128, 64])
                r8 = k.sb(es, "ma_r8", [128, 8])
                rsm = k.sb(es, "ma_rsm", [128, 16])
                si = 0
                for ti, (b, t0, n, r) in enumerate(alltiles):
                    nt = n // 128
                    sg = stg[ti % 2]
                    k.dma("sp", sg[:, 0:nt, :], src[b, t0:t0 + n, :].rearrange("(t p) d -> p t d", p=128), r=[(_key(src), b, t0)])
                    for t in range(nt):
                        k.tt("dve", vtk[:, t, :], sg[:, t, :], VB[:, r, 1, :], ALU.mult, w=[(_key(vtk), t)])
                        k.tt("pool", vtk[:, t, :], vtk[:, t, :], VB[:, r, 0, :], ALU.add, r=[(_key(vtk), t), VB], w=[(_key(vtk), t)])
                    vb_ = vtb[ti % 2]
                    for t in range(nt):
                        k.copy("act", vb_[:, t, :], vtk[:, t, :], r=[(_key(vtk), t)], w=[(_key(vb_), t)])
                    k.dma("act", self.VTB[b, t0:t0 + n, :].rearrange("(t p) d -> p t d", p=128), vb_[:, 0:nt, :], store=True,
                          r=[(_key(vb_), t) for t in range(nt)], w=[("VT", b, t0)])
                    for kk in range(8):
                        ps = PS[6 + kk % 2]
                        for t in range(nt):
                            k.tr(ps[:, t * 128:(t + 1) * 128], vtk[:, t, kk * 128:(kk + 1) * 128], self.ident[:],
                                 r=[(_key(vtk), t), self.ident])
                        k.copy("act", v32[:, kk, 0:n], ps[:, 0:n], w=[(_key(v32), kk)])
                    for t in range(nt):
                        lg = PS[5]
                        for kk in range(8):
                            k.mm(lg[:, 0:64], v32[:, kk, t * 128:(t + 1) * 128], wr[:, kk, :], kk == 0, kk == 7,
                                 r=[(_key(v32), kk), wr])
                        self.route(lg, rbg, rbe, rt, r8, rsm, None, oh1=OH1[:, si, :], oh2=OH2[:, si, :], gts=GT[:, si, :])
                        id_ = ind[si % 2]
                        k.tt("dve", id_[:], OH1[:, si, :], OH2[:, si, :], ALU.add)
                        pr = PS[4]
                        k.mm(pr[:, 0:32], U[:], id_[:], True, True)
                        k.mm(pr[:, 32:64], self.onesb[:], id_[:], True, True)
                        k.tt("dve", RK[:, si, :], pr[:, 0:32], cnt[:], ALU.add)
                        k.tt("dve", cnt[:], pr[:, 32:64], cnt[:], ALU.add)
                        si += 1
                sm = k.sb(es, "ma_sm", [128, 6, 32])
                k.ts("dve", sm[:, 0, :], cnt[:], 511.0, 1.0 / 512.0, ALU.add, ALU.mult)
                k.ts("dve", sm[:, 0, :], sm[:, 0, :], -0.499, 8388608.0, ALU.add, ALU.add)
                k.ts("dve", sm[:, 0, :], sm[:, 0, :], -8388608.0, 512.0, ALU.add, ALU.mult)
                k.memset("dve", sm[:, 1, :], 1.0)
                k.fn("dve", lambda e: e.tensor_tensor_scan(out=sm[:, 2, :], data0=sm[:, 1, :], data1=sm[:, 0, :], initial=0.0,
                                                          op0=ALU.mult, op1=ALU.add), [sm], [sm])
                k.tt("dve", sm[:, 3, :], sm[:, 2, :], sm[:, 0, :], ALU.subtract)
                jv = k.sb(es, "ma_jv", [128, NBLK, 32])
                k.fn("pool", lambda e: e.iota(jv[:], pattern=[[512, NBLK], [0, 32]], base=0, channel_multiplier=0,
                                              allow_small_or_imprecise_dtypes=True), [], [jv])
                k.tt("dve", jv[:], jv[:], sm[:, 2:3, :].broadcast_to([128, NBLK, 32]), ALU.is_ge)
                eb = k.sb(es, "ma_eb", [128, NBLK])
                k.fn("dve", lambda e: e.tensor_reduce(out=eb[:], in_=jv[:], axis=mybir.AxisListType.X, op=ALU.add), [jv], [eb])
                pid = k.sb(es, "ma_pid", [128, 1])
                k.fn("pool", lambda e: e.iota(pid[:], pattern=[[0, 1]], base=0, channel_multiplier=1,
                                              allow_small_or_imprecise_dtypes=True), [], [pid])
                k.ts("dve", eb[:], eb[:], 31.0, 128.0, ALU.min, ALU.mult)
                k.ts("dve", eb[:], eb[:], pid[:, 0:1], float(li * NE * 128), ALU.add, ALU.add)
                k.copy("dve", WI[:], eb[:])
                k.tt("dve", RK[:], RK[:], sm[:, 3:4, :].broadcast_to([128, NSUB, 32]), ALU.add)
                dd = k.sb(es, "ma_dd", [128, NSUB])
                for (OH, Dd) in ((OH1, D0), (OH2, D1)):
                    k.tt("dve", OH[:], OH[:], RK[:], ALU.mult)
                    k.fn("dve", (lambda e, OH=OH: e.tensor_reduce(out=dd[:], in_=OH[:], axis=mybir.AxisListType.X, op=ALU.add)), [OH], [dd])
                    k.copy("dve", Dd[:], dd[:])
                k.S.barrier()
            with contextlib.ExitStack() as es:
                vtk = [k.sb(es, f"mb_vtk{x}", [128, 4, D], BF16) for x in range(3)]
                si = 0
                for ti, (b, t0, n, r) in enumerate(alltiles):
                    nt = n // 128
                    vt = vtk[ti % 3]
                    k.dma("sp", vt[:, 0:nt, :], self.VTB[b, t0:t0 + n, :].rearrange("(t p) d -> p t d", p=128), r=[("VTall",)])
                    for t in range(nt):
                        for Dd in (D0, D1):
                            k.S.op("pool", (lambda e, vt=vt, t=t, Dd=Dd, si=si: e.indirect_dma_start(
                                out=XS, out_offset=bass.IndirectOffsetOnAxis(ap=Dd[:, si:si + 1], axis=0),
                                in_=vt[:, t, :], in_offset=None)), [vt, Dd], [("XS", si, id(Dd))], dma=_key(vt))
                        si += 1
                k.S.barrier()
            ves.close()
            with contextlib.ExitStack() as es:
                wst = [k.sb(es, f"mc_wst{x}", [128, 4096]) for x in range(3)]
                wb = [[k.sb(es, f"mc_wb{x}_{y}", [128, 4096], BF16) for x in range(3)] for y in range(2)]
                stg = k.sb(es, "mc_stg", [128, 4, D], BF16)
                xT = [k.sb(es, f"mc_xT{x}", [128, 8, 512], BF16) for x in range(2)]
                sil = [k.sb(es, f"mc_sil{x}", [128, 512]) for x in range(2)]
                hdn = [k.sb(es, f"mc_hdn{x}", [128, 4, 512], BF16) for x in range(2)]
                ysb = k.sb(es, "mc_ysb", [128, 4, D])
                wsrc = [I["moe_w1"].rearrange("l e p k n -> (l e p) (k n)"), I["moe_w3"].rearrange("l e p k n -> (l e p) (k n)"),
                        I["moe_w2"].rearrange("l e p k n -> (l e p) (k n)")]
                NB_ = NBLK if dbg_ns is None else dbg_ns

                def gather_w(j):
                    for x in range(3):
                        k.S.op("pool", (lambda e, x=x, j=j: e.indirect_dma_start(
                            out=wst[x][:], out_offset=None, in_=wsrc[x],
                            in_offset=bass.IndirectOffsetOnAxis(ap=WI[:, j:j + 1], axis=0))), [WI], [wst[x]], dma=_key(wst[x]))

                def cast_w(j):
                    wbj = wb[j % 2]
                    k.copy("act", wbj[0][:], wst[0][:])
                    k.copy("pool", wbj[1][:], wst[1][:])
                    k.copy("dve", wbj[2][:], wst[2][:])

                def load_x(j):
                    k.dma("sp", stg[:], XS[j * 512:(j + 1) * 512, :].rearrange("(t p) d -> p t d", p=128), r=[("XSall",)])
                gather_w(0)
                cast_w(0)
                load_x(0)
                for j in range(NB_):
                    wbj = wb[j % 2]
                    w1v = wbj[0][:].rearrange("p (k n) -> p k n", n=512)
                    w3v = wbj[1][:].rearrange("p (k n) -> p k n", n=512)
                    w2v = wbj[2][:].rearrange("p (k n) -> p k n", n=1024)
                    x_ = xT[j % 2]
                    hb = hdn[j % 2]
                    for kk in range(8):
                        psb = PS[6 + kk % 2][:].bitcast(BF16)
                        for t in range(4):
                            k.tr(psb[:, t * 128:(t + 1) * 128], stg[:, t, kk * 128:(kk + 1) * 128], self.identb[:])
                        k.copy("act" if kk % 2 == 0 else "dve", x_[:, kk, :], psb[:, 0:512])
                    if j + 1 < NB_:
                        load_x(j + 1)
                        gather_w(j + 1)
                    for jj in range(4):
                        pa, pb = PS[(2 * jj) % 4], PS[(2 * jj + 1) % 4]
                        for kk in range(8):
                            k.mm(pa[:], w1v[:, kk, jj * 128:(jj + 1) * 128], x_[:, kk, :], kk == 0, kk == 7)
                        for kk in range(8):
                            k.mm(pb[:], w3v[:, kk, jj * 128:(jj + 1) * 128], x_[:, kk, :], kk == 0, kk == 7)
                        sl = sil[jj % 2]
                        k.act(sl[:], pa[:], AF.Silu)
                        k.tt("dve", hb[:, jj, :], sl[:], pb[:], ALU.mult, w=[(_key(hb), jj)])
                    if j + 1 < NB_:
                        cast_w(j + 1)
                    for t in range(4):
                        for half in range(2):
                            po = PS[4 + half]
                            for jj in range(4):
                                k.mm(po[:], hb[:, jj, t * 128:(t + 1) * 128], w2v[:, jj, half * 512:(half + 1) * 512], jj == 0, jj == 3,
                                     r=[(_key(hb), jj), wbj[2]])
                            k.copy("act" if half == 0 else "dve", ysb[:, t, half * 512:(half + 1) * 512], po[:])
                    k.dma("act", YS[j * 512:(j + 1) * 512, :].rearrange("(t p) d -> p t d", p=128), ysb[:], store=True, w=[("YS", j)])
                k.S.barrier()
            with contextlib.ExitStack() as es:
                stg = [k.sb(es, f"md_stg{x}", [128, 4, D]) for x in range(2)]
                y0 = [k.sb(es, f"md_y0{x}", [128, D]) for x in range(2)]
                y1 = [k.sb(es, f"md_y1{x}", [128, D]) for x in range(2)]
                fb = [k.sb(es, f"md_f{x}", [128, 4, D]) for x in range(2)]
                zb = [k.sb(es, f"md_zb{x}", [128, 4, D]) for x in range(2)]
                st = [k.sb(es, f"md_st{x}", [128, 4, 12]) for x in range(2)]
                mv = [k.sb(es, f"md_mv{x}", [128, 4, 2]) for x in range(2)]
                rs = [k.sb(es, f"md_rs{x}", [128, 8]) for x in range(2)]
                si = 0
                for ti, (b, t0, n, r) in enumerate(alltiles):
                    nt = n // 128
                    sg, f_ = stg[ti % 2], fb[ti % 2]
                    k.dma("sp", sg[:, 0:nt, :], src[b, t0:t0 + n, :].rearrange("(t p) d -> p t d", p=128), r=[(_key(src), b, t0)])
                    for t in range(nt):
                        a0, a1 = y0[si % 2], y1[si % 2]
                        for (yy, Dd) in ((a0, D0), (a1, D1)):
                            k.S.op("pool", (lambda e, yy=yy, Dd=Dd, si=si: e.indirect_dma_start(
                                out=yy[:], out_offset=None, in_=YS,
                                in_offset=bass.IndirectOffsetOnAxis(ap=Dd[:, si:si + 1], axis=0))), [Dd], [yy], dma=_key(yy))
                        k.act(f_[:, t, :], a0[:], AF.Identity, scale=GT[:, si, 0:1])
                        k.stt(f_[:, t, :], a1[:], GT[:, si, 1:2], f_[:, t, :], ALU.mult, ALU.add)
                        si += 1
                    if dbg_ns is not None and ti >= 1:
                        break
                    if last:
                        drows = self.out[b, t0 - CTX:t0 - CTX + n, :]
                        dkey = ("out", b, t0)
                    else:
                        drows = dst[b, t0:t0 + n, :]
                        dkey = (_key(dst), b, t0)
                    self.ln_epilogue(lambda t, half, f_=f_: f_[:, t, half * 512:(half + 1) * 512], sg, nt, r, 1, 1,
                                     zb, st, mv, rs, drows, dkey)
                k.S.barrier()

    def route(self, lg, rbg, rbe, rt, r8, rsm, Gout, oh1=None, oh2=None, gts=None):
        k = self.k
        k.tt("dve", rt[:, 0:4], lg[:, 0:4], rbg[:], ALU.add)
        k.tt("dve", rt[:, 8:40], lg[:, 4:36], rbe[:], ALU.add)
        k.fn("dve", lambda e: e.tensor_reduce(out=rsm[:, 0:1], in_=rt[:, 0:4], axis=mybir.AxisListType.X, op=ALU.max), [rt], [rsm])
        k.ts("dve", rt[:, 4:8], rt[:, 0:4], rsm[:, 0:1], None, ALU.subtract)
        k.act(rt[:, 40:44], rt[:, 4:8], AF.Exp)
        k.fn("dve", lambda e: e.tensor_reduce(out=rsm[:, 1:2], in_=rt[:, 40:44], axis=mybir.AxisListType.X, op=ALU.add), [rt], [rsm])
        k.fn("dve", lambda e: e.reciprocal(out=rsm[:, 2:3], in_=rsm[:, 1:2]), [rsm], [rsm])
        k.ts("dve", rt[:, 44:48], rt[:, 4:8], 0.0, -1e30, ALU.is_lt, ALU.mult)
        k.tt("dve", rt[:, 8:40].rearrange("p (g e) -> p g e", e=8), rt[:, 8:40].rearrange("p (g e) -> p g e", e=8),
             rt[:, 44:48].unsqueeze(2).broadcast_to([128, 4, 8]), ALU.add)
        k.fn("dve", lambda e: e.max(out=r8[:], in_=rt[:, 8:40]), [rt], [r8])
        k.tt("dve", rsm[:, 3:4], r8[:, 1:2], r8[:, 0:1], ALU.subtract)
        k.act(rsm[:, 4:5], rsm[:, 3:4], AF.Exp)
        k.ts("dve", rsm[:, 5:6], rsm[:, 4:5], 1.0, None, ALU.add)
        k.fn("dve", lambda e: e.reciprocal(out=rsm[:, 6:7], in_=rsm[:, 5:6]), [rsm], [rsm])
        k.tt("dve", rsm[:, 7:8], rsm[:, 6:7], rsm[:, 2:3], ALU.mult)
        k.tt("dve", rsm[:, 8:9], rsm[:, 7:8], rsm[:, 4:5], ALU.mult)
        if oh1 is not None:
            k.ts("dve", oh1, rt[:, 8:40], r8[:, 0:1], None, ALU.is_equal)
            k.ts("dve", oh2, rt[:, 8:40], r8[:, 1:2], None, ALU.is_equal)
            k.copy("dve", gts, rsm[:, 7:9])
            return
        k.ts("dve", Gout, rt[:, 8:40], r8[:, 0:1], rsm[:, 7:8], ALU.is_equal, ALU.mult, r=[rt, r8, rsm], w=[Gout])
        k.ts("dve", rt[:, 8:40], rt[:, 8:40], r8[:, 1:2], rsm[:, 8:9], ALU.is_equal, ALU.mult)
        k.tt("dve", Gout, Gout, rt[:, 8:40], ALU.add, r=[Gout, rt], w=[Gout])


_CACHE = {}


def _host_inputs(inputs, core):
    b0 = core * NBC
    f = np.float32
    hin = np.concatenate([inputs["ctx"][b0:b0 + NBC], inputs["x"][b0:b0 + NBC]], axis=1)
    cc = np.concatenate([inputs["c"][b0:b0 + NBC], inputs["c_ctx"][None, :]], axis=0)
    return {"hin": np.ascontiguousarray(hin, dtype=f), "cc": np.ascontiguousarray(cc, dtype=f)}


def _shared_inputs(inputs):
    f = np.float32
    wr = np.zeros((DEPTH, D, 64), f)
    wr[:, :, 0:4] = inputs["moe_w_group"]
    wr[:, :, 4:36] = inputs["moe_w_expert"]
    conv_wb = np.concatenate([inputs["conv_w"], inputs["conv_b"][:, None, :]], axis=1)
    rows = np.repeat(np.arange(SEQ // 64, dtype=f), 64)
    cols = np.tile(np.arange(64, dtype=f), SEQ // 64)

    def rope_tabs(dim):
        q = dim // 4
        inv = (np.float32(10000.0) ** (-np.arange(q, dtype=f) / np.float32(q))).astype(f)
        ang = np.concatenate([rows[:, None] * inv[None, :], cols[:, None] * inv[None, :]], axis=-1).astype(f)
        return np.ascontiguousarray(np.cos(ang).T.astype(f)), np.ascontiguousarray(np.sin(ang).T.astype(f))
    rc, rs_ = rope_tabs(256)
    mc, ms = rope_tabs(64)
    c1 = np.ascontiguousarray(np.concatenate([mc, mc, mc, mc], axis=0))
    c2 = np.ascontiguousarray(np.concatenate([-ms, ms, -ms, ms], axis=0))
    wdn = inputs["mla_w_down"][0]
    kr = wdn[:, 640:704]
    mla_wd = np.ascontiguousarray(np.concatenate([wdn[:, 0:640], kr, kr[:, 32:64], kr[:, 0:32]], axis=1))
    wuq = inputs["mla_w_uq"][0].reshape(384, 8, 192)
    rp = wuq[:, :, 128:192]
    mla_wuq = np.ascontiguousarray(np.concatenate([
        wuq[:, :, 0:128].reshape(384, 1024), rp.reshape(384, 512),
        np.concatenate([rp[:, :, 32:64], rp[:, :, 0:32]], axis=2).reshape(384, 512)], axis=1))
    wukv = inputs["mla_w_ukv"][0].reshape(256, 8, 256)
    mla_wukv = np.ascontiguousarray(np.concatenate([wukv[:, :, 0:128].reshape(256, 1024), wukv[:, :, 128:256].reshape(256, 1024)], axis=1))
    norms = np.ascontiguousarray(np.concatenate([inputs["mla_q_norm"][0].reshape(3, 128), inputs["mla_kv_norm"][0].reshape(2, 128)], axis=0))
    return {
        "mla_wd": mla_wd, "mla_norms": norms, "mla_wuq": mla_wuq, "mla_wukv": mla_wukv,
        "mla_w_out": np.ascontiguousarray(inputs["mla_w_out"][0]), "mla_c1": c1, "mla_c2": c2,
        "ret_w_in": inputs["ret_w_in"], "ret_decay": np.ascontiguousarray(inputs["ret_decay"].reshape(2, 8)),
        "ret_gn_g": inputs["ret_gn_g"], "ret_w_out": inputs["ret_w_out"], "ret_cos": rc, "ret_sin": rs_,
        "ada_w": inputs["ada_w"], "ada_b": np.ascontiguousarray(inputs["ada_b"][:, None, :]),
        "ln_g": inputs["ln_g"], "ln_b": inputs["ln_b"],
        "conv_w_in": inputs["conv_w_in"], "conv_wb": np.ascontiguousarray(conv_wb), "conv_w_out": inputs["conv_w_out"],
        "moe_wr": wr, "moe_bg": inputs["moe_b_group"], "moe_be": inputs["moe_b_expert"],
        "moe_w1": np.ascontiguousarray(inputs["moe_w1"].reshape(DEPTH, NE, 8, 128, 512).transpose(0, 1, 3, 2, 4)),
        "moe_w3": np.ascontiguousarray(inputs["moe_w3"].reshape(DEPTH, NE, 8, 128, 512).transpose(0, 1, 3, 2, 4)),
        "moe_w2": np.ascontiguousarray(inputs["moe_w2"].reshape(DEPTH, NE, 4, 128, D).transpose(0, 1, 3, 2, 4)),
    }


def kernel(**inputs):
    inputs = {k_: np.asarray(v) for k_, v in inputs.items()}
    if "prog" not in _CACHE:
        _CACHE["prog"] = Prog()
    prog = _CACHE["prog"]
    shared = _shared_inputs(inputs)
    in_maps = []
    for core in range(8):
        m = dict(shared)
        m.update(_host_inputs(inputs, core))
        in_maps.append({k_: np.ascontiguousarray(v, dtype=np.float32) for k_, v in m.items() if k_ in prog.I})
    res = run_bass_kernel_spmd(prog.nc, in_maps, core_ids=list(range(8)))
    return np.concatenate([r["out"] for r in res.results], axis=0).astype(np.float32)
```

```python
import contextlib
import numpy as np
import concourse.bass as bass
import concourse.mybir as mybir
from concourse.bass_utils import run_bass_kernel_spmd

F32 = mybir.dt.float32
BF16 = mybir.dt.bfloat16
AF = mybir.ActivationFunctionType
ALU = mybir.AluOpType

EPOCH_D = 1500
EPOCH_C = 30000
PH = "__phase__"

D = 1024
NBC = 2
SEQ = 4096
CTX = 256
TT = SEQ + CTX
DEPTH = 4
ALPHA = (2.0 * DEPTH) ** 0.25
LN_EPS = 1e-5
RMS_EPS = 1e-6
NE = 32


def _key(x):
    if isinstance(x, (tuple, str)):
        return x
    t = getattr(x, "tensor", None)
    if t is not None:
        return t.name
    return x.name


class Sched:
    def __init__(self, nc):
        self.nc = nc
        self.ops = []

    def op(self, eng, fn, r=(), w=(), dma=False):
        rk = [_key(k) for k in r]
        wk = [_key(k) for k in w]
        for kk in rk:
            if isinstance(kk, str) and kk.startswith("ps") and kk not in wk:
                wk.append(kk)
        rk.append(PH)
        self.ops.append([eng, fn, tuple(rk), tuple(wk), dma])

    def dma(self, eng, out, in_, r=None, w=None, store=False, skey=None, **kw):
        r = [in_] if r is None else r
        w = [out] if w is None else w
        if skey is None:
            skey = _key(in_) if store else _key(out)
        self.op(eng, lambda e, out=out, in_=in_, kw=kw: e.dma_start(out=out, in_=in_, **kw), r, w, dma=skey)

    def barrier(self):
        self.ops.append(["sp", lambda e: e.nop(), (), (PH,), False])

    def finish(self):
        nc = self.nc
        ops = self.ops
        n = len(ops)
        last_w = {}
        rd_eng = {}
        rd_dma = {}
        deps_all = [None] * n
        for i, (eng, fn, r, w, dma) in enumerate(ops):
            deps = set()
            for k in r:
                j = last_w.get(k)
                if j is not None:
                    deps.add(j)
            for k in w:
                j = last_w.get(k)
                if j is not None:
                    deps.add(j)
                d = rd_eng.get(k)
                if d:
                    deps.update(d.values())
                d = rd_dma.get(k)
                if d:
                    deps.update(d)
            for k in r:
                if dma:
                    rd_dma.setdefault(k, []).append(i)
                else:
                    rd_eng.setdefault(k, {})[eng] = i
            for k in w:
                last_w[k] = i
                rd_eng[k] = {}
                rd_dma[k] = []
            deps.discard(i)
            deps_all[i] = deps
        need = [False] * n
        for i, (eng, fn, r, w, dma) in enumerate(ops):
            keep = set()
            for j in deps_all[i]:
                je, _, _, _, jd = ops[j]
                if jd:
                    keep.add(j)
                elif je != eng or eng != "pe":
                    keep.add(j)
            deps_all[i] = keep
            for j in keep:
                need[j] = True
        pos = [None] * n
        cnt = {}
        for i, (eng, fn, r, w, dma) in enumerate(ops):
            if dma:
                stream = ("dma", dma)
            elif need[i]:
                stream = ("eng", eng)
            else:
                continue
            c = cnt.get(stream, 0)
            pos[i] = (stream, c)
            cnt[stream] = c + 1
        phase = [0] * n
        ph = 0
        for i, o in enumerate(ops):
            if o[3] == (PH,):
                ph += 1
            phase[i] = ph
        seg = {}
        for i in range(n):
            if pos[i] is None:
                continue
            stream, c = pos[i]
            EP = EPOCH_D if stream[0] == "dma" else EPOCH_C
            sk = (stream, c // EP)
            g = seg.get(sk)
            if g is None:
                seg[sk] = [phase[i], phase[i], 1]
            else:
                g[1] = phase[i]
                g[2] += 1
        es = contextlib.ExitStack()
        hw = []
        base = {}
        semof = {}
        for sk in sorted(seg, key=lambda q: seg[q][0]):
            pf, pl, c = seg[sk]
            inc = 16 if sk[0][0] == "dma" else 1
            pick = None
            for hwi in hw:
                if hwi[2] < pf and hwi[1] + c * inc <= 30000:
                    pick = hwi
                    break
            if pick is None:
                pick = [es.enter_context(nc.semaphore(f"s{len(hw)}")), 0, -1]
                hw.append(pick)
            base[sk] = pick[1]
            semof[sk] = pick[0]
            pick[1] += c * inc
            pick[2] = pl
        self.nsem = len(hw)
        by_eng = {}
        for i, o in enumerate(ops):
            by_eng.setdefault(o[0], []).append(i)

        def emit(e, eng):
            waited = {}
            for i in by_eng.get(eng, []):
                _, fn, r, w, dma = ops[i]
                reqs = {}
                for j in deps_all[i]:
                    stream, c = pos[j]
                    isd = stream[0] == "dma"
                    EP = EPOCH_D if isd else EPOCH_C
                    sk = (stream, c // EP)
                    v = base[sk] + (c % EP + 1) * (16 if isd else 1)
                    sm = semof[sk]
                    if reqs.get(id(sm), (0, None))[0] < v:
                        reqs[id(sm)] = (v, sm)
                for sid, (v, sm) in reqs.items():
                    if waited.get(sid, 0) < v:
                        e.wait_ge(sm, v)
                        waited[sid] = v
                ins = fn(e)
                if pos[i] is not None:
                    stream, c = pos[i]
                    isd = stream[0] == "dma"
                    EP = EPOCH_D if isd else EPOCH_C
                    ins.then_inc(semof[(stream, c // EP)], 16 if isd else 1)

        with nc.Block() as block:
            @block.tensor
            def _(e): emit(e, "pe")

            @block.scalar
            def _(e): emit(e, "act")

            @block.vector
            def _(e): emit(e, "dve")

            @block.gpsimd
            def _(e): emit(e, "pool")

            @block.sync
            def _(e): emit(e, "sp")
        es.close()


class KB:
    def __init__(self, nc):
        self.nc = nc
        self.S = Sched(nc)
        self.gs = contextlib.ExitStack()

    def sb(self, es, name, shape, dt=F32):
        self.uid = getattr(self, "uid", 0) + 1
        return es.enter_context(self.nc.sbuf_tensor(f"{name}_u{self.uid}", list(shape), dt))

    def dram(self, name, shape, dt, kind):
        return self.nc.dram_tensor(name, list(shape), dt, kind=kind).ap()

    @staticmethod
    def _rw(out, ins, r, w):
        if r is None:
            r = [a for a in ins if not isinstance(a, (int, float)) and a is not None]
        if w is None:
            w = [out]
        return r, w

    def mm(self, out, lhsT, rhs, start, stop, r=None, w=None):
        r, w = self._rw(out, [lhsT, rhs], r, w)
        self.S.op("pe", lambda e: e.matmul(out, lhsT=lhsT, rhs=rhs, start=start, stop=stop), r, w)

    def tr(self, out, in_, ident, r=None, w=None):
        r, w = self._rw(out, [in_, ident], r, w)
        self.S.op("pe", lambda e: e.transpose(out, in_, ident), r, w)

    def act(self, out, in_, func, scale=None, bias=None, r=None, w=None):
        r, w = self._rw(out, [in_, scale, bias], r, w)
        kw = {}
        if scale is not None:
            kw["scale"] = scale
        if bias is not None:
            kw["bias"] = bias
        self.S.op("act", lambda e: e.activation(out=out, in_=in_, func=func, **kw), r, w)

    def tt(self, eng, out, in0, in1, op, r=None, w=None):
        r, w = self._rw(out, [in0, in1], r, w)
        self.S.op(eng, lambda e: e.tensor_tensor(out=out, in0=in0, in1=in1, op=op), r, w)

    def ts(self, eng, out, in0, s1, s2, op0, op1=None, r=None, w=None):
        r, w = self._rw(out, [in0, s1, s2], r, w)
        if op1 is None:
            self.S.op(eng, lambda e: e.tensor_scalar(out=out, in0=in0, scalar1=s1, scalar2=None, op0=op0), r, w)
        else:
            self.S.op(eng, lambda e: e.tensor_scalar(out=out, in0=in0, scalar1=s1, scalar2=s2, op0=op0, op1=op1), r, w)

    def stt(self, out, in0, scalar, in1, op0, op1, r=None, w=None):
        r, w = self._rw(out, [in0, scalar, in1], r, w)
        self.S.op("dve", lambda e: e.scalar_tensor_tensor(out=out, in0=in0, scalar=scalar, in1=in1, op0=op0, op1=op1), r, w)

    def copy(self, eng, out, in_, r=None, w=None):
        r, w = self._rw(out, [in_], r, w)
        if eng == "act":
            self.S.op("act", lambda e: e.copy(out=out, in_=in_), r, w)
        else:
            self.S.op(eng, lambda e: e.tensor_copy(out=out, in_=in_), r, w)

    def memset(self, eng, ap, val, w=None):
        self.S.op(eng, lambda e: e.memset(ap, val), (), [ap] if w is None else w)

    def fn(self, eng, f, r, w):
        self.S.op(eng, f, r, w)

    def dma(self, eng, out, in_, **kw):
        self.S.dma(eng, out, in_, **kw)


def tiles_of(b, with_ctx=True):
    t = []
    if with_ctx:
        t.append((b, 0, CTX, 2))
    for j in range(SEQ // 512):
        t.append((b, CTX + 512 * j, 512, b))
    return t


class Prog:
    def __init__(self, layers=(0, 1, 2, 3), h_from_input=True, dbg=None):
        self.layers = list(layers)
        self.dbg = dbg
        NL = len(self.layers)
        self.li = {l: j for j, l in enumerate(self.layers)}
        nc = bass.Bass("TRN2", target_bir_lowering=False)
        self.nc = nc
        k = KB(nc)
        self.k = k
        NEd = dbg[2] if isinstance(dbg, tuple) else NE
        shapes = {
            "hin": [NBC, TT, D], "cc": [3, D],
            "ada_w": [NL, D, 6 * D], "ada_b": [NL, 1, 6 * D], "ln_g": [NL, 2, D], "ln_b": [NL, 2, D],
            "ret_w_in": [2, D, 6 * D], "ret_decay": [2, 8], "ret_gn_g": [2, 2 * D], "ret_w_out": [2, 2 * D, D],
            "ret_cos": [128, SEQ], "ret_sin": [128, SEQ],
            "mla_wd": [D, 768], "mla_norms": [5, 128], "mla_wuq": [384, 2048], "mla_wukv": [256, 2048],
            "mla_w_out": [D, D], "mla_c1": [128, SEQ], "mla_c2": [128, SEQ],
            "conv_w_in": [1, D, 3 * D], "conv_wb": [1, 4, D], "conv_w_out": [1, D, D],
            "moe_wr": [NL, D, 64], "moe_bg": [NL, 4], "moe_be": [NL, 32],
            "moe_w1": [NL, NE, 128, 8, 512], "moe_w3": [NL, NE, 128, 8, 512], "moe_w2": [NL, NE, 128, 4, D],
        }

        class Lazy(dict):
            def __missing__(d, name):
                d[name] = k.dram(name, shapes[name], F32, "ExternalInput")
                return d[name]
        I = Lazy()
        self.I = I
        self.out = k.dram("out", [NBC, SEQ, D], F32, "ExternalOutput")
        self.HA = k.dram("HA", [NBC, TT, D], F32, "ExternalOutput" if dbg else "Internal")
        self.HB = k.dram("HB", [NBC, TT, D], F32, "ExternalOutput" if dbg else "Internal")
        self.SC1 = k.dram("SC1", [NBC, D, SEQ + 2], F32, "Internal")
        self.SC2 = k.dram("SC2", [NBC, D, SEQ], F32, "Internal")
        self.SC1c = k.dram("SC1c", [NBC, D, CTX + 2], F32, "Internal")
        self.SC2c = k.dram("SC2c", [NBC, D, CTX], F32, "Internal")
        if any(l % 3 == 0 for l in self.layers):
            for nm in ("QT", "KT", "QTF", "QTB"):
                setattr(self, nm, k.dram(nm, [NBC, D, TT], BF16, "Internal"))
            for nm in ("KF", "KB"):
                setattr(self, nm, k.dram(nm, [NBC, TT, D], BF16, "Internal"))
            for nm in ("RV", "RG"):
                setattr(self, nm, k.dram(nm, [NBC, TT, 2 * D], BF16, "Internal"))
            self.SBD = k.dram("SBD", [NBC, TT // 128, 128, 8 * 512], BF16, "Internal")
        if any(l % 3 == 1 for l in self.layers):
            self.QN = k.dram("QN", [NBC, 8, 128, TT], BF16, "Internal")
            self.QR = k.dram("QR", [NBC, 4, 128, TT], BF16, "Internal")
            self.KN = k.dram("KN", [NBC, 8, 128, TT], BF16, "Internal")
            self.KR = k.dram("KR", [NBC, 64, TT], BF16, "Internal")
            self.MV = k.dram("MV", [NBC, TT, D], BF16, "Internal")
            self.OT = k.dram("OT", [NBC, D, TT], BF16, "Internal")
        NROWS = ((2 * (TT // 128) * NBC * 128 + 511) // 512 + NE) * 512
        self.XS = k.dram("XS", [NROWS, D], BF16, "Internal")
        self.VTB = k.dram("VTB", [NBC, TT, D], BF16, "Internal")
        self.YS = k.dram("YS", [NROWS, D], F32, "Internal")
        gs = k.gs
        self.PS = [gs.enter_context(nc.psum_tensor(f"ps{i}", [128, 512], F32)) for i in range(8)]
        self.ident = k.sb(gs, "ident", [128, 128])
        self.actT = k.sb(gs, "actT", [128, 8, 4])
        self.ones = k.sb(gs, "ones", [1, 512])
        self.negh = k.sb(gs, "negh", [128, 512])
        self.onesf = k.sb(gs, "onesf", [128, 128])
        self.onesb = k.sb(gs, "onesb", [128, 128], BF16)
        self.identb = k.sb(gs, "identb", [128, 128], BF16)
        self.MP = k.sb(gs, "MP", [128, 4, 8, 4])
        self.GB = k.sb(gs, "GB", [128, 3, 2, 1024])
        self.LNP = k.sb(gs, "LNP", [128, 4, 1024])
        self.setup()
        first = True
        for i in self.layers:
            src = I["hin"] if (first and h_from_input) else self.HA
            first = False
            self.mod_params(i)
            if isinstance(dbg, tuple) and dbg[0] == "moe":
                self.moe_layer(i, src, self.HA, False, dbg_ns=dbg[1], dbg_ne=dbg[2])
                break
            if dbg == "mod":
                d1 = k.dram("dbg_MP", [128, 4 * 8 * 4], F32, "ExternalOutput")
                d2 = k.dram("dbg_GB", [128, 3 * 2 * 1024], F32, "ExternalOutput")
                d3 = k.dram("dbg_LNP", [128, 4 * 1024], F32, "ExternalOutput")
                k.dma("sp", d1, self.MP[:].rearrange("p a b c -> p (a b c)"), store=True)
                k.dma("sp", d2, self.GB[:].rearrange("p a b c -> p (a b c)"), store=True)
                k.dma("sp", d3, self.LNP[:].rearrange("p a c -> p (a c)"), store=True)
                break
            kind = i % 3
            if kind == 2:
                self.conv_layer(i, src, self.HB)
            elif kind == 0:
                self.ret_layer(i, src, self.HB)
            elif kind == 1:
                self.mla_layer(i, src, self.HB)
            else:
                raise NotImplementedError
            if dbg == "mix":
                break
            last = (i == DEPTH - 1)
            self.moe_layer(i, self.HB, self.HA, last)
        import os
        mo = int(os.environ.get("MAXOPS", "0"))
        if mo:
            for o in k.S.ops[mo:mo + 3]:
                print("TRUNC next ops:", o[0], o[2][:3], o[3])
            del k.S.ops[mo:]
        k.S.barrier()
        k.S.op("sp", lambda e: e.nop(), (), ())
        k.S.finish()
        gs.close()

    def setup(self):
        k = self.k
        nc = self.nc
        ident = self.ident
        k.memset("pool", ident[:], 0.0)
        k.fn("pool", lambda e: e.affine_select(out=ident[:], in_=ident[:], pattern=[[-1, 128]], compare_op=ALU.not_equal,
                                               fill=1.0, base=0, channel_multiplier=1), [ident], [ident])
        k.copy("dve", self.identb[:], ident[:])
        k.memset("dve", self.ones[:], 1.0)
        k.memset("dve", self.negh[:], -0.5)
        k.memset("dve", self.onesf[:], 1.0)
        k.memset("dve", self.onesb[:], 1.0)
        with contextlib.ExitStack() as es:
            craw = k.sb(es, "craw", [4, D])
            k.memset("dve", craw[:], 0.0)
            k.dma("sp", craw[0:3, :], self.I["cc"][:, :])
            k.act(craw[:], craw[:], AF.Silu)
            ps = self.PS[0]
            for kk in range(8):
                k.tr(ps[:, kk * 4:(kk + 1) * 4], craw[0:4, kk * 128:(kk + 1) * 128], ident[0:4, 0:4])
            k.copy("dve", self.actT[:], ps[:, 0:32].rearrange("p (k f) -> p k f", f=4))
            k.S.barrier()

    def mod_params(self, i):
        k = self.k
        I = self.I
        i = self.li[i]
        with contextlib.ExitStack() as es:
            wblk = [k.sb(es, f"mp_w{j}", [128, 8, 1024]) for j in range(2)]
            brow = k.sb(es, "mp_brow", [1, 6 * D])
            REP = k.sb(es, "mp_REP", [128, 3, 8, 128])
            for r in range(3):
                for kk in range(8):
                    k.copy("dve", REP[:, r, kk, :], self.actT[:, kk, r:r + 1].broadcast_to([128, 128]))
            k.dma("sp", brow[:], I["ada_b"][i])
            for j in range(4):
                k.dma("sp", self.LNP[:, j, :], (I["ln_g"] if j % 2 == 0 else I["ln_b"])[i, j // 2].partition_broadcast(128))
            pi = 0
            for wi in (0, 1, 2, 5):
                buf = wblk[wi % 2]
                k.dma("sp", buf[:], I["ada_w"][i][:, wi * 1024:(wi + 1) * 1024].rearrange("(k p) n -> p k n", p=128))
                if wi in (0, 1, 3, 4):
                    slot = {0: 0, 1: 1, 3: 2, 4: 3}[wi]
                    ps = self.PS[pi % 8]
                    pi += 1
                    for ko in range(8):
                        o = ps[:, ko * 4:ko * 4 + 3]
                        for ki in range(8):
                            k.mm(o, buf[:, ki, ko * 128:(ko + 1) * 128], self.actT[:, ki, 0:3], ki == 0, False)
                        k.mm(o, brow[0:1, wi * 1024 + ko * 128: wi * 1024 + (ko + 1) * 128], self.ones[0:1, 0:3], False, True)
                    src = ps[:, 0:32].rearrange("p (k f) -> p k f", f=4)[:, :, 0:3]
                    if wi in (1, 4):
                        k.ts("dve", self.MP[:, slot, :, 0:3], src, 1.0, None, ALU.add)
                    else:
                        k.copy("dve", self.MP[:, slot, :, 0:3], src)
                else:
                    gi = 0 if wi == 2 else 1
                    for r in range(3):
                        for half in range(2):
                            ps = self.PS[pi % 8]
                            pi += 1
                            for ki in range(8):
                                k.mm(ps[:], REP[:, r, ki, :], buf[:, ki, half * 512:(half + 1) * 512], ki == 0, False)
                            k.mm(ps[:], self.ones[0:1, 0:128], brow[0:1, wi * 1024 + half * 512: wi * 1024 + (half + 1) * 512], False, True)
                            k.copy("act", self.GB[:, r, gi, half * 512:(half + 1) * 512], ps[:])
            k.S.barrier()

    def prologue(self, src, b, t0, n, r, slot_sh, stg, uT, psb, v32=None):
        k = self.k
        nt = n // 128
        k.dma("sp", stg[:, 0:nt, :], src[b, t0:t0 + n, :].rearrange("(t p) d -> p t d", p=128),
              r=[(_key(src), b, t0)])
        for kk in range(8):
            ps = psb[kk % len(psb)]
            for t in range(nt):
                k.tr(ps[:, t * 128:(t + 1) * 128], stg[:, t, kk * 128:(kk + 1) * 128], self.ident[:])
            k.act(uT[:, kk, 0:n], ps[:, 0:n], AF.Identity, scale=self.MP[:, slot_sh + 1, kk, r:r + 1],
                  bias=self.MP[:, slot_sh, kk, r:r + 1])
            if v32 is not None:
                k.ts("dve", v32[:, kk, 0:n], ps[:, 0:n], self.MP[:, slot_sh + 1, kk, r:r + 1],
                     self.MP[:, slot_sh, kk, r:r + 1], ALU.mult, ALU.add)

    def ln_epilogue(self, ysrc, stg, nt, r, gi, li, zb, st, mv, rs, dst_rows, dst_key, mid=None):
        k = self.k
        if isinstance(zb, list):
            self._lnc = getattr(self, "_lnc", 0) + 1
            x = self._lnc % len(zb)
            zb, st, mv, rs = zb[x], st[x], mv[x], rs[x]
        zk = lambda t: (_key(zb), t)
        for t in range(nt):
            z = zb[:, t, :]
            for half in range(2):
                y = ysrc(t, half)
                k.tt("dve", zb[:, t, half * 512:(half + 1) * 512], y, self.GB[:, r, gi, half * 512:(half + 1) * 512], ALU.mult,
                     r=[y, self.GB], w=[zk(t)])
            k.stt(z, stg[:, t, :], ALPHA, z, ALU.mult, ALU.add, r=[stg, zk(t)], w=[zk(t)])
            for c in range(2):
                k.fn("dve", (lambda e, t=t, c=c: e.bn_stats(out=st[:, t, c * 6:(c + 1) * 6], in_=zb[:, t, c * 512:(c + 1) * 512])),
                     [zk(t)], [(_key(st), t, c)])
            k.fn("dve", (lambda e, t=t: e.bn_aggr(out=mv[:, t, :], in_=st[:, t, :])), [(_key(st), t, 0), (_key(st), t, 1)], [(_key(mv), t)])
        k.ts("pool", rs[:, 0:nt], mv[:, 0:nt, 1], LN_EPS, None, ALU.add, r=[(_key(mv), t) for t in range(nt)], w=[rs])
        if mid is not None:
            mid()
        k.tt("pool", rs[:, 0:nt], rs[:, 0:nt], self.negh[:, 0:nt], ALU.pow)
        k.tt("pool", rs[:, 4:4 + nt], mv[:, 0:nt, 0], rs[:, 0:nt], ALU.mult, r=[(_key(mv), t) for t in range(nt)] + [rs], w=[rs])
        k.ts("pool", rs[:, 4:4 + nt], rs[:, 4:4 + nt], -1.0, None, ALU.mult)
        for t in range(nt):
            z = zb[:, t, :]
            k.act(z, z, AF.Identity, scale=rs[:, t:t + 1], bias=rs[:, 4 + t:5 + t], r=[zk(t), rs], w=[zk(t)])
            k.tt("dve", z, z, self.LNP[:, 2 * li, :], ALU.mult, r=[zk(t), self.LNP], w=[zk(t)])
            k.tt("pool", z, z, self.LNP[:, 2 * li + 1, :], ALU.add, r=[zk(t), self.LNP], w=[zk(t)])
        k.dma("pool", dst_rows.rearrange("(t p) d -> p t d", p=128), zb[:, 0:nt, :], store=True,
              r=[zk(t) for t in range(nt)], w=[dst_key])

    def conv_layer(self, i, src, dst):
        k = self.k
        I = self.I
        PS = self.PS
        need_ctx = i < DEPTH - 1
        with contextlib.ExitStack() as es:
            win = k.sb(es, "cv_win", [128, 8, 3 * D], BF16)
            k.dma("pool", win[:], I["conv_w_in"][0].rearrange("(k p) n -> p k n", p=128))
            stg = [k.sb(es, "cv_stg0", [128, 4, D])]
            uT = [k.sb(es, f"cv_uT{j}", [128, 8, 512], BF16) for j in range(2)]
            sbuf = [k.sb(es, f"cv_s{j}", [128, 8, 512]) for j in range(2)]
            gbuf = [k.sb(es, f"cv_g{j}", [128, 8, 512]) for j in range(2)]
            gct = [k.sb(es, f"cv_gc{j}", [128, 512]) for j in range(2)]
            zero = k.sb(es, "cv_zero", [128, 8, 1])
            k.memset("dve", zero[:], 0.0)
            it = 0
            for b in range(NBC):
                for (bb, t0, n, r) in tiles_of(b, need_ctx):
                    isctx = (r == 2)
                    s1 = (self.SC1c if isctx else self.SC1)
                    s2 = (self.SC2c if isctx else self.SC2)
                    c0 = t0 if isctx else t0 - CTX
                    sg, u, sbf, gbf = stg[0], uT[it % 2], sbuf[it % 2], gbuf[it % 2]
                    self.prologue(src, b, t0, n, r, 0, sg, u, [PS[6], PS[7]])
                    for j in range(8):
                        pb, pc, ph = PS[(3 * j) % 6], PS[(3 * j + 1) % 6], PS[(3 * j + 2) % 6]
                        for (pp, col) in ((pb, j), (pc, 8 + j), (ph, 16 + j)):
                            for kk in range(8):
                                k.mm(pp[:, 0:n], win[:, kk, col * 128:(col + 1) * 128], u[:, kk, 0:n], kk == 0, kk == 7)
                        g = gct[j % 2]
                        k.copy("act", gbf[:, j, 0:n], pb[:, 0:n], w=[(_key(gbf), j)])
                        k.copy("act", g[:, 0:n], pc[:, 0:n])
                        k.tt("dve", sbf[:, j, 0:n], g[:, 0:n], ph[:, 0:n], ALU.mult, w=[(_key(sbf), j)])
                    k.dma("pool", s1[b, :, 1 + c0:1 + c0 + n].rearrange("(k p) t -> p k t", p=128), sbf[:, :, 0:n], store=True,
                          r=[(_key(sbf), j) for j in range(8)], w=[("SC1", b, isctx, c0)])
                    k.dma("act", s2[b, :, c0:c0 + n].rearrange("(k p) t -> p k t", p=128), gbf[:, :, 0:n], store=True,
                          r=[(_key(gbf), j) for j in range(8)], w=[("SC2", b, isctx, c0)])
                    it += 1
                for (s1, L) in (((self.SC1c, CTX), (self.SC1, SEQ)) if need_ctx else ((self.SC1, SEQ),)):
                    for col in (0, L + 1):
                        k.dma("sp", s1[b, :, col:col + 1].rearrange("(k p) t -> p k t", p=128), zero[:], store=True,
                              w=[("SC1h", b, L, col)], allow_slow_non_contiguous=True)
            k.S.barrier()
        with contextlib.ExitStack() as es:
            wout = k.sb(es, "cv_wout", [128, 8, D], BF16)
            cw = k.sb(es, "cv_cw", [128, 4, 8])
            craw = k.sb(es, "cv_craw", [32, 128])
            k.dma("pool", wout[:], I["conv_w_out"][0].rearrange("(k p) n -> p k n", p=128))
            k.dma("sp", craw[:], I["conv_wb"][0].rearrange("j (k p) -> (j k) p", p=128))
            k.tr(PS[7][:, 0:32], craw[:, :], self.ident[0:32, 0:32])
            k.copy("dve", cw[:], PS[7][:, 0:32].rearrange("p (j k) -> p j k", k=8))
            stg = [k.sb(es, f"cv_stg{j}", [128, 4, D]) for j in range(2)]
            gbuf = [k.sb(es, f"cv_g{j}", [128, 8, 512]) for j in range(2)]
            zb = [k.sb(es, f"cv_zb{x}", [128, 4, D]) for x in range(1)]
            st = [k.sb(es, f"cv_st{x}", [128, 4, 12]) for x in range(1)]
            mv = [k.sb(es, f"cv_mv{x}", [128, 4, 2]) for x in range(1)]
            rs = [k.sb(es, f"cv_rs{x}", [128, 8]) for x in range(1)]
            sx = [k.sb(es, f"cv_sx{j}", [128, 8, 514]) for j in range(2)]
            gT = [k.sb(es, f"cv_gT{j}", [128, 8, 512], BF16) for j in range(2)]
            ctmp = [k.sb(es, f"cv_ct{j}", [128, 512]) for j in range(2)]
            it = 0
            for b in range(NBC):
                for (bb, t0, n, r) in tiles_of(b, need_ctx):
                    isctx = (r == 2)
                    s1 = (self.SC1c if isctx else self.SC1)
                    s2 = (self.SC2c if isctx else self.SC2)
                    c0 = t0 if isctx else t0 - CTX
                    nt = n // 128
                    sg, sxx, gbf, g = stg[it % 2], sx[it % 2], gbuf[it % 2], gT[it % 2]
                    k.dma("sp", sg[:, 0:nt, :], src[b, t0:t0 + n, :].rearrange("(t p) d -> p t d", p=128), r=[(_key(src), b, t0)])
                    k.dma("sp", sxx[:, :, 0:n + 2], s1[b, :, c0:c0 + n + 2].rearrange("(k p) t -> p k t", p=128), r=[("SC1all",)])
                    k.dma("sp", gbf[:, :, 0:n], s2[b, :, c0:c0 + n].rearrange("(k p) t -> p k t", p=128), r=[("SC2all",)])
                    for kk in range(8):
                        c = ctmp[kk % 2]
                        k.act(c[:, 0:n], sxx[:, kk, 1:n + 1], AF.Identity, scale=cw[:, 1, kk:kk + 1], bias=cw[:, 3, kk:kk + 1])
                        k.stt(c[:, 0:n], sxx[:, kk, 0:n], cw[:, 0, kk:kk + 1], c[:, 0:n], ALU.mult, ALU.add)
                        k.stt(c[:, 0:n], sxx[:, kk, 2:n + 2], cw[:, 2, kk:kk + 1], c[:, 0:n], ALU.mult, ALU.add)
                        k.tt("dve", g[:, kk, 0:n], c[:, 0:n], gbf[:, kk, 0:n], ALU.mult)
                    for t in range(nt):
                        for half in range(2):
                            ps = PS[(t * 2 + half) % 8]
                            for kk in range(8):
                                k.mm(ps[:], g[:, kk, t * 128:(t + 1) * 128], wout[:, kk, half * 512:(half + 1) * 512], kk == 0, kk == 7)
                    self.ln_epilogue(lambda t, half: PS[(t * 2 + half) % 8][:], sg, nt, r, 0, 0, zb, st, mv, rs,
                                     dst[b, t0:t0 + n, :], (_key(dst), b, t0))
                    it += 1
            k.S.barrier()

    def ret_layer(self, i, src, dst):
        k = self.k
        I = self.I
        PS = self.PS
        need_ctx = i < DEPTH - 1
        j = i // 3
        NCH = TT // 128
        with contextlib.ExitStack() as tes:
            lg = k.sb(tes, "rt_lg", [128, 8])
            GL = k.sb(tes, "rt_GL", [128, 8])
            MT = k.sb(tes, "rt_MT", [128, 4, 128])
            DF = k.sb(tes, "rt_DF", [128, 4, 128])
            DB = k.sb(tes, "rt_DB", [128, 4, 128])
            KFd = k.sb(tes, "rt_KFd", [128, 4])
            KBd = k.sb(tes, "rt_KBd", [128, 4])
            with contextlib.ExitStack() as es:
                Dm = k.sb(es, "rt_D", [128, 128])
                Dp = k.sb(es, "rt_Dp", [128, 128])
                Dn = k.sb(es, "rt_Dn", [128, 128])
                mf = k.sb(es, "rt_mf", [128, 128])
                mb = k.sb(es, "rt_mb", [128, 128])
                Ef = k.sb(es, "rt_Ef", [128, 128])
                Eb = k.sb(es, "rt_Eb", [128, 128])
                I1 = k.sb(es, "rt_I1", [128, 128])
                I2 = k.sb(es, "rt_I2", [128, 128])
                P1 = k.sb(es, "rt_P1", [128, 2])
                k.dma("sp", lg[:], I["ret_decay"][j].partition_broadcast(128))
                k.act(lg[:], lg[:], AF.Exp, scale=-1.0)
                k.act(lg[:], lg[:], AF.Ln, bias=1.0)
                k.ts("dve", lg[:], lg[:], -1.0, None, ALU.mult)
                k.act(GL[:], lg[:], AF.Exp, scale=128.0)
                k.fn("pool", lambda e: e.iota(Dm[:], pattern=[[1, 128]], base=0, channel_multiplier=-1,
                                              allow_small_or_imprecise_dtypes=True), [], [Dm])
                k.fn("pool", lambda e: e.iota(I1[:], pattern=[[1, 128]], base=1, channel_multiplier=0,
                                              allow_small_or_imprecise_dtypes=True), [], [I1])
                k.fn("pool", lambda e: e.iota(I2[:], pattern=[[-1, 128]], base=128, channel_multiplier=0,
                                              allow_small_or_imprecise_dtypes=True), [], [I2])
                k.fn("pool", lambda e: e.iota(P1[:, 0:1], pattern=[[0, 1]], base=127, channel_multiplier=-1,
                                              allow_small_or_imprecise_dtypes=True), [], [P1])
                k.fn("pool", lambda e: e.iota(P1[:, 1:2], pattern=[[0, 1]], base=0, channel_multiplier=1,
                                              allow_small_or_imprecise_dtypes=True), [P1], [P1])
                k.ts("dve", Dp[:], Dm[:], 0.0, None, ALU.max)
                k.ts("dve", Dn[:], Dm[:], -1.0, 0.0, ALU.mult, ALU.max)
                k.ts("dve", mf[:], Dm[:], 0.0, None, ALU.is_ge)
                k.ts("dve", mb[:], Dm[:], 0.0, None, ALU.is_le)
                for h in range(4):
                    k.act(Ef[:], Dp[:], AF.Exp, scale=lg[:, h:h + 1])
                    k.tt("dve", Ef[:], Ef[:], mf[:], ALU.mult)
                    k.act(Eb[:], Dn[:], AF.Exp, scale=lg[:, 4 + h:5 + h])
                    k.tt("dve", Eb[:], Eb[:], mb[:], ALU.mult)
                    k.tt("dve", Ef[:], Ef[:], Eb[:], ALU.add)
                    k.ts("dve", MT[:, h, :], Ef[:], 0.0625, None, ALU.mult)
                    k.act(DF[:, h, :], I1[:], AF.Exp, scale=lg[:, h:h + 1])
                    k.act(DB[:, h, :], I2[:], AF.Exp, scale=lg[:, 4 + h:5 + h])
                    k.act(KFd[:, h:h + 1], P1[:, 0:1], AF.Exp, scale=lg[:, h:h + 1])
                    k.act(KBd[:, h:h + 1], P1[:, 1:2], AF.Exp, scale=lg[:, 4 + h:5 + h])
                k.ts("dve", KFd[:], KFd[:], 0.0625, None, ALU.mult)
                k.ts("dve", KBd[:], KBd[:], 0.0625, None, ALU.mult)
                k.S.barrier()
            for part in range(2):
              with contextlib.ExitStack() as es:
                wqk = k.sb(es, "r1_wqk", [128, 8, D], BF16)
                k.dma("pool", wqk[:], I["ret_w_in"][j][:, part * D:(part + 1) * D].rearrange("(k p) n -> p k n", p=128))
                cosT = k.sb(es, "r1_cos", [128, SEQ])
                sinT = k.sb(es, "r1_sin", [128, SEQ])
                k.dma("sp", cosT[:], I["ret_cos"])
                k.dma("sp", sinT[:], I["ret_sin"])
                stg = k.sb(es, "r1_stg", [128, 4, D])
                uT = [k.sb(es, f"r1_uT{x}", [128, 8, 512], BF16) for x in range(2)]
                tm = [k.sb(es, f"r1_tm{x}", [128, 512]) for x in range(4)]
                if part == 0:
                    qT = k.sb(es, "r1_qT", [128, 8, 512], BF16)
                    qf = k.sb(es, "r1_qf", [128, 8, 512], BF16)
                    qb = k.sb(es, "r1_qb", [128, 8, 512], BF16)
                    q32 = [k.sb(es, f"r1_q32{x}", [128, 512]) for x in range(2)]
                else:
                    kT = k.sb(es, "r1_kT", [128, 8, 512], BF16)
                    k32 = k.sb(es, "r1_k32", [128, 8, 512])
                    kfb = k.sb(es, "r1_kf", [128, 4, D], BF16)
                    kbb = k.sb(es, "r1_kb", [128, 4, D], BF16)
                it = 0
                for b in range(NBC):
                    for (bb, t0, n, r) in tiles_of(b, True):
                        isctx = (r == 2)
                        c0 = t0 - CTX
                        nt = n // 128
                        u = uT[it % 2]
                        it += 1
                        self.prologue(src, b, t0, n, r, 0, stg, u, [PS[6], PS[7]])
                        for hh in range(part * 4, part * 4 + 4):
                            isq = hh < 4
                            h = hh % 4
                            c1, c2 = 2 * h, 2 * h + 1
                            x1, x2 = PS[(2 * hh) % 4], PS[(2 * hh + 1) % 4]
                            for kk in range(8):
                                k.mm(x1[:, 0:n], wqk[:, kk, c1 * 128:(c1 + 1) * 128], u[:, kk, 0:n], kk == 0, kk == 7)
                            for kk in range(8):
                                k.mm(x2[:, 0:n], wqk[:, kk, c2 * 128:(c2 + 1) * 128], u[:, kk, 0:n], kk == 0, kk == 7)
                            d1, d2 = 2 * h, 2 * h + 1
                            if isq:
                                o1, o2 = q32[0][:, 0:n], q32[1][:, 0:n]
                            else:
                                o1, o2 = k32[:, d1, 0:n], k32[:, d2, 0:n]
                            if isctx:
                                k.copy("act", o1, x1[:, 0:n])
                                k.copy("act", o2, x2[:, 0:n])
                            else:
                                cs, sn = cosT[:, c0:c0 + n], sinT[:, c0:c0 + n]
                                k.tt("dve", tm[0][:, 0:n], x1[:, 0:n], cs, ALU.mult)
                                k.tt("dve", tm[1][:, 0:n], x2[:, 0:n], sn, ALU.mult)
                                k.tt("dve", tm[2][:, 0:n], x1[:, 0:n], sn, ALU.mult)
                                k.tt("dve", tm[3][:, 0:n], x2[:, 0:n], cs, ALU.mult)
                                k.tt("pool", o1, tm[0][:, 0:n], tm[1][:, 0:n], ALU.subtract)
                                k.tt("pool", o2, tm[2][:, 0:n], tm[3][:, 0:n], ALU.add)
                            for (o, d) in ((o1, d1), (o2, d2)):
                                if isq:
                                    k.copy("act", qT[:, d, 0:n], o)
                                    o3 = o.rearrange("p (s i) -> p s i", i=128)
                                    k.tt("dve", qf[:, d, 0:n].rearrange("p (s i) -> p s i", i=128), o3,
                                         DF[:, h:h + 1, :].broadcast_to([128, nt, 128]), ALU.mult)
                                    k.tt("dve", qb[:, d, 0:n].rearrange("p (s i) -> p s i", i=128), o3,
                                         DB[:, h:h + 1, :].broadcast_to([128, nt, 128]), ALU.mult)
                                else:
                                    k.copy("act", kT[:, d, 0:n], o)
                        for s_ in range(nt if part == 1 else 0):
                            pa, pb = PS[4], PS[5]
                            for c in range(8):
                                pp = pa if c < 4 else pb
                                k.tr(pp[:, (c % 4) * 128:(c % 4 + 1) * 128], k32[:, c, s_ * 128:(s_ + 1) * 128], self.ident[:])
                            for h in range(4):
                                pp = pa if h < 2 else pb
                                sl = pp[:, (h % 2) * 256:(h % 2 + 1) * 256]
                                k.act(kfb[:, s_, h * 256:(h + 1) * 256], sl, AF.Identity, scale=KFd[:, h:h + 1])
                                k.ts("dve", kbb[:, s_, h * 256:(h + 1) * 256], sl, KBd[:, h:h + 1], None, ALU.mult)
                        for (buf, dr, qe) in (((qT, self.QT, "act"), (qf, self.QTF, "pool"), (qb, self.QTB, "pool")) if part == 0 else ((kT, self.KT, "act"),)):
                            k.dma(qe, dr[b, :, t0:t0 + n].rearrange("(k p) t -> p k t", p=128), buf[:, :, 0:n], store=True,
                                  w=[(_key(dr), b, t0)])
                        for (buf, dr, qe) in (((kfb, self.KF, "act"), (kbb, self.KB, "pool")) if part == 1 else ()):
                            k.dma(qe, dr[b, t0:t0 + n, :].rearrange("(s p) d -> p s d", p=128), buf[:, 0:nt, :], store=True,
                                  w=[(_key(dr), b, t0)])
                k.S.barrier()
            with contextlib.ExitStack() as es:
                wvg = k.sb(es, "r1_wvg", [128, 8, 4 * D], BF16)
                k.dma("pool", wvg[:], I["ret_w_in"][j][:, 2 * D:6 * D].rearrange("(k p) n -> p k n", p=128))
                stg = k.sb(es, "r1b_stg", [128, 4, D])
                uT = [k.sb(es, f"r1b_uT{x}", [128, 8, 512], BF16) for x in range(2)]
                vt = [k.sb(es, "r1b_vt0", [128, 4, 2 * D], BF16)] * 2
                gt = [k.sb(es, "r1b_gt0", [128, 4, 2 * D], BF16)] * 2
                it = 0
                for b in range(NBC):
                    for (bb, t0, n, r) in tiles_of(b, True):
                        nt = n // 128
                        u, vv, gg = uT[it % 2], vt[it % 2], gt[it % 2]
                        it += 1
                        self.prologue(src, b, t0, n, r, 0, stg, u, [PS[6], PS[7]])
                        pi = 0
                        for s_ in range(nt):
                            for nb in range(8):
                                ps = PS[pi % 6]
                                pi += 1
                                for kk in range(8):
                                    k.mm(ps[:], u[:, kk, s_ * 128:(s_ + 1) * 128], wvg[:, kk, nb * 512:(nb + 1) * 512], kk == 0, kk == 7)
                                if nb < 4:
                                    k.copy("act", vv[:, s_, nb * 512:(nb + 1) * 512], ps[:])
                                else:
                                    k.act(gg[:, s_, (nb - 4) * 512:(nb - 3) * 512], ps[:], AF.Silu)
                        k.dma("act", self.RV[b, t0:t0 + n, :].rearrange("(s p) d -> p s d", p=128), vv[:, 0:nt, :], store=True, w=[("RV", b, t0)])
                        k.dma("act", self.RG[b, t0:t0 + n, :].rearrange("(s p) d -> p s d", p=128), gg[:, 0:nt, :], store=True, w=[("RG", b, t0)])
                k.S.barrier()
            with contextlib.ExitStack() as es:
                Sb = k.sb(es, "r2_S", [128, 8, 512])
                sbf = [k.sb(es, f"r2_sbf{x}", [128, 8 * 512], BF16) for x in range(2)]
                kbc = [k.sb(es, f"r2_kb{x}", [128, D], BF16) for x in range(2)]
                vc = [k.sb(es, f"r2_v{x}", [128, 2 * D], BF16) for x in range(2)]
                it = 0
                for b in range(NBC):
                    for h in range(4):
                        for a in range(2):
                            k.memset("dve", Sb[:, h * 2 + a, :], 0.0, w=[(_key(Sb), h, a)])
                    order = [1, 0] + list(range(NCH - 1, 1, -1))
                    k.memset("pool", sbf[it % 2][:], 0.0)
                    for oi, g in enumerate(order):
                        sf, kb_, v_ = sbf[it % 2], kbc[it % 2], vc[it % 2]
                        sfn = sbf[(it + 1) % 2]
                        it += 1
                        k.dma("act", self.SBD[b, g], sf[:], store=True, w=[("SBD", b, g)])
                        if oi == len(order) - 1:
                            break
                        k.dma("sp", kb_[:], self.KB[b, g * 128:(g + 1) * 128, :], r=[("KBall",)])
                        k.dma("sp", v_[:], self.RV[b, g * 128:(g + 1) * 128, :], r=[("RVall",)])
                        for h in range(4):
                            for a in range(2):
                                ps = PS[(h * 2 + a) % 8]
                                k.mm(ps[:], kb_[:, h * 256 + a * 128:h * 256 + (a + 1) * 128], v_[:, h * 512:(h + 1) * 512], True, True)
                                k.stt(Sb[:, h * 2 + a, :], Sb[:, h * 2 + a, :], GL[:, 4 + h:5 + h], ps[:], ALU.mult, ALU.add,
                                      r=[(_key(Sb), h, a), GL, ps], w=[(_key(Sb), h, a)])
                                k.copy("act", sfn[:, (h * 2 + a) * 512:(h * 2 + a + 1) * 512], Sb[:, h * 2 + a, :],
                                       r=[(_key(Sb), h, a)], w=[sfn])
                k.S.barrier()
            with contextlib.ExitStack() as es:
                wout = k.sb(es, "r3_wout", [128, 16, D], BF16)
                k.dma("pool", wout[:], I["ret_w_out"][j].rearrange("(k p) n -> p k n", p=128))
                gng = k.sb(es, "r3_gng", [128, 2 * D])
                k.dma("sp", gng[:], I["ret_gn_g"][j].partition_broadcast(128))
                Sf = k.sb(es, "r3_Sf", [128, 8, 512])
                Sfb = k.sb(es, "r3_Sfb", [128, 8, 512], BF16)
                L = []
                for x in range(2):
                    L.append(dict(
                        qT=k.sb(es, f"r3_qT{x}", [128, 8, 128], BF16), kT=k.sb(es, f"r3_kT{x}", [128, 8, 128], BF16),
                        qf=k.sb(es, f"r3_qf{x}", [128, 8, 128], BF16), qb=k.sb(es, f"r3_qb{x}", [128, 8, 128], BF16),
                        kf=k.sb(es, f"r3_kf{x}", [128, D], BF16), v=k.sb(es, f"r3_v{x}", [128, 2 * D], BF16),
                        g=(k.sb(es, f"r3_g{x}", [128, 2 * D], BF16) if x == 0 else None),
                        sb=(k.sb(es, f"r3_sb{x}", [128, 8, 512], BF16) if x == 0 else None),
                        h=k.sb(es, f"r3_h{x}", [128, 1, D])))
                L[1]["sb"] = L[0]["sb"]
                L[1]["g"] = L[0]["g"]
                z32 = k.sb(es, "r3_z32", [128, 2 * D])
                zT = k.sb(es, "r3_zT", [128, 16, 128], BF16)
                on = [k.sb(es, f"r3_on{x}", [128, 512]) for x in range(2)]
                Pm = [k.sb(es, f"r3_P{x}", [128, 128], BF16) for x in range(2)]
                gst = k.sb(es, "r3_gst", [128, 4, 12])
                gmv = k.sb(es, "r3_gmv", [128, 4, 2])
                grs = k.sb(es, "r3_grs", [128, 8])
                zb = [k.sb(es, f"r3_zb{x}", [128, 1, D]) for x in range(2)]
                st = [k.sb(es, f"r3_st{x}", [128, 1, 12]) for x in range(2)]
                mv = [k.sb(es, f"r3_mv{x}", [128, 1, 2]) for x in range(2)]
                rs = [k.sb(es, f"r3_rs{x}", [128, 8]) for x in range(2)]
                z32s = [z32, k.sb(es, "r3_z32b", [128, 2 * D])]
                seq = [(b, g) for b in range(NBC) for g in range(NCH)]

                def loads(ci):
                    b, g = seq[ci]
                    B_ = L[ci % 2]
                    isctx = g < 2
                    want_out = (not isctx) or need_ctx
                    cs = slice(g * 128, (g + 1) * 128)
                    k.dma("sp", B_["kf"][:], self.KF[b, cs, :], r=[("KFall",)])
                    k.dma("sp", B_["v"][:], self.RV[b, cs, :], r=[("RVall",)])
                    if want_out:
                        for nm, dr in (("qT", self.QT), ("kT", self.KT), ("qf", self.QTF), ("qb", self.QTB)):
                            k.dma("sp", B_[nm][:], dr[b, :, cs].rearrange("(k p) t -> p k t", p=128), r=[(nm + "all",)])
                        k.dma("sp", B_["h"][:, 0, :], src[b, cs, :], r=[(_key(src), b, (g * 128 // 512) * 512 if not isctx else 0)])
                        k.dma("sp", B_["g"][:], self.RG[b, cs, :], r=[("RGall",)])
                        k.dma("sp", B_["sb"][:].rearrange("p a c -> p (a c)"), self.SBD[b, g], r=[("SBDall",)])

                def front(ci):
                    b, g = seq[ci]
                    B_ = L[ci % 2]
                    zz = z32s[ci % 2]
                    isctx = g < 2
                    want_out = (not isctx) or need_ctx
                    if g == 0:
                        k.memset("dve", Sf[:], 0.0)
                        k.memset("pool", Sfb[:], 0.0)
                    for h in range(4):
                        vh = B_["v"][:, h * 512:(h + 1) * 512]
                        if want_out:
                            sc = PS[0]
                            for a in range(2):
                                k.mm(sc[:, 0:128], B_["kT"][:, 2 * h + a, :], B_["qT"][:, 2 * h + a, :], a == 0, a == 1)
                        for a in range(2):
                            k.mm(PS[3 + a][:], B_["kf"][:, h * 256 + a * 128:h * 256 + (a + 1) * 128], vh, True, True)
                        if want_out:
                            P_ = Pm[h % 2]
                            k.tt("dve", P_[:], sc[:, 0:128], MT[:, h, :], ALU.mult)
                            O = PS[1 + h % 2]
                            k.mm(O[:], P_[:], vh, True, False)
                            for a in range(2):
                                k.mm(O[:], B_["qf"][:, 2 * h + a, :], Sfb[:, 2 * h + a, :], False, False)
                            for a in range(2):
                                k.mm(O[:], B_["qb"][:, 2 * h + a, :], B_["sb"][:, 2 * h + a, :], False, a == 1)
                        for a in range(2):
                            ps = PS[3 + a]
                            k.stt(Sf[:, 2 * h + a, :], Sf[:, 2 * h + a, :], GL[:, h:h + 1], ps[:], ALU.mult, ALU.add)
                            k.copy("act", Sfb[:, 2 * h + a, :], Sf[:, 2 * h + a, :])
                        if want_out:
                            k.fn("dve", (lambda e, h=h, O=O: e.bn_stats(out=gst[:, h, 0:6], in_=O[:])), [O], [gst])
                            k.fn("dve", (lambda e, h=h: e.bn_aggr(out=gmv[:, h, :], in_=gst[:, h, 0:6])), [gst], [gmv])
                            k.ts("pool", grs[:, h:h + 1], gmv[:, h, 1:2], LN_EPS, None, ALU.add)
                            k.tt("pool", grs[:, h:h + 1], grs[:, h:h + 1], self.negh[:, 0:1], ALU.pow)
                            o_ = on[h % 2]
                            k.ts("dve", o_[:], O[:], gmv[:, h, 0:1], grs[:, h:h + 1], ALU.subtract, ALU.mult)
                            k.tt("pool", o_[:], o_[:], gng[:, h * 512:(h + 1) * 512], ALU.mult)
                            k.tt("pool", zz[:, h * 512:(h + 1) * 512], o_[:], B_["g"][:, h * 512:(h + 1) * 512], ALU.mult,
                                 w=[(_key(zz), h)])

                def back(ci):
                    b, g = seq[ci]
                    B_ = L[ci % 2]
                    zz = z32s[ci % 2]
                    isctx = g < 2
                    want_out = (not isctx) or need_ctx
                    if not want_out:
                        return
                    r = 2 if isctx else b
                    cs = slice(g * 128, (g + 1) * 128)
                    for c in range(16):
                        pp = PS[5]
                        k.tr(pp[:, (c % 4) * 128:(c % 4 + 1) * 128], zz[:, c * 128:(c + 1) * 128], self.ident[:],
                             r=[(_key(zz), c // 4), self.ident])
                        if c % 4 == 3:
                            k.copy("act", zT[:, c - 3:c + 1, :], pp[:].rearrange("p (c i) -> p c i", i=128))
                    for half in range(2):
                        y = PS[6 + half]
                        for c in range(16):
                            k.mm(y[:], zT[:, c, :], wout[:, c, half * 512:(half + 1) * 512], c == 0, c == 15)
                    t0 = 0 if isctx else (g * 128 // 512) * 512
                    self.ln_epilogue(lambda t, half: PS[6 + half][:], B_["h"], 1, r, 0, 0, zb, st, mv, rs,
                                     dst[b, cs, :], (_key(dst), b, t0, g))
                loads(0)
                front(0)
                for ci in range(len(seq)):
                    if ci + 1 < len(seq):
                        loads(ci + 1)
                        front(ci + 1)
                    back(ci)
                k.S.barrier()

    def mla_layer(self, i, src, dst):
        k = self.k
        I = self.I
        PS = self.PS
        need_ctx = i < DEPTH - 1
        NKT = TT // 128
        with contextlib.ExitStack() as es:
            wd = k.sb(es, "m1_wd", [128, 8, 768], BF16)
            wuq = k.sb(es, "m1_wuq", [128, 3, 2048], BF16)
            wukv = k.sb(es, "m1_wukv", [128, 2, 2048], BF16)
            k.dma("pool", wd[:], I["mla_wd"].rearrange("(k p) n -> p k n", p=128))
            k.dma("pool", wuq[:], I["mla_wuq"].rearrange("(k p) n -> p k n", p=128))
            k.dma("pool", wukv[:], I["mla_wukv"].rearrange("(k p) n -> p k n", p=128))
            C1 = k.sb(es, "m1_c1", [128, SEQ])
            C2 = k.sb(es, "m1_c2", [128, SEQ])
            k.dma("sp", C1[:], I["mla_c1"])
            k.dma("sp", C2[:], I["mla_c2"])
            nraw = k.sb(es, "m1_nraw", [5, 128])
            npp = k.sb(es, "m1_npp", [128, 5])
            k.dma("sp", nraw[:], I["mla_norms"])
            k.tr(PS[7][:, 0:5], nraw[:, :], self.ident[0:5, 0:5])
            k.copy("dve", npp[:], PS[7][:, 0:5])
            stg = k.sb(es, "m1_stg", [128, 4, D])
            u = k.sb(es, "m1_uT", [128, 8, 512], BF16)
            d32 = k.sb(es, "m1_d32", [128, 5, 512])
            sq = [k.sb(es, f"m1_sq{x}", [128, 512]) for x in range(2)]
            rr = k.sb(es, "m1_rr", [128, 2, 512])
            dn = k.sb(es, "m1_dn", [128, 5, 512], BF16)
            qnb = k.sb(es, "m1_qnb", [128, 8, 512], BF16)
            qrb = k.sb(es, "m1_qrb", [128, 4, 512], BF16)
            knb = k.sb(es, "m1_knb", [128, 8, 512], BF16)
            vb = k.sb(es, "m1_vb", [128, 4, D], BF16)
            krb = k.sb(es, "m1_krb", [64, 512], BF16)
            tm = [k.sb(es, f"m1_tm{x}", [128, 512]) for x in range(2)]
            for b in range(NBC):
                for (bb, t0, n, r) in tiles_of(b, True):
                    isctx = (r == 2)
                    c0 = t0 - CTX
                    nt = n // 128
                    self.prologue(src, b, t0, n, r, 0, stg, u, [PS[6], PS[7]])
                    for c in range(5):
                        ps = PS[c]
                        for kk in range(8):
                            k.mm(ps[:, 0:n], wd[:, kk, c * 128:(c + 1) * 128], u[:, kk, 0:n], kk == 0, kk == 7)
                        k.copy("act", d32[:, c, 0:n], ps[:, 0:n])
                    for (x, ps) in ((0, PS[5]), (1, PS[6])):
                        for kk in range(8):
                            k.mm(ps[0:64, 0:n], wd[:, kk, 640 + 64 * x:704 + 64 * x], u[:, kk, 0:n], kk == 0, kk == 7)
                    if isctx:
                        k.copy("act", krb[:, 0:n], PS[5][0:64, 0:n])
                    else:
                        k.tt("dve", tm[0][0:64, 0:n], PS[5][0:64, 0:n], C1[0:64, c0:c0 + n], ALU.mult)
                        k.tt("dve", tm[1][0:64, 0:n], PS[6][0:64, 0:n], C2[0:64, c0:c0 + n], ALU.mult)
                        k.tt("pool", krb[:, 0:n], tm[0][0:64, 0:n], tm[1][0:64, 0:n], ALU.add)
                    k.dma("pool", self.KR[b, :, t0:t0 + n], krb[:, 0:n], store=True, w=[("KR", b, t0)])
                    for (gi_, cl, dim) in ((0, (0, 1, 2), 384.0), (1, (3, 4), 256.0)):
                        ps = PS[7]
                        for ci, c in enumerate(cl):
                            sqq = sq[ci % 2]
                            k.tt("dve", sqq[:, 0:n], d32[:, c, 0:n], d32[:, c, 0:n], ALU.mult)
                            k.mm(ps[:, 0:n], self.onesf[:], sqq[:, 0:n], ci == 0, ci == len(cl) - 1)
                        k.ts("dve", rr[:, gi_, 0:n], ps[:, 0:n], 1.0 / dim, RMS_EPS, ALU.mult, ALU.add)
                        k.act(rr[:, gi_, 0:n], rr[:, gi_, 0:n], AF.Ln)
                        k.act(rr[:, gi_, 0:n], rr[:, gi_, 0:n], AF.Exp, scale=-0.5)
                        for c in cl:
                            k.stt(dn[:, c, 0:n], d32[:, c, 0:n], npp[:, c:c + 1], rr[:, gi_, 0:n], ALU.mult, ALU.mult)
                    for h in range(8):
                        ps = PS[h % 4]
                        for c in range(3):
                            k.mm(ps[:, 0:n], wuq[:, c, h * 128:(h + 1) * 128], dn[:, c, 0:n], c == 0, c == 2)
                        k.copy("act", qnb[:, h, 0:n], ps[:, 0:n])
                    k.dma("act", self.QN[b, :, :, t0:t0 + n].rearrange("h p t -> p h t"), qnb[:, :, 0:n], store=True, w=[("QN", b, t0)])
                    for hp in range(4):
                        pa, pb = PS[4 + (2 * hp) % 2], PS[4 + (2 * hp + 1) % 2]
                        for c in range(3):
                            k.mm(pa[:, 0:n], wuq[:, c, 1024 + hp * 128:1024 + (hp + 1) * 128], dn[:, c, 0:n], c == 0, c == 2)
                        if isctx:
                            k.copy("act", qrb[:, hp, 0:n], pa[:, 0:n])
                        else:
                            for c in range(3):
                                k.mm(pb[:, 0:n], wuq[:, c, 1536 + hp * 128:1536 + (hp + 1) * 128], dn[:, c, 0:n], c == 0, c == 2)
                            k.tt("dve", tm[0][:, 0:n], pa[:, 0:n], C1[:, c0:c0 + n], ALU.mult)
                            k.tt("dve", tm[1][:, 0:n], pb[:, 0:n], C2[:, c0:c0 + n], ALU.mult)
                            k.tt("pool", qrb[:, hp, 0:n], tm[0][:, 0:n], tm[1][:, 0:n], ALU.add)
                    k.dma("pool", self.QR[b, :, :, t0:t0 + n].rearrange("h p t -> p h t"), qrb[:, :, 0:n], store=True, w=[("QR", b, t0)])
                    for h in range(8):
                        ps = PS[h % 4]
                        for c in range(2):
                            k.mm(ps[:, 0:n], wukv[:, c, h * 128:(h + 1) * 128], dn[:, 3 + c, 0:n], c == 0, c == 1)
                        k.copy("act", knb[:, h, 0:n], ps[:, 0:n])
                    k.dma("act", self.KN[b, :, :, t0:t0 + n].rearrange("h p t -> p h t"), knb[:, :, 0:n], store=True, w=[("KN", b, t0)])
                    for s_ in range(nt):
                        for half in range(2):
                            ps = PS[4 + half]
                            for c in range(2):
                                k.mm(ps[:], dn[:, 3 + c, s_ * 128:(s_ + 1) * 128], wukv[:, c, 1024 + half * 512:1024 + (half + 1) * 512], c == 0, c == 1)
                            k.copy("act", vb[:, s_, half * 512:(half + 1) * 512], ps[:])
                    k.dma("act", self.MV[b, t0:t0 + n, :].rearrange("(s p) d -> p s d", p=128), vb[:, 0:nt, :], store=True, w=[("MV", b, t0)])
            k.S.barrier()
        with contextlib.ExitStack() as es:
            kra = k.sb(es, "m2_kr", [64, TT], BF16)
            knh = [k.sb(es, f"m2_kn{x}", [128, TT], BF16) for x in range(2)]
            vh = [k.sb(es, f"m2_v{x}", [128, NKT, 128], BF16) for x in range(2)]
            qn = [k.sb(es, f"m2_qn{x}", [128, 512], BF16) for x in range(2)]
            qr = [k.sb(es, f"m2_qr{x}", [64, 512], BF16) for x in range(2)]
            PT = [k.sb(es, f"m2_PT{x}", [128, 512], BF16) for x in range(3)]
            rec = [k.sb(es, f"m2_rec{x}", [128, 512]) for x in range(2)]
            otb = [k.sb(es, f"m2_ot{x}", [128, 512], BF16) for x in range(2)]
            dac = [k.sb(es, f"m2_dac{x}", [128, 512]) for x in range(2)]
            scale = float(192.0 ** -0.5)
            it = 0
            ih = 0
            for b in range(NBC):
                k.dma("sp", kra[:], self.KR[b], r=[("KRall",)])
                for h in range(8):
                    kn_, v_ = knh[ih % 2], vh[ih % 2]
                    ih += 1
                    k.dma("sp", kn_[:], self.KN[b, h], r=[("KNall",)])
                    k.dma("sp", v_[:], self.MV[b, :, h * 128:(h + 1) * 128].rearrange("(t p) d -> p t d", p=128), r=[("MVall",)])
                    for (bb, t0, n, r) in tiles_of(b, need_ctx):
                        isctx = (r == 2)
                        kts = [0, 1] if isctx else list(range(NKT))
                        q_, r_ = qn[it % 2], qr[it % 2]
                        O, DEN = PS[4 + it % 2], PS[6 + it % 2]
                        rc, ot = rec[it % 2], otb[it % 2]
                        dacc = dac[it % 2]
                        it += 1
                        k.dma("sp", q_[:, 0:n], self.QN[b, h, :, t0:t0 + n], r=[("QNall",)])
                        k.dma("sp", r_[:, 0:n], self.QR[b, h // 2, (h % 2) * 64:(h % 2) * 64 + 64, t0:t0 + n], r=[("QRall",)])

                        def scores(idx):
                            kt = kts[idx]
                            sc = PS[idx % 4]
                            k.mm(sc[:, 0:n], kn_[:, kt * 128:(kt + 1) * 128], q_[:, 0:n], True, False)
                            k.mm(sc[:, 0:n], kra[:, kt * 128:(kt + 1) * 128], r_[:, 0:n], False, True)
                        scores(0)
                        for idx, kt in enumerate(kts):
                            if idx + 1 < len(kts):
                                scores(idx + 1)
                            p_ = PT[idx % 3]
                            k.act(p_[:, 0:n], PS[idx % 4][:, 0:n], AF.Exp, scale=scale)
                            k.mm(O[:, 0:n], v_[:, kt, :], p_[:, 0:n], idx == 0, idx == len(kts) - 1)
                            k.mm(DEN[:, 0:n], self.onesb[:], p_[:, 0:n], idx == 0, idx == len(kts) - 1)
                        k.fn("dve", (lambda e, rc=rc, DEN=DEN, n=n: e.reciprocal(out=rc[:, 0:n], in_=DEN[:, 0:n])), [DEN], [rc])
                        k.tt("dve", ot[:, 0:n], O[:, 0:n], rc[:, 0:n], ALU.mult)
                        k.dma("pool", self.OT[b, h * 128:(h + 1) * 128, t0:t0 + n], ot[:, 0:n], store=True, w=[("OT", b, h, t0)])
            k.S.barrier()
        with contextlib.ExitStack() as es:
            wout = k.sb(es, "m3_wout", [128, 8, D], BF16)
            k.dma("pool", wout[:], I["mla_w_out"].rearrange("(k p) n -> p k n", p=128))
            stg = [k.sb(es, f"m3_stg{x}", [128, 4, D]) for x in range(2)]
            oT = [k.sb(es, f"m3_oT{x}", [128, 8, 512], BF16) for x in range(2)]
            zb = [k.sb(es, f"m3_zb{x}", [128, 4, D]) for x in range(2)]
            st = [k.sb(es, f"m3_st{x}", [128, 4, 12]) for x in range(2)]
            mv = [k.sb(es, f"m3_mv{x}", [128, 4, 2]) for x in range(2)]
            rs = [k.sb(es, f"m3_rs{x}", [128, 8]) for x in range(2)]
            it = 0
            for b in range(NBC):
                for (bb, t0, n, r) in tiles_of(b, need_ctx):
                    nt = n // 128
                    sg, o_ = stg[it % 2], oT[it % 2]
                    it += 1
                    k.dma("sp", sg[:, 0:nt, :], src[b, t0:t0 + n, :].rearrange("(t p) d -> p t d", p=128), r=[(_key(src), b, t0)])
                    k.dma("sp", o_[:, :, 0:n], self.OT[b, :, t0:t0 + n].rearrange("(k p) t -> p k t", p=128), r=[("OTall",)])
                    for t in range(nt):
                        for half in range(2):
                            ps = PS[(t * 2 + half) % 8]
                            for kk in range(8):
                                k.mm(ps[:], o_[:, kk, t * 128:(t + 1) * 128], wout[:, kk, half * 512:(half + 1) * 512], kk == 0, kk == 7)
                    self.ln_epilogue(lambda t, half: PS[(t * 2 + half) % 8][:], sg, nt, r, 0, 0, zb, st, mv, rs,
                                     dst[b, t0:t0 + n, :], (_key(dst), b, t0))
            k.S.barrier()

    def moe_layer_dense(self, i, src, dst, last, dbg_ns=None, dbg_ne=NE):
        k = self.k
        I = self.I
        PS = self.PS
        need_ctx = not last
        i = self.li[i]
        alltiles = []
        for b in range(NBC):
            alltiles += tiles_of(b, need_ctx)
        NS = 2
        with contextlib.ExitStack() as es:
            wr = k.sb(es, "mo_wr", [128, 8, 64])
            rbg = k.sb(es, "mo_rbg", [128, 4])
            rbe = k.sb(es, "mo_rbe", [128, 32])
            k.dma("sp", wr[:], I["moe_wr"][i].rearrange("(k p) n -> p k n", p=128))
            k.dma("sp", rbg[:], I["moe_bg"][i].partition_broadcast(128))
            k.dma("sp", rbe[:], I["moe_be"][i].partition_broadcast(128))
            vT = k.sb(es, "mo_vT", [128, 8, NS * 512], BF16)
            acc = [[k.sb(es, f"mo_acc{s}_{h}", [128, 512]) for h in range(2)] for s in range(NS * 4)]
            G = k.sb(es, "mo_G", [128, NS * 4, 32])
            stg = k.sb(es, "mo_stg", [128, 4, D])
            w1b = [k.sb(es, f"mo_w1_{j}", [128, 8, 512], BF16) for j in range(2)]
            w3b = [k.sb(es, f"mo_w3_{j}", [128, 8, 512], BF16) for j in range(2)]
            w2b = [k.sb(es, f"mo_w2_{j}", [128, 4, D], BF16) for j in range(2)]
            sil = [k.sb(es, f"mo_sil{j}", [128, 512]) for j in range(2)]
            hdn = [k.sb(es, f"mo_hdn{j}", [128, 4, 512], BF16) for j in range(2)]
            rt = k.sb(es, "mo_rt", [128, 64])
            r8 = k.sb(es, "mo_r8", [128, 8])
            rsm = k.sb(es, "mo_rsm", [128, 16])
            zb = k.sb(es, "mo_zb", [128, 4, D])
            v32 = zb[:].rearrange("p a (b c) -> p (a b) c", c=512)
            st = k.sb(es, "mo_st", [128, 4, 12])
            mv = k.sb(es, "mo_mv", [128, 4, 2])
            rs = k.sb(es, "mo_rs", [128, 8])
            wi = 0
            for s0 in range(0, len(alltiles) if dbg_ns is None else dbg_ns * NS, NS):
                tl = alltiles[s0:s0 + NS]
                subs = []
                for ti, (b, t0, n, r) in enumerate(tl):
                    self.prologue(src, b, t0, n, r, 2, stg, vT[:, :, ti * 512:(ti + 1) * 512], [PS[6], PS[7]], v32=v32)
                    for t in range(n // 128):
                        si = ti * 4 + t
                        subs.append((ti, t, ti * 512 + t * 128, si))
                        lg = PS[5]
                        for kk in range(8):
                            k.mm(lg[:, 0:64], v32[:, kk, t * 128:(t + 1) * 128], wr[:, kk, :], kk == 0, kk == 7)
                        self.route(lg, rbg, rbe, rt, r8, rsm, G[:, si, :])
                for e in range(dbg_ne):
                    wb1, wb3, wb2 = w1b[wi % 2], w3b[wi % 2], w2b[wi % 2]
                    wi += 1
                    k.dma("pool", wb1[:], I["moe_w1"][i, e].rearrange("(k p) n -> p k n", p=128))
                    k.dma("pool", wb3[:], I["moe_w3"][i, e].rearrange("(k p) n -> p k n", p=128))
                    k.dma("pool", wb2[:], I["moe_w2"][i, e].rearrange("(k p) n -> p k n", p=128))
                    for ti, (b, t0, n, r) in enumerate(tl):
                        hb = hdn[ti % 2]
                        cs = ti * 512
                        for j in range(4):
                            pa, pb = PS[(2 * j) % 4], PS[(2 * j + 1) % 4]
                            for kk in range(8):
                                k.mm(pa[:, 0:n], wb1[:, kk, j * 128:(j + 1) * 128], vT[:, kk, cs:cs + n], kk == 0, kk == 7)
                            for kk in range(8):
                                k.mm(pb[:, 0:n], wb3[:, kk, j * 128:(j + 1) * 128], vT[:, kk, cs:cs + n], kk == 0, kk == 7)
                            sl = sil[j % 2]
                            k.act(sl[:, 0:n], pa[:, 0:n], AF.Silu)
                            k.tt("dve", hb[:, j, 0:n], sl[:, 0:n], pb[:, 0:n], ALU.mult, w=[(_key(hb), j)])
                        for t in range(n // 128):
                            si = ti * 4 + t
                            for half in range(2):
                                po = PS[4 + (t * 2 + half) % 2]
                                for j in range(4):
                                    k.mm(po[:], hb[:, j, t * 128:(t + 1) * 128], wb2[:, j, half * 512:(half + 1) * 512], j == 0, j == 3,
                                         r=[(_key(hb), j), wb2])
                                a = acc[si][half][:]
                                if e == 0:
                                    k.ts("dve", a, po[:], G[:, si, e:e + 1], None, ALU.mult)
                                else:
                                    k.stt(a, po[:], G[:, si, e:e + 1], a, ALU.mult, ALU.add)
                for ti, (b, t0, n, r) in enumerate(tl):
                    nt = n // 128
                    k.dma("sp", stg[:, 0:nt, :], src[b, t0:t0 + n, :].rearrange("(t p) d -> p t d", p=128), r=[(_key(src), b, t0)])
                    if last:
                        drows = self.out[b, t0 - CTX:t0 - CTX + n, :]
                        dkey = ("out", b, t0)
                    else:
                        drows = dst[b, t0:t0 + n, :]
                        dkey = (_key(dst), b, t0)
                    self.ln_epilogue(lambda t, half, ti=ti: acc[ti * 4 + t][half][:], stg, nt, r, 1, 1,
                                     zb, st, mv, rs, drows, dkey)
            k.S.barrier()

    def moe_layer(self, i, src, dst, last, dbg_ns=None, dbg_ne=NE):
        k = self.k
        I = self.I
        PS = self.PS
        need_ctx = not last
        li = self.li[i]
        alltiles = []
        for b in range(NBC):
            alltiles += tiles_of(b, need_ctx)
        nsub_all = sum(n // 128 for (_, _, n, _) in alltiles)
        NBLK = (2 * nsub_all * 128 + 511) // 512 + NE
        NSUB = nsub_all
        XS, YS = self.XS, self.YS
        I32 = mybir.dt.int32
        with contextlib.ExitStack() as mes:
            GT = k.sb(mes, "ms_GT", [128, NSUB, 2])
            D0 = k.sb(mes, "ms_D0", [128, NSUB], I32)
            D1 = k.sb(mes, "ms_D1", [128, NSUB], I32)
            WI = k.sb(mes, "ms_WI", [128, NBLK], I32)
            ves = contextlib.ExitStack()
            VB = k.sb(ves, "ms_VB", [128, 3, 2, D])
            with contextlib.ExitStack() as es:
                wblk = [k.sb(es, f"mv_w{x}", [128, 8, 1024]) for x in range(2)]
                brow = k.sb(es, "mv_brow", [1, 6 * D])
                REP = k.sb(es, "mv_REP", [128, 3, 8, 128])
                for r in range(3):
                    for kk in range(8):
                        k.copy("dve", REP[:, r, kk, :], self.actT[:, kk, r:r + 1].broadcast_to([128, 128]))
                k.dma("sp", brow[:], I["ada_b"][li])
                pi = 0
                for wi in (3, 4):
                    buf = wblk[wi % 2]
                    k.dma("sp", buf[:], I["ada_w"][li][:, wi * 1024:(wi + 1) * 1024].rearrange("(k p) n -> p k n", p=128))
                    for r in range(3):
                        for half in range(2):
                            ps = PS[pi % 8]
                            pi += 1
                            for ki in range(8):
                                k.mm(ps[:], REP[:, r, ki, :], buf[:, ki, half * 512:(half + 1) * 512], ki == 0, False)
                            k.mm(ps[:], self.ones[0:1, 0:128], brow[0:1, wi * 1024 + half * 512: wi * 1024 + (half + 1) * 512], False, True)
                            if wi == 3:
                                k.copy("act", VB[:, r, 0, half * 512:(half + 1) * 512], ps[:])
                            else:
                                k.act(VB[:, r, 1, half * 512:(half + 1) * 512], ps[:], AF.Identity, bias=1.0)
                k.S.barrier()
            with contextlib.ExitStack() as es:
                wr = k.sb(es, "ma_wr", [128, 8, 64])
                rbg = k.sb(es, "ma_rbg", [128, 4])
                rbe = k.sb(es, "ma_rbe", [128, 32])
                k.dma("sp", wr[:], I["moe_wr"][li].rearrange("(k p) n -> p k n", p=128))
                k.dma("sp", rbg[:], I["moe_bg"][li].partition_broadcast(128))
                k.dma("sp", rbe[:], I["moe_be"][li].partition_broadcast(128))
                U = k.sb(es, "ma_U", [128, 128], BF16)
                k.memset("pool", U[:], 1.0)
                k.fn("pool", lambda e: e.affine_select(out=U[:], in_=U[:], pattern=[[1, 128]], compare_op=ALU.is_gt,
                                                       fill=0.0, base=0, channel_multiplier=-1), [U], [U])
                OH1 = k.sb(es, "ms_OH1", [128, NSUB, 32])
                OH2 = k.sb(es, "ms_OH2", [128, NSUB, 32])
                RK = k.sb(es, "ms_RK", [128, NSUB, 32])
                cnt = k.sb(es, "ma_cnt", [128, 32])
                k.memset("dve", cnt[:], 0.0)
                stg = [k.sb(es, f"ma_stg{x}", [128, 4, D]) for x in range(2)]
                vtk = k.sb(es, "ma_vtk", [128, 4, D])
                vtb = [k.sb(es, f"ma_vtb{x}", [128, 4, D], BF16) for x in range(2)]
                v32 = k.sb(es, "ma_v32", [128, 8, 512])
                ind = [k.sb(es, f"ma_ind{x}", [128, 32], BF16) for x in range(2)]
                rt4 = k.sb(es, "ma_rt4", [128, 4, 64])
                r84 = k.sb(es, "ma_r84", [128, 4, 8])
                rsm4 = k.sb(es, "ma_rsm4", [128, 16, 4])
                si = 0
                for ti, (b, t0, n, r) in enumerate(alltiles):
                    nt = n // 128
                    sg = stg[ti % 2]
                    k.dma("sp", sg[:, 0:nt, :], src[b, t0:t0 + n, :].rearrange("(t p) d -> p t d", p=128), r=[(_key(src), b, t0)])
                    for t in range(nt):
                        k.tt("dve", vtk[:, t, :], sg[:, t, :], VB[:, r, 1, :], ALU.mult, w=[(_key(vtk), t)])
                        k.tt("pool", vtk[:, t, :], vtk[:, t, :], VB[:, r, 0, :], ALU.add, r=[(_key(vtk), t), VB], w=[(_key(vtk), t)])
                    vb_ = vtb[ti % 2]
                    for t in range(nt):
                        k.copy("act", vb_[:, t, :], vtk[:, t, :], r=[(_key(vtk), t)], w=[(_key(vb_), t)])
                    k.dma("act", self.VTB[b, t0:t0 + n, :].rearrange("(t p) d -> p t d", p=128), vb_[:, 0:nt, :], store=True,
                          r=[(_key(vb_), t) for t in range(nt)], w=[("VT", b, t0)])
                    for kk in range(8):
                        ps = PS[6 + kk % 2]
                        for t in range(nt):
                            k.tr(ps[:, t * 128:(t + 1) * 128], vtk[:, t, kk * 128:(kk + 1) * 128], self.ident[:],
                                 r=[(_key(vtk), t), self.ident])
                        k.copy("act", v32[:, kk, 0:n], ps[:, 0:n], w=[(_key(v32), kk)])
                    lg = PS[5]
                    for t in range(nt):
                        for kk in range(8):
                            k.mm(lg[:, t * 64:(t + 1) * 64], v32[:, kk, t * 128:(t + 1) * 128], wr[:, kk, :], kk == 0, kk == 7,
                                 r=[(_key(v32), kk), wr])
                    self.route4(lg, nt, rbg, rbe, rt4, r84, rsm4, OH1[:, si:si + nt, :], OH2[:, si:si + nt, :], GT[:, si:si + nt, :])
                    for t in range(nt):
                        id_ = ind[si % 2]
                        k.tt("dve", id_[:], OH1[:, si, :], OH2[:, si, :], ALU.add)
                        pr = PS[4]
                        k.mm(pr[:, 0:32], U[:], id_[:], True, True)
                        k.mm(pr[:, 32:64], self.onesb[:], id_[:], True, True)
                        k.tt("dve", RK[:, si, :], pr[:, 0:32], cnt[:], ALU.add)
                        k.tt("dve", cnt[:], pr[:, 32:64], cnt[:], ALU.add)
                        si += 1
                sm = k.sb(es, "ma_sm", [128, 6, 32])
                k.ts("dve", sm[:, 0, :], cnt[:], 511.0, 1.0 / 512.0, ALU.add, ALU.mult)
                k.ts("dve", sm[:, 0, :], sm[:, 0, :], -0.499, 8388608.0, ALU.add, ALU.add)
                k.ts("dve", sm[:, 0, :], sm[:, 0, :], -8388608.0, 512.0, ALU.add, ALU.mult)
                k.memset("dve", sm[:, 1, :], 1.0)
                k.fn("dve", lambda e: e.tensor_tensor_scan(out=sm[:, 2, :], data0=sm[:, 1, :], data1=sm[:, 0, :], initial=0.0,
                                                          op0=ALU.mult, op1=ALU.add), [sm], [sm])
                k.tt("dve", sm[:, 3, :], sm[:, 2, :], sm[:, 0, :], ALU.subtract)
                jv = k.sb(es, "ma_jv", [128, NBLK, 32])
                k.fn("pool", lambda e: e.iota(jv[:], pattern=[[512, NBLK], [0, 32]], base=0, channel_multiplier=0,
                                              allow_small_or_imprecise_dtypes=True), [], [jv])
                k.tt("dve", jv[:], jv[:], sm[:, 2:3, :].broadcast_to([128, NBLK, 32]), ALU.is_ge)
                eb = k.sb(es, "ma_eb", [128, NBLK])
                k.fn("dve", lambda e: e.tensor_reduce(out=eb[:], in_=jv[:], axis=mybir.AxisListType.X, op=ALU.add), [jv], [eb])
                pid = k.sb(es, "ma_pid", [128, 1])
                k.fn("pool", lambda e: e.iota(pid[:], pattern=[[0, 1]], base=0, channel_multiplier=1,
                                              allow_small_or_imprecise_dtypes=True), [], [pid])
                k.ts("dve", eb[:], eb[:], 31.0, 128.0, ALU.min, ALU.mult)
                k.ts("dve", eb[:], eb[:], pid[:, 0:1], float(li * NE * 128), ALU.add, ALU.add)
                k.copy("dve", WI[:], eb[:])
                k.tt("dve", RK[:], RK[:], sm[:, 3:4, :].broadcast_to([128, NSUB, 32]), ALU.add)
                dd = k.sb(es, "ma_dd", [128, NSUB])
                for (OH, Dd) in ((OH1, D0), (OH2, D1)):
                    k.tt("dve", OH[:], OH[:], RK[:], ALU.mult)
                    k.fn("dve", (lambda e, OH=OH: e.tensor_reduce(out=dd[:], in_=OH[:], axis=mybir.AxisListType.X, op=ALU.add)), [OH], [dd])
                    k.copy("dve", Dd[:], dd[:])
                k.S.barrier()
            with contextlib.ExitStack() as es:
                vtk = [k.sb(es, f"mb_vtk{x}", [128, 4, D], BF16) for x in range(3)]
                si = 0
                for ti, (b, t0, n, r) in enumerate(alltiles):
                    nt = n // 128
                    vt = vtk[ti % 3]
                    k.dma("sp", vt[:, 0:nt, :], self.VTB[b, t0:t0 + n, :].rearrange("(t p) d -> p t d", p=128), r=[("VTall",)])
                    for t in range(nt):
                        for Dd in (D0, D1):
                            k.S.op("pool", (lambda e, vt=vt, t=t, Dd=Dd, si=si: e.indirect_dma_start(
                                out=XS, out_offset=bass.IndirectOffsetOnAxis(ap=Dd[:, si:si + 1], axis=0),
                                in_=vt[:, t, :], in_offset=None)), [vt, Dd], [("XS", si, id(Dd))], dma=_key(vt))
                        si += 1
                k.S.barrier()
            ves.close()
            with contextlib.ExitStack() as es:
                wst = [k.sb(es, f"mc_wst{x}", [128, 4096]) for x in range(3)]
                wb = [[k.sb(es, f"mc_wb{x}_{y}", [128, 4096], BF16) for x in range(3)] for y in range(2)]
                stg = k.sb(es, "mc_stg", [128, 4, D], BF16)
                xT = [k.sb(es, f"mc_xT{x}", [128, 8, 512], BF16) for x in range(2)]
                sil = [k.sb(es, f"mc_sil{x}", [128, 512]) for x in range(2)]
                hdn = [k.sb(es, f"mc_hdn{x}", [128, 4, 512], BF16) for x in range(2)]
                ysb = k.sb(es, "mc_ysb", [128, 4, D])
                wsrc = [I["moe_w1"].rearrange("l e p k n -> (l e p) (k n)"), I["moe_w3"].rearrange("l e p k n -> (l e p) (k n)"),
                        I["moe_w2"].rearrange("l e p k n -> (l e p) (k n)")]
                NB_ = NBLK if dbg_ns is None else dbg_ns

                def gather_w(j):
                    for x in range(3):
                        k.S.op("pool", (lambda e, x=x, j=j: e.indirect_dma_start(
                            out=wst[x][:], out_offset=None, in_=wsrc[x],
                            in_offset=bass.IndirectOffsetOnAxis(ap=WI[:, j:j + 1], axis=0))), [WI], [wst[x]], dma=_key(wst[x]))

                def cast_w(j):
                    wbj = wb[j % 2]
                    k.copy("act", wbj[0][:], wst[0][:])
                    k.copy("pool", wbj[1][:], wst[1][:])
                    k.copy("dve", wbj[2][:], wst[2][:])

                def load_x(j):
                    k.dma("sp", stg[:], XS[j * 512:(j + 1) * 512, :].rearrange("(t p) d -> p t d", p=128), r=[("XSall",)])
                gather_w(0)
                cast_w(0)
                load_x(0)
                for j in range(NB_):
                    wbj = wb[j % 2]
                    w1v = wbj[0][:].rearrange("p (k n) -> p k n", n=512)
                    w3v = wbj[1][:].rearrange("p (k n) -> p k n", n=512)
                    w2v = wbj[2][:].rearrange("p (k n) -> p k n", n=1024)
                    x_ = xT[j % 2]
                    hb = hdn[j % 2]
                    for kk in range(8):
                        psb = PS[6 + kk % 2][:].bitcast(BF16)
                        for t in range(4):
                            k.tr(psb[:, t * 128:(t + 1) * 128], stg[:, t, kk * 128:(kk + 1) * 128], self.identb[:])
                        k.copy("act" if kk % 2 == 0 else "dve", x_[:, kk, :], psb[:, 0:512])
                    if j + 1 < NB_:
                        load_x(j + 1)
                        gather_w(j + 1)
                    for jj in range(4):
                        pa, pb = PS[(2 * jj) % 4], PS[(2 * jj + 1) % 4]
                        for kk in range(8):
                            k.mm(pa[:], w1v[:, kk, jj * 128:(jj + 1) * 128], x_[:, kk, :], kk == 0, kk == 7)
                        for kk in range(8):
                            k.mm(pb[:], w3v[:, kk, jj * 128:(jj + 1) * 128], x_[:, kk, :], kk == 0, kk == 7)
                        sl = sil[jj % 2]
                        k.act(sl[:], pa[:], AF.Silu)
                        k.tt("dve", hb[:, jj, :], sl[:], pb[:], ALU.mult, w=[(_key(hb), jj)])
                    if j + 1 < NB_:
                        cast_w(j + 1)
                    for t in range(4):
                        for half in range(2):
                            po = PS[4 + half]
                            for jj in range(4):
                                k.mm(po[:], hb[:, jj, t * 128:(t + 1) * 128], w2v[:, jj, half * 512:(half + 1) * 512], jj == 0, jj == 3,
                                     r=[(_key(hb), jj), wbj[2]])
                            k.copy("act" if half == 0 else "dve", ysb[:, t, half * 512:(half + 1) * 512], po[:])
                    k.dma("act", YS[j * 512:(j + 1) * 512, :].rearrange("(t p) d -> p t d", p=128), ysb[:], store=True, w=[("YS", j)])
                k.S.barrier()
            with contextlib.ExitStack() as es:
                stg = [k.sb(es, f"md_stg{x}", [128, 4, D]) for x in range(2)]
                fb = [k.sb(es, f"md_f{x}", [128, 4, D]) for x in range(2)]
                zb = [k.sb(es, f"md_zb{x}", [128, 4, D]) for x in range(2)]
                st = [k.sb(es, f"md_st{x}", [128, 4, 12]) for x in range(2)]
                mv = [k.sb(es, f"md_mv{x}", [128, 4, 2]) for x in range(2)]
                rs = [k.sb(es, f"md_rs{x}", [128, 8]) for x in range(2)]
                ybuf = [[k.sb(es, f"md_yb{x}_{y}", [128, D]) for y in range(2)] for x in range(6)]
                sis = []
                acc_ = 0
                for (_, _, n_, _) in alltiles:
                    sis.append(acc_)
                    acc_ += n_ // 128
                yc = [0]

                def pre(ti):
                    b, t0, n, r = alltiles[ti]
                    nt = n // 128
                    sg, f_ = stg[ti % 2], fb[ti % 2]
                    k.dma("sp", sg[:, 0:nt, :], src[b, t0:t0 + n, :].rearrange("(t p) d -> p t d", p=128), r=[(_key(src), b, t0)])
                    for t in range(nt):
                        si = sis[ti] + t
                        a0, a1 = ybuf[yc[0] % 6]
                        yc[0] += 1
                        for (yy, Dd) in ((a0, D0), (a1, D1)):
                            k.S.op("pool", (lambda e, yy=yy, Dd=Dd, si=si: e.indirect_dma_start(
                                out=yy[:], out_offset=None, in_=YS,
                                in_offset=bass.IndirectOffsetOnAxis(ap=Dd[:, si:si + 1], axis=0))), [Dd], [yy], dma=_key(yy))
                        k.act(f_[:, t, :], a0[:], AF.Identity, scale=GT[:, si, 0:1])
                        k.stt(f_[:, t, :], a1[:], GT[:, si, 1:2], f_[:, t, :], ALU.mult, ALU.add)
                ntl = len(alltiles) if dbg_ns is None else 2
                pre(0)
                for ti in range(ntl):
                    b, t0, n, r = alltiles[ti]
                    nt = n // 128
                    sg, f_ = stg[ti % 2], fb[ti % 2]
                    if last:
                        drows = self.out[b, t0 - CTX:t0 - CTX + n, :]
                        dkey = ("out", b, t0)
                    else:
                        drows = dst[b, t0:t0 + n, :]
                        dkey = (_key(dst), b, t0)
                    nxt = (lambda ti=ti: pre(ti + 1)) if ti + 1 < ntl else None
                    self.ln_epilogue(lambda t, half, f_=f_: f_[:, t, half * 512:(half + 1) * 512], sg, nt, r, 1, 1,
                                     zb, st, mv, rs, drows, dkey, mid=nxt)
                k.S.barrier()

    def route4(self, lg4, nt, rbg, rbe, rt, r8, rsm, oh1, oh2, gts):
        k = self.k
        X = mybir.AxisListType.X
        L = lg4[:, 0:nt * 64].rearrange("p (t c) -> p t c", c=64)
        R = rt[:, 0:nt, :]
        k.tt("dve", R[:, :, 0:4], L[:, :, 0:4], rbg[:].unsqueeze(1).broadcast_to([128, nt, 4]), ALU.add, r=[lg4, rbg], w=[rt])
        k.tt("dve", R[:, :, 8:40], L[:, :, 4:36], rbe[:].unsqueeze(1).broadcast_to([128, nt, 32]), ALU.add, r=[lg4, rbe], w=[rt])
        S = lambda j: rsm[:, j, 0:nt]
        k.fn("dve", lambda e: e.tensor_reduce(out=S(0), in_=R[:, :, 0:4], axis=X, op=ALU.max), [rt], [rsm])
        k.tt("dve", R[:, :, 4:8], R[:, :, 0:4], S(0).unsqueeze(2).broadcast_to([128, nt, 4]), ALU.subtract, r=[rt, rsm], w=[rt])
        k.act(R[:, :, 40:44], R[:, :, 4:8], AF.Exp, r=[rt], w=[rt])
        k.fn("dve", lambda e: e.tensor_reduce(out=S(1), in_=R[:, :, 40:44], axis=X, op=ALU.add), [rt], [rsm])
        k.fn("dve", lambda e: e.reciprocal(out=S(2), in_=S(1)), [rsm], [rsm])
        k.ts("dve", R[:, :, 44:48], R[:, :, 4:8], 0.0, -1e30, ALU.is_lt, ALU.mult, r=[rt], w=[rt])
        E4 = lambda: R[:, :, 8:40].rearrange("p t (g e) -> p t g e", e=8)
        k.tt("dve", E4(), E4(), R[:, :, 44:48].unsqueeze(3).broadcast_to([128, nt, 4, 8]), ALU.add, r=[rt], w=[rt])
        for t in range(nt):
            k.fn("dve", (lambda e, t=t: e.max(out=r8[:, t, :], in_=rt[:, t, 8:40])), [rt], [r8])
        k.tt("dve", S(3), r8[:, 0:nt, 1], r8[:, 0:nt, 0], ALU.subtract, r=[r8], w=[rsm])
        k.act(S(4), S(3), AF.Exp, r=[rsm], w=[rsm])
        k.ts("dve", S(5), S(4), 1.0, None, ALU.add, r=[rsm], w=[rsm])
        k.fn("dve", lambda e: e.reciprocal(out=S(6), in_=S(5)), [rsm], [rsm])
        k.tt("dve", gts[:, :, 0], S(6), S(2), ALU.mult, r=[rsm], w=[gts])
        k.tt("dve", gts[:, :, 1], gts[:, :, 0], S(4), ALU.mult, r=[gts, rsm], w=[gts])
        k.tt("dve", oh1, R[:, :, 8:40], r8[:, 0:nt, 0:1].broadcast_to([128, nt, 32]), ALU.is_equal, r=[rt, r8], w=[oh1])
        k.tt("dve", oh2, R[:, :, 8:40], r8[:, 0:nt, 1:2].broadcast_to([128, nt, 32]), ALU.is_equal, r=[rt, r8], w=[oh2])

    def route(self, lg, rbg, rbe, rt, r8, rsm, Gout, oh1=None, oh2=None, gts=None):
        k = self.k
        k.tt("dve", rt[:, 0:4], lg[:, 0:4], rbg[:], ALU.add)
        k.tt("dve", rt[:, 8:40], lg[:, 4:36], rbe[:], ALU.add)
        k.fn("dve", lambda e: e.tensor_reduce(out=rsm[:, 0:1], in_=rt[:, 0:4], axis=mybir.AxisListType.X, op=ALU.max), [rt], [rsm])
        k.ts("dve", rt[:, 4:8], rt[:, 0:4], rsm[:, 0:1], None, ALU.subtract)
        k.act(rt[:, 40:44], rt[:, 4:8], AF.Exp)
        k.fn("dve", lambda e: e.tensor_reduce(out=rsm[:, 1:2], in_=rt[:, 40:44], axis=mybir.AxisListType.X, op=ALU.add), [rt], [rsm])
        k.fn("dve", lambda e: e.reciprocal(out=rsm[:, 2:3], in_=rsm[:, 1:2]), [rsm], [rsm])
        k.ts("dve", rt[:, 44:48], rt[:, 4:8], 0.0, -1e30, ALU.is_lt, ALU.mult)
        k.tt("dve", rt[:, 8:40].rearrange("p (g e) -> p g e", e=8), rt[:, 8:40].rearrange("p (g e) -> p g e", e=8),
             rt[:, 44:48].unsqueeze(2).broadcast_to([128, 4, 8]), ALU.add)
        k.fn("dve", lambda e: e.max(out=r8[:], in_=rt[:, 8:40]), [rt], [r8])
        k.tt("dve", rsm[:, 3:4], r8[:, 1:2], r8[:, 0:1], ALU.subtract)
        k.act(rsm[:, 4:5], rsm[:, 3:4], AF.Exp)
        k.ts("dve", rsm[:, 5:6], rsm[:, 4:5], 1.0, None, ALU.add)
        k.fn("dve", lambda e: e.reciprocal(out=rsm[:, 6:7], in_=rsm[:, 5:6]), [rsm], [rsm])
        k.tt("dve", rsm[:, 7:8], rsm[:, 6:7], rsm[:, 2:3], ALU.mult)
        k.tt("dve", rsm[:, 8:9], rsm[:, 7:8], rsm[:, 4:5], ALU.mult)
        if oh1 is not None:
            k.ts("dve", oh1, rt[:, 8:40], r8[:, 0:1], None, ALU.is_equal)
            k.ts("dve", oh2, rt[:, 8:40], r8[:, 1:2], None, ALU.is_equal)
            k.copy("dve", gts, rsm[:, 7:9])
            return
        k.ts("dve", Gout, rt[:, 8:40], r8[:, 0:1], rsm[:, 7:8], ALU.is_equal, ALU.mult, r=[rt, r8, rsm], w=[Gout])
        k.ts("dve", rt[:, 8:40], rt[:, 8:40], r8[:, 1:2], rsm[:, 8:9], ALU.is_equal, ALU.mult)
        k.tt("dve", Gout, Gout, rt[:, 8:40], ALU.add, r=[Gout, rt], w=[Gout])


_CACHE = {}


def _host_inputs(inputs, core):
    b0 = core * NBC
    f = np.float32
    hin = np.concatenate([inputs["ctx"][b0:b0 + NBC], inputs["x"][b0:b0 + NBC]], axis=1)
    cc = np.concatenate([inputs["c"][b0:b0 + NBC], inputs["c_ctx"][None, :]], axis=0)
    return {"hin": np.ascontiguousarray(hin, dtype=f), "cc": np.ascontiguousarray(cc, dtype=f)}


def _shared_inputs(inputs):
    f = np.float32
    wr = np.zeros((DEPTH, D, 64), f)
    wr[:, :, 0:4] = inputs["moe_w_group"]
    wr[:, :, 4:36] = inputs["moe_w_expert"]
    conv_wb = np.concatenate([inputs["conv_w"], inputs["conv_b"][:, None, :]], axis=1)
    rows = np.repeat(np.arange(SEQ // 64, dtype=f), 64)
    cols = np.tile(np.arange(64, dtype=f), SEQ // 64)

    def rope_tabs(dim):
        q = dim // 4
        inv = (np.float32(10000.0) ** (-np.arange(q, dtype=f) / np.float32(q))).astype(f)
        ang = np.concatenate([rows[:, None] * inv[None, :], cols[:, None] * inv[None, :]], axis=-1).astype(f)
        return np.ascontiguousarray(np.cos(ang).T.astype(f)), np.ascontiguousarray(np.sin(ang).T.astype(f))
    rc, rs_ = rope_tabs(256)
    mc, ms = rope_tabs(64)
    c1 = np.ascontiguousarray(np.concatenate([mc, mc, mc, mc], axis=0))
    c2 = np.ascontiguousarray(np.concatenate([-ms, ms, -ms, ms], axis=0))
    wdn = inputs["mla_w_down"][0]
    kr = wdn[:, 640:704]
    mla_wd = np.ascontiguousarray(np.concatenate([wdn[:, 0:640], kr, kr[:, 32:64], kr[:, 0:32]], axis=1))
    wuq = inputs["mla_w_uq"][0].reshape(384, 8, 192)
    rp = wuq[:, :, 128:192]
    mla_wuq = np.ascontiguousarray(np.concatenate([
        wuq[:, :, 0:128].reshape(384, 1024), rp.reshape(384, 512),
        np.concatenate([rp[:, :, 32:64], rp[:, :, 0:32]], axis=2).reshape(384, 512)], axis=1))
    wukv = inputs["mla_w_ukv"][0].reshape(256, 8, 256)
    mla_wukv = np.ascontiguousarray(np.concatenate([wukv[:, :, 0:128].reshape(256, 1024), wukv[:, :, 128:256].reshape(256, 1024)], axis=1))
    norms = np.ascontiguousarray(np.concatenate([inputs["mla_q_norm"][0].reshape(3, 128), inputs["mla_kv_norm"][0].reshape(2, 128)], axis=0))
    return {
        "mla_wd": mla_wd, "mla_norms": norms, "mla_wuq": mla_wuq, "mla_wukv": mla_wukv,
        "mla_w_out": np.ascontiguousarray(inputs["mla_w_out"][0]), "mla_c1": c1, "mla_c2": c2,
        "ret_w_in": inputs["ret_w_in"], "ret_decay": np.ascontiguousarray(inputs["ret_decay"].reshape(2, 8)),
        "ret_gn_g": inputs["ret_gn_g"], "ret_w_out": inputs["ret_w_out"], "ret_cos": rc, "ret_sin": rs_,
        "ada_w": inputs["ada_w"], "ada_b": np.ascontiguousarray(inputs["ada_b"][:, None, :]),
        "ln_g": inputs["ln_g"], "ln_b": inputs["ln_b"],
        "conv_w_in": inputs["conv_w_in"], "conv_wb": np.ascontiguousarray(conv_wb), "conv_w_out": inputs["conv_w_out"],
        "moe_wr": wr, "moe_bg": inputs["moe_b_group"], "moe_be": inputs["moe_b_expert"],
        "moe_w1": np.ascontiguousarray(inputs["moe_w1"].reshape(DEPTH, NE, 8, 128, 512).transpose(0, 1, 3, 2, 4)),
        "moe_w3": np.ascontiguousarray(inputs["moe_w3"].reshape(DEPTH, NE, 8, 128, 512).transpose(0, 1, 3, 2, 4)),
        "moe_w2": np.ascontiguousarray(inputs["moe_w2"].reshape(DEPTH, NE, 4, 128, D).transpose(0, 1, 3, 2, 4)),
    }


def kernel(**inputs):
    inputs = {k_: np.asarray(v) for k_, v in inputs.items()}
    if "prog" not in _CACHE:
        _CACHE["prog"] = Prog()
    prog = _CACHE["prog"]
    shared = _shared_inputs(inputs)
    in_maps = []
    for core in range(8):
        m = dict(shared)
        m.update(_host_inputs(inputs, core))
        in_maps.append({k_: np.ascontiguousarray(v, dtype=np.float32) for k_, v in m.items() if k_ in prog.I})
    res = run_bass_kernel_spmd(prog.nc, in_maps, core_ids=list(range(8)))
    return np.concatenate([r["out"] for r in res.results], axis=0).astype(np.float32)
```
